# Optimizing a Trainium2 kernel written in Bass

```python
import jax, jax.numpy as jnp
from jax import lax
import numpy as np

D_MODEL = 1024
BATCH = 8
SEQ = 2048
DEPTH = 4

CHUNK = 64
HEAD_DIM = 64
RWKV_WIDTH = D_MODEL // 2
RWKV_HEADS = RWKV_WIDTH // HEAD_DIM
SC_WIDTH = D_MODEL // 4
SC_KERNEL = 3
CF_WIDTH = D_MODEL // 4
CF_KERNEL = 31
MIX_WIDTH = RWKV_WIDTH + SC_WIDTH + CF_WIDTH
IN_WIDTH = 3 * RWKV_WIDTH + 3 * SC_WIDTH + 2 * CF_WIDTH
DECAY_RANK = 64
ICLR_RANK = 64
GATE_RANK = 128
FFN_HIDDEN = ((8 * D_MODEL + 3 * 256 - 1) // (3 * 256)) * 256
NORM_EPS = 1e-6
LN_EPS = 1e-5
GN_EPS = 64e-5

kernel_name = "hymba_rwkv7_shortconv_conformer_trunk"


def rms_norm(x, g):
    x32 = x.astype(jnp.float32)
    y = x32 * lax.rsqrt(jnp.mean(x32 * x32, axis=-1, keepdims=True) + NORM_EPS)
    return (y * g.astype(jnp.float32)).astype(x.dtype)


def layer_norm(x, g, b):
    x32 = x.astype(jnp.float32)
    mu = jnp.mean(x32, axis=-1, keepdims=True)
    var = jnp.mean(jnp.square(x32 - mu), axis=-1, keepdims=True)
    y = (x32 - mu) * lax.rsqrt(var + LN_EPS)
    return (y * g.astype(jnp.float32) + b.astype(jnp.float32)).astype(x.dtype)


def token_shift(u):
    return jnp.pad(u[:, :-1], ((0, 0), (1, 0), (0, 0)))


def causal_dwconv(u, w):
    k_width, ch = w.shape
    return lax.conv_general_dilated(
        u, w[:, None, :].astype(u.dtype), window_strides=(1,), padding=[(k_width - 1, 0)],
        dimension_numbers=("NWC", "WIO", "NWC"), feature_group_count=ch)


def rwkv7_scan(r, w, k, v, a_vec, b_vec):
    xs = tuple(jnp.moveaxis(t, 1, 0) for t in (r, w, k, v, a_vec, b_vec))

    def step(S, inp):
        r_t, w_t, k_t, v_t, a_t, b_t = inp
        sa = jnp.einsum("bhij,bhj->bhi", S, a_t)
        S = S * w_t[:, :, None, :] + sa[..., None] * b_t[:, :, None, :] + v_t[..., None] * k_t[:, :, None, :]
        y = jnp.einsum("bhij,bhj->bhi", S, r_t)
        return S, y

    bsz, _, nh, nd = r.shape
    s0 = jnp.zeros((bsz, nh, nd, nd), jnp.float32)
    _, ys = lax.scan(step, s0, xs)
    return jnp.moveaxis(ys, 0, 1)


def rwkv7_group(h, rkv, mu_rkv, mu_wag, w0, w1, w2, a0, a1, a2, g1, g2, k_k, k_a, r_k, lnx_g, lnx_b):
    bsz, seq, _ = h.shape
    rkv = rkv + (token_shift(rkv) - rkv) * mu_rkv
    r, k, v = jnp.split(rkv, 3, axis=-1)
    dh = token_shift(h) - h
    xw = h + dh * mu_wag[0]
    xa = h + dh * mu_wag[1]
    xg = h + dh * mu_wag[2]
    w_log = -jax.nn.softplus(-(w0 + jnp.tanh(xw @ w1) @ w2).astype(jnp.float32)) - 0.5
    decay = jnp.exp(-jnp.exp(w_log))
    a = jax.nn.sigmoid((a0 + (xa @ a1) @ a2).astype(jnp.float32))
    g = jax.nn.sigmoid(xg @ g1) @ g2
    hs = lambda t: t.reshape(bsz, seq, RWKV_HEADS, HEAD_DIM)
    hp = lambda p: p.reshape(RWKV_HEADS, HEAD_DIM).astype(jnp.float32)
    r32, k32, v32 = (hs(t).astype(jnp.float32) for t in (r, k, v))
    a_h, w_h = hs(a), hs(decay)
    kk = k32 * hp(k_k)
    kk = kk / jnp.maximum(jnp.sqrt(jnp.sum(kk * kk, axis=-1, keepdims=True)), 1e-12)
    k32 = k32 * (1.0 + (a_h - 1.0) * hp(k_a))
    y = rwkv7_scan(r32, w_h, k32, v32, -kk, kk * a_h)
    mu = jnp.mean(y, axis=-1, keepdims=True)
    var = jnp.mean(jnp.square(y - mu), axis=-1, keepdims=True)
    y = (y - mu) * lax.rsqrt(var + GN_EPS)
    y = y * hp(lnx_g) + hp(lnx_b)
    bonus = jnp.sum(r32 * k32 * r_k.astype(jnp.float32), axis=-1, keepdims=True) * v32
    y = (y + bonus).reshape(bsz, seq, RWKV_WIDTH).astype(h.dtype)
    return y * g


def shortconv_group(sc_in, conv_w):
    gate_b, gate_c, u = jnp.split(sc_in, 3, axis=-1)
    return gate_b * causal_dwconv(gate_c * u, conv_w)


def conformer_group(cf_in, conv_w, conv_b, ln_g, ln_b):
    val, gate = jnp.split(cf_in, 2, axis=-1)
    u = val * jax.nn.sigmoid(gate)
    u = causal_dwconv(u, conv_w) + conv_b
    return jax.nn.silu(layer_norm(u, ln_g, ln_b))


def setup_inputs(seed: int = 0) -> dict:
    key = jax.random.key(seed)
    ks = iter(jax.random.split(key, 40))
    nrm = lambda shape, s: jax.random.normal(next(ks), shape, jnp.float32) * s
    unif = lambda shape: jax.random.uniform(next(ks), shape, jnp.float32)
    L = DEPTH
    return {
        "x": nrm((BATCH, SEQ, D_MODEL), 1.0),
        "w_in": nrm((L, D_MODEL, IN_WIDTH), D_MODEL ** -0.5),
        "mu_rkv": unif((L, 3 * RWKV_WIDTH)),
        "mu_wag": unif((L, 3, D_MODEL)),
        "w0": nrm((L, RWKV_WIDTH), 0.5),
        "w1": nrm((L, D_MODEL, DECAY_RANK), D_MODEL ** -0.5),
        "w2": nrm((L, DECAY_RANK, RWKV_WIDTH), 0.5 * DECAY_RANK ** -0.5),
        "a0": nrm((L, RWKV_WIDTH), 0.5),
        "a1": nrm((L, D_MODEL, ICLR_RANK), D_MODEL ** -0.5),
        "a2": nrm((L, ICLR_RANK, RWKV_WIDTH), 0.5 * ICLR_RANK ** -0.5),
        "g1": nrm((L, D_MODEL, GATE_RANK), D_MODEL ** -0.5),
        "g2": nrm((L, GATE_RANK, RWKV_WIDTH), GATE_RANK ** -0.5),
        "k_k": 0.85 + nrm((L, RWKV_WIDTH), 0.05),
        "k_a": 1.0 + nrm((L, RWKV_WIDTH), 0.05),
        "r_k": nrm((L, RWKV_HEADS, HEAD_DIM), 0.1),
        "lnx_g": 1.0 + nrm((L, RWKV_WIDTH), 0.05),
        "lnx_b": nrm((L, RWKV_WIDTH), 0.01),
        "sc_conv_w": nrm((L, SC_KERNEL, SC_WIDTH), SC_KERNEL ** -0.5),
        "cf_conv_w": nrm((L, CF_KERNEL, CF_WIDTH), CF_KERNEL ** -0.5),
        "cf_conv_b": nrm((L, CF_WIDTH), 0.01),
        "cf_ln_g": 1.0 + nrm((L, CF_WIDTH), 0.05),
        "cf_ln_b": nrm((L, CF_WIDTH), 0.01),
        "w_o": nrm((L, MIX_WIDTH, D_MODEL), MIX_WIDTH ** -0.5),
        "w_gate": nrm((L, D_MODEL, FFN_HIDDEN), D_MODEL ** -0.5),
        "w_up": nrm((L, D_MODEL, FFN_HIDDEN), D_MODEL ** -0.5),
        "w_down": nrm((L, FFN_HIDDEN, D_MODEL), FFN_HIDDEN ** -0.5),
        "pre_mix_g": 1.0 + nrm((L, D_MODEL), 0.05),
        "post_mix_g": 1.0 + nrm((L, D_MODEL), 0.05),
        "pre_ffn_g": 1.0 + nrm((L, D_MODEL), 0.05),
        "post_ffn_g": 1.0 + nrm((L, D_MODEL), 0.05),
    }


def reference(x, w_in, mu_rkv, mu_wag, w0, w1, w2, a0, a1, a2, g1, g2, k_k, k_a, r_k,
              lnx_g, lnx_b, sc_conv_w, cf_conv_w, cf_conv_b, cf_ln_g, cf_ln_b, w_o,
              w_gate, w_up, w_down, pre_mix_g, post_mix_g, pre_ffn_g, post_ffn_g):
    split_at = (3 * RWKV_WIDTH, 3 * RWKV_WIDTH + 3 * SC_WIDTH)
    for l in range(DEPTH):
        h = rms_norm(x, pre_mix_g[l])
        proj = h @ w_in[l]
        rkv, sc_in, cf_in = jnp.split(proj, split_at, axis=-1)
        y_rwkv = rwkv7_group(h, rkv, mu_rkv[l], mu_wag[l], w0[l], w1[l], w2[l], a0[l], a1[l], a2[l],
                             g1[l], g2[l], k_k[l], k_a[l], r_k[l], lnx_g[l], lnx_b[l])
        y_sc = shortconv_group(sc_in, sc_conv_w[l])
        y_cf = conformer_group(cf_in, cf_conv_w[l], cf_conv_b[l], cf_ln_g[l], cf_ln_b[l])
        mix = jnp.concatenate([y_rwkv, y_sc, y_cf], axis=-1) @ w_o[l]
        x = x + rms_norm(mix, post_mix_g[l])
        h2 = rms_norm(x, pre_ffn_g[l])
        f = (jax.nn.silu(h2 @ w_gate[l]) * (h2 @ w_up[l])) @ w_down[l]
        x = x + rms_norm(f, post_ffn_g[l])
    return x
```

```python
import contextlib
import numpy as np
import concourse.bass as bass
import concourse.mybir as mybir
from concourse.bass_utils import run_bass_kernel_spmd

F32 = mybir.dt.float32
BF16 = mybir.dt.bfloat16
AF = mybir.ActivationFunctionType
ALU = mybir.AluOpType

P = 128
T = 2048
D = 1024
TT = 512
NT = T // TT
KC = 8
NJ = 22
NCORES = 8
C0 = -0.6065306597126334
SAME_ENGINE_SYNC = True

PV = {}
_o = 0
for _n, _c in [("premix", 8), ("postmix", 8), ("preffn", 8), ("postffn", 8), ("muw", 8), ("mua", 8),
               ("mug", 8), ("murkv", 12), ("w0", 4), ("a0", 4), ("kk", 4), ("ka", 4), ("rk", 4),
               ("lng", 4), ("lnb", 4), ("scw", 6), ("cfw", 62), ("cfb", 2), ("cflg", 2), ("cflb", 2)]:
    PV[_n] = _o
    _o += _c
NPV = _o
PD = {"omuw": 0, "omua": 8, "omug": 16, "omurkv": 24, "omka": 36}
NPD = 40
CF = {"ident": 0, "ones": 128, "bones": 256, "msu": 384, "miu": 896, "msl": 1408, "irep": 1920, "scan": 2432}
NCF = 2944


class Tok:
    __slots__ = ("w", "r")

    def __init__(self):
        self.w = None
        self.r = []


class _Rec:
    def __init__(self):
        self.calls = []

    def __getattr__(self, name):
        def f(*a, **k):
            self.calls.append((name, a, k))
        return f


class Builder:
    ENGS = ("pe", "act", "dve", "pool", "sp")

    def __init__(self, nc, es, ndma=48):
        self.nc = nc
        self.q = {e: [] for e in self.ENGS}
        self.cnt = {e: 0 for e in self.ENGS}
        self.seen = {e: {} for e in self.ENGS}
        self.sems = {}
        for e in ("pe", "act", "dve", "pool"):
            self.sems[e] = es.enter_context(nc.semaphore("s_" + e))
        self.ndma = ndma
        self.dma_uses = [0] * ndma
        self.dma_next = 0
        for k in range(ndma):
            self.sems["dma%d" % k] = es.enter_context(nc.semaphore("s_dma%d" % k))

    def _collect(self, r, w):
        deps = {}

        def add(d):
            if d is None:
                return
            k, v = d
            if deps.get(k, 0) < v:
                deps[k] = v
        for t in r:
            add(t.w)
        for t in w:
            add(t.w)
            for d in t.r:
                add(d)
        return deps

    def _waits(self, eng, deps):
        out = []
        seen = self.seen[eng]
        for k, v in deps.items():
            if k == eng and (eng == "pe" or not SAME_ENGINE_SYNC):
                continue
            if seen.get(k, 0) >= v:
                continue
            seen[k] = v
            out.append((k, v))
        return out

    def op(self, eng, fn, r=(), w=()):
        rec = _Rec()
        fn(rec)
        assert len(rec.calls) == 1
        call = rec.calls[0]
        fn = lambda e, call=call: getattr(e, call[0])(*call[1], **call[2])
        deps = self._collect(r, w)
        waits = self._waits(eng, deps)
        self.cnt[eng] += 1
        me = (eng, self.cnt[eng])
        self.q[eng].append((fn, waits, (eng, 1)))
        for t in r:
            t.r.append(me)
        for t in w:
            t.w = me
            t.r = []

    def dma(self, qe, out_ap, in_ap, r=(), w=()):
        deps = self._collect(r, w)
        k = self.dma_next
        self.dma_next = (k + 1) % self.ndma
        key = "dma%d" % k
        if self.dma_uses[k] > 0:
            v = 16 * self.dma_uses[k]
            if deps.get(key, 0) < v:
                deps[key] = v
        waits = self._waits(qe, deps)
        self.dma_uses[k] += 1
        me = (key, 16 * self.dma_uses[k])
        self.q[qe].append((lambda e: e.dma_start(out=out_ap, in_=in_ap), waits, (key, 16)))
        for t in r:
            t.r.append(me)
        for t in w:
            t.w = me
            t.r = []

    def wait_only(self, eng, toks):
        deps = self._collect(toks, ())
        waits = self._waits(eng, deps)
        self.q[eng].append((None, waits, None))

    def barrier(self):
        allv = {e: self.cnt[e] for e in ("pe", "act", "dve", "pool") if self.cnt[e] > 0}
        for k in range(self.ndma):
            if self.dma_uses[k] > 0:
                allv["dma%d" % k] = 16 * self.dma_uses[k]
        for e in self.ENGS:
            deps = {k: v for k, v in allv.items() if k != e}
            waits = self._waits(e, deps)
            if waits:
                self.q[e].append((None, waits, None))

    def emit(self, block):
        sems = self.sems

        def run(e, lst):
            for fn, waits, inc in lst:
                for k, v in waits:
                    e.wait_ge(sems[k], v)
                if fn is not None:
                    ins = fn(e)
                    if inc is not None:
                        ins.then_inc(sems[inc[0]], inc[1])

        @block.tensor
        def _(e):
            run(e, self.q["pe"])

        @block.scalar
        def _(e):
            run(e, self.q["act"])

        @block.vector
        def _(e):
            run(e, self.q["dve"])

        @block.gpsimd
        def _(e):
            run(e, self.q["pool"])

        @block.sync
        def _(e):
            run(e, self.q["sp"])


class Arena:
    def __init__(self, ap, nbytes):
        self.ap = ap
        self.n = nbytes
        self.off = 0

    def alloc(self, cols, dtype):
        sz = cols * (4 if dtype == F32 else 2)
        sz = (sz + 3) // 4 * 4
        assert self.off + sz <= self.n, ("arena overflow", self.off, sz, self.n)
        a = self.ap[:, self.off // 4:(self.off + sz) // 4]
        self.off += sz
        if dtype != F32:
            a = a.bitcast(dtype)
            a = a[:, 0:cols]
        return a


def build_program(L=4, dbg=False):
    nc = bass.Bass("TRN2", target_bir_lowering=False)
    dt_in = lambda n, s: nc.dram_tensor(n, s, F32, kind="ExternalInput").ap()
    x_d = dt_in("xT", [P, KC * T])
    win_d = dt_in("w_in", [L, NJ, P, KC * 128])
    wg_d = dt_in("w_gate", [L, NJ, P, KC * 128])
    wu_d = dt_in("w_up", [L, NJ, P, KC * 128])
    wo_d = dt_in("w_o", [L, KC, P, KC * 128])
    wd_d = dt_in("w_down", [L, KC, P, NJ * 128])
    wlr_d = dt_in("wlr", [L, P, KC * 256])
    w2a2_d = dt_in("w2a2", [L, P, 512])
    g2_d = dt_in("g2", [L, P, 512])
    pv_d = dt_in("pv", [L, P, NPV])
    cf_d = dt_in("cf", [P, NCF])
    y_d = nc.dram_tensor("yT", [P, KC * T], F32, kind="ExternalOutput").ap()
    if dbg:
        dbg_d = nc.dram_tensor("dbg", [P, KC * TT], F32, kind="ExternalOutput").ap()

    with contextlib.ExitStack() as es:
        def sb(name, cols, dtype):
            return es.enter_context(nc.sbuf_tensor(name, [P, cols], dtype))
        xT = sb("xT_sb", KC * T, F32)
        pv = sb("pv_sb", NPV, F32)
        pd = sb("pd_sb", NPD, F32)
        cb = sb("cb", NCF, BF16)
        cmask = cb[:, 384:NCF]
        smallc = sb("smallc", 4, F32)
        wlra = sb("wlra", KC * 256, BF16)
        wlrb = sb("wlrb", KC * 256, BF16)
        w2a2 = sb("w2a2_sb", 512, BF16)
        g2 = sb("g2_sb", 512, BF16)
        rkvcarry = sb("rkvcarry", 12, F32)
        cu = sb("cu", 2 * 514, F32)
        cfu = sb("cfu", 2 * 542, F32)
        ST32 = sb("ST32", 4 * 64, F32)
        STb = sb("STb", 4 * 64, BF16)
        hT = sb("hT", KC * 513, BF16)
        hcarry = sb("hcarry", KC, BF16)
        mixcat = sb("mixcat", KC * TT, BF16)
        fo = sb("fo", KC * TT, F32)
        wlr = fo[:, 0:KC * 128].bitcast(BF16)
        wos = [sb("wos%d" % i, KC * 128, BF16) for i in range(2)]
        ARENA_BYTES = 80 * 1024
        arena_t = sb("arena", ARENA_BYTES // 4, F32)
        psum = [es.enter_context(nc.psum_tensor("ps%d" % i, [P, 512], F32)) for i in range(8)]

        B = Builder(nc, es)
        pst = [Tok() for _ in range(8)]
        ps_rr = [0]

        held = set()

        ALLB = tuple(range(8))
        ps_ptr = {}

        def getps(banks=None, hold=False):
            key = banks or ALLB
            while True:
                p_ = ps_ptr.get(key, 0)
                ps_ptr[key] = p_ + 1
                i = key[p_ % len(key)]
                if i not in held:
                    break
            if hold:
                held.add(i)
            return psum[i], pst[i]

        def release(ps):
            for i in range(8):
                if psum[i] is ps:
                    held.discard(i)

        t_x = [Tok() for _ in range(NT)]
        t_pv, t_pd, t_c, t_wlr, t_wlrab, t_w2, t_g2 = Tok(), Tok(), Tok(), Tok(), Tok(), Tok(), Tok()
        t_carry, t_cu, t_cfu = Tok(), [Tok(), Tok()], [Tok(), Tok()]
        t_ST = [Tok() for _ in range(4)]
        t_hT, t_mix, t_fo = Tok(), [Tok() for _ in range(KC)], [Tok() for _ in range(KC)]
        t_wos = [Tok(), Tok()]

        def pvc(name, i=0):
            o = PV[name] + i
            return pv[:, o:o + 1]

        def pdc(name, i=0):
            o = PD[name] + i
            return pd[:, o:o + 1]

        ident = cb[:, 0:128]
        ones = cb[:, 128:256]
        bones = cb[:, 256:384]
        msu = cmask[:, 0:512]
        miu = cmask[:, 512:1024]
        msl = cmask[:, 1024:1536]
        irep = cmask[:, 1536:2048]
        scanm = cmask[:, 2048:2560]
        eps_n = smallc[:, 0:1]
        eps_ln = smallc[:, 1:2]
        eps_gn = smallc[:, 2:3]

        def xs(kc, tt):
            return xT[:, kc * T + tt * TT: kc * T + (tt + 1) * TT]

        def hs(kc, a, b):
            return hT[:, kc * 513 + a: kc * 513 + b]

        for kc in range(KC):
            for tt in range(NT):
                B.dma("sp", xs(kc, tt), x_d[:, kc * T + tt * TT: kc * T + (tt + 1) * TT], w=[t_x[tt]])
        B.dma("pool", cb[:, :], cf_d[:, :], w=[t_c])
        B.op("dve", lambda e: e.memset(smallc[:, 0:1], 1e-6), w=[t_c])
        B.op("dve", lambda e: e.memset(smallc[:, 1:2], 1e-5), w=[t_c])
        B.op("dve", lambda e: e.memset(smallc[:, 2:3], 64e-5), w=[t_c])
        B.op("dve", lambda e: e.memset(smallc[:, 3:4], 1e-24), w=[t_c])

        wslot_rr = {}

        def rr(name, n):
            i = wslot_rr.get(name, 0)
            wslot_rr[name] = (i + 1) % n
            return i

        def rmsnorm_to_hT(tt, gname):
            pss, tss = getps()
            sq, t_sq = NORM["sq"], NORM["t_sq"]
            for kc in range(KC):
                i = kc % 2
                B.op("act", lambda e, kc=kc, i=i: e.activation(out=sq[i], in_=xs(kc, tt), func=AF.Square),
                     r=[t_x[tt]], w=[t_sq[i]])
                B.op("pe", lambda e, kc=kc, i=i: e.matmul(pss[:, :], ones, sq[i], start=(kc == 0), stop=(kc == KC - 1)),
                     r=[t_sq[i], t_c], w=[tss])
            rstd, t_rstd = NORM["rstd"], NORM["t_rstd"]
            B.op("act", lambda e: e.activation(out=rstd, in_=pss[:, :], func=AF.Ln, scale=1.0 / D, bias=eps_n),
                 r=[tss, t_c], w=[t_rstd])
            B.op("act", lambda e: e.activation(out=rstd, in_=rstd, func=AF.Exp, scale=-0.5), r=[t_rstd], w=[t_rstd])
            for kc in range(KC):
                B.op("dve", lambda e, kc=kc: e.scalar_tensor_tensor(
                    out=hs(kc, 1, 513), in0=xs(kc, tt), scalar=pvc(gname, kc), in1=rstd,
                    op0=ALU.mult, op1=ALU.mult), r=[t_x[tt], t_rstd, t_pv], w=[t_hT])

        def post_norm_residual(tt, gname, pss, tss):
            rstd, t_rstd = NORM["rstd"], NORM["t_rstd"]
            B.op("act", lambda e: e.activation(out=rstd, in_=pss[:, :], func=AF.Ln, scale=1.0 / D, bias=eps_n),
                 r=[tss, t_c], w=[t_rstd])
            B.op("act", lambda e: e.activation(out=rstd, in_=rstd, func=AF.Exp, scale=-0.5), r=[t_rstd], w=[t_rstd])
            for dg in range(KC):
                f = fo[:, dg * TT:(dg + 1) * TT]
                B.op("dve", lambda e, f=f, dg=dg: e.scalar_tensor_tensor(
                    out=f, in0=f, scalar=pvc(gname, dg), in1=rstd, op0=ALU.mult, op1=ALU.mult),
                    r=[t_rstd, t_pv], w=[t_fo[dg]])
                B.op("dve", lambda e, f=f, dg=dg: e.tensor_tensor(out=xs(dg, tt), in0=xs(dg, tt), in1=f, op=ALU.add),
                     r=[t_fo[dg]], w=[t_x[tt]])

        NORM = {}

        for l in range(L):
            B.barrier()
            ar = Arena(arena_t[:, :], ARENA_BYTES)
            NORM["sq"] = [ar.alloc(TT, BF16), ar.alloc(TT, BF16)]
            NORM["t_sq"] = [Tok(), Tok()]
            NORM["rstd"] = ar.alloc(TT, F32)
            NORM["t_rstd"] = Tok()
            base_off = ar.off
            B.dma("sp", pv[:, :], pv_d[l], w=[t_pv])
            B.dma("pool", wlr[:, :], wlr_d[l], w=[t_wlr])
            B.dma("pool", w2a2[:, :], w2a2_d[l], w=[t_w2])
            B.dma("pool", g2[:, :], g2_d[l], w=[t_g2])
            for nm, src, n in [("omuw", "muw", 8), ("omua", "mua", 8), ("omug", "mug", 8), ("omurkv", "murkv", 12),
                               ("omka", "ka", 4)]:
                B.op("dve", lambda e, nm=nm, src=src, n=n: e.tensor_scalar(
                    out=pd[:, PD[nm]:PD[nm] + n], in0=pv[:, PV[src]:PV[src] + n], scalar1=-1.0, scalar2=1.0,
                    op0=ALU.mult, op1=ALU.add), r=[t_pv], w=[t_pd])
            for kc in range(KC):
                for (c0, c1, mn, on) in [(0, 64, "muw", "omuw"), (64, 128, "mua", "omua"), (128, 256, "mug", "omug")]:
                    src = wlr[:, kc * 256 + c0: kc * 256 + c1]
                    B.op("dve", lambda e, src=src, kc=kc, c0=c0, c1=c1, on=on: e.tensor_scalar(
                        out=wlra[:, kc * 256 + c0: kc * 256 + c1], in0=src, scalar1=pdc(on, kc), scalar2=None,
                        op0=ALU.mult), r=[t_wlr, t_pd], w=[t_wlrab])
                    B.op("dve", lambda e, src=src, kc=kc, c0=c0, c1=c1, mn=mn: e.tensor_scalar(
                        out=wlrb[:, kc * 256 + c0: kc * 256 + c1], in0=src, scalar1=pvc(mn, kc), scalar2=None,
                        op0=ALU.mult), r=[t_wlr, t_pv], w=[t_wlrab])
            B.op("dve", lambda e: e.memset(rkvcarry[:, :], 0.0), w=[t_carry])
            B.op("dve", lambda e: e.memset(cu[:, :], 0.0), w=t_cu)
            B.op("dve", lambda e: e.memset(cfu[:, :], 0.0), w=t_cfu)
            B.op("dve", lambda e: e.memset(ST32[:, :], 0.0), w=t_ST)
            B.op("dve", lambda e: e.memset(STb[:, :], 0.0), w=t_ST)
            B.op("dve", lambda e: e.memset(hT[:, :], 0.0), w=[t_hT])

            for tt in range(NT):
                B.barrier()
                ar.off = base_off
                if tt > 0:
                    for kc in range(KC):
                        B.op("dve", lambda e, kc=kc: e.tensor_copy(hs(kc, 0, 1), hcarry[:, kc:kc + 1]), w=[t_hT])
                rmsnorm_to_hT(tt, "premix")
                for kc in range(KC):
                    B.op("dve", lambda e, kc=kc: e.tensor_copy(hcarry[:, kc:kc + 1], hs(kc, 512, 513)), r=[t_hT], w=[t_hT])

                ar2 = Arena(fo[:, :], KC * TT * 4)

                def A_(cols, dtype):
                    sz = (cols * (4 if dtype == F32 else 2) + 3) // 4 * 4
                    if ar.off + sz <= ar.n:
                        return ar.alloc(cols, dtype)
                    return ar2.alloc(cols, dtype)
                wins = [A_(KC * 128, BF16) for _ in range(2)]
                t_wins = [Tok() for _ in range(2)]
                raw = [A_(513, F32)]
                t_raw = [Tok()]
                f32t = lambda: A_(TT, F32)
                bft = lambda: A_(TT, BF16)
                r_t, k_t, v_t, sgw, av = f32t(), f32t(), f32t(), f32t(), f32t()
                twa, tg = bft(), bft()
                tA, tB, tC, tD, tE = f32t(), f32t(), f32t(), f32t(), f32t()
                rkb = bft()
                scr2 = tg
                SETS = []
                for _i in range(2):
                    S = dict(bt=bft(), kt=bft(), Bh=bft(), Kh=bft(), vb=bft())
                    S["tk"] = {n: Tok() for n in ["bt", "kt", "Bh", "Kh", "vb"]}
                    SETS.append(S)
                Rm = [bft(), bft()]
                Lm = [bft(), bft()]
                Gm = [bft(), bft()]
                CH = []
                for _i in range(4):
                    S = dict(art=A_(8 * 128, BF16), bonus=bft(), gg=bft(), PC=A_(8, F32), TT=bft(),
                             LakT=bft(), LrbT=bft(), LrkT=bft(), VT=bft(), BhT=bft(), KhT=bft(),
                             Xb=A_(64, BF16), Ub=A_(64, BF16))
                    S["tk"] = {n: Tok() for n in ["art", "bonus", "gg", "PC", "TT", "LakT", "LrbT", "LrkT", "VT", "BhT", "KhT", "Xb", "Ub"]}
                    CH.append(S)
                gA, gB, gC = tE, r_t, k_t
                tk = {n: Tok() for n in ["r", "k", "v", "sgw", "av", "twa", "tg", "A", "B", "C", "D", "E", "rkb",
                                         "R0", "R1", "L0", "L1", "G0", "G1"]}
                tk["scr2"] = tk["tg"]
                tk["gA"], tk["gB"], tk["gC"] = tk["E"], tk["r"], tk["k"]
                XB = (0, 1, 2)
                YB = (3, 4, 5, 6, 7)
                eh = [slice(0, 64), slice(64, 128)]
                v3 = lambda a: a.rearrange("p (c x) -> p c x", c=8)
                mk3 = lambda m, n: m[:, 0:n * 64].rearrange("p (c x) -> p c x", c=n)

                def proj_group(g, consume):
                    si = rr("win", 2)
                    B.dma("pool", wins[si], win_d[l, g], w=[t_wins[si]])
                    ps, tps = getps(XB)
                    for kc in range(KC):
                        B.op("pe", lambda e, kc=kc, ps=ps, si=si: e.matmul(
                            ps[:, :], wins[si][:, kc * 128:(kc + 1) * 128], hs(kc, 1, 513),
                            start=(kc == 0), stop=(kc == KC - 1)), r=[t_wins[si], t_hT], w=[tps])
                    consume(ps, tps)

                def lowrank(c0, c1, consume):
                    ps, tps = getps(XB)
                    n = 0
                    for kc in range(KC):
                        for (wsb, a, b) in [(wlra, 1, 513), (wlrb, 0, 512)]:
                            B.op("pe", lambda e, kc=kc, wsb=wsb, a=a, b=b, n=n, ps=ps: e.matmul(
                                ps[:, :], wsb[:, kc * 256 + c0: kc * 256 + c1], hs(kc, a, b),
                                start=(n == 0), stop=(n == 2 * KC - 1)), r=[t_wlrab, t_hT], w=[tps])
                            n += 1
                    consume(ps, tps)

                def x_lowrank():
                    def lr0(ps, tps):
                        B.op("act", lambda e: e.activation(out=twa[0:64, :], in_=ps[0:64, :], func=AF.Tanh),
                             r=[tps], w=[tk["twa"]])
                        B.op("dve", lambda e: e.tensor_copy(twa[64:128, :], ps[64:128, :]), r=[tps], w=[tk["twa"]])
                    lowrank(0, 128, lr0)
                    yield

                    def lr1(ps, tps):
                        B.op("act", lambda e: e.activation(out=tg, in_=ps[:, :], func=AF.Sigmoid), r=[tps], w=[tk["tg"]])
                    lowrank(128, 256, lr1)
                    yield

                def x_sc(gi):
                    cuv = lambda a, b: cu[:, gi * 514 + a: gi * 514 + b]
                    csb = tA
                    acc = tB

                    def c_cons(ps, tps):
                        B.op("act", lambda e: e.activation(out=csb, in_=ps[:, :], func=AF.Copy), r=[tps], w=[tk["A"]])
                    proj_group(14 + gi, c_cons)
                    yield

                    def u_cons(ps, tps):
                        B.op("dve", lambda e: e.tensor_tensor(out=cuv(2, 514), in0=ps[:, :], in1=csb, op=ALU.mult),
                             r=[tps, tk["A"]], w=[t_cu[gi]])
                    proj_group(16 + gi, u_cons)
                    yield
                    B.op("dve", lambda e: e.tensor_scalar(
                        out=acc, in0=cuv(0, 512), scalar1=pvc("scw", 0 * 2 + gi), scalar2=None, op0=ALU.mult),
                        r=[t_cu[gi], t_pv], w=[tk["B"]])
                    for kk_ in (1, 2):
                        B.op("dve", lambda e, kk_=kk_: e.scalar_tensor_tensor(
                            out=acc, in0=cuv(kk_, kk_ + 512), scalar=pvc("scw", kk_ * 2 + gi), in1=acc,
                            op0=ALU.mult, op1=ALU.add), r=[t_cu[gi], t_pv], w=[tk["B"]])
                    B.op("dve", lambda e: e.tensor_copy(cuv(0, 2), cuv(512, 514)), w=[t_cu[gi]])
                    yield

                    def b_cons(ps, tps):
                        B.op("dve", lambda e: e.tensor_tensor(
                            out=mixcat[:, (4 + gi) * TT:(5 + gi) * TT], in0=ps[:, :], in1=acc, op=ALU.mult),
                            r=[tps, tk["B"]], w=[t_mix[4 + gi]])
                    proj_group(12 + gi, b_cons)
                    yield

                cres = [tC, tD]
                tcres = [tk["C"], tk["D"]]

                def x_cf(gi):
                    fv = lambda a, b: cfu[:, gi * 542 + a: gi * 542 + b]
                    sg = tA
                    acc = cres[gi]

                    def g_cons(ps, tps):
                        B.op("act", lambda e: e.activation(out=sg, in_=ps[:, :], func=AF.Sigmoid), r=[tps], w=[tk["A"]])
                    proj_group(20 + gi, g_cons)
                    yield

                    def v_cons(ps, tps):
                        B.op("dve", lambda e: e.tensor_tensor(out=fv(30, 542), in0=ps[:, :], in1=sg, op=ALU.mult),
                             r=[tps, tk["A"]], w=[t_cfu[gi]])
                    proj_group(18 + gi, v_cons)
                    yield
                    B.op("dve", lambda e: e.tensor_scalar(
                        out=acc, in0=fv(0, 512), scalar1=pvc("cfw", gi), scalar2=pvc("cfb", gi),
                        op0=ALU.mult, op1=ALU.add), r=[t_cfu[gi], t_pv], w=[tcres[gi]])
                    for kk_ in range(1, 31):
                        B.op("dve", lambda e, kk_=kk_: e.scalar_tensor_tensor(
                            out=acc, in0=fv(kk_, kk_ + 512), scalar=pvc("cfw", kk_ * 2 + gi), in1=acc,
                            op0=ALU.mult, op1=ALU.add), r=[t_cfu[gi], t_pv], w=[tcres[gi]])
                        if kk_ % 2 == 0:
                            yield
                    B.op("dve", lambda e: e.tensor_copy(fv(0, 30), fv(512, 542)), w=[t_cfu[gi]])
                    yield

                def x_cfln():
                    psm, tpsm = getps(XB)
                    psq, tpsq = getps(XB)
                    for gi in range(2):
                        B.op("act", lambda e, gi=gi: e.activation(out=rkb, in_=cres[gi], func=AF.Copy),
                             r=[tcres[gi]], w=[tk["rkb"]])
                        B.op("pe", lambda e, gi=gi: e.matmul(psm[:, :], ones, rkb, start=(gi == 0), stop=(gi == 1)),
                             r=[tk["rkb"], t_c], w=[tpsm])
                        B.op("act", lambda e, gi=gi: e.activation(out=scr2, in_=cres[gi], func=AF.Square),
                             r=[tcres[gi]], w=[tk["scr2"]])
                        B.op("pe", lambda e, gi=gi: e.matmul(psq[:, :], ones, scr2, start=(gi == 0), stop=(gi == 1)),
                             r=[tk["scr2"], t_c], w=[tpsq])
                        yield
                    B.op("dve", lambda e: e.tensor_scalar(out=tA, in0=psm[:, :], scalar1=1.0 / 256, scalar2=None, op0=ALU.mult),
                         r=[tpsm], w=[tk["A"]])
                    B.op("dve", lambda e: e.tensor_tensor(out=tB, in0=tA, in1=tA, op=ALU.mult), r=[tk["A"]], w=[tk["B"]])
                    B.op("dve", lambda e: e.scalar_tensor_tensor(out=tB, in0=psq[:, :], scalar=1.0 / 256, in1=tB,
                                                                 op0=ALU.mult, op1=ALU.subtract),
                         r=[tpsq], w=[tk["B"]])
                    yield
                    B.op("act", lambda e: e.activation(out=tB, in_=tB, func=AF.Ln, bias=eps_ln), r=[t_c], w=[tk["B"]])
                    B.op("act", lambda e: e.activation(out=tB, in_=tB, func=AF.Exp, scale=-0.5), w=[tk["B"]])
                    yield
                    for gi in range(2):
                        B.op("dve", lambda e, gi=gi: e.tensor_tensor(out=cres[gi], in0=cres[gi], in1=tA, op=ALU.subtract),
                             r=[tk["A"]], w=[tcres[gi]])
                        B.op("dve", lambda e, gi=gi: e.tensor_tensor(out=cres[gi], in0=cres[gi], in1=tB, op=ALU.mult),
                             r=[tk["B"]], w=[tcres[gi]])
                        B.op("act", lambda e, gi=gi: e.activation(
                            out=mixcat[:, (6 + gi) * TT:(7 + gi) * TT], in_=cres[gi], func=AF.Silu,
                            scale=pvc("cflg", gi), bias=pvc("cflb", gi)), r=[tcres[gi], t_pv], w=[t_mix[6 + gi]])
                        yield

                def x_prep(hp):
                    S = {**SETS[hp % 2], **CH[hp]}
                    stk = {**SETS[hp % 2]["tk"], **CH[hp]["tk"]}
                    art, bt_, kt_, Bh, Kh, vb, bonus, gg, PCt = (S[n] for n in ["art", "bt", "kt", "Bh", "Kh", "vb", "bonus", "gg", "PC"])
                    art3 = art.rearrange("p (c x) -> p c x", c=8)
                    ps, tps = getps(XB)
                    B.op("pe", lambda e: e.matmul(ps[:, :], w2a2[0:64, hp * 128:(hp + 1) * 128], twa[0:64, :],
                                                  start=True, stop=True), r=[t_w2, tk["twa"]], w=[tps])
                    B.op("act", lambda e: e.activation(out=sgw, in_=ps[:, :], func=AF.Sigmoid, bias=pvc("w0", hp)),
                         r=[tps, t_pv], w=[tk["sgw"]])
                    ps2, tps2 = getps(XB)
                    B.op("pe", lambda e: e.matmul(ps2[:, :], w2a2[64:128, hp * 128:(hp + 1) * 128], twa[64:128, :],
                                                  start=True, stop=True), r=[t_w2, tk["twa"]], w=[tps2])
                    B.op("act", lambda e: e.activation(out=av, in_=ps2[:, :], func=AF.Sigmoid, bias=pvc("a0", hp)),
                         r=[tps2, t_pv], w=[tk["av"]])
                    ps3, tps3 = getps(XB)
                    B.op("pe", lambda e: e.matmul(ps3[:, :], g2[:, hp * 128:(hp + 1) * 128], tg,
                                                  start=True, stop=True), r=[t_g2, tk["tg"]], w=[tps3])
                    B.op("act", lambda e: e.activation(out=gg, in_=ps3[:, :], func=AF.Copy), r=[tps3], w=[stk["gg"]])
                    yield
                    for (which, dst, tkn) in [(0, r_t, "r"), (1, k_t, "k"), (2, v_t, "v")]:
                        g = which * 4 + hp

                        def rkv_cons(ps, tps, g=g, dst=dst, tkn=tkn):
                            ri = 0
                            rw = raw[ri]
                            B.op("dve", lambda e: e.tensor_copy(rw[:, 0:1], rkvcarry[:, g:g + 1]),
                                 r=[t_carry], w=[t_raw[ri]])
                            B.op("act", lambda e: e.activation(out=rw[:, 1:513], in_=ps[:, :], func=AF.Copy),
                                 r=[tps], w=[t_raw[ri]])
                            B.op("dve", lambda e: e.tensor_copy(rkvcarry[:, g:g + 1], rw[:, 512:513]),
                                 r=[t_raw[ri]], w=[t_carry])
                            B.op("dve", lambda e: e.tensor_scalar(out=dst, in0=rw[:, 0:512], scalar1=pvc("murkv", g),
                                                                  scalar2=None, op0=ALU.mult),
                                 r=[t_raw[ri], t_pv], w=[tk[tkn]])
                            B.op("dve", lambda e: e.scalar_tensor_tensor(
                                out=dst, in0=rw[:, 1:513], scalar=pdc("omurkv", g), in1=dst, op0=ALU.mult, op1=ALU.add),
                                r=[t_raw[ri], t_pd], w=[tk[tkn]])
                        proj_group(g, rkv_cons)
                        yield
                    Pb, Pexb, Pinvb, PCrb = S["VT"], S["BhT"], S["KhT"], S["TT"]
                    tPb, tPexb, tPinvb, tPCrb = stk["VT"], stk["BhT"], stk["KhT"], stk["TT"]
                    rn = raw[0][:, 0:512]
                    B.op("dve", lambda e: e.tensor_tensor_scan(tA, scanm, sgw, 0.0, ALU.mult, ALU.add),
                         r=[tk["sgw"], t_c], w=[tk["A"]])
                    B.op("dve", lambda e: e.tensor_scalar(out=tC, in0=k_t, scalar1=pvc("kk", hp), scalar2=None, op0=ALU.mult),
                         r=[tk["k"], t_pv], w=[tk["C"]])
                    B.op("act", lambda e: e.activation(out=rkb, in_=tC, func=AF.Square), r=[tk["C"]], w=[tk["rkb"]])
                    ps4, tps4 = getps(XB)
                    B.op("pe", lambda e: e.matmul(ps4[:, :], bones, rkb, start=True, stop=True),
                         r=[tk["rkb"], t_c], w=[tps4])
                    B.op("act", lambda e: e.activation(out=vb, in_=v_t, func=AF.Copy), r=[tk["v"]], w=[stk["vb"]])
                    yield
                    B.op("act", lambda e: e.activation(out=Pb, in_=tA, func=AF.Exp, scale=C0), r=[tk["A"]], w=[tPb])
                    B.op("act", lambda e: e.activation(out=Pinvb, in_=tA, func=AF.Exp, scale=-C0), r=[tk["A"]], w=[tPinvb])
                    B.op("act", lambda e: e.activation(out=PCt.rearrange("p (c x) -> p c x", x=1), in_=v3(tA)[:, :, 63:64],
                                                       func=AF.Exp, scale=C0), r=[tk["A"]], w=[stk["PC"]])
                    B.op("dve", lambda e: e.tensor_tensor(out=tD, in0=tA, in1=sgw, op=ALU.subtract),
                         r=[tk["A"], tk["sgw"]], w=[tk["D"]])
                    B.op("dve", lambda e: e.tensor_tensor(out=v3(tB), in0=v3(tA), in1=v3(tA)[:, :, 63:64].to_broadcast([P, 8, 64]),
                                                          op=ALU.subtract), r=[tk["A"]], w=[tk["B"]])
                    yield
                    B.op("act", lambda e: e.activation(out=Pexb, in_=tD, func=AF.Exp, scale=C0), r=[tk["D"]], w=[tPexb])
                    B.op("act", lambda e: e.activation(out=PCrb, in_=tB, func=AF.Exp, scale=-C0), r=[tk["B"]], w=[tPCrb])
                    B.op("act", lambda e: e.activation(out=rn, in_=ps4[:, :], func=AF.Ln, bias=smallc[:, 3:4]), r=[tps4, t_c], w=[t_raw[0]])
                    B.op("act", lambda e: e.activation(out=rn, in_=rn, func=AF.Exp, scale=-0.5), w=[t_raw[0]])
                    B.op("dve", lambda e: e.tensor_tensor(out=art3[:, :, 64:128], in0=v3(r_t), in1=v3(Pb), op=ALU.mult),
                         r=[tk["r"], tPb], w=[stk["art"]])
                    B.op("dve", lambda e: e.tensor_scalar(out=tE, in0=av, scalar1=pvc("ka", hp), scalar2=pdc("omka", hp),
                                                          op0=ALU.mult, op1=ALU.add),
                         r=[tk["av"], t_pv, t_pd], w=[tk["E"]])
                    yield
                    B.op("dve", lambda e: e.tensor_tensor(out=tE, in0=tE, in1=k_t, op=ALU.mult), r=[tk["k"]], w=[tk["E"]])
                    B.op("dve", lambda e: e.tensor_tensor(out=tC, in0=tC, in1=rn, op=ALU.mult), r=[t_raw[0]], w=[tk["C"]])
                    yield
                    B.op("dve", lambda e: e.scalar_tensor_tensor(out=art3[:, :, 0:64], in0=v3(tC), scalar=-1.0, in1=v3(Pexb),
                                                                 op0=ALU.mult, op1=ALU.mult),
                         r=[tk["C"], tPexb], w=[stk["art"]])
                    B.op("dve", lambda e: e.scalar_tensor_tensor(out=rkb, in0=r_t, scalar=pvc("rk", hp), in1=tE,
                                                                 op0=ALU.mult, op1=ALU.mult),
                         r=[tk["r"], tk["E"], t_pv], w=[tk["rkb"]])
                    ps5, tps5 = getps(XB)
                    B.op("pe", lambda e: e.matmul(ps5[:, :], bones, rkb, start=True, stop=True),
                         r=[tk["rkb"], t_c], w=[tps5])
                    yield
                    B.op("dve", lambda e: e.tensor_tensor(out=tC, in0=tC, in1=av, op=ALU.mult), r=[tk["av"]], w=[tk["C"]])
                    B.op("dve", lambda e: e.tensor_tensor(out=kt_, in0=tE, in1=Pinvb, op=ALU.mult),
                         r=[tk["E"], tPinvb], w=[stk["kt"]])
                    yield
                    B.op("dve", lambda e: e.tensor_tensor(out=Kh, in0=tE, in1=PCrb, op=ALU.mult),
                         r=[tk["E"], tPCrb], w=[stk["Kh"]])
                    B.op("dve", lambda e: e.tensor_tensor(out=bt_, in0=tC, in1=Pinvb, op=ALU.mult),
                         r=[tk["C"], tPinvb], w=[stk["bt"]])
                    yield
                    B.op("dve", lambda e: e.tensor_tensor(out=Bh, in0=tC, in1=PCrb, op=ALU.mult),
                         r=[tk["C"], tPCrb], w=[stk["Bh"]])
                    B.op("dve", lambda e: e.tensor_tensor(out=bonus, in0=ps5[:, :], in1=v_t, op=ALU.mult),
                         r=[tps5, tk["v"]], w=[stk["bonus"]])
                    yield

                def ysetup(hp):
                    S = {**SETS[hp % 2], **CH[hp]}
                    stk = {**SETS[hp % 2]["tk"], **CH[hp]["tk"]}
                    return S, stk

                def y2a(hp):
                    S, stk = ysetup(hp)
                    TK = lambda n: stk[n] if n in stk else tk[n]
                    art, bt_, kt_, Bh, Kh, vb = (S[n] for n in ["art", "bt", "kt", "Bh", "Kh", "vb"])
                    VT, BhT, KhT, LakT, LrbT, LrkT, TTf = (S[n] for n in ["VT", "BhT", "KhT", "LakT", "LrbT", "LrkT", "TT"])
                    for (src, sk, dst, dtk) in [(vb, "vb", VT, "VT"), (Bh, "Bh", BhT, "BhT"), (Kh, "Kh", KhT, "KhT")]:
                        ps, tps = getps(YB)
                        for c in range(8):
                            for e_ in range(2):
                                B.op("pe", lambda e, ps=ps, c=c, e_=e_, src=src: e.matmul(
                                    ps[eh[e_], c * 64:(c + 1) * 64], src[eh[e_], c * 64:(c + 1) * 64],
                                    ident[eh[e_], e_ * 64:(e_ + 1) * 64], start=True, stop=True,
                                    tile_position=(64 * e_, 64 * e_)), r=[stk[sk], t_c], w=[tps])
                        B.op("act", lambda e, ps=ps, dst=dst: e.activation(out=dst, in_=ps[:, :], func=AF.Copy),
                             r=[tps], w=[stk[dtk]])
                        yield
                    for (stat, sk, dA, dAk, dB, dBk) in [(bt_, "bt", Rm[0], "R0", LrbT, "LrbT"),
                                                        (kt_, "kt", LakT, "LakT", LrkT, "LrkT")]:
                        for half in range(2):
                            ps, tps = getps(YB)
                            for c4 in range(4):
                                c = half * 4 + c4
                                for e_ in range(2):
                                    B.op("pe", lambda e, ps=ps, c=c, c4=c4, e_=e_, stat=stat: e.matmul(
                                        ps[eh[e_], c4 * 128:(c4 + 1) * 128], stat[eh[e_], c * 64:(c + 1) * 64],
                                        art[eh[e_], c * 128:(c + 1) * 128], start=True, stop=True,
                                        tile_position=(64 * e_, 64 * e_)), r=[stk[sk], stk["art"]], w=[tps])
                            ps3 = ps[:, :].rearrange("p (c x) -> p c x", c=4)
                            B.op("dve", lambda e, ps3=ps3, dA=dA, half=half: e.tensor_tensor(
                                out=v3(dA)[:, half * 4:(half + 1) * 4, :], in0=ps3[:, :, 0:64], in1=mk3(msu, 4), op=ALU.mult),
                                r=[tps, t_c], w=[TK(dAk)])
                            B.op("dve", lambda e, ps3=ps3, dB=dB, half=half: e.tensor_tensor(
                                out=v3(dB)[:, half * 4:(half + 1) * 4, :], in0=ps3[:, :, 64:128], in1=mk3(miu, 4), op=ALU.mult),
                                r=[tps, t_c], w=[TK(dBk)])
                            yield
                    ps, tps = getps(YB)
                    for c in range(8):
                        for e_ in range(2):
                            B.op("pe", lambda e, ps=ps, c=c, e_=e_: e.matmul(
                                ps[eh[e_], c * 64:(c + 1) * 64], art[eh[e_], c * 128: c * 128 + 64],
                                bt_[eh[e_], c * 64:(c + 1) * 64], start=True, stop=True,
                                tile_position=(64 * e_, 64 * e_)), r=[stk["bt"], stk["art"]], w=[tps])
                    B.op("dve", lambda e, ps=ps: e.tensor_tensor(out=Lm[0], in0=ps[:, :], in1=msl, op=ALU.mult),
                         r=[tps, t_c], w=[tk["L0"]])
                    B.op("dve", lambda e: e.tensor_tensor(out=Gm[0], in0=Rm[0], in1=irep, op=ALU.add),
                         r=[tk["R0"], t_c], w=[tk["G0"]])
                    yield
                    cur = 0
                    for lev in range(1, 6):
                        nxt = 1 - cur
                        Rc, Lc, Gc = Rm[cur], Lm[cur], Gm[cur]
                        Rn, Ln, Gn = Rm[nxt], Lm[nxt], Gm[nxt]
                        tRc, tLc, tGc = tk["R%d" % cur], tk["L%d" % cur], tk["G%d" % cur]
                        tRn, tLn, tGn = tk["R%d" % nxt], tk["L%d" % nxt], tk["G%d" % nxt]
                        if lev == 5:
                            Gn, tGn = TTf, stk["TT"]
                        ps, tps = getps(YB)
                        for c in range(8):
                            for e_ in range(2):
                                sl = slice(c * 64, (c + 1) * 64)
                                B.op("pe", lambda e, ps=ps, sl=sl, e_=e_: e.matmul(
                                    ps[eh[e_], sl], Rc[eh[e_], sl], Lc[eh[e_], sl], start=True, stop=True,
                                    tile_position=(64 * e_, 64 * e_)), r=[tRc, tLc], w=[tps])
                        B.op("act", lambda e, ps=ps: e.activation(out=Ln, in_=ps[:, :], func=AF.Copy),
                             r=[tps], w=[tLn])
                        if lev < 5:
                            ps2, tps2 = getps(YB)
                            for c in range(8):
                                for e_ in range(2):
                                    sl = slice(c * 64, (c + 1) * 64)
                                    B.op("pe", lambda e, ps2=ps2, sl=sl, e_=e_: e.matmul(
                                        ps2[eh[e_], sl], Lc[eh[e_], sl], Rc[eh[e_], sl], start=True, stop=True,
                                        tile_position=(64 * e_, 64 * e_)), r=[tRc, tLc], w=[tps2])
                            B.op("dve", lambda e, ps2=ps2: e.tensor_copy(Rn, ps2[:, :]), r=[tps2], w=[tRn])
                        yield
                        ps3_, tps3 = getps(YB)
                        for c in range(8):
                            for e_ in range(2):
                                sl = slice(c * 64, (c + 1) * 64)
                                B.op("pe", lambda e, ps3_=ps3_, sl=sl, e_=e_: e.matmul(
                                    ps3_[eh[e_], sl], Ln[eh[e_], sl], Gc[eh[e_], sl], start=True, stop=True,
                                    tile_position=(64 * e_, 64 * e_)), r=[tLn, tGc], w=[tps3])
                        B.op("dve", lambda e, ps3_=ps3_: e.tensor_tensor(out=Gn, in0=ps3_[:, :], in1=Gc, op=ALU.add),
                             r=[tps3, tGc], w=[tGn])
                        yield
                        cur = nxt

                PSY = {}

                def y_chain(hp):
                    S, stk = ysetup(hp)
                    art, bonus, gg, PCt = (S[n] for n in ["art", "bonus", "gg", "PC"])
                    VT, BhT, KhT, LakT, LrbT, LrkT, TTm, Xb, Ub = (S[n] for n in ["VT", "BhT", "KhT", "LakT", "LrbT", "LrkT", "TT", "Xb", "Ub"])
                    tTT = stk["TT"]
                    YB = (0, 1, 2, 3)
                    psY, tpsY = getps((4 + hp,), hold=True)
                    PSY[hp] = (psY, tpsY)
                    Sb = STb[:, hp * 64:(hp + 1) * 64]
                    S32 = ST32[:, hp * 64:(hp + 1) * 64]
                    tS = t_ST[hp]
                    for c in range(8):
                        sl = slice(c * 64, (c + 1) * 64)
                        psX, tpsX = getps(YB)
                        for e_ in range(2):
                            B.op("pe", lambda e, e_=e_: e.matmul(
                                psX[eh[e_], 0:64], art[eh[e_], c * 128:c * 128 + 64], Sb[eh[e_], :], start=True, stop=False,
                                tile_position=(64 * e_, 64 * e_)), r=[stk["art"], tS], w=[tpsX])
                            B.op("pe", lambda e, e_=e_: e.matmul(
                                psX[eh[e_], 0:64], LakT[eh[e_], sl], VT[eh[e_], sl], start=False, stop=True,
                                tile_position=(64 * e_, 64 * e_)), r=[stk["LakT"], stk["VT"]], w=[tpsX])
                        B.op("act", lambda e: e.activation(out=Xb, in_=psX[:, 0:64], func=AF.Copy),
                             r=[tpsX], w=[stk["Xb"]])
                        yield
                        psU, tpsU = getps(YB)
                        for e_ in range(2):
                            B.op("pe", lambda e, e_=e_: e.matmul(
                                psU[eh[e_], 0:64], TTm[eh[e_], sl], Xb[eh[e_], :], start=True, stop=True,
                                tile_position=(64 * e_, 64 * e_)), r=[tTT, stk["Xb"]], w=[tpsU])
                        B.op("dve", lambda e: e.tensor_copy(Ub, psU[:, 0:64]), r=[tpsU], w=[stk["Ub"]])
                        yield
                        for e_ in range(2):
                            B.op("pe", lambda e, e_=e_: e.matmul(
                                psY[eh[e_], sl], Sb[eh[e_], :], art[eh[e_], c * 128 + 64:(c + 1) * 128], start=True, stop=False,
                                tile_position=(64 * e_, 64 * e_)), r=[tS, stk["art"]], w=[tpsY])
                            B.op("pe", lambda e, e_=e_: e.matmul(
                                psY[eh[e_], sl], Ub[eh[e_], :], LrbT[eh[e_], sl], start=False, stop=False,
                                tile_position=(64 * e_, 64 * e_)), r=[stk["Ub"], stk["LrbT"]], w=[tpsY])
                            B.op("pe", lambda e, e_=e_: e.matmul(
                                psY[eh[e_], sl], VT[eh[e_], sl], LrkT[eh[e_], sl], start=False, stop=True,
                                tile_position=(64 * e_, 64 * e_)), r=[stk["VT"], stk["LrkT"]], w=[tpsY])
                        psS, tpsS = getps(YB)
                        for e_ in range(2):
                            B.op("pe", lambda e, e_=e_: e.matmul(
                                psS[eh[e_], 0:64], BhT[eh[e_], sl], Ub[eh[e_], :], start=True, stop=False,
                                tile_position=(64 * e_, 64 * e_)), r=[stk["BhT"], stk["Ub"]], w=[tpsS])
                            B.op("pe", lambda e, e_=e_: e.matmul(
                                psS[eh[e_], 0:64], KhT[eh[e_], sl], VT[eh[e_], sl], start=False, stop=True,
                                tile_position=(64 * e_, 64 * e_)), r=[stk["KhT"], stk["VT"]], w=[tpsS])
                        B.op("dve", lambda e: e.scalar_tensor_tensor(
                            out=S32, in0=S32, scalar=PCt[:, c:c + 1], in1=psS[:, 0:64], op0=ALU.mult, op1=ALU.add),
                            r=[tpsS, stk["PC"]], w=[tS])
                        B.op("act", lambda e: e.activation(out=Sb, in_=S32, func=AF.Copy), w=[tS])
                        yield

                def y_tail(hp):
                    S, stk = ysetup(hp)
                    bonus, gg, VT, BhT = (S[n] for n in ["bonus", "gg", "VT", "BhT"])
                    YB = (0, 1, 2, 3)
                    psY, tpsY = PSY[hp]
                    B.op("act", lambda e: e.activation(out=gA, in_=psY[:, :], func=AF.Copy), r=[tpsY], w=[tk["gA"]])
                    B.op("act", lambda e: e.activation(out=VT, in_=psY[:, :], func=AF.Copy), r=[tpsY], w=[stk["VT"]])
                    B.op("act", lambda e: e.activation(out=BhT, in_=psY[:, :], func=AF.Square), r=[tpsY], w=[stk["BhT"]])
                    release(psY)
                    yield
                    psm, tpsm = getps(YB)
                    psq, tpsq = getps(YB)
                    B.op("pe", lambda e: e.matmul(psm[:, :], bones, VT, start=True, stop=True),
                         r=[stk["VT"], t_c], w=[tpsm])
                    B.op("pe", lambda e: e.matmul(psq[:, :], bones, BhT, start=True, stop=True),
                         r=[stk["BhT"], t_c], w=[tpsq])
                    B.op("dve", lambda e: e.tensor_scalar(out=gB, in0=psm[:, :], scalar1=1.0 / 64, scalar2=None, op0=ALU.mult),
                         r=[tpsm], w=[tk["gB"]])
                    B.op("dve", lambda e: e.tensor_tensor(out=gC, in0=gB, in1=gB, op=ALU.mult), r=[tk["gB"]], w=[tk["gC"]])
                    B.op("dve", lambda e: e.scalar_tensor_tensor(out=gC, in0=psq[:, :], scalar=1.0 / 64, in1=gC,
                                                                 op0=ALU.mult, op1=ALU.subtract),
                         r=[tpsq], w=[tk["gC"]])
                    yield
                    B.op("act", lambda e: e.activation(out=gC, in_=gC, func=AF.Ln, bias=eps_gn), r=[t_c], w=[tk["gC"]])
                    B.op("act", lambda e: e.activation(out=gC, in_=gC, func=AF.Exp, scale=-0.5), w=[tk["gC"]])
                    B.op("pool", lambda e: e.tensor_tensor(out=gA, in0=gA, in1=gB, op=ALU.subtract), r=[tk["gB"]], w=[tk["gA"]])
                    yield
                    B.op("pool", lambda e: e.tensor_tensor(out=gA, in0=gA, in1=gC, op=ALU.mult), r=[tk["gC"]], w=[tk["gA"]])
                    B.op("pool", lambda e: e.tensor_scalar(out=gA, in0=gA, scalar1=pvc("lng", hp), scalar2=pvc("lnb", hp),
                                                          op0=ALU.mult, op1=ALU.add), r=[t_pv], w=[tk["gA"]])
                    yield
                    B.op("pool", lambda e: e.tensor_tensor(out=gA, in0=gA, in1=bonus, op=ALU.add), r=[stk["bonus"]], w=[tk["gA"]])
                    B.op("pool", lambda e: e.tensor_tensor(out=mixcat[:, hp * TT:(hp + 1) * TT], in0=gA, in1=gg, op=ALU.mult),
                         r=[tk["gA"], stk["gg"]], w=[t_mix[hp]])
                    yield

                def chain_gens(gs):
                    for g_ in gs:
                        yield from g_

                def interleave(gs):
                    gs = list(gs)
                    while gs:
                        for g_ in list(gs):
                            try:
                                next(g_)
                            except StopIteration:
                                gs.remove(g_)
                for _ in x_lowrank():
                    pass
                for _ in x_prep(0):
                    pass
                for hp in range(1, 4):
                    interleave([x_prep(hp), y2a(hp - 1)])
                interleave([chain_gens([x_sc(0), x_sc(1)]), y2a(3)])
                for dg in range(2):
                    B.dma("pool", wos[dg][:, :], wo_d[l, dg], w=[t_wos[dg]])
                XB = (0, 1, 2, 3)
                interleave([y_chain(0), y_chain(1), y_chain(2), y_chain(3), chain_gens([x_cf(0), x_cf(1), x_cfln()])])
                for hp in range(4):
                    for _ in y_tail(hp):
                        pass
                B.barrier()

                if dbg and l == 0 and tt == 0:
                    B.dma("pool", dbg_d[:, :], mixcat[:, :], r=t_mix)

                pss, tss = getps(hold=True)
                for dg in range(KC):
                    si = dg % 2
                    if dg >= 2:
                        B.dma("pool", wos[si][:, :], wo_d[l, dg], w=[t_wos[si]])
                    ps, tps = getps()
                    for kc in range(KC):
                        B.op("pe", lambda e, ps=ps, kc=kc, si=si: e.matmul(
                            ps[:, :], wos[si][:, kc * 128:(kc + 1) * 128], mixcat[:, kc * TT:(kc + 1) * TT],
                            start=(kc == 0), stop=(kc == KC - 1)), r=[t_wos[si], t_mix[kc]], w=[tps])
                    B.op("act", lambda e, ps=ps, dg=dg: e.activation(out=fo[:, dg * TT:(dg + 1) * TT], in_=ps[:, :], func=AF.Copy),
                         r=[tps], w=[t_fo[dg]])
                    i = dg % 2
                    B.op("act", lambda e, ps=ps, i=i: e.activation(out=NORM["sq"][i], in_=ps[:, :], func=AF.Square),
                         r=[tps], w=[NORM["t_sq"][i]])
                    B.op("pe", lambda e, i=i, dg=dg, pss=pss: e.matmul(pss[:, :], ones, NORM["sq"][i], start=(dg == 0), stop=(dg == KC - 1)),
                         r=[NORM["t_sq"][i], t_c], w=[tss])
                release(pss)
                post_norm_residual(tt, "postmix", pss, tss)

                B.barrier()
                ar.off = base_off
                act_t = ar.alloc(NJ * TT, BF16)
                t_act = [Tok() for _ in range(NJ)]
                NGUS, NWDS = 5, 3
                gus = [ar.alloc(2 * KC * 128, BF16) for _ in range(NGUS)]
                t_gus = [Tok() for _ in range(NGUS)]
                wds = [ar.alloc(NJ * 128, BF16) for _ in range(NWDS)]
                t_wds = [Tok() for _ in range(NWDS)]
                sil = [ar.alloc(TT, F32), ar.alloc(TT, F32)]
                t_sil = [Tok(), Tok()]
                for dg in range(NWDS):
                    B.dma("pool", wds[dg], wd_d[l, dg], w=[t_wds[dg]])
                rmsnorm_to_hT(tt, "preffn")
                for j in range(NJ):
                    si = rr("gus", NGUS)
                    B.dma("pool", gus[si][:, 0:KC * 128], wg_d[l, j], w=[t_gus[si]])
                    B.dma("pool", gus[si][:, KC * 128:2 * KC * 128], wu_d[l, j], w=[t_gus[si]])
                    psg, tpsg = getps()
                    psu, tpsu = getps()
                    for kc in range(KC):
                        B.op("pe", lambda e, kc=kc, si=si, psg=psg: e.matmul(
                            psg[:, :], gus[si][:, kc * 128:(kc + 1) * 128], hs(kc, 1, 513),
                            start=(kc == 0), stop=(kc == KC - 1)), r=[t_gus[si], t_hT], w=[tpsg])
                    for kc in range(KC):
                        B.op("pe", lambda e, kc=kc, si=si, psu=psu: e.matmul(
                            psu[:, :], gus[si][:, (KC + kc) * 128:(KC + kc + 1) * 128], hs(kc, 1, 513),
                            start=(kc == 0), stop=(kc == KC - 1)), r=[t_gus[si], t_hT], w=[tpsu])
                    i = j % 2
                    B.op("act", lambda e, i=i, psg=psg: e.activation(out=sil[i], in_=psg[:, :], func=AF.Silu),
                         r=[tpsg], w=[t_sil[i]])
                    B.op("dve", lambda e, i=i, j=j, psu=psu: e.tensor_tensor(
                        out=act_t[:, j * TT:(j + 1) * TT], in0=psu[:, :], in1=sil[i], op=ALU.mult),
                        r=[tpsu, t_sil[i]], w=[t_act[j]])
                pss, tss = getps(hold=True)
                for dg in range(KC):
                    si = dg % NWDS
                    if dg >= NWDS:
                        B.dma("pool", wds[si], wd_d[l, dg], w=[t_wds[si]])
                    ps, tps = getps()
                    for j in range(NJ):
                        B.op("pe", lambda e, ps=ps, j=j, si=si: e.matmul(
                            ps[:, :], wds[si][:, j * 128:(j + 1) * 128], act_t[:, j * TT:(j + 1) * TT],
                            start=(j == 0), stop=(j == NJ - 1)), r=[t_wds[si], t_act[j]], w=[tps])
                    B.op("act", lambda e, ps=ps, dg=dg: e.activation(out=fo[:, dg * TT:(dg + 1) * TT], in_=ps[:, :], func=AF.Copy),
                         r=[tps], w=[t_fo[dg]])
                    i = dg % 2
                    B.op("act", lambda e, ps=ps, i=i: e.activation(out=NORM["sq"][i], in_=ps[:, :], func=AF.Square),
                         r=[tps], w=[NORM["t_sq"][i]])
                    B.op("pe", lambda e, i=i, dg=dg, pss=pss: e.matmul(pss[:, :], ones, NORM["sq"][i], start=(dg == 0), stop=(dg == KC - 1)),
                         r=[NORM["t_sq"][i], t_c], w=[tss])
                release(pss)
                post_norm_residual(tt, "postffn", pss, tss)

        t_out = Tok()
        for tt in range(NT):
            for kc in range(KC):
                B.dma("sp", y_d[:, kc * T + tt * TT: kc * T + (tt + 1) * TT], xs(kc, tt), r=[t_x[tt]], w=[t_out])
        B.barrier()

        with nc.Block() as block:
            B.emit(block)
    return nc


def _consts():
    c = np.zeros((P, NCF), np.float32)
    p = np.arange(P)[:, None]
    c[:, CF["ident"]:CF["ident"] + 128] = (p == np.arange(128)[None, :])
    c[:, CF["ones"]:CF["ones"] + 128] = 1.0
    c[:, CF["bones"]:CF["bones"] + 128] = ((p // 64) == (np.arange(128)[None, :] // 64))
    col = (np.arange(512) % 64)[None, :]
    row = p % 64
    c[:, CF["msu"]:CF["msu"] + 512] = (row < col)
    c[:, CF["miu"]:CF["miu"] + 512] = (row <= col)
    c[:, CF["msl"]:CF["msl"] + 512] = (col < row)
    c[:, CF["irep"]:CF["irep"] + 512] = (row == col)
    c[:, CF["scan"]:CF["scan"] + 512] = (col != 0) * np.ones((P, 1))
    return c


def _colvec(v):
    return np.ascontiguousarray(np.asarray(v, np.float32).reshape(-1, P).T)


def _prep_inputs(inp, L):
    f = lambda a: np.asarray(a, np.float32)
    out = {}

    def grp_cols(w, ncol_groups):
        Lw, K, N = w.shape
        a = w.reshape(Lw, K // P, P, N // P, P)
        a = a.transpose(0, 3, 2, 1, 4)
        return np.ascontiguousarray(a.reshape(Lw, N // P, P, (K // P) * P))
    out["w_in"] = grp_cols(f(inp["w_in"])[:L], NJ)
    out["w_gate"] = grp_cols(f(inp["w_gate"])[:L], NJ)
    out["w_up"] = grp_cols(f(inp["w_up"])[:L], NJ)
    out["w_o"] = grp_cols(f(inp["w_o"])[:L], KC)
    out["w_down"] = grp_cols(f(inp["w_down"])[:L], KC)
    wl = np.concatenate([f(inp["w1"]), f(inp["a1"]), f(inp["g1"])], axis=2)[:L]
    wl = wl.reshape(L, KC, P, 256).transpose(0, 2, 1, 3).reshape(L, P, KC * 256)
    out["wlr"] = np.ascontiguousarray(wl)
    out["w2a2"] = np.ascontiguousarray(np.concatenate([f(inp["w2"]), f(inp["a2"])], axis=1)[:L])
    out["g2"] = np.ascontiguousarray(f(inp["g2"])[:L])
    pv = np.zeros((L, P, NPV), np.float32)
    for l in range(L):
        def put(name, arr):
            a = _colvec(arr)
            pv[l, :, PV[name]:PV[name] + a.shape[1]] = a
        put("premix", inp["pre_mix_g"][l]); put("postmix", inp["post_mix_g"][l])
        put("preffn", inp["pre_ffn_g"][l]); put("postffn", inp["post_ffn_g"][l])
        put("muw", inp["mu_wag"][l][0]); put("mua", inp["mu_wag"][l][1]); put("mug", inp["mu_wag"][l][2])
        put("murkv", inp["mu_rkv"][l])
        put("w0", inp["w0"][l]); put("a0", inp["a0"][l]); put("kk", inp["k_k"][l]); put("ka", inp["k_a"][l])
        put("rk", np.asarray(inp["r_k"][l]).reshape(-1)); put("lng", inp["lnx_g"][l]); put("lnb", inp["lnx_b"][l])
        scw = f(inp["sc_conv_w"][l])
        for k_ in range(3):
            pv[l, :, PV["scw"] + k_ * 2: PV["scw"] + k_ * 2 + 2] = _colvec(scw[k_])
        cfw = f(inp["cf_conv_w"][l])
        for k_ in range(31):
            pv[l, :, PV["cfw"] + k_ * 2: PV["cfw"] + k_ * 2 + 2] = _colvec(cfw[k_])
        put("cfb", inp["cf_conv_b"][l]); put("cflg", inp["cf_ln_g"][l]); put("cflb", inp["cf_ln_b"][l])
    out["pv"] = pv
    out["cf"] = _consts()
    return out


_CACHE = {}


def run(inputs, L=4, dbg=False):
    x = np.asarray(inputs["x"], np.float32)
    shared = _prep_inputs(inputs, L)
    key = (L, dbg)
    if key not in _CACHE:
        _CACHE[key] = build_program(L, dbg)
    nc = _CACHE[key]
    in_maps = []
    for b in range(NCORES):
        xt = np.ascontiguousarray(x[b].T.reshape(KC, P, T).transpose(1, 0, 2).reshape(P, KC * T))
        m = dict(shared)
        m["xT"] = xt
        in_maps.append(m)
    res = run_bass_kernel_spmd(nc, in_maps, core_ids=list(range(NCORES)))
    outs = []
    for b in range(NCORES):
        yt = np.asarray(res.results[b]["yT"]).reshape(P, KC, T).transpose(1, 0, 2).reshape(D, T)
        outs.append(yt.T)
    y = np.stack(outs, 0).astype(np.float32)
    if dbg:
        return y, [np.asarray(r["dbg"]) for r in res.results]
    return y


def kernel(**inputs):
    return run(inputs, L=4)
```

```python
import contextlib
import numpy as np
import concourse.bass as bass
import concourse.mybir as mybir
from concourse.bass_utils import run_bass_kernel_spmd

F32 = mybir.dt.float32
BF16 = mybir.dt.bfloat16
AF = mybir.ActivationFunctionType
ALU = mybir.AluOpType

P = 128
T = 2048
D = 1024
TT = 512
NT = T // TT
KC = 8
NJ = 22
NCORES = 8
C0 = -0.6065306597126334
SAME_ENGINE_SYNC = True

PV = {}
_o = 0
for _n, _c in [("premix", 8), ("postmix", 8), ("preffn", 8), ("postffn", 8), ("muw", 8), ("mua", 8),
               ("mug", 8), ("murkv", 12), ("w0", 4), ("a0", 4), ("kk", 4), ("ka", 4), ("rk", 4),
               ("lng", 4), ("lnb", 4), ("scw", 6), ("cfw", 62), ("cfb", 2), ("cflg", 2), ("cflb", 2)]:
    PV[_n] = _o
    _o += _c
NPV = _o
PD = {"omuw": 0, "omua": 8, "omug": 16, "omurkv": 24, "omka": 36}
NPD = 40
CF = {"ident": 0, "ones": 128, "bones": 256, "msu": 384, "miu": 896, "msl": 1408, "irep": 1920, "scan": 2432}
NCF = 2944


class Tok:
    __slots__ = ("w", "r")

    def __init__(self):
        self.w = None
        self.r = []


class _Rec:
    def __init__(self):
        self.calls = []

    def __getattr__(self, name):
        def f(*a, **k):
            self.calls.append((name, a, k))
        return f


class Builder:
    ENGS = ("pe", "act", "dve", "pool", "sp")

    def __init__(self, nc, es, ndma=48):
        self.nc = nc
        self.q = {e: [] for e in self.ENGS}
        self.cnt = {e: 0 for e in self.ENGS}
        self.seen = {e: {} for e in self.ENGS}
        self.sems = {}
        for e in ("pe", "act", "dve", "pool"):
            self.sems[e] = es.enter_context(nc.semaphore("s_" + e))
        self.ndma = ndma
        self.dma_uses = [0] * ndma
        self.dma_next = 0
        for k in range(ndma):
            self.sems["dma%d" % k] = es.enter_context(nc.semaphore("s_dma%d" % k))

    def _collect(self, r, w):
        deps = {}

        def add(d):
            if d is None:
                return
            k, v = d
            if deps.get(k, 0) < v:
                deps[k] = v
        for t in r:
            add(t.w)
        for t in w:
            add(t.w)
            for d in t.r:
                add(d)
        return deps

    def _waits(self, eng, deps):
        out = []
        seen = self.seen[eng]
        for k, v in deps.items():
            if k == eng and (eng == "pe" or not SAME_ENGINE_SYNC):
                continue
            if seen.get(k, 0) >= v:
                continue
            seen[k] = v
            out.append((k, v))
        return out

    def op(self, eng, fn, r=(), w=()):
        rec = _Rec()
        fn(rec)
        assert len(rec.calls) == 1
        call = rec.calls[0]
        fn = lambda e, call=call: getattr(e, call[0])(*call[1], **call[2])
        deps = self._collect(r, w)
        waits = self._waits(eng, deps)
        self.cnt[eng] += 1
        me = (eng, self.cnt[eng])
        self.q[eng].append((fn, waits, (eng, 1)))
        for t in r:
            t.r.append(me)
        for t in w:
            t.w = me
            t.r = []

    def dma(self, qe, out_ap, in_ap, r=(), w=()):
        deps = self._collect(r, w)
        k = self.dma_next
        self.dma_next = (k + 1) % self.ndma
        key = "dma%d" % k
        if self.dma_uses[k] > 0:
            v = 16 * self.dma_uses[k]
            if deps.get(key, 0) < v:
                deps[key] = v
        waits = self._waits(qe, deps)
        self.dma_uses[k] += 1
        me = (key, 16 * self.dma_uses[k])
        self.q[qe].append((lambda e: e.dma_start(out=out_ap, in_=in_ap), waits, (key, 16)))
        for t in r:
            t.r.append(me)
        for t in w:
            t.w = me
            t.r = []

    def wait_only(self, eng, toks):
        deps = self._collect(toks, ())
        waits = self._waits(eng, deps)
        self.q[eng].append((None, waits, None))

    def barrier(self):
        allv = {e: self.cnt[e] for e in ("pe", "act", "dve", "pool") if self.cnt[e] > 0}
        for k in range(self.ndma):
            if self.dma_uses[k] > 0:
                allv["dma%d" % k] = 16 * self.dma_uses[k]
        for e in self.ENGS:
            deps = {k: v for k, v in allv.items() if k != e}
            waits = self._waits(e, deps)
            if waits:
                self.q[e].append((None, waits, None))

    def emit(self, block):
        sems = self.sems

        def run(e, lst):
            for fn, waits, inc in lst:
                for k, v in waits:
                    e.wait_ge(sems[k], v)
                if fn is not None:
                    ins = fn(e)
                    if inc is not None:
                        ins.then_inc(sems[inc[0]], inc[1])

        @block.tensor
        def _(e):
            run(e, self.q["pe"])

        @block.scalar
        def _(e):
            run(e, self.q["act"])

        @block.vector
        def _(e):
            run(e, self.q["dve"])

        @block.gpsimd
        def _(e):
            run(e, self.q["pool"])

        @block.sync
        def _(e):
            run(e, self.q["sp"])


class Arena:
    def __init__(self, ap, nbytes):
        self.ap = ap
        self.n = nbytes
        self.off = 0

    def alloc(self, cols, dtype):
        sz = cols * (4 if dtype == F32 else 2)
        sz = (sz + 3) // 4 * 4
        assert self.off + sz <= self.n, ("arena overflow", self.off, sz, self.n)
        a = self.ap[:, self.off // 4:(self.off + sz) // 4]
        self.off += sz
        if dtype != F32:
            a = a.bitcast(dtype)
            a = a[:, 0:cols]
        return a


def build_program(L=4, dbg=False):
    nc = bass.Bass("TRN2", target_bir_lowering=False)
    dt_in = lambda n, s: nc.dram_tensor(n, s, F32, kind="ExternalInput").ap()
    x_d = dt_in("xT", [P, KC * T])
    win_d = dt_in("w_in", [L, NJ, P, KC * 128])
    wg_d = dt_in("w_gate", [L, NJ, P, KC * 128])
    wu_d = dt_in("w_up", [L, NJ, P, KC * 128])
    wo_d = dt_in("w_o", [L, KC, P, KC * 128])
    wd_d = dt_in("w_down", [L, KC, P, NJ * 128])
    wlr_d = dt_in("wlr", [L, P, KC * 256])
    w2a2_d = dt_in("w2a2", [L, P, 512])
    g2_d = dt_in("g2", [L, P, 512])
    pv_d = dt_in("pv", [L, P, NPV])
    cf_d = dt_in("cf", [P, NCF])
    y_d = nc.dram_tensor("yT", [P, KC * T], F32, kind="ExternalOutput").ap()
    if dbg:
        dbg_d = nc.dram_tensor("dbg", [P, KC * TT], F32, kind="ExternalOutput").ap()

    with contextlib.ExitStack() as es:
        def sb(name, cols, dtype):
            return es.enter_context(nc.sbuf_tensor(name, [P, cols], dtype))
        xT = sb("xT_sb", KC * T, F32)
        pv = sb("pv_sb", NPV, F32)
        pd = sb("pd_sb", NPD, F32)
        cb = sb("cb", NCF, BF16)
        cmask = cb[:, 384:NCF]
        smallc = sb("smallc", 4, F32)
        wlra = sb("wlra", KC * 256, BF16)
        wlrb = sb("wlrb", KC * 256, BF16)
        w2a2 = sb("w2a2_sb", 512, BF16)
        g2 = sb("g2_sb", 512, BF16)
        rkvcarry = sb("rkvcarry", 12, F32)
        cu = sb("cu", 2 * 514, F32)
        cfu = sb("cfu", 2 * 542, F32)
        ST32 = sb("ST32", 4 * 64, F32)
        STb = sb("STb", 4 * 64, BF16)
        hT = sb("hT", KC * 513, BF16)
        hcarry = sb("hcarry", KC, BF16)
        mixcat = sb("mixcat", KC * TT, BF16)
        fo = sb("fo", KC * TT, F32)
        wlr = fo[:, 0:KC * 128].bitcast(BF16)
        wos = [sb("wos%d" % i, KC * 128, BF16) for i in range(2)]
        ARENA_BYTES = 80 * 1024
        arena_t = sb("arena", ARENA_BYTES // 4, F32)
        psum = [es.enter_context(nc.psum_tensor("ps%d" % i, [P, 512], F32)) for i in range(8)]

        B = Builder(nc, es)
        pst = [Tok() for _ in range(8)]
        ps_rr = [0]

        held = set()

        ALLB = tuple(range(8))
        ps_ptr = {}

        def getps(banks=None, hold=False):
            key = banks or ALLB
            while True:
                p_ = ps_ptr.get(key, 0)
                ps_ptr[key] = p_ + 1
                i = key[p_ % len(key)]
                if i not in held:
                    break
            if hold:
                held.add(i)
            return psum[i], pst[i]

        def release(ps):
            for i in range(8):
                if psum[i] is ps:
                    held.discard(i)

        t_x = [[Tok() for _ in range(KC)] for _ in range(NT)]
        t_pv, t_pd, t_c, t_wlr, t_wlrab, t_w2, t_g2 = Tok(), Tok(), Tok(), Tok(), Tok(), Tok(), Tok()
        t_carry, t_cu, t_cfu = Tok(), [Tok(), Tok()], [Tok(), Tok()]
        t_ST = [Tok() for _ in range(4)]
        t_hT, t_mix, t_fo = [Tok() for _ in range(KC)], [Tok() for _ in range(KC)], [Tok() for _ in range(KC)]
        t_wos = [Tok(), Tok()]
        t_hc = [Tok() for _ in range(KC)]

        def pvc(name, i=0):
            o = PV[name] + i
            return pv[:, o:o + 1]

        def pdc(name, i=0):
            o = PD[name] + i
            return pd[:, o:o + 1]

        ident = cb[:, 0:128]
        ones = cb[:, 128:256]
        bones = cb[:, 256:384]
        msu = cmask[:, 0:512]
        miu = cmask[:, 512:1024]
        msl = cmask[:, 1024:1536]
        irep = cmask[:, 1536:2048]
        scanm = cmask[:, 2048:2560]
        eps_n = smallc[:, 0:1]
        eps_ln = smallc[:, 1:2]
        eps_gn = smallc[:, 2:3]

        def xs(kc, tt):
            return xT[:, kc * T + tt * TT: kc * T + (tt + 1) * TT]

        def hs(kc, a, b):
            return hT[:, kc * 513 + a: kc * 513 + b]

        for kc in range(KC):
            for tt in range(NT):
                B.dma("sp", xs(kc, tt), x_d[:, kc * T + tt * TT: kc * T + (tt + 1) * TT], w=[t_x[tt][kc]])
        B.dma("pool", cb[:, :], cf_d[:, :], w=[t_c])
        B.op("dve", lambda e: e.memset(smallc[:, 0:1], 1e-6), w=[t_c])
        B.op("dve", lambda e: e.memset(smallc[:, 1:2], 1e-5), w=[t_c])
        B.op("dve", lambda e: e.memset(smallc[:, 2:3], 64e-5), w=[t_c])
        B.op("dve", lambda e: e.memset(smallc[:, 3:4], 1e-24), w=[t_c])

        wslot_rr = {}

        def rr(name, n):
            i = wslot_rr.get(name, 0)
            wslot_rr[name] = (i + 1) % n
            return i

        def rmsnorm_to_hT(tt, gname):
            pss, tss = getps()
            sq, t_sq = NORM["sq"], NORM["t_sq"]
            for kc in range(KC):
                i = kc % 2
                B.op("act", lambda e, kc=kc, i=i: e.activation(out=sq[i], in_=xs(kc, tt), func=AF.Square),
                     r=[t_x[tt][kc]], w=[t_sq[i]])
                B.op("pe", lambda e, kc=kc, i=i: e.matmul(pss[:, :], ones, sq[i], start=(kc == 0), stop=(kc == KC - 1)),
                     r=[t_sq[i], t_c], w=[tss])
            rstd, t_rstd = NORM["rstd"], NORM["t_rstd"]
            B.op("act", lambda e: e.activation(out=rstd, in_=pss[:, :], func=AF.Ln, scale=1.0 / D, bias=eps_n),
                 r=[tss, t_c], w=[t_rstd])
            B.op("act", lambda e: e.activation(out=rstd, in_=rstd, func=AF.Exp, scale=-0.5), r=[t_rstd], w=[t_rstd])
            for kc in range(KC):
                B.op("dve", lambda e, kc=kc: e.scalar_tensor_tensor(
                    out=hs(kc, 1, 513), in0=xs(kc, tt), scalar=pvc(gname, kc), in1=rstd,
                    op0=ALU.mult, op1=ALU.mult), r=[t_x[tt][kc], t_rstd, t_pv], w=[t_hT[kc]])

        def post_norm_residual(tt, gname, pss, tss):
            rstd, t_rstd = NORM["rstd"], NORM["t_rstd"]
            B.op("act", lambda e: e.activation(out=rstd, in_=pss[:, :], func=AF.Ln, scale=1.0 / D, bias=eps_n),
                 r=[tss, t_c], w=[t_rstd])
            B.op("act", lambda e: e.activation(out=rstd, in_=rstd, func=AF.Exp, scale=-0.5), r=[t_rstd], w=[t_rstd])
            for dg in range(KC):
                f = fo[:, dg * TT:(dg + 1) * TT]
                B.op("dve", lambda e, f=f, dg=dg: e.scalar_tensor_tensor(
                    out=f, in0=f, scalar=pvc(gname, dg), in1=rstd, op0=ALU.mult, op1=ALU.mult),
                    r=[t_rstd, t_pv], w=[t_fo[dg]])
                B.op("dve", lambda e, f=f, dg=dg: e.tensor_tensor(out=xs(dg, tt), in0=xs(dg, tt), in1=f, op=ALU.add),
                     r=[t_fo[dg]], w=[t_x[tt][dg]])

        NORM = {}

        for l in range(L):
            B.barrier()
            ar = Arena(arena_t[:, :], ARENA_BYTES)
            NORM["sq"] = [ar.alloc(TT, BF16), ar.alloc(TT, BF16)]
            NORM["t_sq"] = [Tok(), Tok()]
            NORM["rstd"] = ar.alloc(TT, F32)
            NORM["t_rstd"] = Tok()
            base_off = ar.off
            B.dma("sp", pv[:, :], pv_d[l], w=[t_pv])
            B.dma("pool", wlr[:, :], wlr_d[l], w=[t_wlr])
            B.dma("pool", w2a2[:, :], w2a2_d[l], w=[t_w2])
            B.dma("pool", g2[:, :], g2_d[l], w=[t_g2])
            for nm, src, n in [("omuw", "muw", 8), ("omua", "mua", 8), ("omug", "mug", 8), ("omurkv", "murkv", 12),
                               ("omka", "ka", 4)]:
                B.op("dve", lambda e, nm=nm, src=src, n=n: e.tensor_scalar(
                    out=pd[:, PD[nm]:PD[nm] + n], in0=pv[:, PV[src]:PV[src] + n], scalar1=-1.0, scalar2=1.0,
                    op0=ALU.mult, op1=ALU.add), r=[t_pv], w=[t_pd])
            for kc in range(KC):
                for (c0, c1, mn, on) in [(0, 64, "muw", "omuw"), (64, 128, "mua", "omua"), (128, 256, "mug", "omug")]:
                    src = wlr[:, kc * 256 + c0: kc * 256 + c1]
                    B.op("dve", lambda e, src=src, kc=kc, c0=c0, c1=c1, on=on: e.tensor_scalar(
                        out=wlra[:, kc * 256 + c0: kc * 256 + c1], in0=src, scalar1=pdc(on, kc), scalar2=None,
                        op0=ALU.mult), r=[t_wlr, t_pd], w=[t_wlrab])
                    B.op("dve", lambda e, src=src, kc=kc, c0=c0, c1=c1, mn=mn: e.tensor_scalar(
                        out=wlrb[:, kc * 256 + c0: kc * 256 + c1], in0=src, scalar1=pvc(mn, kc), scalar2=None,
                        op0=ALU.mult), r=[t_wlr, t_pv], w=[t_wlrab])
            B.op("dve", lambda e: e.memset(rkvcarry[:, :], 0.0), w=[t_carry])
            B.op("dve", lambda e: e.memset(cu[:, :], 0.0), w=t_cu)
            B.op("dve", lambda e: e.memset(cfu[:, :], 0.0), w=t_cfu)
            B.op("dve", lambda e: e.memset(ST32[:, :], 0.0), w=t_ST)
            B.op("dve", lambda e: e.memset(STb[:, :], 0.0), w=t_ST)
            B.op("dve", lambda e: e.memset(hT[:, :], 0.0), w=t_hT)

            for tt in range(NT):
                B.barrier()
                ar.off = base_off
                if tt > 0:
                    for kc in range(KC):
                        B.op("dve", lambda e, kc=kc: e.tensor_copy(hs(kc, 0, 1), hcarry[:, kc:kc + 1]), r=[t_hc[kc]], w=[t_hT[kc]])
                rmsnorm_to_hT(tt, "premix")
                for kc in range(KC):
                    B.op("dve", lambda e, kc=kc: e.tensor_copy(hcarry[:, kc:kc + 1], hs(kc, 512, 513)), r=[t_hT[kc]], w=[t_hc[kc]])

                ar2 = Arena(fo[:, :], KC * TT * 4)

                def A_(cols, dtype):
                    sz = (cols * (4 if dtype == F32 else 2) + 3) // 4 * 4
                    if ar.off + sz <= ar.n:
                        return ar.alloc(cols, dtype)
                    return ar2.alloc(cols, dtype)
                wins = [A_(KC * 128, BF16) for _ in range(2)]
                t_wins = [Tok() for _ in range(2)]
                raw = [A_(513, F32)]
                t_raw = [Tok()]
                f32t = lambda: A_(TT, F32)
                bft = lambda: A_(TT, BF16)
                r_t, k_t, v_t, sgw, av = f32t(), f32t(), f32t(), f32t(), f32t()
                twa, tg = bft(), bft()
                tA, tB, tC, tD, tE = f32t(), f32t(), f32t(), f32t(), f32t()
                rkb = bft()
                scr2 = tg
                SETS = []
                for _i in range(2):
                    S = dict(bt=bft(), kt=bft(), Bh=bft(), Kh=bft(), vb=bft())
                    S["tk"] = {n: Tok() for n in ["bt", "kt", "Bh", "Kh", "vb"]}
                    SETS.append(S)
                Rm = [bft(), bft()]
                Lm = [bft(), bft()]
                Gm = [bft(), bft()]
                CH = []
                for _i in range(4):
                    S = dict(art=A_(8 * 128, BF16), bonus=bft(), gg=bft(), PC=A_(8, F32), TT=bft(),
                             LakT=bft(), LrbT=bft(), LrkT=bft(), VT=bft(), BhT=bft(), KhT=bft(),
                             Xb=A_(64, BF16), Ub=A_(64, BF16))
                    S["tk"] = {n: Tok() for n in ["art", "bonus", "gg", "PC", "TT", "LakT", "LrbT", "LrkT", "VT", "BhT", "KhT", "Xb", "Ub"]}
                    CH.append(S)
                gA, gB, gC = tE, r_t, k_t
                tk = {n: Tok() for n in ["r", "k", "v", "sgw", "av", "twa", "tg", "A", "B", "C", "D", "E", "rkb",
                                         "R0", "R1", "L0", "L1", "G0", "G1"]}
                tk["scr2"] = tk["tg"]
                tk["gA"], tk["gB"], tk["gC"] = tk["E"], tk["r"], tk["k"]
                XB = (0, 1, 2)
                YB = (3, 4, 5, 6, 7)
                eh = [slice(0, 64), slice(64, 128)]
                v3 = lambda a: a.rearrange("p (c x) -> p c x", c=8)
                mk3 = lambda m, n: m[:, 0:n * 64].rearrange("p (c x) -> p c x", c=n)

                def proj_group(g, consume):
                    si = rr("win", 2)
                    B.dma("pool", wins[si], win_d[l, g], w=[t_wins[si]])
                    ps, tps = getps(XB)
                    for kc in range(KC):
                        B.op("pe", lambda e, kc=kc, ps=ps, si=si: e.matmul(
                            ps[:, :], wins[si][:, kc * 128:(kc + 1) * 128], hs(kc, 1, 513),
                            start=(kc == 0), stop=(kc == KC - 1)), r=[t_wins[si], t_hT[kc]], w=[tps])
                    consume(ps, tps)

                def lowrank(c0, c1, consume):
                    ps, tps = getps(XB)
                    n = 0
                    for kc in range(KC):
                        for (wsb, a, b) in [(wlra, 1, 513), (wlrb, 0, 512)]:
                            B.op("pe", lambda e, kc=kc, wsb=wsb, a=a, b=b, n=n, ps=ps: e.matmul(
                                ps[:, :], wsb[:, kc * 256 + c0: kc * 256 + c1], hs(kc, a, b),
                                start=(n == 0), stop=(n == 2 * KC - 1)), r=[t_wlrab, t_hT[kc]], w=[tps])
                            n += 1
                    consume(ps, tps)

                def x_lowrank():
                    def lr0(ps, tps):
                        B.op("act", lambda e: e.activation(out=twa[0:64, :], in_=ps[0:64, :], func=AF.Tanh),
                             r=[tps], w=[tk["twa"]])
                        B.op("dve", lambda e: e.tensor_copy(twa[64:128, :], ps[64:128, :]), r=[tps], w=[tk["twa"]])
                    lowrank(0, 128, lr0)
                    yield

                    def lr1(ps, tps):
                        B.op("act", lambda e: e.activation(out=tg, in_=ps[:, :], func=AF.Sigmoid), r=[tps], w=[tk["tg"]])
                    lowrank(128, 256, lr1)
                    yield

                def x_sc(gi):
                    cuv = lambda a, b: cu[:, gi * 514 + a: gi * 514 + b]
                    csb = tA
                    acc = tB

                    def c_cons(ps, tps):
                        B.op("act", lambda e: e.activation(out=csb, in_=ps[:, :], func=AF.Copy), r=[tps], w=[tk["A"]])
                    proj_group(14 + gi, c_cons)
                    yield

                    def u_cons(ps, tps):
                        B.op("dve", lambda e: e.tensor_tensor(out=cuv(2, 514), in0=ps[:, :], in1=csb, op=ALU.mult),
                             r=[tps, tk["A"]], w=[t_cu[gi]])
                    proj_group(16 + gi, u_cons)
                    yield
                    B.op("dve", lambda e: e.tensor_scalar(
                        out=acc, in0=cuv(0, 512), scalar1=pvc("scw", 0 * 2 + gi), scalar2=None, op0=ALU.mult),
                        r=[t_cu[gi], t_pv], w=[tk["B"]])
                    for kk_ in (1, 2):
                        B.op("dve", lambda e, kk_=kk_: e.scalar_tensor_tensor(
                            out=acc, in0=cuv(kk_, kk_ + 512), scalar=pvc("scw", kk_ * 2 + gi), in1=acc,
                            op0=ALU.mult, op1=ALU.add), r=[t_cu[gi], t_pv], w=[tk["B"]])
                    B.op("dve", lambda e: e.tensor_copy(cuv(0, 2), cuv(512, 514)), w=[t_cu[gi]])
                    yield

                    def b_cons(ps, tps):
                        B.op("dve", lambda e: e.tensor_tensor(
                            out=mixcat[:, (4 + gi) * TT:(5 + gi) * TT], in0=ps[:, :], in1=acc, op=ALU.mult),
                            r=[tps, tk["B"]], w=[t_mix[4 + gi]])
                    proj_group(12 + gi, b_cons)
                    yield

                cres = [tC, tD]
                tcres = [tk["C"], tk["D"]]

                def x_cf(gi):
                    fv = lambda a, b: cfu[:, gi * 542 + a: gi * 542 + b]
                    sg = tA
                    acc = cres[gi]

                    def g_cons(ps, tps):
                        B.op("act", lambda e: e.activation(out=sg, in_=ps[:, :], func=AF.Sigmoid), r=[tps], w=[tk["A"]])
                    proj_group(20 + gi, g_cons)
                    yield

                    def v_cons(ps, tps):
                        B.op("dve", lambda e: e.tensor_tensor(out=fv(30, 542), in0=ps[:, :], in1=sg, op=ALU.mult),
                             r=[tps, tk["A"]], w=[t_cfu[gi]])
                    proj_group(18 + gi, v_cons)
                    yield
                    B.op("dve", lambda e: e.tensor_scalar(
                        out=acc, in0=fv(0, 512), scalar1=pvc("cfw", gi), scalar2=pvc("cfb", gi),
                        op0=ALU.mult, op1=ALU.add), r=[t_cfu[gi], t_pv], w=[tcres[gi]])
                    for kk_ in range(1, 31):
                        B.op("dve", lambda e, kk_=kk_: e.scalar_tensor_tensor(
                            out=acc, in0=fv(kk_, kk_ + 512), scalar=pvc("cfw", kk_ * 2 + gi), in1=acc,
                            op0=ALU.mult, op1=ALU.add), r=[t_cfu[gi], t_pv], w=[tcres[gi]])
                        if kk_ % 2 == 0:
                            yield
                    B.op("dve", lambda e: e.tensor_copy(fv(0, 30), fv(512, 542)), w=[t_cfu[gi]])
                    yield

                def x_cfln():
                    psm, tpsm = getps(XB)
                    psq, tpsq = getps(XB)
                    for gi in range(2):
                        B.op("act", lambda e, gi=gi: e.activation(out=rkb, in_=cres[gi], func=AF.Copy),
                             r=[tcres[gi]], w=[tk["rkb"]])
                        B.op("pe", lambda e, gi=gi: e.matmul(psm[:, :], ones, rkb, start=(gi == 0), stop=(gi == 1)),
                             r=[tk["rkb"], t_c], w=[tpsm])
                        B.op("act", lambda e, gi=gi: e.activation(out=scr2, in_=cres[gi], func=AF.Square),
                             r=[tcres[gi]], w=[tk["scr2"]])
                        B.op("pe", lambda e, gi=gi: e.matmul(psq[:, :], ones, scr2, start=(gi == 0), stop=(gi == 1)),
                             r=[tk["scr2"], t_c], w=[tpsq])
                        yield
                    B.op("dve", lambda e: e.tensor_scalar(out=tA, in0=psm[:, :], scalar1=1.0 / 256, scalar2=None, op0=ALU.mult),
                         r=[tpsm], w=[tk["A"]])
                    B.op("dve", lambda e: e.tensor_tensor(out=tB, in0=tA, in1=tA, op=ALU.mult), r=[tk["A"]], w=[tk["B"]])
                    B.op("dve", lambda e: e.scalar_tensor_tensor(out=tB, in0=psq[:, :], scalar=1.0 / 256, in1=tB,
                                                                 op0=ALU.mult, op1=ALU.subtract),
                         r=[tpsq], w=[tk["B"]])
                    yield
                    B.op("act", lambda e: e.activation(out=tB, in_=tB, func=AF.Ln, bias=eps_ln), r=[t_c], w=[tk["B"]])
                    B.op("act", lambda e: e.activation(out=tB, in_=tB, func=AF.Exp, scale=-0.5), w=[tk["B"]])
                    yield
                    for gi in range(2):
                        B.op("dve", lambda e, gi=gi: e.tensor_tensor(out=cres[gi], in0=cres[gi], in1=tA, op=ALU.subtract),
                             r=[tk["A"]], w=[tcres[gi]])
                        B.op("dve", lambda e, gi=gi: e.tensor_tensor(out=cres[gi], in0=cres[gi], in1=tB, op=ALU.mult),
                             r=[tk["B"]], w=[tcres[gi]])
                        B.op("act", lambda e, gi=gi: e.activation(
                            out=mixcat[:, (6 + gi) * TT:(7 + gi) * TT], in_=cres[gi], func=AF.Silu,
                            scale=pvc("cflg", gi), bias=pvc("cflb", gi)), r=[tcres[gi], t_pv], w=[t_mix[6 + gi]])
                        yield

                def x_prep(hp):
                    S = {**SETS[hp % 2], **CH[hp]}
                    stk = {**SETS[hp % 2]["tk"], **CH[hp]["tk"]}
                    art, bt_, kt_, Bh, Kh, vb, bonus, gg, PCt = (S[n] for n in ["art", "bt", "kt", "Bh", "Kh", "vb", "bonus", "gg", "PC"])
                    art3 = art.rearrange("p (c x) -> p c x", c=8)
                    ps, tps = getps(XB)
                    B.op("pe", lambda e: e.matmul(ps[:, :], w2a2[0:64, hp * 128:(hp + 1) * 128], twa[0:64, :],
                                                  start=True, stop=True), r=[t_w2, tk["twa"]], w=[tps])
                    B.op("act", lambda e: e.activation(out=sgw, in_=ps[:, :], func=AF.Sigmoid, bias=pvc("w0", hp)),
                         r=[tps, t_pv], w=[tk["sgw"]])
                    ps2, tps2 = getps(XB)
                    B.op("pe", lambda e: e.matmul(ps2[:, :], w2a2[64:128, hp * 128:(hp + 1) * 128], twa[64:128, :],
                                                  start=True, stop=True), r=[t_w2, tk["twa"]], w=[tps2])
                    B.op("act", lambda e: e.activation(out=av, in_=ps2[:, :], func=AF.Sigmoid, bias=pvc("a0", hp)),
                         r=[tps2, t_pv], w=[tk["av"]])
                    ps3, tps3 = getps(XB)
                    B.op("pe", lambda e: e.matmul(ps3[:, :], g2[:, hp * 128:(hp + 1) * 128], tg,
                                                  start=True, stop=True), r=[t_g2, tk["tg"]], w=[tps3])
                    B.op("act", lambda e: e.activation(out=gg, in_=ps3[:, :], func=AF.Copy), r=[tps3], w=[stk["gg"]])
                    yield
                    for (which, dst, tkn) in [(0, r_t, "r"), (1, k_t, "k"), (2, v_t, "v")]:
                        g = which * 4 + hp

                        def rkv_cons(ps, tps, g=g, dst=dst, tkn=tkn):
                            ri = 0
                            rw = raw[ri]
                            B.op("dve", lambda e: e.tensor_copy(rw[:, 0:1], rkvcarry[:, g:g + 1]),
                                 r=[t_carry], w=[t_raw[ri]])
                            B.op("act", lambda e: e.activation(out=rw[:, 1:513], in_=ps[:, :], func=AF.Copy),
                                 r=[tps], w=[t_raw[ri]])
                            B.op("dve", lambda e: e.tensor_copy(rkvcarry[:, g:g + 1], rw[:, 512:513]),
                                 r=[t_raw[ri]], w=[t_carry])
                            B.op("dve", lambda e: e.tensor_scalar(out=dst, in0=rw[:, 0:512], scalar1=pvc("murkv", g),
                                                                  scalar2=None, op0=ALU.mult),
                                 r=[t_raw[ri], t_pv], w=[tk[tkn]])
                            B.op("dve", lambda e: e.scalar_tensor_tensor(
                                out=dst, in0=rw[:, 1:513], scalar=pdc("omurkv", g), in1=dst, op0=ALU.mult, op1=ALU.add),
                                r=[t_raw[ri], t_pd], w=[tk[tkn]])
                        proj_group(g, rkv_cons)
                        yield
                    Pb, Pexb, Pinvb, PCrb = S["VT"], S["BhT"], S["KhT"], S["TT"]
                    tPb, tPexb, tPinvb, tPCrb = stk["VT"], stk["BhT"], stk["KhT"], stk["TT"]
                    rn = raw[0][:, 0:512]
                    B.op("dve", lambda e: e.tensor_tensor_scan(tA, scanm, sgw, 0.0, ALU.mult, ALU.add),
                         r=[tk["sgw"], t_c], w=[tk["A"]])
                    B.op("dve", lambda e: e.tensor_scalar(out=tC, in0=k_t, scalar1=pvc("kk", hp), scalar2=None, op0=ALU.mult),
                         r=[tk["k"], t_pv], w=[tk["C"]])
                    B.op("act", lambda e: e.activation(out=rkb, in_=tC, func=AF.Square), r=[tk["C"]], w=[tk["rkb"]])
                    ps4, tps4 = getps(XB)
                    B.op("pe", lambda e: e.matmul(ps4[:, :], bones, rkb, start=True, stop=True),
                         r=[tk["rkb"], t_c], w=[tps4])
                    B.op("act", lambda e: e.activation(out=vb, in_=v_t, func=AF.Copy), r=[tk["v"]], w=[stk["vb"]])
                    yield
                    B.op("act", lambda e: e.activation(out=Pb, in_=tA, func=AF.Exp, scale=C0), r=[tk["A"]], w=[tPb])
                    B.op("act", lambda e: e.activation(out=Pinvb, in_=tA, func=AF.Exp, scale=-C0), r=[tk["A"]], w=[tPinvb])
                    B.op("act", lambda e: e.activation(out=PCt.rearrange("p (c x) -> p c x", x=1), in_=v3(tA)[:, :, 63:64],
                                                       func=AF.Exp, scale=C0), r=[tk["A"]], w=[stk["PC"]])
                    B.op("dve", lambda e: e.tensor_tensor(out=tD, in0=tA, in1=sgw, op=ALU.subtract),
                         r=[tk["A"], tk["sgw"]], w=[tk["D"]])
                    B.op("dve", lambda e: e.tensor_tensor(out=v3(tB), in0=v3(tA), in1=v3(tA)[:, :, 63:64].to_broadcast([P, 8, 64]),
                                                          op=ALU.subtract), r=[tk["A"]], w=[tk["B"]])
                    yield
                    B.op("act", lambda e: e.activation(out=Pexb, in_=tD, func=AF.Exp, scale=C0), r=[tk["D"]], w=[tPexb])
                    B.op("act", lambda e: e.activation(out=PCrb, in_=tB, func=AF.Exp, scale=-C0), r=[tk["B"]], w=[tPCrb])
                    B.op("act", lambda e: e.activation(out=rn, in_=ps4[:, :], func=AF.Ln, bias=smallc[:, 3:4]), r=[tps4, t_c], w=[t_raw[0]])
                    B.op("act", lambda e: e.activation(out=rn, in_=rn, func=AF.Exp, scale=-0.5), w=[t_raw[0]])
                    B.op("dve", lambda e: e.tensor_tensor(out=art3[:, :, 64:128], in0=v3(r_t), in1=v3(Pb), op=ALU.mult),
                         r=[tk["r"], tPb], w=[stk["art"]])
                    B.op("dve", lambda e: e.tensor_scalar(out=tE, in0=av, scalar1=pvc("ka", hp), scalar2=pdc("omka", hp),
                                                          op0=ALU.mult, op1=ALU.add),
                         r=[tk["av"], t_pv, t_pd], w=[tk["E"]])
                    yield
                    B.op("dve", lambda e: e.tensor_tensor(out=tE, in0=tE, in1=k_t, op=ALU.mult), r=[tk["k"]], w=[tk["E"]])
                    B.op("dve", lambda e: e.tensor_tensor(out=tC, in0=tC, in1=rn, op=ALU.mult), r=[t_raw[0]], w=[tk["C"]])
                    yield
                    B.op("dve", lambda e: e.scalar_tensor_tensor(out=art3[:, :, 0:64], in0=v3(tC), scalar=-1.0, in1=v3(Pexb),
                                                                 op0=ALU.mult, op1=ALU.mult),
                         r=[tk["C"], tPexb], w=[stk["art"]])
                    B.op("dve", lambda e: e.scalar_tensor_tensor(out=rkb, in0=r_t, scalar=pvc("rk", hp), in1=tE,
                                                                 op0=ALU.mult, op1=ALU.mult),
                         r=[tk["r"], tk["E"], t_pv], w=[tk["rkb"]])
                    ps5, tps5 = getps(XB)
                    B.op("pe", lambda e: e.matmul(ps5[:, :], bones, rkb, start=True, stop=True),
                         r=[tk["rkb"], t_c], w=[tps5])
                    yield
                    B.op("dve", lambda e: e.tensor_tensor(out=tC, in0=tC, in1=av, op=ALU.mult), r=[tk["av"]], w=[tk["C"]])
                    B.op("dve", lambda e: e.tensor_tensor(out=kt_, in0=tE, in1=Pinvb, op=ALU.mult),
                         r=[tk["E"], tPinvb], w=[stk["kt"]])
                    yield
                    B.op("dve", lambda e: e.tensor_tensor(out=Kh, in0=tE, in1=PCrb, op=ALU.mult),
                         r=[tk["E"], tPCrb], w=[stk["Kh"]])
                    B.op("dve", lambda e: e.tensor_tensor(out=bt_, in0=tC, in1=Pinvb, op=ALU.mult),
                         r=[tk["C"], tPinvb], w=[stk["bt"]])
                    yield
                    B.op("dve", lambda e: e.tensor_tensor(out=Bh, in0=tC, in1=PCrb, op=ALU.mult),
                         r=[tk["C"], tPCrb], w=[stk["Bh"]])
                    B.op("dve", lambda e: e.tensor_tensor(out=bonus, in0=ps5[:, :], in1=v_t, op=ALU.mult),
                         r=[tps5, tk["v"]], w=[stk["bonus"]])
                    yield

                def ysetup(hp):
                    S = {**SETS[hp % 2], **CH[hp]}
                    stk = {**SETS[hp % 2]["tk"], **CH[hp]["tk"]}
                    return S, stk

                def y2a(hp):
                    S, stk = ysetup(hp)
                    TK = lambda n: stk[n] if n in stk else tk[n]
                    art, bt_, kt_, Bh, Kh, vb = (S[n] for n in ["art", "bt", "kt", "Bh", "Kh", "vb"])
                    VT, BhT, KhT, LakT, LrbT, LrkT, TTf = (S[n] for n in ["VT", "BhT", "KhT", "LakT", "LrbT", "LrkT", "TT"])
                    for (src, sk, dst, dtk) in [(vb, "vb", VT, "VT"), (Bh, "Bh", BhT, "BhT"), (Kh, "Kh", KhT, "KhT")]:
                        ps, tps = getps(YB)
                        for c in range(8):
                            for e_ in range(2):
                                B.op("pe", lambda e, ps=ps, c=c, e_=e_, src=src: e.matmul(
                                    ps[eh[e_], c * 64:(c + 1) * 64], src[eh[e_], c * 64:(c + 1) * 64],
                                    ident[eh[e_], e_ * 64:(e_ + 1) * 64], start=True, stop=True,
                                    tile_position=(64 * e_, 64 * e_)), r=[stk[sk], t_c], w=[tps])
                        B.op("act", lambda e, ps=ps, dst=dst: e.activation(out=dst, in_=ps[:, :], func=AF.Copy),
                             r=[tps], w=[stk[dtk]])
                        yield
                    for (stat, sk, dA, dAk, dB, dBk) in [(bt_, "bt", Rm[0], "R0", LrbT, "LrbT"),
                                                        (kt_, "kt", LakT, "LakT", LrkT, "LrkT")]:
                        for half in range(2):
                            ps, tps = getps(YB)
                            for c4 in range(4):
                                c = half * 4 + c4
                                for e_ in range(2):
                                    B.op("pe", lambda e, ps=ps, c=c, c4=c4, e_=e_, stat=stat: e.matmul(
                                        ps[eh[e_], c4 * 128:(c4 + 1) * 128], stat[eh[e_], c * 64:(c + 1) * 64],
                                        art[eh[e_], c * 128:(c + 1) * 128], start=True, stop=True,
                                        tile_position=(64 * e_, 64 * e_)), r=[stk[sk], stk["art"]], w=[tps])
                            ps3 = ps[:, :].rearrange("p (c x) -> p c x", c=4)
                            B.op("dve", lambda e, ps3=ps3, dA=dA, half=half: e.tensor_tensor(
                                out=v3(dA)[:, half * 4:(half + 1) * 4, :], in0=ps3[:, :, 0:64], in1=mk3(msu, 4), op=ALU.mult),
                                r=[tps, t_c], w=[TK(dAk)])
                            B.op("dve", lambda e, ps3=ps3, dB=dB, half=half: e.tensor_tensor(
                                out=v3(dB)[:, half * 4:(half + 1) * 4, :], in0=ps3[:, :, 64:128], in1=mk3(miu, 4), op=ALU.mult),
                                r=[tps, t_c], w=[TK(dBk)])
                            yield
                    ps, tps = getps(YB)
                    for c in range(8):
                        for e_ in range(2):
                            B.op("pe", lambda e, ps=ps, c=c, e_=e_: e.matmul(
                                ps[eh[e_], c * 64:(c + 1) * 64], art[eh[e_], c * 128: c * 128 + 64],
                                bt_[eh[e_], c * 64:(c + 1) * 64], start=True, stop=True,
                                tile_position=(64 * e_, 64 * e_)), r=[stk["bt"], stk["art"]], w=[tps])
                    B.op("dve", lambda e, ps=ps: e.tensor_tensor(out=Lm[0], in0=ps[:, :], in1=msl, op=ALU.mult),
                         r=[tps, t_c], w=[tk["L0"]])
                    B.op("dve", lambda e: e.tensor_tensor(out=Gm[0], in0=Rm[0], in1=irep, op=ALU.add),
                         r=[tk["R0"], t_c], w=[tk["G0"]])
                    yield
                    cur = 0
                    for lev in range(1, 6):
                        nxt = 1 - cur
                        Rc, Lc, Gc = Rm[cur], Lm[cur], Gm[cur]
                        Rn, Ln, Gn = Rm[nxt], Lm[nxt], Gm[nxt]
                        tRc, tLc, tGc = tk["R%d" % cur], tk["L%d" % cur], tk["G%d" % cur]
                        tRn, tLn, tGn = tk["R%d" % nxt], tk["L%d" % nxt], tk["G%d" % nxt]
                        if lev == 5:
                            Gn, tGn = TTf, stk["TT"]
                        ps, tps = getps(YB)
                        for c in range(8):
                            for e_ in range(2):
                                sl = slice(c * 64, (c + 1) * 64)
                                B.op("pe", lambda e, ps=ps, sl=sl, e_=e_: e.matmul(
                                    ps[eh[e_], sl], Rc[eh[e_], sl], Lc[eh[e_], sl], start=True, stop=True,
                                    tile_position=(64 * e_, 64 * e_)), r=[tRc, tLc], w=[tps])
                        B.op("act", lambda e, ps=ps: e.activation(out=Ln, in_=ps[:, :], func=AF.Copy),
                             r=[tps], w=[tLn])
                        if lev < 5:
                            ps2, tps2 = getps(YB)
                            for c in range(8):
                                for e_ in range(2):
                                    sl = slice(c * 64, (c + 1) * 64)
                                    B.op("pe", lambda e, ps2=ps2, sl=sl, e_=e_: e.matmul(
                                        ps2[eh[e_], sl], Lc[eh[e_], sl], Rc[eh[e_], sl], start=True, stop=True,
                                        tile_position=(64 * e_, 64 * e_)), r=[tRc, tLc], w=[tps2])
                            B.op("dve", lambda e, ps2=ps2: e.tensor_copy(Rn, ps2[:, :]), r=[tps2], w=[tRn])
                        yield
                        ps3_, tps3 = getps(YB)
                        for c in range(8):
                            for e_ in range(2):
                                sl = slice(c * 64, (c + 1) * 64)
                                B.op("pe", lambda e, ps3_=ps3_, sl=sl, e_=e_: e.matmul(
                                    ps3_[eh[e_], sl], Ln[eh[e_], sl], Gc[eh[e_], sl], start=True, stop=True,
                                    tile_position=(64 * e_, 64 * e_)), r=[tLn, tGc], w=[tps3])
                        B.op("dve", lambda e, ps3_=ps3_: e.tensor_tensor(out=Gn, in0=ps3_[:, :], in1=Gc, op=ALU.add),
                             r=[tps3, tGc], w=[tGn])
                        yield
                        cur = nxt

                PSY = {}

                def y_chain(hp):
                    S, stk = ysetup(hp)
                    art, bonus, gg, PCt = (S[n] for n in ["art", "bonus", "gg", "PC"])
                    VT, BhT, KhT, LakT, LrbT, LrkT, TTm, Xb, Ub = (S[n] for n in ["VT", "BhT", "KhT", "LakT", "LrbT", "LrkT", "TT", "Xb", "Ub"])
                    tTT = stk["TT"]
                    YB = (0, 1, 2, 3)
                    psY, tpsY = getps((4 + hp,), hold=True)
                    PSY[hp] = (psY, tpsY)
                    Sb = STb[:, hp * 64:(hp + 1) * 64]
                    S32 = ST32[:, hp * 64:(hp + 1) * 64]
                    tS = t_ST[hp]
                    for c in range(8):
                        sl = slice(c * 64, (c + 1) * 64)
                        psX, tpsX = getps(YB)
                        for e_ in range(2):
                            B.op("pe", lambda e, e_=e_: e.matmul(
                                psX[eh[e_], 0:64], art[eh[e_], c * 128:c * 128 + 64], Sb[eh[e_], :], start=True, stop=False,
                                tile_position=(64 * e_, 64 * e_)), r=[stk["art"], tS], w=[tpsX])
                            B.op("pe", lambda e, e_=e_: e.matmul(
                                psX[eh[e_], 0:64], LakT[eh[e_], sl], VT[eh[e_], sl], start=False, stop=True,
                                tile_position=(64 * e_, 64 * e_)), r=[stk["LakT"], stk["VT"]], w=[tpsX])
                        B.op("act", lambda e: e.activation(out=Xb, in_=psX[:, 0:64], func=AF.Copy),
                             r=[tpsX], w=[stk["Xb"]])
                        yield
                        psU, tpsU = getps(YB)
                        for e_ in range(2):
                            B.op("pe", lambda e, e_=e_: e.matmul(
                                psU[eh[e_], 0:64], TTm[eh[e_], sl], Xb[eh[e_], :], start=True, stop=True,
                                tile_position=(64 * e_, 64 * e_)), r=[tTT, stk["Xb"]], w=[tpsU])
                        B.op("dve", lambda e: e.tensor_copy(Ub, psU[:, 0:64]), r=[tpsU], w=[stk["Ub"]])
                        yield
                        for e_ in range(2):
                            B.op("pe", lambda e, e_=e_: e.matmul(
                                psY[eh[e_], sl], Sb[eh[e_], :], art[eh[e_], c * 128 + 64:(c + 1) * 128], start=True, stop=False,
                                tile_position=(64 * e_, 64 * e_)), r=[tS, stk["art"]], w=[tpsY])
                            B.op("pe", lambda e, e_=e_: e.matmul(
                                psY[eh[e_], sl], Ub[eh[e_], :], LrbT[eh[e_], sl], start=False, stop=False,
                                tile_position=(64 * e_, 64 * e_)), r=[stk["Ub"], stk["LrbT"]], w=[tpsY])
                            B.op("pe", lambda e, e_=e_: e.matmul(
                                psY[eh[e_], sl], VT[eh[e_], sl], LrkT[eh[e_], sl], start=False, stop=True,
                                tile_position=(64 * e_, 64 * e_)), r=[stk["VT"], stk["LrkT"]], w=[tpsY])
                        psS, tpsS = getps(YB)
                        for e_ in range(2):
                            B.op("pe", lambda e, e_=e_: e.matmul(
                                psS[eh[e_], 0:64], BhT[eh[e_], sl], Ub[eh[e_], :], start=True, stop=False,
                                tile_position=(64 * e_, 64 * e_)), r=[stk["BhT"], stk["Ub"]], w=[tpsS])
                            B.op("pe", lambda e, e_=e_: e.matmul(
                                psS[eh[e_], 0:64], KhT[eh[e_], sl], VT[eh[e_], sl], start=False, stop=True,
                                tile_position=(64 * e_, 64 * e_)), r=[stk["KhT"], stk["VT"]], w=[tpsS])
                        B.op("dve", lambda e: e.scalar_tensor_tensor(
                            out=S32, in0=S32, scalar=PCt[:, c:c + 1], in1=psS[:, 0:64], op0=ALU.mult, op1=ALU.add),
                            r=[tpsS, stk["PC"]], w=[tS])
                        B.op("act", lambda e: e.activation(out=Sb, in_=S32, func=AF.Copy), w=[tS])
                        yield

                def y_tail(hp):
                    S, stk = ysetup(hp)
                    bonus, gg, VT, BhT = (S[n] for n in ["bonus", "gg", "VT", "BhT"])
                    YB = (0, 1, 2, 3)
                    psY, tpsY = PSY[hp]
                    B.op("act", lambda e: e.activation(out=gA, in_=psY[:, :], func=AF.Copy), r=[tpsY], w=[tk["gA"]])
                    B.op("act", lambda e: e.activation(out=VT, in_=psY[:, :], func=AF.Copy), r=[tpsY], w=[stk["VT"]])
                    B.op("act", lambda e: e.activation(out=BhT, in_=psY[:, :], func=AF.Square), r=[tpsY], w=[stk["BhT"]])
                    release(psY)
                    yield
                    psm, tpsm = getps(YB)
                    psq, tpsq = getps(YB)
                    B.op("pe", lambda e: e.matmul(psm[:, :], bones, VT, start=True, stop=True),
                         r=[stk["VT"], t_c], w=[tpsm])
                    B.op("pe", lambda e: e.matmul(psq[:, :], bones, BhT, start=True, stop=True),
                         r=[stk["BhT"], t_c], w=[tpsq])
                    B.op("dve", lambda e: e.tensor_scalar(out=gB, in0=psm[:, :], scalar1=1.0 / 64, scalar2=None, op0=ALU.mult),
                         r=[tpsm], w=[tk["gB"]])
                    B.op("dve", lambda e: e.tensor_tensor(out=gC, in0=gB, in1=gB, op=ALU.mult), r=[tk["gB"]], w=[tk["gC"]])
                    B.op("dve", lambda e: e.scalar_tensor_tensor(out=gC, in0=psq[:, :], scalar=1.0 / 64, in1=gC,
                                                                 op0=ALU.mult, op1=ALU.subtract),
                         r=[tpsq], w=[tk["gC"]])
                    yield
                    B.op("act", lambda e: e.activation(out=gC, in_=gC, func=AF.Ln, bias=eps_gn), r=[t_c], w=[tk["gC"]])
                    B.op("act", lambda e: e.activation(out=gC, in_=gC, func=AF.Exp, scale=-0.5), w=[tk["gC"]])
                    B.op("pool", lambda e: e.tensor_tensor(out=gA, in0=gA, in1=gB, op=ALU.subtract), r=[tk["gB"]], w=[tk["gA"]])
                    yield
                    B.op("pool", lambda e: e.tensor_tensor(out=gA, in0=gA, in1=gC, op=ALU.mult), r=[tk["gC"]], w=[tk["gA"]])
                    B.op("pool", lambda e: e.tensor_scalar(out=gA, in0=gA, scalar1=pvc("lng", hp), scalar2=pvc("lnb", hp),
                                                          op0=ALU.mult, op1=ALU.add), r=[t_pv], w=[tk["gA"]])
                    yield
                    B.op("pool", lambda e: e.tensor_tensor(out=gA, in0=gA, in1=bonus, op=ALU.add), r=[stk["bonus"]], w=[tk["gA"]])
                    B.op("pool", lambda e: e.tensor_tensor(out=mixcat[:, hp * TT:(hp + 1) * TT], in0=gA, in1=gg, op=ALU.mult),
                         r=[tk["gA"], stk["gg"]], w=[t_mix[hp]])
                    yield

                def chain_gens(gs):
                    for g_ in gs:
                        yield from g_

                def interleave(gs):
                    gs = list(gs)
                    while gs:
                        for g_ in list(gs):
                            try:
                                next(g_)
                            except StopIteration:
                                gs.remove(g_)
                for _ in x_lowrank():
                    pass
                for _ in x_prep(0):
                    pass
                for hp in range(1, 4):
                    interleave([x_prep(hp), y2a(hp - 1)])
                interleave([chain_gens([x_sc(0), x_sc(1)]), y2a(3)])
                for dg in range(2):
                    B.dma("pool", wos[dg][:, :], wo_d[l, dg], w=[t_wos[dg]])
                XB = (0, 1, 2, 3)
                interleave([y_chain(0), y_chain(1), y_chain(2), y_chain(3), chain_gens([x_cf(0), x_cf(1), x_cfln()])])
                for hp in range(4):
                    for _ in y_tail(hp):
                        pass
                B.barrier()

                if dbg and l == 0 and tt == 0:
                    B.dma("pool", dbg_d[:, :], mixcat[:, :], r=t_mix)

                pss, tss = getps(hold=True)
                for dg in range(KC):
                    si = dg % 2
                    if dg >= 2:
                        B.dma("pool", wos[si][:, :], wo_d[l, dg], w=[t_wos[si]])
                    ps, tps = getps()
                    for kc in range(KC):
                        B.op("pe", lambda e, ps=ps, kc=kc, si=si: e.matmul(
                            ps[:, :], wos[si][:, kc * 128:(kc + 1) * 128], mixcat[:, kc * TT:(kc + 1) * TT],
                            start=(kc == 0), stop=(kc == KC - 1)), r=[t_wos[si], t_mix[kc]], w=[tps])
                    B.op("act", lambda e, ps=ps, dg=dg: e.activation(out=fo[:, dg * TT:(dg + 1) * TT], in_=ps[:, :], func=AF.Copy),
                         r=[tps], w=[t_fo[dg]])
                    i = dg % 2
                    B.op("act", lambda e, ps=ps, i=i: e.activation(out=NORM["sq"][i], in_=ps[:, :], func=AF.Square),
                         r=[tps], w=[NORM["t_sq"][i]])
                    B.op("pe", lambda e, i=i, dg=dg, pss=pss: e.matmul(pss[:, :], ones, NORM["sq"][i], start=(dg == 0), stop=(dg == KC - 1)),
                         r=[NORM["t_sq"][i], t_c], w=[tss])
                release(pss)
                post_norm_residual(tt, "postmix", pss, tss)

                B.barrier()
                ar.off = base_off
                act_t = ar.alloc(NJ * TT, BF16)
                t_act = [Tok() for _ in range(NJ)]
                NGUS, NWDS = 5, 3
                gus = [ar.alloc(2 * KC * 128, BF16) for _ in range(NGUS)]
                t_gus = [Tok() for _ in range(NGUS)]
                t_guu = [Tok() for _ in range(NGUS)]
                wds = [ar.alloc(NJ * 128, BF16) for _ in range(NWDS)]
                t_wds = [Tok() for _ in range(NWDS)]
                sil = [ar.alloc(TT, F32), ar.alloc(TT, F32)]
                t_sil = [Tok(), Tok()]
                for dg in range(NWDS):
                    B.dma("pool", wds[dg], wd_d[l, dg], w=[t_wds[dg]])
                rmsnorm_to_hT(tt, "preffn")
                for j in range(NJ):
                    si = rr("gus", NGUS)
                    B.dma("pool", gus[si][:, 0:KC * 128], wg_d[l, j], w=[t_gus[si]])
                    B.dma("pool", gus[si][:, KC * 128:2 * KC * 128], wu_d[l, j], w=[t_guu[si]])
                    psg, tpsg = getps()
                    psu, tpsu = getps()
                    for kc in range(KC):
                        B.op("pe", lambda e, kc=kc, si=si, psg=psg: e.matmul(
                            psg[:, :], gus[si][:, kc * 128:(kc + 1) * 128], hs(kc, 1, 513),
                            start=(kc == 0), stop=(kc == KC - 1)), r=[t_gus[si], t_hT[kc]], w=[tpsg])
                    for kc in range(KC):
                        B.op("pe", lambda e, kc=kc, si=si, psu=psu: e.matmul(
                            psu[:, :], gus[si][:, (KC + kc) * 128:(KC + kc + 1) * 128], hs(kc, 1, 513),
                            start=(kc == 0), stop=(kc == KC - 1)), r=[t_guu[si], t_hT[kc]], w=[tpsu])
                    i = j % 2
                    B.op("act", lambda e, i=i, psg=psg: e.activation(out=sil[i], in_=psg[:, :], func=AF.Silu),
                         r=[tpsg], w=[t_sil[i]])
                    B.op("dve", lambda e, i=i, j=j, psu=psu: e.tensor_tensor(
                        out=act_t[:, j * TT:(j + 1) * TT], in0=psu[:, :], in1=sil[i], op=ALU.mult),
                        r=[tpsu, t_sil[i]], w=[t_act[j]])
                pss, tss = getps(hold=True)
                for dg in range(KC):
                    si = dg % NWDS
                    if dg >= NWDS:
                        B.dma("pool", wds[si], wd_d[l, dg], w=[t_wds[si]])
                    ps, tps = getps()
                    for j in range(NJ):
                        B.op("pe", lambda e, ps=ps, j=j, si=si: e.matmul(
                            ps[:, :], wds[si][:, j * 128:(j + 1) * 128], act_t[:, j * TT:(j + 1) * TT],
                            start=(j == 0), stop=(j == NJ - 1)), r=[t_wds[si], t_act[j]], w=[tps])
                    B.op("act", lambda e, ps=ps, dg=dg: e.activation(out=fo[:, dg * TT:(dg + 1) * TT], in_=ps[:, :], func=AF.Copy),
                         r=[tps], w=[t_fo[dg]])
                    i = dg % 2
                    B.op("act", lambda e, ps=ps, i=i: e.activation(out=NORM["sq"][i], in_=ps[:, :], func=AF.Square),
                         r=[tps], w=[NORM["t_sq"][i]])
                    B.op("pe", lambda e, i=i, dg=dg, pss=pss: e.matmul(pss[:, :], ones, NORM["sq"][i], start=(dg == 0), stop=(dg == KC - 1)),
                         r=[NORM["t_sq"][i], t_c], w=[tss])
                release(pss)
                post_norm_residual(tt, "postffn", pss, tss)

        t_out = Tok()
        for tt in range(NT):
            for kc in range(KC):
                B.dma("sp", y_d[:, kc * T + tt * TT: kc * T + (tt + 1) * TT], xs(kc, tt), r=[t_x[tt][kc]], w=[Tok()])
        B.barrier()

        with nc.Block() as block:
            B.emit(block)
    return nc


def _consts():
    c = np.zeros((P, NCF), np.float32)
    p = np.arange(P)[:, None]
    c[:, CF["ident"]:CF["ident"] + 128] = (p == np.arange(128)[None, :])
    c[:, CF["ones"]:CF["ones"] + 128] = 1.0
    c[:, CF["bones"]:CF["bones"] + 128] = ((p // 64) == (np.arange(128)[None, :] // 64))
    col = (np.arange(512) % 64)[None, :]
    row = p % 64
    c[:, CF["msu"]:CF["msu"] + 512] = (row < col)
    c[:, CF["miu"]:CF["miu"] + 512] = (row <= col)
    c[:, CF["msl"]:CF["msl"] + 512] = (col < row)
    c[:, CF["irep"]:CF["irep"] + 512] = (row == col)
    c[:, CF["scan"]:CF["scan"] + 512] = (col != 0) * np.ones((P, 1))
    return c


def _colvec(v):
    return np.ascontiguousarray(np.asarray(v, np.float32).reshape(-1, P).T)


def _prep_inputs(inp, L):
    f = lambda a: np.asarray(a, np.float32)
    out = {}

    def grp_cols(w, ncol_groups):
        Lw, K, N = w.shape
        a = w.reshape(Lw, K // P, P, N // P, P)
        a = a.transpose(0, 3, 2, 1, 4)
        return np.ascontiguousarray(a.reshape(Lw, N // P, P, (K // P) * P))
    out["w_in"] = grp_cols(f(inp["w_in"])[:L], NJ)
    out["w_gate"] = grp_cols(f(inp["w_gate"])[:L], NJ)
    out["w_up"] = grp_cols(f(inp["w_up"])[:L], NJ)
    out["w_o"] = grp_cols(f(inp["w_o"])[:L], KC)
    out["w_down"] = grp_cols(f(inp["w_down"])[:L], KC)
    wl = np.concatenate([f(inp["w1"]), f(inp["a1"]), f(inp["g1"])], axis=2)[:L]
    wl = wl.reshape(L, KC, P, 256).transpose(0, 2, 1, 3).reshape(L, P, KC * 256)
    out["wlr"] = np.ascontiguousarray(wl)
    out["w2a2"] = np.ascontiguousarray(np.concatenate([f(inp["w2"]), f(inp["a2"])], axis=1)[:L])
    out["g2"] = np.ascontiguousarray(f(inp["g2"])[:L])
    pv = np.zeros((L, P, NPV), np.float32)
    for l in range(L):
        def put(name, arr):
            a = _colvec(arr)
            pv[l, :, PV[name]:PV[name] + a.shape[1]] = a
        put("premix", inp["pre_mix_g"][l]); put("postmix", inp["post_mix_g"][l])
        put("preffn", inp["pre_ffn_g"][l]); put("postffn", inp["post_ffn_g"][l])
        put("muw", inp["mu_wag"][l][0]); put("mua", inp["mu_wag"][l][1]); put("mug", inp["mu_wag"][l][2])
        put("murkv", inp["mu_rkv"][l])
        put("w0", inp["w0"][l]); put("a0", inp["a0"][l]); put("kk", inp["k_k"][l]); put("ka", inp["k_a"][l])
        put("rk", np.asarray(inp["r_k"][l]).reshape(-1)); put("lng", inp["lnx_g"][l]); put("lnb", inp["lnx_b"][l])
        scw = f(inp["sc_conv_w"][l])
        for k_ in range(3):
            pv[l, :, PV["scw"] + k_ * 2: PV["scw"] + k_ * 2 + 2] = _colvec(scw[k_])
        cfw = f(inp["cf_conv_w"][l])
        for k_ in range(31):
            pv[l, :, PV["cfw"] + k_ * 2: PV["cfw"] + k_ * 2 + 2] = _colvec(cfw[k_])
        put("cfb", inp["cf_conv_b"][l]); put("cflg", inp["cf_ln_g"][l]); put("cflb", inp["cf_ln_b"][l])
    out["pv"] = pv
    out["cf"] = _consts()
    return out


_CACHE = {}


def run(inputs, L=4, dbg=False):
    x = np.asarray(inputs["x"], np.float32)
    shared = _prep_inputs(inputs, L)
    key = (L, dbg)
    if key not in _CACHE:
        _CACHE[key] = build_program(L, dbg)
    nc = _CACHE[key]
    in_maps = []
    for b in range(NCORES):
        xt = np.ascontiguousarray(x[b].T.reshape(KC, P, T).transpose(1, 0, 2).reshape(P, KC * T))
        m = dict(shared)
        m["xT"] = xt
        in_maps.append(m)
    res = run_bass_kernel_spmd(nc, in_maps, core_ids=list(range(NCORES)))
    outs = []
    for b in range(NCORES):
        yt = np.asarray(res.results[b]["yT"]).reshape(P, KC, T).transpose(1, 0, 2).reshape(D, T)
        outs.append(yt.T)
    y = np.stack(outs, 0).astype(np.float32)
    if dbg:
        return y, [np.asarray(r["dbg"]) for r in res.results]
    return y


def kernel(**inputs):
    return run(inputs, L=4)
```

```python
import contextlib
import numpy as np
import concourse.bass as bass
import concourse.mybir as mybir
from concourse.bass_utils import run_bass_kernel_spmd

F32 = mybir.dt.float32
BF16 = mybir.dt.bfloat16
AF = mybir.ActivationFunctionType
ALU = mybir.AluOpType

P = 128
T = 2048
D = 1024
TT = 512
NT = T // TT
KC = 8
NJ = 22
NCORES = 8
C0 = -0.6065306597126334
SAME_ENGINE_SYNC = True

PV = {}
_o = 0
for _n, _c in [("premix", 8), ("postmix", 8), ("preffn", 8), ("postffn", 8), ("muw", 8), ("mua", 8),
               ("mug", 8), ("murkv", 12), ("w0", 4), ("a0", 4), ("kk", 4), ("ka", 4), ("rk", 4),
               ("lng", 4), ("lnb", 4), ("scw", 6), ("cfw", 62), ("cfb", 2), ("cflg", 2), ("cflb", 2)]:
    PV[_n] = _o
    _o += _c
NPV = _o
PD = {"omuw": 0, "omua": 8, "omug": 16, "omurkv": 24, "omka": 36}
NPD = 40
CF = {"ident": 0, "ones": 128, "bones": 256, "msu": 384, "miu": 896, "msl": 1408, "irep": 1920, "scan": 2432}
NCF = 2944


class Tok:
    __slots__ = ("w", "r")

    def __init__(self):
        self.w = None
        self.r = []


class _Rec:
    def __init__(self):
        self.calls = []

    def __getattr__(self, name):
        def f(*a, **k):
            self.calls.append((name, a, k))
        return f


class Builder:
    ENGS = ("pe", "act", "dve", "pool", "sp")

    def __init__(self, nc, es, ndma=48):
        self.nc = nc
        self.q = {e: [] for e in self.ENGS}
        self.cnt = {e: 0 for e in self.ENGS}
        self.seen = {e: {} for e in self.ENGS}
        self.sems = {}
        for e in ("pe", "act", "dve", "pool"):
            self.sems[e] = es.enter_context(nc.semaphore("s_" + e))
        self.ndma = ndma
        self.dma_uses = [0] * ndma
        self.dma_next = 0
        for k in range(ndma):
            self.sems["dma%d" % k] = es.enter_context(nc.semaphore("s_dma%d" % k))

    def _collect(self, r, w):
        deps = {}

        def add(d):
            if d is None:
                return
            k, v = d
            if deps.get(k, 0) < v:
                deps[k] = v
        for t in r:
            add(t.w)
        for t in w:
            add(t.w)
            for d in t.r:
                add(d)
        return deps

    def _waits(self, eng, deps):
        out = []
        seen = self.seen[eng]
        for k, v in deps.items():
            if k == eng and (eng == "pe" or not SAME_ENGINE_SYNC):
                continue
            if seen.get(k, 0) >= v:
                continue
            seen[k] = v
            out.append((k, v))
        return out

    def op(self, eng, fn, r=(), w=()):
        rec = _Rec()
        fn(rec)
        assert len(rec.calls) == 1
        call = rec.calls[0]
        fn = lambda e, call=call: getattr(e, call[0])(*call[1], **call[2])
        deps = self._collect(r, w)
        waits = self._waits(eng, deps)
        self.cnt[eng] += 1
        me = (eng, self.cnt[eng])
        self.q[eng].append((fn, waits, (eng, 1)))
        for t in r:
            t.r.append(me)
        for t in w:
            t.w = me
            t.r = []

    def dma(self, qe, out_ap, in_ap, r=(), w=()):
        deps = self._collect(r, w)
        k = self.dma_next
        self.dma_next = (k + 1) % self.ndma
        key = "dma%d" % k
        if self.dma_uses[k] > 0:
            v = 16 * self.dma_uses[k]
            if deps.get(key, 0) < v:
                deps[key] = v
        waits = self._waits(qe, deps)
        self.dma_uses[k] += 1
        me = (key, 16 * self.dma_uses[k])
        self.q[qe].append((lambda e: e.dma_start(out=out_ap, in_=in_ap), waits, (key, 16)))
        for t in r:
            t.r.append(me)
        for t in w:
            t.w = me
            t.r = []

    def wait_only(self, eng, toks):
        deps = self._collect(toks, ())
        waits = self._waits(eng, deps)
        self.q[eng].append((None, waits, None))

    def barrier(self):
        allv = {e: self.cnt[e] for e in ("pe", "act", "dve", "pool") if self.cnt[e] > 0}
        for k in range(self.ndma):
            if self.dma_uses[k] > 0:
                allv["dma%d" % k] = 16 * self.dma_uses[k]
        for e in self.ENGS:
            deps = {k: v for k, v in allv.items() if k != e}
            waits = self._waits(e, deps)
            if waits:
                self.q[e].append((None, waits, None))

    def emit(self, block):
        sems = self.sems

        def run(e, lst):
            for fn, waits, inc in lst:
                for k, v in waits:
                    e.wait_ge(sems[k], v)
                if fn is not None:
                    ins = fn(e)
                    if inc is not None:
                        ins.then_inc(sems[inc[0]], inc[1])

        @block.tensor
        def _(e):
            run(e, self.q["pe"])

        @block.scalar
        def _(e):
            run(e, self.q["act"])

        @block.vector
        def _(e):
            run(e, self.q["dve"])

        @block.gpsimd
        def _(e):
            run(e, self.q["pool"])

        @block.sync
        def _(e):
            run(e, self.q["sp"])


class Arena:
    def __init__(self, ap, nbytes):
        self.ap = ap
        self.n = nbytes
        self.off = 0

    def alloc(self, cols, dtype):
        sz = cols * (4 if dtype == F32 else 2)
        sz = (sz + 3) // 4 * 4
        assert self.off + sz <= self.n, ("arena overflow", self.off, sz, self.n)
        a = self.ap[:, self.off // 4:(self.off + sz) // 4]
        self.off += sz
        if dtype != F32:
            a = a.bitcast(dtype)
            a = a[:, 0:cols]
        return a


def build_program(L=4, dbg=False):
    nc = bass.Bass("TRN2", target_bir_lowering=False)
    dt_in = lambda n, s: nc.dram_tensor(n, s, F32, kind="ExternalInput").ap()
    x_d = dt_in("xT", [P, KC * T])
    win_d = dt_in("w_in", [L, NJ, P, KC * 128])
    wg_d = dt_in("w_gate", [L, NJ, P, KC * 128])
    wu_d = dt_in("w_up", [L, NJ, P, KC * 128])
    wo_d = dt_in("w_o", [L, KC, P, KC * 128])
    wd_d = dt_in("w_down", [L, KC, P, NJ * 128])
    wlr_d = dt_in("wlr", [L, P, KC * 256])
    w2a2_d = dt_in("w2a2", [L, P, 512])
    g2_d = dt_in("g2", [L, P, 512])
    pv_d = dt_in("pv", [L, P, NPV])
    cf_d = dt_in("cf", [P, NCF])
    y_d = nc.dram_tensor("yT", [P, KC * T], F32, kind="ExternalOutput").ap()
    if dbg:
        dbg_d = nc.dram_tensor("dbg", [P, KC * TT], F32, kind="ExternalOutput").ap()

    with contextlib.ExitStack() as es:
        def sb(name, cols, dtype):
            return es.enter_context(nc.sbuf_tensor(name, [P, cols], dtype))
        xT = sb("xT_sb", KC * T, F32)
        pv = sb("pv_sb", NPV, F32)
        pd = sb("pd_sb", NPD, F32)
        cb = sb("cb", NCF, BF16)
        cmask = cb[:, 384:NCF]
        smallc = sb("smallc", 4, F32)
        wlra = sb("wlra", KC * 256, BF16)
        wlrb = sb("wlrb", KC * 256, BF16)
        w2a2 = sb("w2a2_sb", 512, BF16)
        g2 = sb("g2_sb", 512, BF16)
        rkvcarry = sb("rkvcarry", 12, F32)
        cu = sb("cu", 2 * 514, F32)
        cfu = sb("cfu", 2 * 542, F32)
        ST32 = sb("ST32", 4 * 64, F32)
        STb = sb("STb", 4 * 64, BF16)
        hT = sb("hT", KC * 513, BF16)
        hcarry = sb("hcarry", KC, BF16)
        mixcat = sb("mixcat", KC * TT, BF16)
        fo = sb("fo", KC * TT, F32)
        wlr = fo[:, 0:KC * 128].bitcast(BF16)
        wos = [sb("wos%d" % i, KC * 128, BF16) for i in range(2)]
        ARENA_BYTES = 80 * 1024
        arena_t = sb("arena", ARENA_BYTES // 4, F32)
        psum = [es.enter_context(nc.psum_tensor("ps%d" % i, [P, 512], F32)) for i in range(8)]

        B = Builder(nc, es)
        pst = [Tok() for _ in range(8)]
        ps_rr = [0]

        held = set()

        ALLB = tuple(range(8))
        ps_ptr = {}

        def getps(banks=None, hold=False):
            key = banks or ALLB
            while True:
                p_ = ps_ptr.get(key, 0)
                ps_ptr[key] = p_ + 1
                i = key[p_ % len(key)]
                if i not in held:
                    break
            if hold:
                held.add(i)
            return psum[i], pst[i]

        def release(ps):
            for i in range(8):
                if psum[i] is ps:
                    held.discard(i)

        t_x = [[Tok() for _ in range(KC)] for _ in range(NT)]
        t_pv, t_pd, t_c, t_wlr, t_wlrab, t_w2, t_g2 = Tok(), Tok(), Tok(), Tok(), Tok(), Tok(), Tok()
        t_carry, t_cu, t_cfu = Tok(), [Tok(), Tok()], [Tok(), Tok()]
        t_ST = [Tok() for _ in range(4)]
        t_hT, t_mix, t_fo = [Tok() for _ in range(KC)], [Tok() for _ in range(KC)], [Tok() for _ in range(KC)]
        t_wos = [Tok(), Tok()]
        t_hc = [Tok() for _ in range(KC)]

        def pvc(name, i=0):
            o = PV[name] + i
            return pv[:, o:o + 1]

        def pdc(name, i=0):
            o = PD[name] + i
            return pd[:, o:o + 1]

        ident = cb[:, 0:128]
        ones = cb[:, 128:256]
        bones = cb[:, 256:384]
        msu = cmask[:, 0:512]
        miu = cmask[:, 512:1024]
        msl = cmask[:, 1024:1536]
        irep = cmask[:, 1536:2048]
        scanm = cmask[:, 2048:2560]
        eps_n = smallc[:, 0:1]
        eps_ln = smallc[:, 1:2]
        eps_gn = smallc[:, 2:3]

        def xs(kc, tt):
            return xT[:, kc * T + tt * TT: kc * T + (tt + 1) * TT]

        def hs(kc, a, b):
            return hT[:, kc * 513 + a: kc * 513 + b]

        for kc in range(KC):
            for tt in range(NT):
                B.dma("sp", xs(kc, tt), x_d[:, kc * T + tt * TT: kc * T + (tt + 1) * TT], w=[t_x[tt][kc]])
        B.dma("pool", cb[:, :], cf_d[:, :], w=[t_c])
        B.op("dve", lambda e: e.memset(smallc[:, 0:1], 1e-6), w=[t_c])
        B.op("dve", lambda e: e.memset(smallc[:, 1:2], 1e-5), w=[t_c])
        B.op("dve", lambda e: e.memset(smallc[:, 2:3], 64e-5), w=[t_c])
        B.op("dve", lambda e: e.memset(smallc[:, 3:4], 1e-24), w=[t_c])

        wslot_rr = {}

        def rr(name, n):
            i = wslot_rr.get(name, 0)
            wslot_rr[name] = (i + 1) % n
            return i

        def rmsnorm_to_hT(tt, gname):
            pss, tss = getps()
            sq, t_sq = NORM["sq"], NORM["t_sq"]
            for kc in range(KC):
                i = kc % 2
                B.op("act", lambda e, kc=kc, i=i: e.activation(out=sq[i], in_=xs(kc, tt), func=AF.Square),
                     r=[t_x[tt][kc]], w=[t_sq[i]])
                B.op("pe", lambda e, kc=kc, i=i: e.matmul(pss[:, :], ones, sq[i], start=(kc == 0), stop=(kc == KC - 1)),
                     r=[t_sq[i], t_c], w=[tss])
            rstd, t_rstd = NORM["rstd"], NORM["t_rstd"]
            B.op("act", lambda e: e.activation(out=rstd, in_=pss[:, :], func=AF.Ln, scale=1.0 / D, bias=eps_n),
                 r=[tss, t_c], w=[t_rstd])
            B.op("act", lambda e: e.activation(out=rstd, in_=rstd, func=AF.Exp, scale=-0.5), r=[t_rstd], w=[t_rstd])
            for kc in range(KC):
                B.op("dve", lambda e, kc=kc: e.scalar_tensor_tensor(
                    out=hs(kc, 1, 513), in0=xs(kc, tt), scalar=pvc(gname, kc), in1=rstd,
                    op0=ALU.mult, op1=ALU.mult), r=[t_x[tt][kc], t_rstd, t_pv], w=[t_hT[kc]])

        def post_norm_residual(tt, gname, pss, tss):
            rstd, t_rstd = NORM["rstd"], NORM["t_rstd"]
            B.op("act", lambda e: e.activation(out=rstd, in_=pss[:, :], func=AF.Ln, scale=1.0 / D, bias=eps_n),
                 r=[tss, t_c], w=[t_rstd])
            B.op("act", lambda e: e.activation(out=rstd, in_=rstd, func=AF.Exp, scale=-0.5), r=[t_rstd], w=[t_rstd])
            for dg in range(KC):
                f = fo[:, dg * TT:(dg + 1) * TT]
                B.op("dve", lambda e, f=f, dg=dg: e.scalar_tensor_tensor(
                    out=f, in0=f, scalar=pvc(gname, dg), in1=rstd, op0=ALU.mult, op1=ALU.mult),
                    r=[t_rstd, t_pv], w=[t_fo[dg]])
                B.op("dve", lambda e, f=f, dg=dg: e.tensor_tensor(out=xs(dg, tt), in0=xs(dg, tt), in1=f, op=ALU.add),
                     r=[t_fo[dg]], w=[t_x[tt][dg]])

        NORM = {}

        for l in range(L):
            B.barrier()
            ar = Arena(arena_t[:, :], ARENA_BYTES)
            NORM["sq"] = [ar.alloc(TT, BF16), ar.alloc(TT, BF16)]
            NORM["t_sq"] = [Tok(), Tok()]
            NORM["rstd"] = ar.alloc(TT, F32)
            NORM["t_rstd"] = Tok()
            base_off = ar.off
            B.dma("sp", pv[:, :], pv_d[l], w=[t_pv])
            B.dma("pool", wlr[:, :], wlr_d[l], w=[t_wlr])
            B.dma("pool", w2a2[:, :], w2a2_d[l], w=[t_w2])
            B.dma("pool", g2[:, :], g2_d[l], w=[t_g2])
            for nm, src, n in [("omuw", "muw", 8), ("omua", "mua", 8), ("omug", "mug", 8), ("omurkv", "murkv", 12),
                               ("omka", "ka", 4)]:
                B.op("dve", lambda e, nm=nm, src=src, n=n: e.tensor_scalar(
                    out=pd[:, PD[nm]:PD[nm] + n], in0=pv[:, PV[src]:PV[src] + n], scalar1=-1.0, scalar2=1.0,
                    op0=ALU.mult, op1=ALU.add), r=[t_pv], w=[t_pd])
            for kc in range(KC):
                for (c0, c1, mn, on) in [(0, 64, "muw", "omuw"), (64, 128, "mua", "omua"), (128, 256, "mug", "omug")]:
                    src = wlr[:, kc * 256 + c0: kc * 256 + c1]
                    B.op("dve", lambda e, src=src, kc=kc, c0=c0, c1=c1, on=on: e.tensor_scalar(
                        out=wlra[:, kc * 256 + c0: kc * 256 + c1], in0=src, scalar1=pdc(on, kc), scalar2=None,
                        op0=ALU.mult), r=[t_wlr, t_pd], w=[t_wlrab])
                    B.op("dve", lambda e, src=src, kc=kc, c0=c0, c1=c1, mn=mn: e.tensor_scalar(
                        out=wlrb[:, kc * 256 + c0: kc * 256 + c1], in0=src, scalar1=pvc(mn, kc), scalar2=None,
                        op0=ALU.mult), r=[t_wlr, t_pv], w=[t_wlrab])
            B.op("dve", lambda e: e.memset(rkvcarry[:, :], 0.0), w=[t_carry])
            B.op("dve", lambda e: e.memset(cu[:, :], 0.0), w=t_cu)
            B.op("dve", lambda e: e.memset(cfu[:, :], 0.0), w=t_cfu)
            B.op("dve", lambda e: e.memset(ST32[:, :], 0.0), w=t_ST)
            B.op("dve", lambda e: e.memset(STb[:, :], 0.0), w=t_ST)
            B.op("dve", lambda e: e.memset(hT[:, :], 0.0), w=t_hT)

            for tt in range(NT):
                B.barrier()
                ar.off = base_off
                if tt > 0:
                    for kc in range(KC):
                        B.op("dve", lambda e, kc=kc: e.tensor_copy(hs(kc, 0, 1), hcarry[:, kc:kc + 1]), r=[t_hc[kc]], w=[t_hT[kc]])
                rmsnorm_to_hT(tt, "premix")
                for kc in range(KC):
                    B.op("dve", lambda e, kc=kc: e.tensor_copy(hcarry[:, kc:kc + 1], hs(kc, 512, 513)), r=[t_hT[kc]], w=[t_hc[kc]])

                ar2 = Arena(fo[:, :], KC * TT * 4)

                def A_(cols, dtype):
                    sz = (cols * (4 if dtype == F32 else 2) + 3) // 4 * 4
                    if ar.off + sz <= ar.n:
                        return ar.alloc(cols, dtype)
                    return ar2.alloc(cols, dtype)
                wins = [A_(KC * 128, BF16) for _ in range(2)]
                t_wins = [Tok() for _ in range(2)]
                raw = [A_(513, F32)]
                t_raw = [Tok()]
                f32t = lambda: A_(TT, F32)
                bft = lambda: A_(TT, BF16)
                r_t, k_t, v_t, sgw, av = f32t(), f32t(), f32t(), f32t(), f32t()
                twa, tg = bft(), bft()
                tA, tB, tC, tD, tE = f32t(), f32t(), f32t(), f32t(), f32t()
                rkb = bft()
                scr2 = tg
                SETS = []
                for _i in range(2):
                    S = dict(bt=bft(), kt=bft(), Bh=bft(), Kh=bft(), vb=bft())
                    S["tk"] = {n: Tok() for n in ["bt", "kt", "Bh", "Kh", "vb"]}
                    SETS.append(S)
                Rm = [bft(), bft()]
                Lm = [bft(), bft()]
                Gm = [bft(), bft()]
                CH = []
                for _i in range(4):
                    S = dict(art=A_(8 * 128, BF16), bonus=bft(), gg=bft(), PC=A_(8, F32), TT=bft(),
                             LakT=bft(), LrbT=bft(), LrkT=bft(), VT=bft(), BhT=bft(), KhT=bft(),
                             Xb=A_(64, BF16), Ub=A_(64, BF16))
                    S["tk"] = {n: Tok() for n in ["art", "bonus", "gg", "PC", "TT", "LakT", "LrbT", "LrkT", "VT", "BhT", "KhT", "Xb", "Ub"]}
                    CH.append(S)
                gA, gB, gC = tE, r_t, k_t
                tk = {n: Tok() for n in ["r", "k", "v", "sgw", "av", "twa", "tg", "A", "B", "C", "D", "E", "rkb",
                                         "R0", "R1", "L0", "L1", "G0", "G1"]}
                tk["scr2"] = tk["tg"]
                tk["gA"], tk["gB"], tk["gC"] = tk["E"], tk["r"], tk["k"]
                XB = (0, 1, 2)
                YB = (3, 4, 5, 6, 7)
                eh = [slice(0, 64), slice(64, 128)]
                v3 = lambda a: a.rearrange("p (c x) -> p c x", c=8)
                mk3 = lambda m, n: m[:, 0:n * 64].rearrange("p (c x) -> p c x", c=n)

                def proj_group(g, consume):
                    si = rr("win", 2)
                    B.dma("pool", wins[si], win_d[l, g], w=[t_wins[si]])
                    ps, tps = getps(XB)
                    for kc in range(KC):
                        B.op("pe", lambda e, kc=kc, ps=ps, si=si: e.matmul(
                            ps[:, :], wins[si][:, kc * 128:(kc + 1) * 128], hs(kc, 1, 513),
                            start=(kc == 0), stop=(kc == KC - 1)), r=[t_wins[si], t_hT[kc]], w=[tps])
                    consume(ps, tps)

                def lowrank(c0, c1, consume):
                    ps, tps = getps(XB)
                    n = 0
                    for kc in range(KC):
                        for (wsb, a, b) in [(wlra, 1, 513), (wlrb, 0, 512)]:
                            B.op("pe", lambda e, kc=kc, wsb=wsb, a=a, b=b, n=n, ps=ps: e.matmul(
                                ps[:, :], wsb[:, kc * 256 + c0: kc * 256 + c1], hs(kc, a, b),
                                start=(n == 0), stop=(n == 2 * KC - 1)), r=[t_wlrab, t_hT[kc]], w=[tps])
                            n += 1
                    consume(ps, tps)

                def x_lowrank():
                    def lr0(ps, tps):
                        B.op("act", lambda e: e.activation(out=twa[0:64, :], in_=ps[0:64, :], func=AF.Tanh),
                             r=[tps], w=[tk["twa"]])
                        B.op("act", lambda e: e.activation(out=twa[64:128, :], in_=ps[64:128, :], func=AF.Copy), r=[tps], w=[tk["twa"]])
                    lowrank(0, 128, lr0)
                    yield

                    def lr1(ps, tps):
                        B.op("act", lambda e: e.activation(out=tg, in_=ps[:, :], func=AF.Sigmoid), r=[tps], w=[tk["tg"]])
                    lowrank(128, 256, lr1)
                    yield

                def x_sc(gi):
                    cuv = lambda a, b: cu[:, gi * 514 + a: gi * 514 + b]
                    csb = tA
                    acc = tB

                    def c_cons(ps, tps):
                        B.op("act", lambda e: e.activation(out=csb, in_=ps[:, :], func=AF.Copy), r=[tps], w=[tk["A"]])
                    proj_group(14 + gi, c_cons)
                    yield

                    def u_cons(ps, tps):
                        B.op("dve", lambda e: e.tensor_tensor(out=cuv(2, 514), in0=ps[:, :], in1=csb, op=ALU.mult),
                             r=[tps, tk["A"]], w=[t_cu[gi]])
                    proj_group(16 + gi, u_cons)
                    yield
                    B.op("dve", lambda e: e.tensor_scalar(
                        out=acc, in0=cuv(0, 512), scalar1=pvc("scw", 0 * 2 + gi), scalar2=None, op0=ALU.mult),
                        r=[t_cu[gi], t_pv], w=[tk["B"]])
                    for kk_ in (1, 2):
                        B.op("dve", lambda e, kk_=kk_: e.scalar_tensor_tensor(
                            out=acc, in0=cuv(kk_, kk_ + 512), scalar=pvc("scw", kk_ * 2 + gi), in1=acc,
                            op0=ALU.mult, op1=ALU.add), r=[t_cu[gi], t_pv], w=[tk["B"]])
                    B.op("dve", lambda e: e.tensor_copy(cuv(0, 2), cuv(512, 514)), w=[t_cu[gi]])
                    yield

                    def b_cons(ps, tps):
                        B.op("dve", lambda e: e.tensor_tensor(
                            out=mixcat[:, (4 + gi) * TT:(5 + gi) * TT], in0=ps[:, :], in1=acc, op=ALU.mult),
                            r=[tps, tk["B"]], w=[t_mix[4 + gi]])
                    proj_group(12 + gi, b_cons)
                    yield

                cres = [tC, tD]
                tcres = [tk["C"], tk["D"]]

                def x_cfall():
                    sg = tA
                    fvs = []
                    for gi in range(2):
                        fv = lambda a_, b_, gi=gi: cfu[:, gi * 542 + a_: gi * 542 + b_]
                        fvs.append(fv)

                        def g_cons(ps, tps):
                            B.op("act", lambda e: e.activation(out=sg, in_=ps[:, :], func=AF.Sigmoid), r=[tps], w=[tk["A"]])
                        proj_group(20 + gi, g_cons)
                        yield

                        def v_cons(ps, tps, gi=gi, fv=fv):
                            B.op("dve", lambda e: e.tensor_tensor(out=fv(30, 542), in0=ps[:, :], in1=sg, op=ALU.mult),
                                 r=[tps, tk["A"]], w=[t_cfu[gi]])
                        proj_group(18 + gi, v_cons)
                        yield
                    for gi in range(2):
                        B.op("dve", lambda e, gi=gi: e.tensor_scalar(
                            out=cres[gi], in0=fvs[gi](0, 512), scalar1=pvc("cfw", gi), scalar2=pvc("cfb", gi),
                            op0=ALU.mult, op1=ALU.add), r=[t_cfu[gi], t_pv], w=[tcres[gi]])
                    for kk_ in range(1, 31):
                        for gi in range(2):
                            B.op("dve", lambda e, kk_=kk_, gi=gi: e.scalar_tensor_tensor(
                                out=cres[gi], in0=fvs[gi](kk_, kk_ + 512), scalar=pvc("cfw", kk_ * 2 + gi), in1=cres[gi],
                                op0=ALU.mult, op1=ALU.add), r=[t_cfu[gi], t_pv], w=[tcres[gi]])
                        yield
                    for gi in range(2):
                        B.op("dve", lambda e, gi=gi: e.tensor_copy(fvs[gi](0, 30), fvs[gi](512, 542)), w=[t_cfu[gi]])
                    yield

                def x_cfln():
                    psm, tpsm = getps(XB)
                    psq, tpsq = getps(XB)
                    for gi in range(2):
                        B.op("act", lambda e, gi=gi: e.activation(out=rkb, in_=cres[gi], func=AF.Copy),
                             r=[tcres[gi]], w=[tk["rkb"]])
                        B.op("pe", lambda e, gi=gi: e.matmul(psm[:, :], ones, rkb, start=(gi == 0), stop=(gi == 1)),
                             r=[tk["rkb"], t_c], w=[tpsm])
                        B.op("act", lambda e, gi=gi: e.activation(out=scr2, in_=cres[gi], func=AF.Square),
                             r=[tcres[gi]], w=[tk["scr2"]])
                        B.op("pe", lambda e, gi=gi: e.matmul(psq[:, :], ones, scr2, start=(gi == 0), stop=(gi == 1)),
                             r=[tk["scr2"], t_c], w=[tpsq])
                        yield
                    B.op("dve", lambda e: e.tensor_scalar(out=tA, in0=psm[:, :], scalar1=1.0 / 256, scalar2=None, op0=ALU.mult),
                         r=[tpsm], w=[tk["A"]])
                    B.op("dve", lambda e: e.tensor_tensor(out=tB, in0=tA, in1=tA, op=ALU.mult), r=[tk["A"]], w=[tk["B"]])
                    B.op("dve", lambda e: e.scalar_tensor_tensor(out=tB, in0=psq[:, :], scalar=1.0 / 256, in1=tB,
                                                                 op0=ALU.mult, op1=ALU.subtract),
                         r=[tpsq], w=[tk["B"]])
                    yield
                    B.op("act", lambda e: e.activation(out=tB, in_=tB, func=AF.Ln, bias=eps_ln), r=[t_c], w=[tk["B"]])
                    B.op("act", lambda e: e.activation(out=tB, in_=tB, func=AF.Exp, scale=-0.5), w=[tk["B"]])
                    yield
                    for gi in range(2):
                        B.op("dve", lambda e, gi=gi: e.tensor_tensor(out=cres[gi], in0=cres[gi], in1=tA, op=ALU.subtract),
                             r=[tk["A"]], w=[tcres[gi]])
                        B.op("dve", lambda e, gi=gi: e.tensor_tensor(out=cres[gi], in0=cres[gi], in1=tB, op=ALU.mult),
                             r=[tk["B"]], w=[tcres[gi]])
                        B.op("act", lambda e, gi=gi: e.activation(
                            out=mixcat[:, (6 + gi) * TT:(7 + gi) * TT], in_=cres[gi], func=AF.Silu,
                            scale=pvc("cflg", gi), bias=pvc("cflb", gi)), r=[tcres[gi], t_pv], w=[t_mix[6 + gi]])
                        yield

                def x_prep(hp):
                    S = {**SETS[hp % 2], **CH[hp]}
                    stk = {**SETS[hp % 2]["tk"], **CH[hp]["tk"]}
                    art, bt_, kt_, Bh, Kh, vb, bonus, gg, PCt = (S[n] for n in ["art", "bt", "kt", "Bh", "Kh", "vb", "bonus", "gg", "PC"])
                    art3 = art.rearrange("p (c x) -> p c x", c=8)
                    ps, tps = getps(XB)
                    B.op("pe", lambda e: e.matmul(ps[:, :], w2a2[0:64, hp * 128:(hp + 1) * 128], twa[0:64, :],
                                                  start=True, stop=True), r=[t_w2, tk["twa"]], w=[tps])
                    B.op("act", lambda e: e.activation(out=sgw, in_=ps[:, :], func=AF.Sigmoid, bias=pvc("w0", hp)),
                         r=[tps, t_pv], w=[tk["sgw"]])
                    ps2, tps2 = getps(XB)
                    B.op("pe", lambda e: e.matmul(ps2[:, :], w2a2[64:128, hp * 128:(hp + 1) * 128], twa[64:128, :],
                                                  start=True, stop=True), r=[t_w2, tk["twa"]], w=[tps2])
                    B.op("act", lambda e: e.activation(out=av, in_=ps2[:, :], func=AF.Sigmoid, bias=pvc("a0", hp)),
                         r=[tps2, t_pv], w=[tk["av"]])
                    ps3, tps3 = getps(XB)
                    B.op("pe", lambda e: e.matmul(ps3[:, :], g2[:, hp * 128:(hp + 1) * 128], tg,
                                                  start=True, stop=True), r=[t_g2, tk["tg"]], w=[tps3])
                    B.op("act", lambda e: e.activation(out=gg, in_=ps3[:, :], func=AF.Copy), r=[tps3], w=[stk["gg"]])
                    yield
                    for (which, dst, tkn) in [(0, r_t, "r"), (1, k_t, "k"), (2, v_t, "v")]:
                        g = which * 4 + hp

                        def rkv_cons(ps, tps, g=g, dst=dst, tkn=tkn):
                            ri = 0
                            rw = raw[ri]
                            B.op("dve", lambda e: e.tensor_copy(rw[:, 0:1], rkvcarry[:, g:g + 1]),
                                 r=[t_carry], w=[t_raw[ri]])
                            B.op("act", lambda e: e.activation(out=rw[:, 1:513], in_=ps[:, :], func=AF.Copy),
                                 r=[tps], w=[t_raw[ri]])
                            B.op("dve", lambda e: e.tensor_copy(rkvcarry[:, g:g + 1], rw[:, 512:513]),
                                 r=[t_raw[ri]], w=[t_carry])
                            B.op("act", lambda e: e.activation(out=dst, in_=rw[:, 0:512], func=AF.Identity, scale=pvc("murkv", g)),
                                 r=[t_raw[ri], t_pv], w=[tk[tkn]])
                            B.op("dve", lambda e: e.scalar_tensor_tensor(
                                out=dst, in0=rw[:, 1:513], scalar=pdc("omurkv", g), in1=dst, op0=ALU.mult, op1=ALU.add),
                                r=[t_raw[ri], t_pd], w=[tk[tkn]])
                        proj_group(g, rkv_cons)
                        yield
                    Pb, Pexb, Pinvb, PCrb = S["VT"], S["BhT"], S["KhT"], S["TT"]
                    tPb, tPexb, tPinvb, tPCrb = stk["VT"], stk["BhT"], stk["KhT"], stk["TT"]
                    rn = raw[0][:, 0:512]
                    B.op("dve", lambda e: e.tensor_tensor_scan(tA, scanm, sgw, 0.0, ALU.mult, ALU.add),
                         r=[tk["sgw"], t_c], w=[tk["A"]])
                    B.op("act", lambda e: e.activation(out=tC, in_=k_t, func=AF.Identity, scale=pvc("kk", hp)),
                         r=[tk["k"], t_pv], w=[tk["C"]])
                    B.op("act", lambda e: e.activation(out=rkb, in_=tC, func=AF.Square), r=[tk["C"]], w=[tk["rkb"]])
                    ps4, tps4 = getps(XB)
                    B.op("pe", lambda e: e.matmul(ps4[:, :], bones, rkb, start=True, stop=True),
                         r=[tk["rkb"], t_c], w=[tps4])
                    B.op("act", lambda e: e.activation(out=vb, in_=v_t, func=AF.Copy), r=[tk["v"]], w=[stk["vb"]])
                    yield
                    B.op("act", lambda e: e.activation(out=Pb, in_=tA, func=AF.Exp, scale=C0), r=[tk["A"]], w=[tPb])
                    B.op("act", lambda e: e.activation(out=Pinvb, in_=tA, func=AF.Exp, scale=-C0), r=[tk["A"]], w=[tPinvb])
                    B.op("act", lambda e: e.activation(out=PCt.rearrange("p (c x) -> p c x", x=1), in_=v3(tA)[:, :, 63:64],
                                                       func=AF.Exp, scale=C0), r=[tk["A"]], w=[stk["PC"]])
                    B.op("dve", lambda e: e.tensor_tensor(out=tD, in0=tA, in1=sgw, op=ALU.subtract),
                         r=[tk["A"], tk["sgw"]], w=[tk["D"]])
                    B.op("dve", lambda e: e.tensor_tensor(out=v3(tB), in0=v3(tA), in1=v3(tA)[:, :, 63:64].to_broadcast([P, 8, 64]),
                                                          op=ALU.subtract), r=[tk["A"]], w=[tk["B"]])
                    yield
                    B.op("act", lambda e: e.activation(out=Pexb, in_=tD, func=AF.Exp, scale=C0), r=[tk["D"]], w=[tPexb])
                    B.op("act", lambda e: e.activation(out=PCrb, in_=tB, func=AF.Exp, scale=-C0), r=[tk["B"]], w=[tPCrb])
                    B.op("act", lambda e: e.activation(out=rn, in_=ps4[:, :], func=AF.Ln, bias=smallc[:, 3:4]), r=[tps4, t_c], w=[t_raw[0]])
                    B.op("act", lambda e: e.activation(out=rn, in_=rn, func=AF.Exp, scale=-0.5), w=[t_raw[0]])
                    B.op("dve", lambda e: e.tensor_tensor(out=art3[:, :, 64:128], in0=v3(r_t), in1=v3(Pb), op=ALU.mult),
                         r=[tk["r"], tPb], w=[stk["art"]])
                    B.op("act", lambda e: e.activation(out=tE, in_=av, func=AF.Identity, scale=pvc("ka", hp), bias=pdc("omka", hp)),
                         r=[tk["av"], t_pv, t_pd], w=[tk["E"]])
                    yield
                    B.op("dve", lambda e: e.tensor_tensor(out=tE, in0=tE, in1=k_t, op=ALU.mult), r=[tk["k"]], w=[tk["E"]])
                    B.op("dve", lambda e: e.tensor_tensor(out=tC, in0=tC, in1=rn, op=ALU.mult), r=[t_raw[0]], w=[tk["C"]])
                    yield
                    B.op("dve", lambda e: e.scalar_tensor_tensor(out=art3[:, :, 0:64], in0=v3(tC), scalar=-1.0, in1=v3(Pexb),
                                                                 op0=ALU.mult, op1=ALU.mult),
                         r=[tk["C"], tPexb], w=[stk["art"]])
                    B.op("dve", lambda e: e.scalar_tensor_tensor(out=rkb, in0=r_t, scalar=pvc("rk", hp), in1=tE,
                                                                 op0=ALU.mult, op1=ALU.mult),
                         r=[tk["r"], tk["E"], t_pv], w=[tk["rkb"]])
                    ps5, tps5 = getps(XB)
                    B.op("pe", lambda e: e.matmul(ps5[:, :], bones, rkb, start=True, stop=True),
                         r=[tk["rkb"], t_c], w=[tps5])
                    yield
                    B.op("dve", lambda e: e.tensor_tensor(out=tC, in0=tC, in1=av, op=ALU.mult), r=[tk["av"]], w=[tk["C"]])
                    B.op("dve", lambda e: e.tensor_tensor(out=kt_, in0=tE, in1=Pinvb, op=ALU.mult),
                         r=[tk["E"], tPinvb], w=[stk["kt"]])
                    yield
                    B.op("dve", lambda e: e.tensor_tensor(out=Kh, in0=tE, in1=PCrb, op=ALU.mult),
                         r=[tk["E"], tPCrb], w=[stk["Kh"]])
                    B.op("dve", lambda e: e.tensor_tensor(out=bt_, in0=tC, in1=Pinvb, op=ALU.mult),
                         r=[tk["C"], tPinvb], w=[stk["bt"]])
                    yield
                    B.op("dve", lambda e: e.tensor_tensor(out=Bh, in0=tC, in1=PCrb, op=ALU.mult),
                         r=[tk["C"], tPCrb], w=[stk["Bh"]])
                    B.op("dve", lambda e: e.tensor_tensor(out=bonus, in0=ps5[:, :], in1=v_t, op=ALU.mult),
                         r=[tps5, tk["v"]], w=[stk["bonus"]])
                    yield

                def ysetup(hp):
                    S = {**SETS[hp % 2], **CH[hp]}
                    stk = {**SETS[hp % 2]["tk"], **CH[hp]["tk"]}
                    return S, stk

                def y2a(hp):
                    S, stk = ysetup(hp)
                    TK = lambda n: stk[n] if n in stk else tk[n]
                    art, bt_, kt_, Bh, Kh, vb = (S[n] for n in ["art", "bt", "kt", "Bh", "Kh", "vb"])
                    VT, BhT, KhT, LakT, LrbT, LrkT, TTf = (S[n] for n in ["VT", "BhT", "KhT", "LakT", "LrbT", "LrkT", "TT"])
                    for (src, sk, dst, dtk) in [(vb, "vb", VT, "VT"), (Bh, "Bh", BhT, "BhT"), (Kh, "Kh", KhT, "KhT")]:
                        ps, tps = getps(YB)
                        for c in range(8):
                            for e_ in range(2):
                                B.op("pe", lambda e, ps=ps, c=c, e_=e_, src=src: e.matmul(
                                    ps[eh[e_], c * 64:(c + 1) * 64], src[eh[e_], c * 64:(c + 1) * 64],
                                    ident[eh[e_], e_ * 64:(e_ + 1) * 64], start=True, stop=True,
                                    tile_position=(64 * e_, 64 * e_)), r=[stk[sk], t_c], w=[tps])
                        B.op("act", lambda e, ps=ps, dst=dst: e.activation(out=dst, in_=ps[:, :], func=AF.Copy),
                             r=[tps], w=[stk[dtk]])
                        yield
                    for (stat, sk, dA, dAk, dB, dBk) in [(bt_, "bt", Rm[0], "R0", LrbT, "LrbT"),
                                                        (kt_, "kt", LakT, "LakT", LrkT, "LrkT")]:
                        for half in range(2):
                            ps, tps = getps(YB)
                            for c4 in range(4):
                                c = half * 4 + c4
                                for e_ in range(2):
                                    B.op("pe", lambda e, ps=ps, c=c, c4=c4, e_=e_, stat=stat: e.matmul(
                                        ps[eh[e_], c4 * 128:(c4 + 1) * 128], stat[eh[e_], c * 64:(c + 1) * 64],
                                        art[eh[e_], c * 128:(c + 1) * 128], start=True, stop=True,
                                        tile_position=(64 * e_, 64 * e_)), r=[stk[sk], stk["art"]], w=[tps])
                            ps3 = ps[:, :].rearrange("p (c x) -> p c x", c=4)
                            B.op("dve", lambda e, ps3=ps3, dA=dA, half=half: e.tensor_tensor(
                                out=v3(dA)[:, half * 4:(half + 1) * 4, :], in0=ps3[:, :, 0:64], in1=mk3(msu, 4), op=ALU.mult),
                                r=[tps, t_c], w=[TK(dAk)])
                            B.op("dve", lambda e, ps3=ps3, dB=dB, half=half: e.tensor_tensor(
                                out=v3(dB)[:, half * 4:(half + 1) * 4, :], in0=ps3[:, :, 64:128], in1=mk3(miu, 4), op=ALU.mult),
                                r=[tps, t_c], w=[TK(dBk)])
                            yield
                    ps, tps = getps(YB)
                    for c in range(8):
                        for e_ in range(2):
                            B.op("pe", lambda e, ps=ps, c=c, e_=e_: e.matmul(
                                ps[eh[e_], c * 64:(c + 1) * 64], art[eh[e_], c * 128: c * 128 + 64],
                                bt_[eh[e_], c * 64:(c + 1) * 64], start=True, stop=True,
                                tile_position=(64 * e_, 64 * e_)), r=[stk["bt"], stk["art"]], w=[tps])
                    B.op("dve", lambda e, ps=ps: e.tensor_tensor(out=Lm[0], in0=ps[:, :], in1=msl, op=ALU.mult),
                         r=[tps, t_c], w=[tk["L0"]])
                    B.op("dve", lambda e: e.tensor_tensor(out=Gm[0], in0=Rm[0], in1=irep, op=ALU.add),
                         r=[tk["R0"], t_c], w=[tk["G0"]])
                    yield
                    cur = 0
                    for lev in range(1, 6):
                        nxt = 1 - cur
                        Rc, Lc, Gc = Rm[cur], Lm[cur], Gm[cur]
                        Rn, Ln, Gn = Rm[nxt], Lm[nxt], Gm[nxt]
                        tRc, tLc, tGc = tk["R%d" % cur], tk["L%d" % cur], tk["G%d" % cur]
                        tRn, tLn, tGn = tk["R%d" % nxt], tk["L%d" % nxt], tk["G%d" % nxt]
                        if lev == 5:
                            Gn, tGn = TTf, stk["TT"]
                        ps, tps = getps(YB)
                        for c in range(8):
                            for e_ in range(2):
                                sl = slice(c * 64, (c + 1) * 64)
                                B.op("pe", lambda e, ps=ps, sl=sl, e_=e_: e.matmul(
                                    ps[eh[e_], sl], Rc[eh[e_], sl], Lc[eh[e_], sl], start=True, stop=True,
                                    tile_position=(64 * e_, 64 * e_)), r=[tRc, tLc], w=[tps])
                        B.op("act", lambda e, ps=ps: e.activation(out=Ln, in_=ps[:, :], func=AF.Copy),
                             r=[tps], w=[tLn])
                        if lev < 5:
                            ps2, tps2 = getps(YB)
                            for c in range(8):
                                for e_ in range(2):
                                    sl = slice(c * 64, (c + 1) * 64)
                                    B.op("pe", lambda e, ps2=ps2, sl=sl, e_=e_: e.matmul(
                                        ps2[eh[e_], sl], Lc[eh[e_], sl], Rc[eh[e_], sl], start=True, stop=True,
                                        tile_position=(64 * e_, 64 * e_)), r=[tRc, tLc], w=[tps2])
                            B.op("dve", lambda e, ps2=ps2: e.tensor_copy(Rn, ps2[:, :]), r=[tps2], w=[tRn])
                        yield
                        ps3_, tps3 = getps(YB)
                        for c in range(8):
                            for e_ in range(2):
                                sl = slice(c * 64, (c + 1) * 64)
                                B.op("pe", lambda e, ps3_=ps3_, sl=sl, e_=e_: e.matmul(
                                    ps3_[eh[e_], sl], Ln[eh[e_], sl], Gc[eh[e_], sl], start=True, stop=True,
                                    tile_position=(64 * e_, 64 * e_)), r=[tLn, tGc], w=[tps3])
                        B.op("dve", lambda e, ps3_=ps3_: e.tensor_tensor(out=Gn, in0=ps3_[:, :], in1=Gc, op=ALU.add),
                             r=[tps3, tGc], w=[tGn])
                        yield
                        cur = nxt

                PSY = {}

                def y_chain(hp):
                    S, stk = ysetup(hp)
                    art, bonus, gg, PCt = (S[n] for n in ["art", "bonus", "gg", "PC"])
                    VT, BhT, KhT, LakT, LrbT, LrkT, TTm, Xb, Ub = (S[n] for n in ["VT", "BhT", "KhT", "LakT", "LrbT", "LrkT", "TT", "Xb", "Ub"])
                    tTT = stk["TT"]
                    YB = (0, 1, 2, 3)
                    psY, tpsY = getps((4 + hp,), hold=True)
                    PSY[hp] = (psY, tpsY)
                    Sb = STb[:, hp * 64:(hp + 1) * 64]
                    S32 = ST32[:, hp * 64:(hp + 1) * 64]
                    tS = t_ST[hp]
                    for c in range(8):
                        sl = slice(c * 64, (c + 1) * 64)
                        psX, tpsX = getps(YB)
                        for e_ in range(2):
                            B.op("pe", lambda e, e_=e_: e.matmul(
                                psX[eh[e_], 0:64], art[eh[e_], c * 128:c * 128 + 64], Sb[eh[e_], :], start=True, stop=False,
                                tile_position=(64 * e_, 64 * e_)), r=[stk["art"], tS], w=[tpsX])
                            B.op("pe", lambda e, e_=e_: e.matmul(
                                psX[eh[e_], 0:64], LakT[eh[e_], sl], VT[eh[e_], sl], start=False, stop=True,
                                tile_position=(64 * e_, 64 * e_)), r=[stk["LakT"], stk["VT"]], w=[tpsX])
                        B.op("act", lambda e: e.activation(out=Xb, in_=psX[:, 0:64], func=AF.Copy),
                             r=[tpsX], w=[stk["Xb"]])
                        yield
                        psU, tpsU = getps(YB)
                        for e_ in range(2):
                            B.op("pe", lambda e, e_=e_: e.matmul(
                                psU[eh[e_], 0:64], TTm[eh[e_], sl], Xb[eh[e_], :], start=True, stop=True,
                                tile_position=(64 * e_, 64 * e_)), r=[tTT, stk["Xb"]], w=[tpsU])
                        B.op("dve", lambda e: e.tensor_copy(Ub, psU[:, 0:64]), r=[tpsU], w=[stk["Ub"]])
                        yield
                        for e_ in range(2):
                            B.op("pe", lambda e, e_=e_: e.matmul(
                                psY[eh[e_], sl], Sb[eh[e_], :], art[eh[e_], c * 128 + 64:(c + 1) * 128], start=True, stop=False,
                                tile_position=(64 * e_, 64 * e_)), r=[tS, stk["art"]], w=[tpsY])
                            B.op("pe", lambda e, e_=e_: e.matmul(
                                psY[eh[e_], sl], Ub[eh[e_], :], LrbT[eh[e_], sl], start=False, stop=False,
                                tile_position=(64 * e_, 64 * e_)), r=[stk["Ub"], stk["LrbT"]], w=[tpsY])
                            B.op("pe", lambda e, e_=e_: e.matmul(
                                psY[eh[e_], sl], VT[eh[e_], sl], LrkT[eh[e_], sl], start=False, stop=True,
                                tile_position=(64 * e_, 64 * e_)), r=[stk["VT"], stk["LrkT"]], w=[tpsY])
                        psS, tpsS = getps(YB)
                        for e_ in range(2):
                            B.op("pe", lambda e, e_=e_: e.matmul(
                                psS[eh[e_], 0:64], BhT[eh[e_], sl], Ub[eh[e_], :], start=True, stop=False,
                                tile_position=(64 * e_, 64 * e_)), r=[stk["BhT"], stk["Ub"]], w=[tpsS])
                            B.op("pe", lambda e, e_=e_: e.matmul(
                                psS[eh[e_], 0:64], KhT[eh[e_], sl], VT[eh[e_], sl], start=False, stop=True,
                                tile_position=(64 * e_, 64 * e_)), r=[stk["KhT"], stk["VT"]], w=[tpsS])
                        B.op("dve", lambda e: e.scalar_tensor_tensor(
                            out=S32, in0=S32, scalar=PCt[:, c:c + 1], in1=psS[:, 0:64], op0=ALU.mult, op1=ALU.add),
                            r=[tpsS, stk["PC"]], w=[tS])
                        B.op("act", lambda e: e.activation(out=Sb, in_=S32, func=AF.Copy), w=[tS])
                        yield

                def y_tail(hp):
                    S, stk = ysetup(hp)
                    bonus, gg, VT, BhT = (S[n] for n in ["bonus", "gg", "VT", "BhT"])
                    YB = (0, 1, 2, 3)
                    psY, tpsY = PSY[hp]
                    B.op("act", lambda e: e.activation(out=gA, in_=psY[:, :], func=AF.Copy), r=[tpsY], w=[tk["gA"]])
                    B.op("act", lambda e: e.activation(out=VT, in_=psY[:, :], func=AF.Copy), r=[tpsY], w=[stk["VT"]])
                    B.op("act", lambda e: e.activation(out=BhT, in_=psY[:, :], func=AF.Square), r=[tpsY], w=[stk["BhT"]])
                    release(psY)
                    yield
                    psm, tpsm = getps(YB)
                    psq, tpsq = getps(YB)
                    B.op("pe", lambda e: e.matmul(psm[:, :], bones, VT, start=True, stop=True),
                         r=[stk["VT"], t_c], w=[tpsm])
                    B.op("pe", lambda e: e.matmul(psq[:, :], bones, BhT, start=True, stop=True),
                         r=[stk["BhT"], t_c], w=[tpsq])
                    B.op("dve", lambda e: e.tensor_scalar(out=gB, in0=psm[:, :], scalar1=1.0 / 64, scalar2=None, op0=ALU.mult),
                         r=[tpsm], w=[tk["gB"]])
                    B.op("dve", lambda e: e.tensor_tensor(out=gC, in0=gB, in1=gB, op=ALU.mult), r=[tk["gB"]], w=[tk["gC"]])
                    B.op("dve", lambda e: e.scalar_tensor_tensor(out=gC, in0=psq[:, :], scalar=1.0 / 64, in1=gC,
                                                                 op0=ALU.mult, op1=ALU.subtract),
                         r=[tpsq], w=[tk["gC"]])
                    yield
                    B.op("act", lambda e: e.activation(out=gC, in_=gC, func=AF.Ln, bias=eps_gn), r=[t_c], w=[tk["gC"]])
                    B.op("act", lambda e: e.activation(out=gC, in_=gC, func=AF.Exp, scale=-0.5), w=[tk["gC"]])
                    B.op("dve", lambda e: e.tensor_tensor(out=gA, in0=gA, in1=gB, op=ALU.subtract), r=[tk["gB"]], w=[tk["gA"]])
                    yield
                    B.op("dve", lambda e: e.tensor_tensor(out=gA, in0=gA, in1=gC, op=ALU.mult), r=[tk["gC"]], w=[tk["gA"]])
                    B.op("act", lambda e: e.activation(out=gA, in_=gA, func=AF.Identity, scale=pvc("lng", hp), bias=pvc("lnb", hp)),
                         r=[t_pv], w=[tk["gA"]])
                    yield
                    B.op("dve", lambda e: e.tensor_tensor(out=gA, in0=gA, in1=bonus, op=ALU.add), r=[stk["bonus"]], w=[tk["gA"]])
                    B.op("dve", lambda e: e.tensor_tensor(out=mixcat[:, hp * TT:(hp + 1) * TT], in0=gA, in1=gg, op=ALU.mult),
                         r=[tk["gA"], stk["gg"]], w=[t_mix[hp]])
                    yield

                def chain_gens(gs):
                    for g_ in gs:
                        yield from g_

                def interleave(gs):
                    gs = list(gs)
                    while gs:
                        for g_ in list(gs):
                            try:
                                next(g_)
                            except StopIteration:
                                gs.remove(g_)
                for _ in x_lowrank():
                    pass
                for _ in x_prep(0):
                    pass
                for hp in range(1, 4):
                    interleave([x_prep(hp), y2a(hp - 1)])
                interleave([chain_gens([x_sc(0), x_sc(1)]), y2a(3)])
                for dg in range(2):
                    B.dma("pool", wos[dg][:, :], wo_d[l, dg], w=[t_wos[dg]])
                XB = (0, 1, 2, 3)
                interleave([y_chain(0), y_chain(1), y_chain(2), y_chain(3), chain_gens([x_cfall(), x_cfln()])])
                for hp in range(4):
                    for _ in y_tail(hp):
                        pass
                B.barrier()

                if dbg and l == 0 and tt == 0:
                    B.dma("pool", dbg_d[:, :], mixcat[:, :], r=t_mix)

                pss, tss = getps(hold=True)
                for dg in range(KC):
                    si = dg % 2
                    if dg >= 2:
                        B.dma("pool", wos[si][:, :], wo_d[l, dg], w=[t_wos[si]])
                    ps, tps = getps()
                    for kc in range(KC):
                        B.op("pe", lambda e, ps=ps, kc=kc, si=si: e.matmul(
                            ps[:, :], wos[si][:, kc * 128:(kc + 1) * 128], mixcat[:, kc * TT:(kc + 1) * TT],
                            start=(kc == 0), stop=(kc == KC - 1)), r=[t_wos[si], t_mix[kc]], w=[tps])
                    B.op("act", lambda e, ps=ps, dg=dg: e.activation(out=fo[:, dg * TT:(dg + 1) * TT], in_=ps[:, :], func=AF.Copy),
                         r=[tps], w=[t_fo[dg]])
                    i = dg % 2
                    B.op("act", lambda e, ps=ps, i=i: e.activation(out=NORM["sq"][i], in_=ps[:, :], func=AF.Square),
                         r=[tps], w=[NORM["t_sq"][i]])
                    B.op("pe", lambda e, i=i, dg=dg, pss=pss: e.matmul(pss[:, :], ones, NORM["sq"][i], start=(dg == 0), stop=(dg == KC - 1)),
                         r=[NORM["t_sq"][i], t_c], w=[tss])
                release(pss)
                post_norm_residual(tt, "postmix", pss, tss)

                B.barrier()
                ar.off = base_off
                act_t = ar.alloc(NJ * TT, BF16)
                t_act = [Tok() for _ in range(NJ)]
                NGUS, NWDS = 5, 3
                gus = [ar.alloc(2 * KC * 128, BF16) for _ in range(NGUS)]
                t_gus = [Tok() for _ in range(NGUS)]
                t_guu = [Tok() for _ in range(NGUS)]
                wds = [ar.alloc(NJ * 128, BF16) for _ in range(NWDS)]
                t_wds = [Tok() for _ in range(NWDS)]
                sil = [ar.alloc(TT, F32), ar.alloc(TT, F32)]
                t_sil = [Tok(), Tok()]
                for dg in range(NWDS):
                    B.dma("pool", wds[dg], wd_d[l, dg], w=[t_wds[dg]])
                rmsnorm_to_hT(tt, "preffn")
                for j in range(NJ):
                    si = rr("gus", NGUS)
                    B.dma("pool", gus[si][:, 0:KC * 128], wg_d[l, j], w=[t_gus[si]])
                    B.dma("pool", gus[si][:, KC * 128:2 * KC * 128], wu_d[l, j], w=[t_guu[si]])
                    psg, tpsg = getps()
                    psu, tpsu = getps()
                    for kc in range(KC):
                        B.op("pe", lambda e, kc=kc, si=si, psg=psg: e.matmul(
                            psg[:, :], gus[si][:, kc * 128:(kc + 1) * 128], hs(kc, 1, 513),
                            start=(kc == 0), stop=(kc == KC - 1)), r=[t_gus[si], t_hT[kc]], w=[tpsg])
                    for kc in range(KC):
                        B.op("pe", lambda e, kc=kc, si=si, psu=psu: e.matmul(
                            psu[:, :], gus[si][:, (KC + kc) * 128:(KC + kc + 1) * 128], hs(kc, 1, 513),
                            start=(kc == 0), stop=(kc == KC - 1)), r=[t_guu[si], t_hT[kc]], w=[tpsu])
                    i = j % 2
                    B.op("act", lambda e, i=i, psg=psg: e.activation(out=sil[i], in_=psg[:, :], func=AF.Silu),
                         r=[tpsg], w=[t_sil[i]])
                    B.op("dve", lambda e, i=i, j=j, psu=psu: e.tensor_tensor(
                        out=act_t[:, j * TT:(j + 1) * TT], in0=psu[:, :], in1=sil[i], op=ALU.mult),
                        r=[tpsu, t_sil[i]], w=[t_act[j]])
                pss, tss = getps(hold=True)
                for dg in range(KC):
                    si = dg % NWDS
                    if dg >= NWDS:
                        B.dma("pool", wds[si], wd_d[l, dg], w=[t_wds[si]])
                    ps, tps = getps()
                    for j in range(NJ):
                        B.op("pe", lambda e, ps=ps, j=j, si=si: e.matmul(
                            ps[:, :], wds[si][:, j * 128:(j + 1) * 128], act_t[:, j * TT:(j + 1) * TT],
                            start=(j == 0), stop=(j == NJ - 1)), r=[t_wds[si], t_act[j]], w=[tps])
                    B.op("act", lambda e, ps=ps, dg=dg: e.activation(out=fo[:, dg * TT:(dg + 1) * TT], in_=ps[:, :], func=AF.Copy),
                         r=[tps], w=[t_fo[dg]])
                    i = dg % 2
                    B.op("act", lambda e, ps=ps, i=i: e.activation(out=NORM["sq"][i], in_=ps[:, :], func=AF.Square),
                         r=[tps], w=[NORM["t_sq"][i]])
                    B.op("pe", lambda e, i=i, dg=dg, pss=pss: e.matmul(pss[:, :], ones, NORM["sq"][i], start=(dg == 0), stop=(dg == KC - 1)),
                         r=[NORM["t_sq"][i], t_c], w=[tss])
                release(pss)
                post_norm_residual(tt, "postffn", pss, tss)

        t_out = Tok()
        for tt in range(NT):
            for kc in range(KC):
                B.dma("sp", y_d[:, kc * T + tt * TT: kc * T + (tt + 1) * TT], xs(kc, tt), r=[t_x[tt][kc]], w=[Tok()])
        B.barrier()

        with nc.Block() as block:
            B.emit(block)
    return nc


def _consts():
    c = np.zeros((P, NCF), np.float32)
    p = np.arange(P)[:, None]
    c[:, CF["ident"]:CF["ident"] + 128] = (p == np.arange(128)[None, :])
    c[:, CF["ones"]:CF["ones"] + 128] = 1.0
    c[:, CF["bones"]:CF["bones"] + 128] = ((p // 64) == (np.arange(128)[None, :] // 64))
    col = (np.arange(512) % 64)[None, :]
    row = p % 64
    c[:, CF["msu"]:CF["msu"] + 512] = (row < col)
    c[:, CF["miu"]:CF["miu"] + 512] = (row <= col)
    c[:, CF["msl"]:CF["msl"] + 512] = (col < row)
    c[:, CF["irep"]:CF["irep"] + 512] = (row == col)
    c[:, CF["scan"]:CF["scan"] + 512] = (col != 0) * np.ones((P, 1))
    return c


def _colvec(v):
    return np.ascontiguousarray(np.asarray(v, np.float32).reshape(-1, P).T)


def _prep_inputs(inp, L):
    f = lambda a: np.asarray(a, np.float32)
    out = {}

    def grp_cols(w, ncol_groups):
        Lw, K, N = w.shape
        a = w.reshape(Lw, K // P, P, N // P, P)
        a = a.transpose(0, 3, 2, 1, 4)
        return np.ascontiguousarray(a.reshape(Lw, N // P, P, (K // P) * P))
    out["w_in"] = grp_cols(f(inp["w_in"])[:L], NJ)
    out["w_gate"] = grp_cols(f(inp["w_gate"])[:L], NJ)
    out["w_up"] = grp_cols(f(inp["w_up"])[:L], NJ)
    out["w_o"] = grp_cols(f(inp["w_o"])[:L], KC)
    out["w_down"] = grp_cols(f(inp["w_down"])[:L], KC)
    wl = np.concatenate([f(inp["w1"]), f(inp["a1"]), f(inp["g1"])], axis=2)[:L]
    wl = wl.reshape(L, KC, P, 256).transpose(0, 2, 1, 3).reshape(L, P, KC * 256)
    out["wlr"] = np.ascontiguousarray(wl)
    out["w2a2"] = np.ascontiguousarray(np.concatenate([f(inp["w2"]), f(inp["a2"])], axis=1)[:L])
    out["g2"] = np.ascontiguousarray(f(inp["g2"])[:L])
    pv = np.zeros((L, P, NPV), np.float32)
    for l in range(L):
        def put(name, arr):
            a = _colvec(arr)
            pv[l, :, PV[name]:PV[name] + a.shape[1]] = a
        put("premix", inp["pre_mix_g"][l]); put("postmix", inp["post_mix_g"][l])
        put("preffn", inp["pre_ffn_g"][l]); put("postffn", inp["post_ffn_g"][l])
        put("muw", inp["mu_wag"][l][0]); put("mua", inp["mu_wag"][l][1]); put("mug", inp["mu_wag"][l][2])
        put("murkv", inp["mu_rkv"][l])
        put("w0", inp["w0"][l]); put("a0", inp["a0"][l]); put("kk", inp["k_k"][l]); put("ka", inp["k_a"][l])
        put("rk", np.asarray(inp["r_k"][l]).reshape(-1)); put("lng", inp["lnx_g"][l]); put("lnb", inp["lnx_b"][l])
        scw = f(inp["sc_conv_w"][l])
        for k_ in range(3):
            pv[l, :, PV["scw"] + k_ * 2: PV["scw"] + k_ * 2 + 2] = _colvec(scw[k_])
        cfw = f(inp["cf_conv_w"][l])
        for k_ in range(31):
            pv[l, :, PV["cfw"] + k_ * 2: PV["cfw"] + k_ * 2 + 2] = _colvec(cfw[k_])
        put("cfb", inp["cf_conv_b"][l]); put("cflg", inp["cf_ln_g"][l]); put("cflb", inp["cf_ln_b"][l])
    out["pv"] = pv
    out["cf"] = _consts()
    return out


_CACHE = {}


def run(inputs, L=4, dbg=False):
    x = np.asarray(inputs["x"], np.float32)
    shared = _prep_inputs(inputs, L)
    key = (L, dbg)
    if key not in _CACHE:
        _CACHE[key] = build_program(L, dbg)
    nc = _CACHE[key]
    in_maps = []
    for b in range(NCORES):
        xt = np.ascontiguousarray(x[b].T.reshape(KC, P, T).transpose(1, 0, 2).reshape(P, KC * T))
        m = dict(shared)
        m["xT"] = xt
        in_maps.append(m)
    res = run_bass_kernel_spmd(nc, in_maps, core_ids=list(range(NCORES)))
    outs = []
    for b in range(NCORES):
        yt = np.asarray(res.results[b]["yT"]).reshape(P, KC, T).transpose(1, 0, 2).reshape(D, T)
        outs.append(yt.T)
    y = np.stack(outs, 0).astype(np.float32)
    if dbg:
        return y, [np.asarray(r["dbg"]) for r in res.results]
    return y


def kernel(**inputs):
    return run(inputs, L=4)
```

```python
import contextlib
import numpy as np
import concourse.bass as bass
import concourse.mybir as mybir
from concourse.bass_utils import run_bass_kernel_spmd

F32 = mybir.dt.float32
BF16 = mybir.dt.bfloat16
AF = mybir.ActivationFunctionType
ALU = mybir.AluOpType

P = 128
T = 2048
D = 1024
TT = 512
NT = T // TT
KC = 8
NJ = 22
NCORES = 8
C0 = -0.6065306597126334
SAME_ENGINE_SYNC = True

PV = {}
_o = 0
for _n, _c in [("premix", 8), ("postmix", 8), ("preffn", 8), ("postffn", 8), ("muw", 8), ("mua", 8),
               ("mug", 8), ("murkv", 12), ("w0", 4), ("a0", 4), ("kk", 4), ("ka", 4), ("rk", 4),
               ("lng", 4), ("lnb", 4), ("scw", 6), ("cfw", 62), ("cfb", 2), ("cflg", 2), ("cflb", 2)]:
    PV[_n] = _o
    _o += _c
NPV = _o
PD = {"omuw": 0, "omua": 8, "omug": 16, "omurkv": 24, "omka": 36}
NPD = 40
CF = {"ident": 0, "ones": 128, "bones": 256, "msu": 384, "miu": 896, "msl": 1408, "irep": 1920, "scan": 2432}
NCF = 2944


class Tok:
    __slots__ = ("w", "r")

    def __init__(self):
        self.w = None
        self.r = []


class _Rec:
    def __init__(self):
        self.calls = []

    def __getattr__(self, name):
        def f(*a, **k):
            self.calls.append((name, a, k))
        return f


class Builder:
    ENGS = ("pe", "act", "dve", "pool", "sp")

    def __init__(self, nc, es, ndma=48):
        self.nc = nc
        self.q = {e: [] for e in self.ENGS}
        self.cnt = {e: 0 for e in self.ENGS}
        self.seen = {e: {} for e in self.ENGS}
        self.sems = {}
        for e in ("pe", "act", "dve", "pool"):
            self.sems[e] = es.enter_context(nc.semaphore("s_" + e))
        self.ndma = ndma
        self.dma_uses = [0] * ndma
        self.dma_next = 0
        for k in range(ndma):
            self.sems["dma%d" % k] = es.enter_context(nc.semaphore("s_dma%d" % k))

    def _collect(self, r, w):
        deps = {}

        def add(d):
            if d is None:
                return
            k, v = d
            if deps.get(k, 0) < v:
                deps[k] = v
        for t in r:
            add(t.w)
        for t in w:
            add(t.w)
            for d in t.r:
                add(d)
        return deps

    def _waits(self, eng, deps):
        out = []
        seen = self.seen[eng]
        for k, v in deps.items():
            if k == eng and (eng == "pe" or not SAME_ENGINE_SYNC):
                continue
            if seen.get(k, 0) >= v:
                continue
            seen[k] = v
            out.append((k, v))
        return out

    def op(self, eng, fn, r=(), w=()):
        rec = _Rec()
        fn(rec)
        assert len(rec.calls) == 1
        call = rec.calls[0]
        fn = lambda e, call=call: getattr(e, call[0])(*call[1], **call[2])
        deps = self._collect(r, w)
        waits = self._waits(eng, deps)
        self.cnt[eng] += 1
        me = (eng, self.cnt[eng])
        self.q[eng].append((fn, waits, (eng, 1)))
        for t in r:
            t.r.append(me)
        for t in w:
            t.w = me
            t.r = []

    def dma(self, qe, out_ap, in_ap, r=(), w=()):
        deps = self._collect(r, w)
        k = self.dma_next
        self.dma_next = (k + 1) % self.ndma
        key = "dma%d" % k
        if self.dma_uses[k] > 0:
            v = 16 * self.dma_uses[k]
            if deps.get(key, 0) < v:
                deps[key] = v
        waits = self._waits(qe, deps)
        self.dma_uses[k] += 1
        me = (key, 16 * self.dma_uses[k])
        self.q[qe].append((lambda e: e.dma_start(out=out_ap, in_=in_ap), waits, (key, 16)))
        for t in r:
            t.r.append(me)
        for t in w:
            t.w = me
            t.r = []

    def wait_only(self, eng, toks):
        deps = self._collect(toks, ())
        waits = self._waits(eng, deps)
        self.q[eng].append((None, waits, None))

    def barrier(self):
        allv = {e: self.cnt[e] for e in ("pe", "act", "dve", "pool") if self.cnt[e] > 0}
        for k in range(self.ndma):
            if self.dma_uses[k] > 0:
                allv["dma%d" % k] = 16 * self.dma_uses[k]
        for e in self.ENGS:
            deps = {k: v for k, v in allv.items() if k != e}
            waits = self._waits(e, deps)
            if waits:
                self.q[e].append((None, waits, None))

    def emit(self, block):
        sems = self.sems

        def run(e, lst):
            for fn, waits, inc in lst:
                for k, v in waits:
                    e.wait_ge(sems[k], v)
                if fn is not None:
                    ins = fn(e)
                    if inc is not None:
                        ins.then_inc(sems[inc[0]], inc[1])

        @block.tensor
        def _(e):
            run(e, self.q["pe"])

        @block.scalar
        def _(e):
            run(e, self.q["act"])

        @block.vector
        def _(e):
            run(e, self.q["dve"])

        @block.gpsimd
        def _(e):
            run(e, self.q["pool"])

        @block.sync
        def _(e):
            run(e, self.q["sp"])


class Arena:
    def __init__(self, ap, nbytes):
        self.ap = ap
        self.n = nbytes
        self.off = 0

    def alloc(self, cols, dtype):
        sz = cols * (4 if dtype == F32 else 2)
        sz = (sz + 3) // 4 * 4
        assert self.off + sz <= self.n, ("arena overflow", self.off, sz, self.n)
        a = self.ap[:, self.off // 4:(self.off + sz) // 4]
        self.off += sz
        if dtype != F32:
            a = a.bitcast(dtype)
            a = a[:, 0:cols]
        return a


def build_program(L=4, dbg=False):
    nc = bass.Bass("TRN2", target_bir_lowering=False)
    dt_in = lambda n, s: nc.dram_tensor(n, s, F32, kind="ExternalInput").ap()
    x_d = dt_in("xT", [P, KC * T])
    win_d = dt_in("w_in", [L, NJ, P, KC * 128])
    wg_d = dt_in("w_gate", [L, NJ, P, KC * 128])
    wu_d = dt_in("w_up", [L, NJ, P, KC * 128])
    wo_d = dt_in("w_o", [L, KC, P, KC * 128])
    wd_d = dt_in("w_down", [L, KC, P, NJ * 128])
    wlr_d = dt_in("wlr", [L, P, KC * 256])
    w2a2_d = dt_in("w2a2", [L, P, 512])
    g2_d = dt_in("g2", [L, P, 512])
    pv_d = dt_in("pv", [L, P, NPV])
    cf_d = dt_in("cf", [P, NCF])
    y_d = nc.dram_tensor("yT", [P, KC * T], F32, kind="ExternalOutput").ap()
    if dbg:
        dbg_d = nc.dram_tensor("dbg", [P, KC * TT], F32, kind="ExternalOutput").ap()

    with contextlib.ExitStack() as es:
        def sb(name, cols, dtype):
            return es.enter_context(nc.sbuf_tensor(name, [P, cols], dtype))
        xT = sb("xT_sb", KC * T, F32)
        pv = sb("pv_sb", NPV, F32)
        pd = sb("pd_sb", NPD, F32)
        cb = sb("cb", NCF, BF16)
        cmask = cb[:, 384:NCF]
        smallc = sb("smallc", 4, F32)
        wlra = sb("wlra", KC * 256, BF16)
        wlrb = sb("wlrb", KC * 256, BF16)
        w2a2 = sb("w2a2_sb", 512, BF16)
        g2 = sb("g2_sb", 512, BF16)
        rkvcarry = sb("rkvcarry", 12, F32)
        cu = sb("cu", 2 * 514, F32)
        cfu = sb("cfu", 2 * 542, F32)
        ST32 = sb("ST32", 4 * 64, F32)
        STb = sb("STb", 4 * 64, BF16)
        hT = sb("hT", KC * 513, BF16)
        hcarry = sb("hcarry", KC, BF16)
        mixcat = sb("mixcat", KC * TT, BF16)
        fo = sb("fo", KC * TT, F32)
        wlr = fo[:, 0:KC * 128].bitcast(BF16)
        wos = [sb("wos%d" % i, KC * 128, BF16) for i in range(2)]
        ARENA_BYTES = 80 * 1024
        arena_t = sb("arena", ARENA_BYTES // 4, F32)
        psum = [es.enter_context(nc.psum_tensor("ps%d" % i, [P, 512], F32)) for i in range(8)]

        B = Builder(nc, es)
        pst = [Tok() for _ in range(8)]
        ps_rr = [0]

        held = set()

        ALLB = tuple(range(8))
        ps_ptr = {}

        def getps(banks=None, hold=False):
            key = banks or ALLB
            while True:
                p_ = ps_ptr.get(key, 0)
                ps_ptr[key] = p_ + 1
                i = key[p_ % len(key)]
                if i not in held:
                    break
            if hold:
                held.add(i)
            return psum[i], pst[i]

        def release(ps):
            for i in range(8):
                if psum[i] is ps:
                    held.discard(i)

        t_x = [[Tok() for _ in range(KC)] for _ in range(NT)]
        t_pv, t_pd, t_c, t_wlr, t_wlrab, t_w2, t_g2 = Tok(), Tok(), Tok(), Tok(), Tok(), Tok(), Tok()
        t_carry, t_cu, t_cfu = Tok(), [Tok(), Tok()], [Tok(), Tok()]
        t_ST = [Tok() for _ in range(4)]
        t_hT, t_mix, t_fo = [Tok() for _ in range(KC)], [Tok() for _ in range(KC)], [Tok() for _ in range(KC)]
        t_wos = [Tok(), Tok()]
        t_hc = [Tok() for _ in range(KC)]

        def pvc(name, i=0):
            o = PV[name] + i
            return pv[:, o:o + 1]

        def pdc(name, i=0):
            o = PD[name] + i
            return pd[:, o:o + 1]

        ident = cb[:, 0:128]
        ones = cb[:, 128:256]
        bones = cb[:, 256:384]
        msu = cmask[:, 0:512]
        miu = cmask[:, 512:1024]
        msl = cmask[:, 1024:1536]
        irep = cmask[:, 1536:2048]
        scanm = cmask[:, 2048:2560]
        eps_n = smallc[:, 0:1]
        eps_ln = smallc[:, 1:2]
        eps_gn = smallc[:, 2:3]

        def xs(kc, tt):
            return xT[:, kc * T + tt * TT: kc * T + (tt + 1) * TT]

        def hs(kc, a, b):
            return hT[:, kc * 513 + a: kc * 513 + b]

        for kc in range(KC):
            for tt in range(NT):
                B.dma("sp", xs(kc, tt), x_d[:, kc * T + tt * TT: kc * T + (tt + 1) * TT], w=[t_x[tt][kc]])
        B.dma("pool", cb[:, :], cf_d[:, :], w=[t_c])
        B.op("dve", lambda e: e.memset(smallc[:, 0:1], 1e-6), w=[t_c])
        B.op("dve", lambda e: e.memset(smallc[:, 1:2], 1e-5), w=[t_c])
        B.op("dve", lambda e: e.memset(smallc[:, 2:3], 64e-5), w=[t_c])
        B.op("dve", lambda e: e.memset(smallc[:, 3:4], 1e-24), w=[t_c])

        wslot_rr = {}

        def rr(name, n):
            i = wslot_rr.get(name, 0)
            wslot_rr[name] = (i + 1) % n
            return i

        def rmsnorm_to_hT(tt, gname):
            pss, tss = getps()
            sq, t_sq = NORM["sq"], NORM["t_sq"]
            for kc in range(KC):
                i = kc % 2
                B.op("act", lambda e, kc=kc, i=i: e.activation(out=sq[i], in_=xs(kc, tt), func=AF.Square),
                     r=[t_x[tt][kc]], w=[t_sq[i]])
                B.op("pe", lambda e, kc=kc, i=i: e.matmul(pss[:, :], ones, sq[i], start=(kc == 0), stop=(kc == KC - 1)),
                     r=[t_sq[i], t_c], w=[tss])
            rstd, t_rstd = NORM["rstd"], NORM["t_rstd"]
            B.op("act", lambda e: e.activation(out=rstd, in_=pss[:, :], func=AF.Ln, scale=1.0 / D, bias=eps_n),
                 r=[tss, t_c], w=[t_rstd])
            B.op("act", lambda e: e.activation(out=rstd, in_=rstd, func=AF.Exp, scale=-0.5), r=[t_rstd], w=[t_rstd])
            for kc in range(KC):
                B.op("dve", lambda e, kc=kc: e.scalar_tensor_tensor(
                    out=hs(kc, 1, 513), in0=xs(kc, tt), scalar=pvc(gname, kc), in1=rstd,
                    op0=ALU.mult, op1=ALU.mult), r=[t_x[tt][kc], t_rstd, t_pv], w=[t_hT[kc]])

        def post_norm_residual(tt, gname, pss, tss):
            rstd, t_rstd = NORM["rstd"], NORM["t_rstd"]
            B.op("act", lambda e: e.activation(out=rstd, in_=pss[:, :], func=AF.Ln, scale=1.0 / D, bias=eps_n),
                 r=[tss, t_c], w=[t_rstd])
            B.op("act", lambda e: e.activation(out=rstd, in_=rstd, func=AF.Exp, scale=-0.5), r=[t_rstd], w=[t_rstd])
            for dg in range(KC):
                f = fo[:, dg * TT:(dg + 1) * TT]
                B.op("dve", lambda e, f=f, dg=dg: e.scalar_tensor_tensor(
                    out=f, in0=f, scalar=pvc(gname, dg), in1=rstd, op0=ALU.mult, op1=ALU.mult),
                    r=[t_rstd, t_pv], w=[t_fo[dg]])
                B.op("dve", lambda e, f=f, dg=dg: e.tensor_tensor(out=xs(dg, tt), in0=xs(dg, tt), in1=f, op=ALU.add),
                     r=[t_fo[dg]], w=[t_x[tt][dg]])

        NORM = {}

        for l in range(L):
            B.barrier()
            ar = Arena(arena_t[:, :], ARENA_BYTES)
            NORM["sq"] = [ar.alloc(TT, BF16), ar.alloc(TT, BF16)]
            NORM["t_sq"] = [Tok(), Tok()]
            NORM["rstd"] = ar.alloc(TT, F32)
            NORM["t_rstd"] = Tok()
            base_off = ar.off
            B.dma("sp", pv[:, :], pv_d[l], w=[t_pv])
            B.dma("pool", wlr[:, :], wlr_d[l], w=[t_wlr])
            B.dma("pool", w2a2[:, :], w2a2_d[l], w=[t_w2])
            B.dma("pool", g2[:, :], g2_d[l], w=[t_g2])
            for nm, src, n in [("omuw", "muw", 8), ("omua", "mua", 8), ("omug", "mug", 8), ("omurkv", "murkv", 12),
                               ("omka", "ka", 4)]:
                B.op("dve", lambda e, nm=nm, src=src, n=n: e.tensor_scalar(
                    out=pd[:, PD[nm]:PD[nm] + n], in0=pv[:, PV[src]:PV[src] + n], scalar1=-1.0, scalar2=1.0,
                    op0=ALU.mult, op1=ALU.add), r=[t_pv], w=[t_pd])
            for kc in range(KC):
                for (c0, c1, mn, on) in [(0, 64, "muw", "omuw"), (64, 128, "mua", "omua"), (128, 256, "mug", "omug")]:
                    src = wlr[:, kc * 256 + c0: kc * 256 + c1]
                    B.op("dve", lambda e, src=src, kc=kc, c0=c0, c1=c1, on=on: e.tensor_scalar(
                        out=wlra[:, kc * 256 + c0: kc * 256 + c1], in0=src, scalar1=pdc(on, kc), scalar2=None,
                        op0=ALU.mult), r=[t_wlr, t_pd], w=[t_wlrab])
                    B.op("dve", lambda e, src=src, kc=kc, c0=c0, c1=c1, mn=mn: e.tensor_scalar(
                        out=wlrb[:, kc * 256 + c0: kc * 256 + c1], in0=src, scalar1=pvc(mn, kc), scalar2=None,
                        op0=ALU.mult), r=[t_wlr, t_pv], w=[t_wlrab])
            B.op("dve", lambda e: e.memset(rkvcarry[:, :], 0.0), w=[t_carry])
            B.op("dve", lambda e: e.memset(cu[:, :], 0.0), w=t_cu)
            B.op("dve", lambda e: e.memset(cfu[:, :], 0.0), w=t_cfu)
            B.op("dve", lambda e: e.memset(ST32[:, :], 0.0), w=t_ST)
            B.op("dve", lambda e: e.memset(STb[:, :], 0.0), w=t_ST)
            B.op("dve", lambda e: e.memset(hT[:, :], 0.0), w=t_hT)

            for tt in range(NT):
                B.barrier()
                ar.off = base_off
                if tt > 0:
                    for kc in range(KC):
                        B.op("dve", lambda e, kc=kc: e.tensor_copy(hs(kc, 0, 1), hcarry[:, kc:kc + 1]), r=[t_hc[kc]], w=[t_hT[kc]])
                rmsnorm_to_hT(tt, "premix")
                for kc in range(KC):
                    B.op("dve", lambda e, kc=kc: e.tensor_copy(hcarry[:, kc:kc + 1], hs(kc, 512, 513)), r=[t_hT[kc]], w=[t_hc[kc]])

                ar2 = Arena(fo[:, :], KC * TT * 4)

                def A_(cols, dtype):
                    sz = (cols * (4 if dtype == F32 else 2) + 3) // 4 * 4
                    if ar.off + sz <= ar.n:
                        return ar.alloc(cols, dtype)
                    return ar2.alloc(cols, dtype)
                wins = [A_(KC * 128, BF16) for _ in range(2)]
                t_wins = [Tok() for _ in range(2)]
                raw = [A_(513, F32)]
                t_raw = [Tok()]
                f32t = lambda: A_(TT, F32)
                bft = lambda: A_(TT, BF16)
                r_t, k_t, v_t, sgw, av = f32t(), f32t(), f32t(), f32t(), f32t()
                twa, tg = bft(), bft()
                tA, tB, tC, tD, tE = f32t(), f32t(), f32t(), f32t(), f32t()
                rkb = bft()
                scr2 = tg
                SETS = []
                for _i in range(2):
                    S = dict(bt=bft(), kt=bft(), Bh=bft(), Kh=bft(), vb=bft())
                    S["tk"] = {n: Tok() for n in ["bt", "kt", "Bh", "Kh", "vb"]}
                    SETS.append(S)
                Rm = [bft(), bft()]
                Lm = [bft(), bft()]
                Gm = [bft(), bft()]
                CH = []
                for _i in range(4):
                    S = dict(art=A_(8 * 128, BF16), bonus=bft(), gg=bft(), PC=A_(8, F32), TT=bft(),
                             LakT=bft(), LrbT=bft(), LrkT=bft(), VT=bft(), BhT=bft(), KhT=bft(),
                             Xb=A_(64, BF16), Ub=A_(64, BF16))
                    S["tk"] = {n: Tok() for n in ["art", "bonus", "gg", "PC", "TT", "LakT", "LrbT", "LrkT", "VT", "BhT", "KhT", "Xb", "Ub"]}
                    CH.append(S)
                gA, gB, gC = tE, r_t, k_t
                tk = {n: Tok() for n in ["r", "k", "v", "sgw", "av", "twa", "tg", "A", "B", "C", "D", "E", "rkb",
                                         "R0", "R1", "L0", "L1", "G0", "G1"]}
                tk["scr2"] = tk["tg"]
                tk["gA"], tk["gB"], tk["gC"] = tk["E"], tk["r"], tk["k"]
                XB = (0, 1, 2)
                YB = (3, 4, 5, 6, 7)
                eh = [slice(0, 64), slice(64, 128)]
                v3 = lambda a: a.rearrange("p (c x) -> p c x", c=8)
                mk3 = lambda m, n: m[:, 0:n * 64].rearrange("p (c x) -> p c x", c=n)

                def proj_group(g, consume):
                    si = rr("win", 2)
                    B.dma("pool", wins[si], win_d[l, g], w=[t_wins[si]])
                    ps, tps = getps(XB)
                    for kc in range(KC):
                        B.op("pe", lambda e, kc=kc, ps=ps, si=si: e.matmul(
                            ps[:, :], wins[si][:, kc * 128:(kc + 1) * 128], hs(kc, 1, 513),
                            start=(kc == 0), stop=(kc == KC - 1)), r=[t_wins[si], t_hT[kc]], w=[tps])
                    consume(ps, tps)

                def lowrank(c0, c1, consume):
                    ps, tps = getps(XB)
                    n = 0
                    for kc in range(KC):
                        for (wsb, a, b) in [(wlra, 1, 513), (wlrb, 0, 512)]:
                            B.op("pe", lambda e, kc=kc, wsb=wsb, a=a, b=b, n=n, ps=ps: e.matmul(
                                ps[:, :], wsb[:, kc * 256 + c0: kc * 256 + c1], hs(kc, a, b),
                                start=(n == 0), stop=(n == 2 * KC - 1)), r=[t_wlrab, t_hT[kc]], w=[tps])
                            n += 1
                    consume(ps, tps)

                def x_lowrank():
                    def lr0(ps, tps):
                        B.op("act", lambda e: e.activation(out=twa[0:64, :], in_=ps[0:64, :], func=AF.Tanh),
                             r=[tps], w=[tk["twa"]])
                        B.op("act", lambda e: e.activation(out=twa[64:128, :], in_=ps[64:128, :], func=AF.Copy), r=[tps], w=[tk["twa"]])
                    lowrank(0, 128, lr0)
                    yield

                    def lr1(ps, tps):
                        B.op("act", lambda e: e.activation(out=tg, in_=ps[:, :], func=AF.Sigmoid), r=[tps], w=[tk["tg"]])
                    lowrank(128, 256, lr1)
                    yield

                def x_sc(gi):
                    cuv = lambda a, b: cu[:, gi * 514 + a: gi * 514 + b]
                    csb = tA
                    acc = tB

                    def c_cons(ps, tps):
                        B.op("act", lambda e: e.activation(out=csb, in_=ps[:, :], func=AF.Copy), r=[tps], w=[tk["A"]])
                    proj_group(14 + gi, c_cons)
                    yield

                    def u_cons(ps, tps):
                        B.op("dve", lambda e: e.tensor_tensor(out=cuv(2, 514), in0=ps[:, :], in1=csb, op=ALU.mult),
                             r=[tps, tk["A"]], w=[t_cu[gi]])
                    proj_group(16 + gi, u_cons)
                    yield
                    B.op("dve", lambda e: e.tensor_scalar(
                        out=acc, in0=cuv(0, 512), scalar1=pvc("scw", 0 * 2 + gi), scalar2=None, op0=ALU.mult),
                        r=[t_cu[gi], t_pv], w=[tk["B"]])
                    for kk_ in (1, 2):
                        B.op("dve", lambda e, kk_=kk_: e.scalar_tensor_tensor(
                            out=acc, in0=cuv(kk_, kk_ + 512), scalar=pvc("scw", kk_ * 2 + gi), in1=acc,
                            op0=ALU.mult, op1=ALU.add), r=[t_cu[gi], t_pv], w=[tk["B"]])
                    B.op("dve", lambda e: e.tensor_copy(cuv(0, 2), cuv(512, 514)), w=[t_cu[gi]])
                    yield

                    def b_cons(ps, tps):
                        B.op("dve", lambda e: e.tensor_tensor(
                            out=mixcat[:, (4 + gi) * TT:(5 + gi) * TT], in0=ps[:, :], in1=acc, op=ALU.mult),
                            r=[tps, tk["B"]], w=[t_mix[4 + gi]])
                    proj_group(12 + gi, b_cons)
                    yield

                cres = [tC, tD]
                tcres = [tk["C"], tk["D"]]

                def x_cfall():
                    sg = tA
                    fvs = []
                    for gi in range(2):
                        fv = lambda a_, b_, gi=gi: cfu[:, gi * 542 + a_: gi * 542 + b_]
                        fvs.append(fv)

                        def g_cons(ps, tps):
                            B.op("act", lambda e: e.activation(out=sg, in_=ps[:, :], func=AF.Sigmoid), r=[tps], w=[tk["A"]])
                        proj_group(20 + gi, g_cons)
                        yield

                        def v_cons(ps, tps, gi=gi, fv=fv):
                            B.op("dve", lambda e: e.tensor_tensor(out=fv(30, 542), in0=ps[:, :], in1=sg, op=ALU.mult),
                                 r=[tps, tk["A"]], w=[t_cfu[gi]])
                        proj_group(18 + gi, v_cons)
                        yield
                    for gi in range(2):
                        B.op("dve", lambda e, gi=gi: e.tensor_scalar(
                            out=cres[gi], in0=fvs[gi](0, 512), scalar1=pvc("cfw", gi), scalar2=pvc("cfb", gi),
                            op0=ALU.mult, op1=ALU.add), r=[t_cfu[gi], t_pv], w=[tcres[gi]])
                    for kk_ in range(1, 31):
                        for gi in range(2):
                            B.op("dve", lambda e, kk_=kk_, gi=gi: e.scalar_tensor_tensor(
                                out=cres[gi], in0=fvs[gi](kk_, kk_ + 512), scalar=pvc("cfw", kk_ * 2 + gi), in1=cres[gi],
                                op0=ALU.mult, op1=ALU.add), r=[t_cfu[gi], t_pv], w=[tcres[gi]])
                        yield
                    for gi in range(2):
                        B.op("dve", lambda e, gi=gi: e.tensor_copy(fvs[gi](0, 30), fvs[gi](512, 542)), w=[t_cfu[gi]])
                    yield

                def x_cfln():
                    psm, tpsm = getps(XB)
                    psq, tpsq = getps(XB)
                    for gi in range(2):
                        B.op("act", lambda e, gi=gi: e.activation(out=rkb, in_=cres[gi], func=AF.Copy),
                             r=[tcres[gi]], w=[tk["rkb"]])
                        B.op("pe", lambda e, gi=gi: e.matmul(psm[:, :], ones, rkb, start=(gi == 0), stop=(gi == 1)),
                             r=[tk["rkb"], t_c], w=[tpsm])
                        B.op("act", lambda e, gi=gi: e.activation(out=scr2, in_=cres[gi], func=AF.Square),
                             r=[tcres[gi]], w=[tk["scr2"]])
                        B.op("pe", lambda e, gi=gi: e.matmul(psq[:, :], ones, scr2, start=(gi == 0), stop=(gi == 1)),
                             r=[tk["scr2"], t_c], w=[tpsq])
                        yield
                    B.op("dve", lambda e: e.tensor_scalar(out=tA, in0=psm[:, :], scalar1=1.0 / 256, scalar2=None, op0=ALU.mult),
                         r=[tpsm], w=[tk["A"]])
                    B.op("dve", lambda e: e.tensor_tensor(out=tB, in0=tA, in1=tA, op=ALU.mult), r=[tk["A"]], w=[tk["B"]])
                    B.op("dve", lambda e: e.scalar_tensor_tensor(out=tB, in0=psq[:, :], scalar=1.0 / 256, in1=tB,
                                                                 op0=ALU.mult, op1=ALU.subtract),
                         r=[tpsq], w=[tk["B"]])
                    yield
                    B.op("act", lambda e: e.activation(out=tB, in_=tB, func=AF.Ln, bias=eps_ln), r=[t_c], w=[tk["B"]])
                    B.op("act", lambda e: e.activation(out=tB, in_=tB, func=AF.Exp, scale=-0.5), w=[tk["B"]])
                    yield
                    for gi in range(2):
                        B.op("dve", lambda e, gi=gi: e.tensor_tensor(out=cres[gi], in0=cres[gi], in1=tA, op=ALU.subtract),
                             r=[tk["A"]], w=[tcres[gi]])
                        B.op("dve", lambda e, gi=gi: e.tensor_tensor(out=cres[gi], in0=cres[gi], in1=tB, op=ALU.mult),
                             r=[tk["B"]], w=[tcres[gi]])
                        B.op("act", lambda e, gi=gi: e.activation(
                            out=mixcat[:, (6 + gi) * TT:(7 + gi) * TT], in_=cres[gi], func=AF.Silu,
                            scale=pvc("cflg", gi), bias=pvc("cflb", gi)), r=[tcres[gi], t_pv], w=[t_mix[6 + gi]])
                        yield

                def x_prep(hp):
                    S = {**SETS[hp % 2], **CH[hp]}
                    stk = {**SETS[hp % 2]["tk"], **CH[hp]["tk"]}
                    art, bt_, kt_, Bh, Kh, vb, bonus, gg, PCt = (S[n] for n in ["art", "bt", "kt", "Bh", "Kh", "vb", "bonus", "gg", "PC"])
                    art3 = art.rearrange("p (c x) -> p c x", c=8)
                    ps, tps = getps(XB)
                    B.op("pe", lambda e: e.matmul(ps[:, :], w2a2[0:64, hp * 128:(hp + 1) * 128], twa[0:64, :],
                                                  start=True, stop=True), r=[t_w2, tk["twa"]], w=[tps])
                    B.op("act", lambda e: e.activation(out=sgw, in_=ps[:, :], func=AF.Sigmoid, bias=pvc("w0", hp)),
                         r=[tps, t_pv], w=[tk["sgw"]])
                    ps2, tps2 = getps(XB)
                    B.op("pe", lambda e: e.matmul(ps2[:, :], w2a2[64:128, hp * 128:(hp + 1) * 128], twa[64:128, :],
                                                  start=True, stop=True), r=[t_w2, tk["twa"]], w=[tps2])
                    B.op("act", lambda e: e.activation(out=av, in_=ps2[:, :], func=AF.Sigmoid, bias=pvc("a0", hp)),
                         r=[tps2, t_pv], w=[tk["av"]])
                    ps3, tps3 = getps(XB)
                    B.op("pe", lambda e: e.matmul(ps3[:, :], g2[:, hp * 128:(hp + 1) * 128], tg,
                                                  start=True, stop=True), r=[t_g2, tk["tg"]], w=[tps3])
                    B.op("act", lambda e: e.activation(out=gg, in_=ps3[:, :], func=AF.Copy), r=[tps3], w=[stk["gg"]])
                    yield
                    for (which, dst, tkn) in [(0, r_t, "r"), (1, k_t, "k"), (2, v_t, "v")]:
                        g = which * 4 + hp

                        def rkv_cons(ps, tps, g=g, dst=dst, tkn=tkn):
                            ri = 0
                            rw = raw[ri]
                            B.op("dve", lambda e: e.tensor_copy(rw[:, 0:1], rkvcarry[:, g:g + 1]),
                                 r=[t_carry], w=[t_raw[ri]])
                            B.op("act", lambda e: e.activation(out=rw[:, 1:513], in_=ps[:, :], func=AF.Copy),
                                 r=[tps], w=[t_raw[ri]])
                            B.op("dve", lambda e: e.tensor_copy(rkvcarry[:, g:g + 1], rw[:, 512:513]),
                                 r=[t_raw[ri]], w=[t_carry])
                            B.op("act", lambda e: e.activation(out=dst, in_=rw[:, 0:512], func=AF.Identity, scale=pvc("murkv", g)),
                                 r=[t_raw[ri], t_pv], w=[tk[tkn]])
                            B.op("dve", lambda e: e.scalar_tensor_tensor(
                                out=dst, in0=rw[:, 1:513], scalar=pdc("omurkv", g), in1=dst, op0=ALU.mult, op1=ALU.add),
                                r=[t_raw[ri], t_pd], w=[tk[tkn]])
                        proj_group(g, rkv_cons)
                        yield
                    Pb, Pexb, Pinvb, PCrb = S["VT"], S["BhT"], S["KhT"], S["TT"]
                    tPb, tPexb, tPinvb, tPCrb = stk["VT"], stk["BhT"], stk["KhT"], stk["TT"]
                    rn = raw[0][:, 0:512]
                    B.op("dve", lambda e: e.tensor_tensor_scan(tA, scanm, sgw, 0.0, ALU.mult, ALU.add),
                         r=[tk["sgw"], t_c], w=[tk["A"]])
                    B.op("act", lambda e: e.activation(out=tC, in_=k_t, func=AF.Identity, scale=pvc("kk", hp)),
                         r=[tk["k"], t_pv], w=[tk["C"]])
                    B.op("act", lambda e: e.activation(out=rkb, in_=tC, func=AF.Square), r=[tk["C"]], w=[tk["rkb"]])
                    ps4, tps4 = getps(XB)
                    B.op("pe", lambda e: e.matmul(ps4[:, :], bones, rkb, start=True, stop=True),
                         r=[tk["rkb"], t_c], w=[tps4])
                    B.op("act", lambda e: e.activation(out=vb, in_=v_t, func=AF.Copy), r=[tk["v"]], w=[stk["vb"]])
                    yield
                    B.op("act", lambda e: e.activation(out=Pb, in_=tA, func=AF.Exp, scale=C0), r=[tk["A"]], w=[tPb])
                    B.op("act", lambda e: e.activation(out=Pinvb, in_=tA, func=AF.Exp, scale=-C0), r=[tk["A"]], w=[tPinvb])
                    B.op("act", lambda e: e.activation(out=PCt.rearrange("p (c x) -> p c x", x=1), in_=v3(tA)[:, :, 63:64],
                                                       func=AF.Exp, scale=C0), r=[tk["A"]], w=[stk["PC"]])
                    B.op("dve", lambda e: e.tensor_tensor(out=tD, in0=tA, in1=sgw, op=ALU.subtract),
                         r=[tk["A"], tk["sgw"]], w=[tk["D"]])
                    B.op("dve", lambda e: e.tensor_tensor(out=v3(tB), in0=v3(tA), in1=v3(tA)[:, :, 63:64].to_broadcast([P, 8, 64]),
                                                          op=ALU.subtract), r=[tk["A"]], w=[tk["B"]])
                    yield
                    B.op("act", lambda e: e.activation(out=Pexb, in_=tD, func=AF.Exp, scale=C0), r=[tk["D"]], w=[tPexb])
                    B.op("act", lambda e: e.activation(out=PCrb, in_=tB, func=AF.Exp, scale=-C0), r=[tk["B"]], w=[tPCrb])
                    B.op("act", lambda e: e.activation(out=rn, in_=ps4[:, :], func=AF.Ln, bias=smallc[:, 3:4]), r=[tps4, t_c], w=[t_raw[0]])
                    B.op("act", lambda e: e.activation(out=rn, in_=rn, func=AF.Exp, scale=-0.5), w=[t_raw[0]])
                    B.op("dve", lambda e: e.tensor_tensor(out=art3[:, :, 64:128], in0=v3(r_t), in1=v3(Pb), op=ALU.mult),
                         r=[tk["r"], tPb], w=[stk["art"]])
                    B.op("act", lambda e: e.activation(out=tE, in_=av, func=AF.Identity, scale=pvc("ka", hp), bias=pdc("omka", hp)),
                         r=[tk["av"], t_pv, t_pd], w=[tk["E"]])
                    yield
                    B.op("dve", lambda e: e.tensor_tensor(out=tE, in0=tE, in1=k_t, op=ALU.mult), r=[tk["k"]], w=[tk["E"]])
                    B.op("dve", lambda e: e.tensor_tensor(out=tC, in0=tC, in1=rn, op=ALU.mult), r=[t_raw[0]], w=[tk["C"]])
                    yield
                    B.op("dve", lambda e: e.scalar_tensor_tensor(out=art3[:, :, 0:64], in0=v3(tC), scalar=-1.0, in1=v3(Pexb),
                                                                 op0=ALU.mult, op1=ALU.mult),
                         r=[tk["C"], tPexb], w=[stk["art"]])
                    B.op("dve", lambda e: e.scalar_tensor_tensor(out=rkb, in0=r_t, scalar=pvc("rk", hp), in1=tE,
                                                                 op0=ALU.mult, op1=ALU.mult),
                         r=[tk["r"], tk["E"], t_pv], w=[tk["rkb"]])
                    ps5, tps5 = getps(XB)
                    B.op("pe", lambda e: e.matmul(ps5[:, :], bones, rkb, start=True, stop=True),
                         r=[tk["rkb"], t_c], w=[tps5])
                    yield
                    B.op("dve", lambda e: e.tensor_tensor(out=tC, in0=tC, in1=av, op=ALU.mult), r=[tk["av"]], w=[tk["C"]])
                    B.op("dve", lambda e: e.tensor_tensor(out=kt_, in0=tE, in1=Pinvb, op=ALU.mult),
                         r=[tk["E"], tPinvb], w=[stk["kt"]])
                    yield
                    B.op("dve", lambda e: e.tensor_tensor(out=Kh, in0=tE, in1=PCrb, op=ALU.mult),
                         r=[tk["E"], tPCrb], w=[stk["Kh"]])
                    B.op("dve", lambda e: e.tensor_tensor(out=bt_, in0=tC, in1=Pinvb, op=ALU.mult),
                         r=[tk["C"], tPinvb], w=[stk["bt"]])
                    yield
                    B.op("dve", lambda e: e.tensor_tensor(out=Bh, in0=tC, in1=PCrb, op=ALU.mult),
                         r=[tk["C"], tPCrb], w=[stk["Bh"]])
                    B.op("dve", lambda e: e.tensor_tensor(out=bonus, in0=ps5[:, :], in1=v_t, op=ALU.mult),
                         r=[tps5, tk["v"]], w=[stk["bonus"]])
                    yield

                def ysetup(hp):
                    S = {**SETS[hp % 2], **CH[hp]}
                    stk = {**SETS[hp % 2]["tk"], **CH[hp]["tk"]}
                    return S, stk

                def y2a(hp):
                    S, stk = ysetup(hp)
                    TK = lambda n: stk[n] if n in stk else tk[n]
                    art, bt_, kt_, Bh, Kh, vb = (S[n] for n in ["art", "bt", "kt", "Bh", "Kh", "vb"])
                    VT, BhT, KhT, LakT, LrbT, LrkT, TTf = (S[n] for n in ["VT", "BhT", "KhT", "LakT", "LrbT", "LrkT", "TT"])
                    for (src, sk, dst, dtk) in [(vb, "vb", VT, "VT"), (Bh, "Bh", BhT, "BhT"), (Kh, "Kh", KhT, "KhT")]:
                        ps, tps = getps(YB)
                        for c in range(8):
                            for e_ in range(2):
                                B.op("pe", lambda e, ps=ps, c=c, e_=e_, src=src: e.matmul(
                                    ps[eh[e_], c * 64:(c + 1) * 64], src[eh[e_], c * 64:(c + 1) * 64],
                                    ident[eh[e_], e_ * 64:(e_ + 1) * 64], start=True, stop=True,
                                    tile_position=(64 * e_, 64 * e_)), r=[stk[sk], t_c], w=[tps])
                        B.op("act", lambda e, ps=ps, dst=dst: e.activation(out=dst, in_=ps[:, :], func=AF.Copy),
                             r=[tps], w=[stk[dtk]])
                        yield
                    for (stat, sk, dA, dAk, dB, dBk) in [(bt_, "bt", Rm[0], "R0", LrbT, "LrbT"),
                                                        (kt_, "kt", LakT, "LakT", LrkT, "LrkT")]:
                        for half in range(2):
                            ps, tps = getps(YB)
                            for c4 in range(4):
                                c = half * 4 + c4
                                for e_ in range(2):
                                    B.op("pe", lambda e, ps=ps, c=c, c4=c4, e_=e_, stat=stat: e.matmul(
                                        ps[eh[e_], c4 * 128:(c4 + 1) * 128], stat[eh[e_], c * 64:(c + 1) * 64],
                                        art[eh[e_], c * 128:(c + 1) * 128], start=True, stop=True,
                                        tile_position=(64 * e_, 64 * e_)), r=[stk[sk], stk["art"]], w=[tps])
                            ps3 = ps[:, :].rearrange("p (c x) -> p c x", c=4)
                            B.op("dve", lambda e, ps3=ps3, dA=dA, half=half: e.tensor_tensor(
                                out=v3(dA)[:, half * 4:(half + 1) * 4, :], in0=ps3[:, :, 0:64], in1=mk3(msu, 4), op=ALU.mult),
                                r=[tps, t_c], w=[TK(dAk)])
                            B.op("dve", lambda e, ps3=ps3, dB=dB, half=half: e.tensor_tensor(
                                out=v3(dB)[:, half * 4:(half + 1) * 4, :], in0=ps3[:, :, 64:128], in1=mk3(miu, 4), op=ALU.mult),
                                r=[tps, t_c], w=[TK(dBk)])
                            yield
                    ps, tps = getps(YB)
                    for c in range(8):
                        for e_ in range(2):
                            B.op("pe", lambda e, ps=ps, c=c, e_=e_: e.matmul(
                                ps[eh[e_], c * 64:(c + 1) * 64], art[eh[e_], c * 128: c * 128 + 64],
                                bt_[eh[e_], c * 64:(c + 1) * 64], start=True, stop=True,
                                tile_position=(64 * e_, 64 * e_)), r=[stk["bt"], stk["art"]], w=[tps])
                    B.op("dve", lambda e, ps=ps: e.tensor_tensor(out=Lm[0], in0=ps[:, :], in1=msl, op=ALU.mult),
                         r=[tps, t_c], w=[tk["L0"]])
                    B.op("dve", lambda e: e.tensor_tensor(out=Gm[0], in0=Rm[0], in1=irep, op=ALU.add),
                         r=[tk["R0"], t_c], w=[tk["G0"]])
                    yield
                    cur = 0
                    for lev in range(1, 6):
                        nxt = 1 - cur
                        Rc, Lc, Gc = Rm[cur], Lm[cur], Gm[cur]
                        Rn, Ln, Gn = Rm[nxt], Lm[nxt], Gm[nxt]
                        tRc, tLc, tGc = tk["R%d" % cur], tk["L%d" % cur], tk["G%d" % cur]
                        tRn, tLn, tGn = tk["R%d" % nxt], tk["L%d" % nxt], tk["G%d" % nxt]
                        if lev == 5:
                            Gn, tGn = TTf, stk["TT"]
                        ps, tps = getps(YB)
                        for c in range(8):
                            for e_ in range(2):
                                sl = slice(c * 64, (c + 1) * 64)
                                B.op("pe", lambda e, ps=ps, sl=sl, e_=e_: e.matmul(
                                    ps[eh[e_], sl], Rc[eh[e_], sl], Lc[eh[e_], sl], start=True, stop=True,
                                    tile_position=(64 * e_, 64 * e_)), r=[tRc, tLc], w=[tps])
                        B.op("act", lambda e, ps=ps: e.activation(out=Ln, in_=ps[:, :], func=AF.Copy),
                             r=[tps], w=[tLn])
                        if lev < 5:
                            ps2, tps2 = getps(YB)
                            for c in range(8):
                                for e_ in range(2):
                                    sl = slice(c * 64, (c + 1) * 64)
                                    B.op("pe", lambda e, ps2=ps2, sl=sl, e_=e_: e.matmul(
                                        ps2[eh[e_], sl], Lc[eh[e_], sl], Rc[eh[e_], sl], start=True, stop=True,
                                        tile_position=(64 * e_, 64 * e_)), r=[tRc, tLc], w=[tps2])
                            B.op("dve", lambda e, ps2=ps2: e.tensor_copy(Rn, ps2[:, :]), r=[tps2], w=[tRn])
                        yield
                        ps3_, tps3 = getps(YB)
                        for c in range(8):
                            for e_ in range(2):
                                sl = slice(c * 64, (c + 1) * 64)
                                B.op("pe", lambda e, ps3_=ps3_, sl=sl, e_=e_: e.matmul(
                                    ps3_[eh[e_], sl], Ln[eh[e_], sl], Gc[eh[e_], sl], start=True, stop=True,
                                    tile_position=(64 * e_, 64 * e_)), r=[tLn, tGc], w=[tps3])
                        B.op("dve", lambda e, ps3_=ps3_: e.tensor_tensor(out=Gn, in0=ps3_[:, :], in1=Gc, op=ALU.add),
                             r=[tps3, tGc], w=[tGn])
                        yield
                        cur = nxt

                PSY = {}

                def y_chain(hp):
                    S, stk = ysetup(hp)
                    art, bonus, gg, PCt = (S[n] for n in ["art", "bonus", "gg", "PC"])
                    VT, BhT, KhT, LakT, LrbT, LrkT, TTm, Xb, Ub = (S[n] for n in ["VT", "BhT", "KhT", "LakT", "LrbT", "LrkT", "TT", "Xb", "Ub"])
                    tTT = stk["TT"]
                    YB = (0, 1, 2, 3)
                    psY, tpsY = getps((4 + hp,), hold=True)
                    PSY[hp] = (psY, tpsY)
                    Sb = STb[:, hp * 64:(hp + 1) * 64]
                    S32 = ST32[:, hp * 64:(hp + 1) * 64]
                    tS = t_ST[hp]
                    for c in range(8):
                        sl = slice(c * 64, (c + 1) * 64)
                        psX, tpsX = getps(YB)
                        for e_ in range(2):
                            B.op("pe", lambda e, e_=e_: e.matmul(
                                psX[eh[e_], 0:64], art[eh[e_], c * 128:c * 128 + 64], Sb[eh[e_], :], start=True, stop=False,
                                tile_position=(64 * e_, 64 * e_)), r=[stk["art"], tS], w=[tpsX])
                            B.op("pe", lambda e, e_=e_: e.matmul(
                                psX[eh[e_], 0:64], LakT[eh[e_], sl], VT[eh[e_], sl], start=False, stop=True,
                                tile_position=(64 * e_, 64 * e_)), r=[stk["LakT"], stk["VT"]], w=[tpsX])
                        B.op("act", lambda e: e.activation(out=Xb, in_=psX[:, 0:64], func=AF.Copy),
                             r=[tpsX], w=[stk["Xb"]])
                        yield
                        psU, tpsU = getps(YB)
                        for e_ in range(2):
                            B.op("pe", lambda e, e_=e_: e.matmul(
                                psU[eh[e_], 0:64], TTm[eh[e_], sl], Xb[eh[e_], :], start=True, stop=True,
                                tile_position=(64 * e_, 64 * e_)), r=[tTT, stk["Xb"]], w=[tpsU])
                        B.op("dve", lambda e: e.tensor_copy(Ub, psU[:, 0:64]), r=[tpsU], w=[stk["Ub"]])
                        yield
                        for e_ in range(2):
                            B.op("pe", lambda e, e_=e_: e.matmul(
                                psY[eh[e_], sl], Sb[eh[e_], :], art[eh[e_], c * 128 + 64:(c + 1) * 128], start=True, stop=False,
                                tile_position=(64 * e_, 64 * e_)), r=[tS, stk["art"]], w=[tpsY])
                            B.op("pe", lambda e, e_=e_: e.matmul(
                                psY[eh[e_], sl], Ub[eh[e_], :], LrbT[eh[e_], sl], start=False, stop=False,
                                tile_position=(64 * e_, 64 * e_)), r=[stk["Ub"], stk["LrbT"]], w=[tpsY])
                            B.op("pe", lambda e, e_=e_: e.matmul(
                                psY[eh[e_], sl], VT[eh[e_], sl], LrkT[eh[e_], sl], start=False, stop=True,
                                tile_position=(64 * e_, 64 * e_)), r=[stk["VT"], stk["LrkT"]], w=[tpsY])
                        psS, tpsS = getps(YB)
                        for e_ in range(2):
                            B.op("pe", lambda e, e_=e_: e.matmul(
                                psS[eh[e_], 0:64], BhT[eh[e_], sl], Ub[eh[e_], :], start=True, stop=False,
                                tile_position=(64 * e_, 64 * e_)), r=[stk["BhT"], stk["Ub"]], w=[tpsS])
                            B.op("pe", lambda e, e_=e_: e.matmul(
                                psS[eh[e_], 0:64], KhT[eh[e_], sl], VT[eh[e_], sl], start=False, stop=True,
                                tile_position=(64 * e_, 64 * e_)), r=[stk["KhT"], stk["VT"]], w=[tpsS])
                        B.op("dve", lambda e: e.scalar_tensor_tensor(
                            out=S32, in0=S32, scalar=PCt[:, c:c + 1], in1=psS[:, 0:64], op0=ALU.mult, op1=ALU.add),
                            r=[tpsS, stk["PC"]], w=[tS])
                        B.op("act", lambda e: e.activation(out=Sb, in_=S32, func=AF.Copy), w=[tS])
                        yield

                def y_tail(hp):
                    S, stk = ysetup(hp)
                    bonus, gg, VT, BhT = (S[n] for n in ["bonus", "gg", "VT", "BhT"])
                    YB = (0, 1, 2, 3)
                    psY, tpsY = PSY[hp]
                    B.op("act", lambda e: e.activation(out=gA, in_=psY[:, :], func=AF.Copy), r=[tpsY], w=[tk["gA"]])
                    B.op("act", lambda e: e.activation(out=VT, in_=psY[:, :], func=AF.Copy), r=[tpsY], w=[stk["VT"]])
                    B.op("act", lambda e: e.activation(out=BhT, in_=psY[:, :], func=AF.Square), r=[tpsY], w=[stk["BhT"]])
                    release(psY)
                    yield
                    psm, tpsm = getps(YB)
                    psq, tpsq = getps(YB)
                    B.op("pe", lambda e: e.matmul(psm[:, :], bones, VT, start=True, stop=True),
                         r=[stk["VT"], t_c], w=[tpsm])
                    B.op("pe", lambda e: e.matmul(psq[:, :], bones, BhT, start=True, stop=True),
                         r=[stk["BhT"], t_c], w=[tpsq])
                    B.op("dve", lambda e: e.tensor_scalar(out=gB, in0=psm[:, :], scalar1=1.0 / 64, scalar2=None, op0=ALU.mult),
                         r=[tpsm], w=[tk["gB"]])
                    B.op("dve", lambda e: e.tensor_tensor(out=gC, in0=gB, in1=gB, op=ALU.mult), r=[tk["gB"]], w=[tk["gC"]])
                    B.op("dve", lambda e: e.scalar_tensor_tensor(out=gC, in0=psq[:, :], scalar=1.0 / 64, in1=gC,
                                                                 op0=ALU.mult, op1=ALU.subtract),
                         r=[tpsq], w=[tk["gC"]])
                    yield
                    B.op("act", lambda e: e.activation(out=gC, in_=gC, func=AF.Ln, bias=eps_gn), r=[t_c], w=[tk["gC"]])
                    B.op("act", lambda e: e.activation(out=gC, in_=gC, func=AF.Exp, scale=-0.5), w=[tk["gC"]])
                    B.op("dve", lambda e: e.tensor_tensor(out=gA, in0=gA, in1=gB, op=ALU.subtract), r=[tk["gB"]], w=[tk["gA"]])
                    yield
                    B.op("dve", lambda e: e.tensor_tensor(out=gA, in0=gA, in1=gC, op=ALU.mult), r=[tk["gC"]], w=[tk["gA"]])
                    B.op("act", lambda e: e.activation(out=gA, in_=gA, func=AF.Identity, scale=pvc("lng", hp), bias=pvc("lnb", hp)),
                         r=[t_pv], w=[tk["gA"]])
                    yield
                    B.op("dve", lambda e: e.tensor_tensor(out=gA, in0=gA, in1=bonus, op=ALU.add), r=[stk["bonus"]], w=[tk["gA"]])
                    B.op("dve", lambda e: e.tensor_tensor(out=mixcat[:, hp * TT:(hp + 1) * TT], in0=gA, in1=gg, op=ALU.mult),
                         r=[tk["gA"], stk["gg"]], w=[t_mix[hp]])
                    yield

                def chain_gens(gs):
                    for g_ in gs:
                        yield from g_

                def interleave(gs):
                    gs = list(gs)
                    while gs:
                        for g_ in list(gs):
                            try:
                                next(g_)
                            except StopIteration:
                                gs.remove(g_)
                for _ in x_lowrank():
                    pass
                for _ in x_prep(0):
                    pass
                for hp in range(1, 4):
                    interleave([x_prep(hp), y2a(hp - 1)])
                interleave([chain_gens([x_sc(0), x_sc(1)]), y2a(3)])
                for dg in range(2):
                    B.dma("pool", wos[dg][:, :], wo_d[l, dg], w=[t_wos[dg]])
                XB = (0, 1, 2, 3)
                interleave([y_chain(0), y_chain(1), y_chain(2), y_chain(3), chain_gens([x_cfall(), x_cfln()])])
                for hp in range(4):
                    for _ in y_tail(hp):
                        pass
                B.barrier()

                if dbg and l == 0 and tt == 0:
                    B.dma("pool", dbg_d[:, :], mixcat[:, :], r=t_mix)

                ar.off = base_off
                wos4 = [wos[0][:, :], wos[1][:, :], ar.alloc(KC * 128, BF16), ar.alloc(KC * 128, BF16)]
                t_wos4 = [t_wos[0], t_wos[1], Tok(), Tok()]
                for dg in (2, 3):
                    B.dma("pool", wos4[dg], wo_d[l, dg], w=[t_wos4[dg]])
                pss, tss = getps(hold=True)
                for dg in range(KC):
                    si = dg % 4
                    if dg >= 4:
                        B.dma("pool", wos4[si], wo_d[l, dg], w=[t_wos4[si]])
                    ps, tps = getps()
                    for kc in range(KC):
                        B.op("pe", lambda e, ps=ps, kc=kc, si=si: e.matmul(
                            ps[:, :], wos4[si][:, kc * 128:(kc + 1) * 128], mixcat[:, kc * TT:(kc + 1) * TT],
                            start=(kc == 0), stop=(kc == KC - 1)), r=[t_wos4[si], t_mix[kc]], w=[tps])
                    B.op("act", lambda e, ps=ps, dg=dg: e.activation(out=fo[:, dg * TT:(dg + 1) * TT], in_=ps[:, :], func=AF.Copy),
                         r=[tps], w=[t_fo[dg]])
                    i = dg % 2
                    B.op("act", lambda e, ps=ps, i=i: e.activation(out=NORM["sq"][i], in_=ps[:, :], func=AF.Square),
                         r=[tps], w=[NORM["t_sq"][i]])
                    B.op("pe", lambda e, i=i, dg=dg, pss=pss: e.matmul(pss[:, :], ones, NORM["sq"][i], start=(dg == 0), stop=(dg == KC - 1)),
                         r=[NORM["t_sq"][i], t_c], w=[tss])
                release(pss)
                post_norm_residual(tt, "postmix", pss, tss)

                B.barrier()
                ar.off = base_off
                act_t = ar.alloc(NJ * TT, BF16)
                t_act = [Tok() for _ in range(NJ)]
                NGUS, NWDS = 5, 3
                gus = [ar.alloc(2 * KC * 128, BF16) for _ in range(NGUS)]
                t_gus = [Tok() for _ in range(NGUS)]
                t_guu = [Tok() for _ in range(NGUS)]
                wds = [ar.alloc(NJ * 128, BF16) for _ in range(NWDS)]
                t_wds = [Tok() for _ in range(NWDS)]
                sil = [ar.alloc(TT, F32), ar.alloc(TT, F32)]
                t_sil = [Tok(), Tok()]
                for dg in range(NWDS):
                    B.dma("pool", wds[dg], wd_d[l, dg], w=[t_wds[dg]])
                rmsnorm_to_hT(tt, "preffn")
                for j in range(NJ):
                    si = rr("gus", NGUS)
                    B.dma("pool", gus[si][:, 0:KC * 128], wg_d[l, j], w=[t_gus[si]])
                    B.dma("pool", gus[si][:, KC * 128:2 * KC * 128], wu_d[l, j], w=[t_guu[si]])
                    psg, tpsg = getps()
                    psu, tpsu = getps()
                    for kc in range(KC):
                        B.op("pe", lambda e, kc=kc, si=si, psg=psg: e.matmul(
                            psg[:, :], gus[si][:, kc * 128:(kc + 1) * 128], hs(kc, 1, 513),
                            start=(kc == 0), stop=(kc == KC - 1)), r=[t_gus[si], t_hT[kc]], w=[tpsg])
                    for kc in range(KC):
                        B.op("pe", lambda e, kc=kc, si=si, psu=psu: e.matmul(
                            psu[:, :], gus[si][:, (KC + kc) * 128:(KC + kc + 1) * 128], hs(kc, 1, 513),
                            start=(kc == 0), stop=(kc == KC - 1)), r=[t_guu[si], t_hT[kc]], w=[tpsu])
                    i = j % 2
                    B.op("act", lambda e, i=i, psg=psg: e.activation(out=sil[i], in_=psg[:, :], func=AF.Silu),
                         r=[tpsg], w=[t_sil[i]])
                    B.op("dve", lambda e, i=i, j=j, psu=psu: e.tensor_tensor(
                        out=act_t[:, j * TT:(j + 1) * TT], in0=psu[:, :], in1=sil[i], op=ALU.mult),
                        r=[tpsu, t_sil[i]], w=[t_act[j]])
                pss, tss = getps(hold=True)
                for dg in range(KC):
                    si = dg % NWDS
                    if dg >= NWDS:
                        B.dma("pool", wds[si], wd_d[l, dg], w=[t_wds[si]])
                    ps, tps = getps()
                    for j in range(NJ):
                        B.op("pe", lambda e, ps=ps, j=j, si=si: e.matmul(
                            ps[:, :], wds[si][:, j * 128:(j + 1) * 128], act_t[:, j * TT:(j + 1) * TT],
                            start=(j == 0), stop=(j == NJ - 1)), r=[t_wds[si], t_act[j]], w=[tps])
                    B.op("act", lambda e, ps=ps, dg=dg: e.activation(out=fo[:, dg * TT:(dg + 1) * TT], in_=ps[:, :], func=AF.Copy),
                         r=[tps], w=[t_fo[dg]])
                    i = dg % 2
                    B.op("act", lambda e, ps=ps, i=i: e.activation(out=NORM["sq"][i], in_=ps[:, :], func=AF.Square),
                         r=[tps], w=[NORM["t_sq"][i]])
                    B.op("pe", lambda e, i=i, dg=dg, pss=pss: e.matmul(pss[:, :], ones, NORM["sq"][i], start=(dg == 0), stop=(dg == KC - 1)),
                         r=[NORM["t_sq"][i], t_c], w=[tss])
                release(pss)
                post_norm_residual(tt, "postffn", pss, tss)

        t_out = Tok()
        for tt in range(NT):
            for kc in range(KC):
                B.dma("sp", y_d[:, kc * T + tt * TT: kc * T + (tt + 1) * TT], xs(kc, tt), r=[t_x[tt][kc]], w=[Tok()])
        B.barrier()

        with nc.Block() as block:
            B.emit(block)
    return nc


def _consts():
    c = np.zeros((P, NCF), np.float32)
    p = np.arange(P)[:, None]
    c[:, CF["ident"]:CF["ident"] + 128] = (p == np.arange(128)[None, :])
    c[:, CF["ones"]:CF["ones"] + 128] = 1.0
    c[:, CF["bones"]:CF["bones"] + 128] = ((p // 64) == (np.arange(128)[None, :] // 64))
    col = (np.arange(512) % 64)[None, :]
    row = p % 64
    c[:, CF["msu"]:CF["msu"] + 512] = (row < col)
    c[:, CF["miu"]:CF["miu"] + 512] = (row <= col)
    c[:, CF["msl"]:CF["msl"] + 512] = (col < row)
    c[:, CF["irep"]:CF["irep"] + 512] = (row == col)
    c[:, CF["scan"]:CF["scan"] + 512] = (col != 0) * np.ones((P, 1))
    return c


def _colvec(v):
    return np.ascontiguousarray(np.asarray(v, np.float32).reshape(-1, P).T)


def _prep_inputs(inp, L):
    f = lambda a: np.asarray(a, np.float32)
    out = {}

    def grp_cols(w, ncol_groups):
        Lw, K, N = w.shape
        a = w.reshape(Lw, K // P, P, N // P, P)
        a = a.transpose(0, 3, 2, 1, 4)
        return np.ascontiguousarray(a.reshape(Lw, N // P, P, (K // P) * P))
    out["w_in"] = grp_cols(f(inp["w_in"])[:L], NJ)
    out["w_gate"] = grp_cols(f(inp["w_gate"])[:L], NJ)
    out["w_up"] = grp_cols(f(inp["w_up"])[:L], NJ)
    out["w_o"] = grp_cols(f(inp["w_o"])[:L], KC)
    out["w_down"] = grp_cols(f(inp["w_down"])[:L], KC)
    wl = np.concatenate([f(inp["w1"]), f(inp["a1"]), f(inp["g1"])], axis=2)[:L]
    wl = wl.reshape(L, KC, P, 256).transpose(0, 2, 1, 3).reshape(L, P, KC * 256)
    out["wlr"] = np.ascontiguousarray(wl)
    out["w2a2"] = np.ascontiguousarray(np.concatenate([f(inp["w2"]), f(inp["a2"])], axis=1)[:L])
    out["g2"] = np.ascontiguousarray(f(inp["g2"])[:L])
    pv = np.zeros((L, P, NPV), np.float32)
    for l in range(L):
        def put(name, arr):
            a = _colvec(arr)
            pv[l, :, PV[name]:PV[name] + a.shape[1]] = a
        put("premix", inp["pre_mix_g"][l]); put("postmix", inp["post_mix_g"][l])
        put("preffn", inp["pre_ffn_g"][l]); put("postffn", inp["post_ffn_g"][l])
        put("muw", inp["mu_wag"][l][0]); put("mua", inp["mu_wag"][l][1]); put("mug", inp["mu_wag"][l][2])
        put("murkv", inp["mu_rkv"][l])
        put("w0", inp["w0"][l]); put("a0", inp["a0"][l]); put("kk", inp["k_k"][l]); put("ka", inp["k_a"][l])
        put("rk", np.asarray(inp["r_k"][l]).reshape(-1)); put("lng", inp["lnx_g"][l]); put("lnb", inp["lnx_b"][l])
        scw = f(inp["sc_conv_w"][l])
        for k_ in range(3):
            pv[l, :, PV["scw"] + k_ * 2: PV["scw"] + k_ * 2 + 2] = _colvec(scw[k_])
        cfw = f(inp["cf_conv_w"][l])
        for k_ in range(31):
            pv[l, :, PV["cfw"] + k_ * 2: PV["cfw"] + k_ * 2 + 2] = _colvec(cfw[k_])
        put("cfb", inp["cf_conv_b"][l]); put("cflg", inp["cf_ln_g"][l]); put("cflb", inp["cf_ln_b"][l])
    out["pv"] = pv
    out["cf"] = _consts()
    return out


_CACHE = {}


def run(inputs, L=4, dbg=False):
    x = np.asarray(inputs["x"], np.float32)
    shared = _prep_inputs(inputs, L)
    key = (L, dbg)
    if key not in _CACHE:
        _CACHE[key] = build_program(L, dbg)
    nc = _CACHE[key]
    in_maps = []
    for b in range(NCORES):
        xt = np.ascontiguousarray(x[b].T.reshape(KC, P, T).transpose(1, 0, 2).reshape(P, KC * T))
        m = dict(shared)
        m["xT"] = xt
        in_maps.append(m)
    res = run_bass_kernel_spmd(nc, in_maps, core_ids=list(range(NCORES)))
    outs = []
    for b in range(NCORES):
        yt = np.asarray(res.results[b]["yT"]).reshape(P, KC, T).transpose(1, 0, 2).reshape(D, T)
        outs.append(yt.T)
    y = np.stack(outs, 0).astype(np.float32)
    if dbg:
        return y, [np.asarray(r["dbg"]) for r in res.results]
    return y


def kernel(**inputs):
    return run(inputs, L=4)
```

```python
import contextlib
import numpy as np
import concourse.bass as bass
import concourse.mybir as mybir
from concourse.bass_utils import run_bass_kernel_spmd

F32 = mybir.dt.float32
BF16 = mybir.dt.bfloat16
AF = mybir.ActivationFunctionType
ALU = mybir.AluOpType

P = 128
T = 2048
D = 1024
TT = 512
NT = T // TT
KC = 8
NJ = 22
NCORES = 8
C0 = -0.6065306597126334
SAME_ENGINE_SYNC = True

PV = {}
_o = 0
for _n, _c in [("premix", 8), ("postmix", 8), ("preffn", 8), ("postffn", 8), ("muw", 8), ("mua", 8),
               ("mug", 8), ("murkv", 12), ("w0", 4), ("a0", 4), ("kk", 4), ("ka", 4), ("rk", 4),
               ("lng", 4), ("lnb", 4), ("scw", 6), ("cfw", 62), ("cfb", 2), ("cflg", 2), ("cflb", 2)]:
    PV[_n] = _o
    _o += _c
NPV = _o
PD = {"omuw": 0, "omua": 8, "omug": 16, "omurkv": 24, "omka": 36}
NPD = 40
CF = {"ident": 0, "ones": 128, "bones": 256, "msu": 384, "miu": 896, "msl": 1408, "irep": 1920, "scan": 2432}
NCF = 2944


class Tok:
    __slots__ = ("w", "r")

    def __init__(self):
        self.w = None
        self.r = []


class _Rec:
    def __init__(self):
        self.calls = []

    def __getattr__(self, name):
        def f(*a, **k):
            self.calls.append((name, a, k))
        return f


class Builder:
    ENGS = ("pe", "act", "dve", "pool", "sp")

    def __init__(self, nc, es, ndma=48):
        self.nc = nc
        self.q = {e: [] for e in self.ENGS}
        self.cnt = {e: 0 for e in self.ENGS}
        self.seen = {e: {} for e in self.ENGS}
        self.sems = {}
        for e in ("pe", "act", "dve", "pool"):
            self.sems[e] = es.enter_context(nc.semaphore("s_" + e))
        self.ndma = ndma
        self.dma_uses = [0] * ndma
        self.dma_next = 0
        for k in range(ndma):
            self.sems["dma%d" % k] = es.enter_context(nc.semaphore("s_dma%d" % k))

    def _collect(self, r, w):
        deps = {}

        def add(d):
            if d is None:
                return
            k, v = d
            if deps.get(k, 0) < v:
                deps[k] = v
        for t in r:
            add(t.w)
        for t in w:
            add(t.w)
            for d in t.r:
                add(d)
        return deps

    def _waits(self, eng, deps):
        out = []
        seen = self.seen[eng]
        for k, v in deps.items():
            if k == eng and (eng == "pe" or not SAME_ENGINE_SYNC):
                continue
            if seen.get(k, 0) >= v:
                continue
            seen[k] = v
            out.append((k, v))
        return out

    def op(self, eng, fn, r=(), w=()):
        rec = _Rec()
        fn(rec)
        assert len(rec.calls) == 1
        call = rec.calls[0]
        fn = lambda e, call=call: getattr(e, call[0])(*call[1], **call[2])
        deps = self._collect(r, w)
        waits = self._waits(eng, deps)
        self.cnt[eng] += 1
        me = (eng, self.cnt[eng])
        self.q[eng].append((fn, waits, (eng, 1)))
        for t in r:
            t.r.append(me)
        for t in w:
            t.w = me
            t.r = []

    def dma(self, qe, out_ap, in_ap, r=(), w=()):
        deps = self._collect(r, w)
        k = self.dma_next
        self.dma_next = (k + 1) % self.ndma
        key = "dma%d" % k
        if self.dma_uses[k] > 0:
            v = 16 * self.dma_uses[k]
            if deps.get(key, 0) < v:
                deps[key] = v
        waits = self._waits(qe, deps)
        self.dma_uses[k] += 1
        me = (key, 16 * self.dma_uses[k])
        self.q[qe].append((lambda e: e.dma_start(out=out_ap, in_=in_ap), waits, (key, 16)))
        for t in r:
            t.r.append(me)
        for t in w:
            t.w = me
            t.r = []

    def wait_only(self, eng, toks):
        deps = self._collect(toks, ())
        waits = self._waits(eng, deps)
        self.q[eng].append((None, waits, None))

    def barrier(self):
        allv = {e: self.cnt[e] for e in ("pe", "act", "dve", "pool") if self.cnt[e] > 0}
        for k in range(self.ndma):
            if self.dma_uses[k] > 0:
                allv["dma%d" % k] = 16 * self.dma_uses[k]
        for e in self.ENGS:
            deps = {k: v for k, v in allv.items() if k != e}
            waits = self._waits(e, deps)
            if waits:
                self.q[e].append((None, waits, None))

    def emit(self, block):
        sems = self.sems

        def run(e, lst):
            for fn, waits, inc in lst:
                for k, v in waits:
                    e.wait_ge(sems[k], v)
                if fn is not None:
                    ins = fn(e)
                    if inc is not None:
                        ins.then_inc(sems[inc[0]], inc[1])

        @block.tensor
        def _(e):
            run(e, self.q["pe"])

        @block.scalar
        def _(e):
            run(e, self.q["act"])

        @block.vector
        def _(e):
            run(e, self.q["dve"])

        @block.gpsimd
        def _(e):
            run(e, self.q["pool"])

        @block.sync
        def _(e):
            run(e, self.q["sp"])


class Arena:
    def __init__(self, ap, nbytes):
        self.ap = ap
        self.n = nbytes
        self.off = 0

    def alloc(self, cols, dtype):
        sz = cols * (4 if dtype == F32 else 2)
        sz = (sz + 3) // 4 * 4
        assert self.off + sz <= self.n, ("arena overflow", self.off, sz, self.n)
        a = self.ap[:, self.off // 4:(self.off + sz) // 4]
        self.off += sz
        if dtype != F32:
            a = a.bitcast(dtype)
            a = a[:, 0:cols]
        return a


def build_program(L=4, dbg=False):
    nc = bass.Bass("TRN2", target_bir_lowering=False)
    dt_in = lambda n, s: nc.dram_tensor(n, s, F32, kind="ExternalInput").ap()
    x_d = dt_in("xT", [P, KC * T])
    win_d = dt_in("w_in", [L, NJ, P, KC * 128])
    wg_d = dt_in("w_gate", [L, NJ, P, KC * 128])
    wu_d = dt_in("w_up", [L, NJ, P, KC * 128])
    wo_d = dt_in("w_o", [L, KC, P, KC * 128])
    wd_d = dt_in("w_down", [L, KC, P, NJ * 128])
    wlr_d = dt_in("wlr", [L, P, KC * 256])
    w2a2_d = dt_in("w2a2", [L, P, 512])
    g2_d = dt_in("g2", [L, P, 512])
    pv_d = dt_in("pv", [L, P, NPV])
    cf_d = dt_in("cf", [P, NCF])
    y_d = nc.dram_tensor("yT", [P, KC * T], F32, kind="ExternalOutput").ap()
    if dbg:
        dbg_d = nc.dram_tensor("dbg", [P, KC * TT], F32, kind="ExternalOutput").ap()

    with contextlib.ExitStack() as es:
        def sb(name, cols, dtype):
            return es.enter_context(nc.sbuf_tensor(name, [P, cols], dtype))
        xT = sb("xT_sb", KC * T, F32)
        pv = sb("pv_sb", NPV, F32)
        pd = sb("pd_sb", NPD, F32)
        cb = sb("cb", NCF, BF16)
        cmask = cb[:, 384:NCF]
        smallc = sb("smallc", 4, F32)
        wlra = sb("wlra", KC * 256, BF16)
        wlrb = sb("wlrb", KC * 256, BF16)
        w2a2 = sb("w2a2_sb", 512, BF16)
        g2 = sb("g2_sb", 512, BF16)
        rkvcarry = sb("rkvcarry", 12, F32)
        cu = sb("cu", 2 * 514, F32)
        cfu = sb("cfu", 2 * 542, F32)
        ST32 = sb("ST32", 4 * 64, F32)
        STb = sb("STb", 4 * 64, BF16)
        hT = sb("hT", KC * 513, BF16)
        hcarry = sb("hcarry", KC, BF16)
        mixcat = sb("mixcat", KC * TT, BF16)
        fo = sb("fo", KC * TT, F32)
        wlr = fo[:, 0:KC * 128].bitcast(BF16)
        wos = [sb("wos%d" % i, KC * 128, BF16) for i in range(2)]
        ARENA_BYTES = 80 * 1024
        arena_t = sb("arena", ARENA_BYTES // 4, F32)
        psum = [es.enter_context(nc.psum_tensor("ps%d" % i, [P, 512], F32)) for i in range(8)]

        B = Builder(nc, es)
        pst = [Tok() for _ in range(8)]
        ps_rr = [0]

        held = set()

        ALLB = tuple(range(8))
        ps_ptr = {}

        def getps(banks=None, hold=False):
            key = banks or ALLB
            while True:
                p_ = ps_ptr.get(key, 0)
                ps_ptr[key] = p_ + 1
                i = key[p_ % len(key)]
                if i not in held:
                    break
            if hold:
                held.add(i)
            return psum[i], pst[i]

        def release(ps):
            for i in range(8):
                if psum[i] is ps:
                    held.discard(i)

        t_x = [[Tok() for _ in range(KC)] for _ in range(NT)]
        t_pv, t_pd, t_c, t_wlr, t_wlrab, t_w2, t_g2 = Tok(), Tok(), Tok(), Tok(), Tok(), Tok(), Tok()
        t_carry, t_cu, t_cfu = Tok(), [Tok(), Tok()], [Tok(), Tok()]
        t_ST = [Tok() for _ in range(4)]
        t_hT, t_mix, t_fo = [Tok() for _ in range(KC)], [Tok() for _ in range(KC)], [Tok() for _ in range(KC)]
        t_wos = [Tok(), Tok()]
        t_hc = [Tok() for _ in range(KC)]

        def pvc(name, i=0):
            o = PV[name] + i
            return pv[:, o:o + 1]

        def pdc(name, i=0):
            o = PD[name] + i
            return pd[:, o:o + 1]

        ident = cb[:, 0:128]
        ones = cb[:, 128:256]
        bones = cb[:, 256:384]
        msu = cmask[:, 0:512]
        miu = cmask[:, 512:1024]
        msl = cmask[:, 1024:1536]
        irep = cmask[:, 1536:2048]
        scanm = cmask[:, 2048:2560]
        eps_n = smallc[:, 0:1]
        eps_ln = smallc[:, 1:2]
        eps_gn = smallc[:, 2:3]

        def xs(kc, tt):
            return xT[:, kc * T + tt * TT: kc * T + (tt + 1) * TT]

        def hs(kc, a, b):
            return hT[:, kc * 513 + a: kc * 513 + b]

        for kc in range(KC):
            for tt in range(NT):
                B.dma("sp", xs(kc, tt), x_d[:, kc * T + tt * TT: kc * T + (tt + 1) * TT], w=[t_x[tt][kc]])
        B.dma("pool", cb[:, :], cf_d[:, :], w=[t_c])
        B.op("dve", lambda e: e.memset(smallc[:, 0:1], 1e-6), w=[t_c])
        B.op("dve", lambda e: e.memset(smallc[:, 1:2], 1e-5), w=[t_c])
        B.op("dve", lambda e: e.memset(smallc[:, 2:3], 64e-5), w=[t_c])
        B.op("dve", lambda e: e.memset(smallc[:, 3:4], 1e-24), w=[t_c])

        wslot_rr = {}

        def rr(name, n):
            i = wslot_rr.get(name, 0)
            wslot_rr[name] = (i + 1) % n
            return i

        def rmsnorm_to_hT(tt, gname):
            pss, tss = getps()
            sq, t_sq = NORM["sq"], NORM["t_sq"]
            for kc in range(KC):
                i = kc % 2
                B.op("act", lambda e, kc=kc, i=i: e.activation(out=sq[i], in_=xs(kc, tt), func=AF.Square),
                     r=[t_x[tt][kc]], w=[t_sq[i]])
                B.op("pe", lambda e, kc=kc, i=i: e.matmul(pss[:, :], ones, sq[i], start=(kc == 0), stop=(kc == KC - 1)),
                     r=[t_sq[i], t_c], w=[tss])
            rstd, t_rstd = NORM["rstd"], NORM["t_rstd"]
            B.op("act", lambda e: e.activation(out=rstd, in_=pss[:, :], func=AF.Ln, scale=1.0 / D, bias=eps_n),
                 r=[tss, t_c], w=[t_rstd])
            B.op("act", lambda e: e.activation(out=rstd, in_=rstd, func=AF.Exp, scale=-0.5), r=[t_rstd], w=[t_rstd])
            for kc in range(KC):
                B.op("dve", lambda e, kc=kc: e.scalar_tensor_tensor(
                    out=hs(kc, 1, 513), in0=xs(kc, tt), scalar=pvc(gname, kc), in1=rstd,
                    op0=ALU.mult, op1=ALU.mult), r=[t_x[tt][kc], t_rstd, t_pv], w=[t_hT[kc]])

        def post_norm_residual(tt, gname, pss, tss):
            rstd, t_rstd = NORM["rstd"], NORM["t_rstd"]
            B.op("act", lambda e: e.activation(out=rstd, in_=pss[:, :], func=AF.Ln, scale=1.0 / D, bias=eps_n),
                 r=[tss, t_c], w=[t_rstd])
            B.op("act", lambda e: e.activation(out=rstd, in_=rstd, func=AF.Exp, scale=-0.5), r=[t_rstd], w=[t_rstd])
            for dg in range(KC):
                f = fo[:, dg * TT:(dg + 1) * TT]
                B.op("dve", lambda e, f=f, dg=dg: e.scalar_tensor_tensor(
                    out=f, in0=f, scalar=pvc(gname, dg), in1=rstd, op0=ALU.mult, op1=ALU.mult),
                    r=[t_rstd, t_pv], w=[t_fo[dg]])
                B.op("dve", lambda e, f=f, dg=dg: e.tensor_tensor(out=xs(dg, tt), in0=xs(dg, tt), in1=f, op=ALU.add),
                     r=[t_fo[dg]], w=[t_x[tt][dg]])

        NORM = {}

        for l in range(L):
            B.barrier()
            ar = Arena(arena_t[:, :], ARENA_BYTES)
            NORM["sq"] = [ar.alloc(TT, BF16), ar.alloc(TT, BF16)]
            NORM["t_sq"] = [Tok(), Tok()]
            NORM["rstd"] = ar.alloc(TT, F32)
            NORM["t_rstd"] = Tok()
            base_off = ar.off
            B.dma("sp", pv[:, :], pv_d[l], w=[t_pv])
            B.dma("pool", wlr[:, :], wlr_d[l], w=[t_wlr])
            B.dma("pool", w2a2[:, :], w2a2_d[l], w=[t_w2])
            B.dma("pool", g2[:, :], g2_d[l], w=[t_g2])
            for nm, src, n in [("omuw", "muw", 8), ("omua", "mua", 8), ("omug", "mug", 8), ("omurkv", "murkv", 12),
                               ("omka", "ka", 4)]:
                B.op("dve", lambda e, nm=nm, src=src, n=n: e.tensor_scalar(
                    out=pd[:, PD[nm]:PD[nm] + n], in0=pv[:, PV[src]:PV[src] + n], scalar1=-1.0, scalar2=1.0,
                    op0=ALU.mult, op1=ALU.add), r=[t_pv], w=[t_pd])
            for kc in range(KC):
                for (c0, c1, mn, on) in [(0, 64, "muw", "omuw"), (64, 128, "mua", "omua"), (128, 256, "mug", "omug")]:
                    src = wlr[:, kc * 256 + c0: kc * 256 + c1]
                    B.op("dve", lambda e, src=src, kc=kc, c0=c0, c1=c1, on=on: e.tensor_scalar(
                        out=wlra[:, kc * 256 + c0: kc * 256 + c1], in0=src, scalar1=pdc(on, kc), scalar2=None,
                        op0=ALU.mult), r=[t_wlr, t_pd], w=[t_wlrab])
                    B.op("dve", lambda e, src=src, kc=kc, c0=c0, c1=c1, mn=mn: e.tensor_scalar(
                        out=wlrb[:, kc * 256 + c0: kc * 256 + c1], in0=src, scalar1=pvc(mn, kc), scalar2=None,
                        op0=ALU.mult), r=[t_wlr, t_pv], w=[t_wlrab])
            B.op("dve", lambda e: e.memset(rkvcarry[:, :], 0.0), w=[t_carry])
            B.op("dve", lambda e: e.memset(cu[:, :], 0.0), w=t_cu)
            B.op("dve", lambda e: e.memset(cfu[:, :], 0.0), w=t_cfu)
            B.op("dve", lambda e: e.memset(ST32[:, :], 0.0), w=t_ST)
            B.op("dve", lambda e: e.memset(STb[:, :], 0.0), w=t_ST)
            B.op("dve", lambda e: e.memset(hT[:, :], 0.0), w=t_hT)

            for tt in range(NT):
                B.barrier()
                ar.off = base_off
                if tt > 0:
                    for kc in range(KC):
                        B.op("dve", lambda e, kc=kc: e.tensor_copy(hs(kc, 0, 1), hcarry[:, kc:kc + 1]), r=[t_hc[kc]], w=[t_hT[kc]])
                rmsnorm_to_hT(tt, "premix")
                for kc in range(KC):
                    B.op("dve", lambda e, kc=kc: e.tensor_copy(hcarry[:, kc:kc + 1], hs(kc, 512, 513)), r=[t_hT[kc]], w=[t_hc[kc]])

                ar2 = Arena(fo[:, :], KC * TT * 4)

                def A_(cols, dtype):
                    sz = (cols * (4 if dtype == F32 else 2) + 3) // 4 * 4
                    if ar.off + sz <= ar.n:
                        return ar.alloc(cols, dtype)
                    return ar2.alloc(cols, dtype)
                wins = [A_(KC * 128, BF16) for _ in range(2)]
                t_wins = [Tok() for _ in range(2)]
                raw = [A_(513, F32)]
                t_raw = [Tok()]
                f32t = lambda: A_(TT, F32)
                bft = lambda: A_(TT, BF16)
                r_t, k_t, v_t, sgw, av = f32t(), f32t(), f32t(), f32t(), f32t()
                twa, tg = bft(), bft()
                tA, tB, tC, tD, tE = f32t(), f32t(), f32t(), f32t(), f32t()
                rkb = bft()
                scr2 = tg
                SETS = []
                for _i in range(2):
                    S = dict(bt=bft(), kt=bft(), Bh=bft(), Kh=bft(), vb=bft())
                    S["tk"] = {n: Tok() for n in ["bt", "kt", "Bh", "Kh", "vb"]}
                    SETS.append(S)
                Rm = [bft(), bft()]
                Lm = [bft(), bft()]
                Gm = [bft(), bft()]
                CH = []
                for _i in range(4):
                    S = dict(art=A_(8 * 128, BF16), bonus=bft(), gg=bft(), PC=A_(8, F32), TT=bft(),
                             LakT=bft(), LrbT=bft(), LrkT=bft(), VT=bft(), BhT=bft(), KhT=bft(),
                             Xb=A_(64, BF16), Ub=A_(64, BF16))
                    S["tk"] = {n: Tok() for n in ["art", "bonus", "gg", "PC", "TT", "LakT", "LrbT", "LrkT", "VT", "BhT", "KhT", "Xb", "Ub"]}
                    CH.append(S)
                gA, gB, gC = tE, r_t, k_t
                tk = {n: Tok() for n in ["r", "k", "v", "sgw", "av", "twa", "tg", "A", "B", "C", "D", "E", "rkb",
                                         "R0", "R1", "L0", "L1", "G0", "G1"]}
                tk["scr2"] = tk["tg"]
                tk["gA"], tk["gB"], tk["gC"] = tk["E"], tk["r"], tk["k"]
                XB = (0, 1, 2)
                YB = (3, 4, 5, 6, 7)
                eh = [slice(0, 64), slice(64, 128)]
                v3 = lambda a: a.rearrange("p (c x) -> p c x", c=8)
                mk3 = lambda m, n: m[:, 0:n * 64].rearrange("p (c x) -> p c x", c=n)

                def proj_group(g, consume):
                    si = rr("win", 2)
                    B.dma("pool", wins[si], win_d[l, g], w=[t_wins[si]])
                    ps, tps = getps(XB)
                    for kc in range(KC):
                        B.op("pe", lambda e, kc=kc, ps=ps, si=si: e.matmul(
                            ps[:, :], wins[si][:, kc * 128:(kc + 1) * 128], hs(kc, 1, 513),
                            start=(kc == 0), stop=(kc == KC - 1)), r=[t_wins[si], t_hT[kc]], w=[tps])
                    consume(ps, tps)

                def lowrank(c0, c1, consume):
                    ps, tps = getps(XB)
                    n = 0
                    for kc in range(KC):
                        for (wsb, a, b) in [(wlra, 1, 513), (wlrb, 0, 512)]:
                            B.op("pe", lambda e, kc=kc, wsb=wsb, a=a, b=b, n=n, ps=ps: e.matmul(
                                ps[:, :], wsb[:, kc * 256 + c0: kc * 256 + c1], hs(kc, a, b),
                                start=(n == 0), stop=(n == 2 * KC - 1)), r=[t_wlrab, t_hT[kc]], w=[tps])
                            n += 1
                    consume(ps, tps)

                def x_lowrank():
                    def lr0(ps, tps):
                        B.op("act", lambda e: e.activation(out=twa[0:64, :], in_=ps[0:64, :], func=AF.Tanh),
                             r=[tps], w=[tk["twa"]])
                        B.op("act", lambda e: e.activation(out=twa[64:128, :], in_=ps[64:128, :], func=AF.Copy), r=[tps], w=[tk["twa"]])
                    lowrank(0, 128, lr0)
                    yield

                    def lr1(ps, tps):
                        B.op("act", lambda e: e.activation(out=tg, in_=ps[:, :], func=AF.Sigmoid), r=[tps], w=[tk["tg"]])
                    lowrank(128, 256, lr1)
                    yield

                def x_sc(gi):
                    cuv = lambda a, b: cu[:, gi * 514 + a: gi * 514 + b]
                    csb = tA
                    acc = tB

                    def c_cons(ps, tps):
                        B.op("act", lambda e: e.activation(out=csb, in_=ps[:, :], func=AF.Copy), r=[tps], w=[tk["A"]])
                    proj_group(14 + gi, c_cons)
                    yield

                    def u_cons(ps, tps):
                        B.op("dve", lambda e: e.tensor_tensor(out=cuv(2, 514), in0=ps[:, :], in1=csb, op=ALU.mult),
                             r=[tps, tk["A"]], w=[t_cu[gi]])
                    proj_group(16 + gi, u_cons)
                    yield
                    B.op("dve", lambda e: e.tensor_scalar(
                        out=acc, in0=cuv(0, 512), scalar1=pvc("scw", 0 * 2 + gi), scalar2=None, op0=ALU.mult),
                        r=[t_cu[gi], t_pv], w=[tk["B"]])
                    for kk_ in (1, 2):
                        B.op("dve", lambda e, kk_=kk_: e.scalar_tensor_tensor(
                            out=acc, in0=cuv(kk_, kk_ + 512), scalar=pvc("scw", kk_ * 2 + gi), in1=acc,
                            op0=ALU.mult, op1=ALU.add), r=[t_cu[gi], t_pv], w=[tk["B"]])
                    B.op("dve", lambda e: e.tensor_copy(cuv(0, 2), cuv(512, 514)), w=[t_cu[gi]])
                    yield

                    def b_cons(ps, tps):
                        B.op("dve", lambda e: e.tensor_tensor(
                            out=mixcat[:, (4 + gi) * TT:(5 + gi) * TT], in0=ps[:, :], in1=acc, op=ALU.mult),
                            r=[tps, tk["B"]], w=[t_mix[4 + gi]])
                    proj_group(12 + gi, b_cons)
                    yield

                cres = [tC, tD]
                tcres = [tk["C"], tk["D"]]

                def x_cfall():
                    sg = tA
                    fvs = []
                    for gi in range(2):
                        fv = lambda a_, b_, gi=gi: cfu[:, gi * 542 + a_: gi * 542 + b_]
                        fvs.append(fv)

                        def g_cons(ps, tps):
                            B.op("act", lambda e: e.activation(out=sg, in_=ps[:, :], func=AF.Sigmoid), r=[tps], w=[tk["A"]])
                        proj_group(20 + gi, g_cons)
                        yield

                        def v_cons(ps, tps, gi=gi, fv=fv):
                            B.op("dve", lambda e: e.tensor_tensor(out=fv(30, 542), in0=ps[:, :], in1=sg, op=ALU.mult),
                                 r=[tps, tk["A"]], w=[t_cfu[gi]])
                        proj_group(18 + gi, v_cons)
                        yield
                    for gi in range(2):
                        B.op("dve", lambda e, gi=gi: e.tensor_scalar(
                            out=cres[gi], in0=fvs[gi](0, 512), scalar1=pvc("cfw", gi), scalar2=pvc("cfb", gi),
                            op0=ALU.mult, op1=ALU.add), r=[t_cfu[gi], t_pv], w=[tcres[gi]])
                    for kk_ in range(1, 31):
                        for gi in range(2):
                            B.op("dve", lambda e, kk_=kk_, gi=gi: e.scalar_tensor_tensor(
                                out=cres[gi], in0=fvs[gi](kk_, kk_ + 512), scalar=pvc("cfw", kk_ * 2 + gi), in1=cres[gi],
                                op0=ALU.mult, op1=ALU.add), r=[t_cfu[gi], t_pv], w=[tcres[gi]])
                        yield
                    for gi in range(2):
                        B.op("dve", lambda e, gi=gi: e.tensor_copy(fvs[gi](0, 30), fvs[gi](512, 542)), w=[t_cfu[gi]])
                    yield

                def x_cfln():
                    psm, tpsm = getps(XB)
                    psq, tpsq = getps(XB)
                    for gi in range(2):
                        B.op("act", lambda e, gi=gi: e.activation(out=rkb, in_=cres[gi], func=AF.Copy),
                             r=[tcres[gi]], w=[tk["rkb"]])
                        B.op("pe", lambda e, gi=gi: e.matmul(psm[:, :], ones, rkb, start=(gi == 0), stop=(gi == 1)),
                             r=[tk["rkb"], t_c], w=[tpsm])
                        B.op("act", lambda e, gi=gi: e.activation(out=scr2, in_=cres[gi], func=AF.Square),
                             r=[tcres[gi]], w=[tk["scr2"]])
                        B.op("pe", lambda e, gi=gi: e.matmul(psq[:, :], ones, scr2, start=(gi == 0), stop=(gi == 1)),
                             r=[tk["scr2"], t_c], w=[tpsq])
                        yield
                    B.op("dve", lambda e: e.tensor_scalar(out=tA, in0=psm[:, :], scalar1=1.0 / 256, scalar2=None, op0=ALU.mult),
                         r=[tpsm], w=[tk["A"]])
                    B.op("dve", lambda e: e.tensor_tensor(out=tB, in0=tA, in1=tA, op=ALU.mult), r=[tk["A"]], w=[tk["B"]])
                    B.op("dve", lambda e: e.scalar_tensor_tensor(out=tB, in0=psq[:, :], scalar=1.0 / 256, in1=tB,
                                                                 op0=ALU.mult, op1=ALU.subtract),
                         r=[tpsq], w=[tk["B"]])
                    yield
                    B.op("act", lambda e: e.activation(out=tB, in_=tB, func=AF.Ln, bias=eps_ln), r=[t_c], w=[tk["B"]])
                    B.op("act", lambda e: e.activation(out=tB, in_=tB, func=AF.Exp, scale=-0.5), w=[tk["B"]])
                    yield
                    for gi in range(2):
                        B.op("dve", lambda e, gi=gi: e.tensor_tensor(out=cres[gi], in0=cres[gi], in1=tA, op=ALU.subtract),
                             r=[tk["A"]], w=[tcres[gi]])
                        B.op("dve", lambda e, gi=gi: e.tensor_tensor(out=cres[gi], in0=cres[gi], in1=tB, op=ALU.mult),
                             r=[tk["B"]], w=[tcres[gi]])
                        B.op("act", lambda e, gi=gi: e.activation(
                            out=mixcat[:, (6 + gi) * TT:(7 + gi) * TT], in_=cres[gi], func=AF.Silu,
                            scale=pvc("cflg", gi), bias=pvc("cflb", gi)), r=[tcres[gi], t_pv], w=[t_mix[6 + gi]])
                        yield

                def x_prep(hp):
                    S = {**SETS[hp % 2], **CH[hp]}
                    stk = {**SETS[hp % 2]["tk"], **CH[hp]["tk"]}
                    art, bt_, kt_, Bh, Kh, vb, bonus, gg, PCt = (S[n] for n in ["art", "bt", "kt", "Bh", "Kh", "vb", "bonus", "gg", "PC"])
                    art3 = art.rearrange("p (c x) -> p c x", c=8)
                    ps, tps = getps(XB)
                    B.op("pe", lambda e: e.matmul(ps[:, :], w2a2[0:64, hp * 128:(hp + 1) * 128], twa[0:64, :],
                                                  start=True, stop=True), r=[t_w2, tk["twa"]], w=[tps])
                    B.op("act", lambda e: e.activation(out=sgw, in_=ps[:, :], func=AF.Sigmoid, bias=pvc("w0", hp)),
                         r=[tps, t_pv], w=[tk["sgw"]])
                    ps2, tps2 = getps(XB)
                    B.op("pe", lambda e: e.matmul(ps2[:, :], w2a2[64:128, hp * 128:(hp + 1) * 128], twa[64:128, :],
                                                  start=True, stop=True), r=[t_w2, tk["twa"]], w=[tps2])
                    B.op("act", lambda e: e.activation(out=av, in_=ps2[:, :], func=AF.Sigmoid, bias=pvc("a0", hp)),
                         r=[tps2, t_pv], w=[tk["av"]])
                    ps3, tps3 = getps(XB)
                    B.op("pe", lambda e: e.matmul(ps3[:, :], g2[:, hp * 128:(hp + 1) * 128], tg,
                                                  start=True, stop=True), r=[t_g2, tk["tg"]], w=[tps3])
                    B.op("act", lambda e: e.activation(out=gg, in_=ps3[:, :], func=AF.Copy), r=[tps3], w=[stk["gg"]])
                    yield
                    for (which, dst, tkn) in [(0, r_t, "r"), (1, k_t, "k"), (2, v_t, "v")]:
                        g = which * 4 + hp

                        def rkv_cons(ps, tps, g=g, dst=dst, tkn=tkn):
                            ri = 0
                            rw = raw[ri]
                            B.op("dve", lambda e: e.tensor_copy(rw[:, 0:1], rkvcarry[:, g:g + 1]),
                                 r=[t_carry], w=[t_raw[ri]])
                            B.op("act", lambda e: e.activation(out=rw[:, 1:513], in_=ps[:, :], func=AF.Copy),
                                 r=[tps], w=[t_raw[ri]])
                            B.op("dve", lambda e: e.tensor_copy(rkvcarry[:, g:g + 1], rw[:, 512:513]),
                                 r=[t_raw[ri]], w=[t_carry])
                            B.op("act", lambda e: e.activation(out=dst, in_=rw[:, 0:512], func=AF.Identity, scale=pvc("murkv", g)),
                                 r=[t_raw[ri], t_pv], w=[tk[tkn]])
                            B.op("dve", lambda e: e.scalar_tensor_tensor(
                                out=dst, in0=rw[:, 1:513], scalar=pdc("omurkv", g), in1=dst, op0=ALU.mult, op1=ALU.add),
                                r=[t_raw[ri], t_pd], w=[tk[tkn]])
                        proj_group(g, rkv_cons)
                        yield
                    Pb, Pexb, Pinvb, PCrb = S["VT"], S["BhT"], S["KhT"], S["TT"]
                    tPb, tPexb, tPinvb, tPCrb = stk["VT"], stk["BhT"], stk["KhT"], stk["TT"]
                    rn = raw[0][:, 0:512]
                    B.op("dve", lambda e: e.tensor_tensor_scan(tA, scanm, sgw, 0.0, ALU.mult, ALU.add),
                         r=[tk["sgw"], t_c], w=[tk["A"]])
                    B.op("act", lambda e: e.activation(out=tC, in_=k_t, func=AF.Identity, scale=pvc("kk", hp)),
                         r=[tk["k"], t_pv], w=[tk["C"]])
                    B.op("act", lambda e: e.activation(out=rkb, in_=tC, func=AF.Square), r=[tk["C"]], w=[tk["rkb"]])
                    ps4, tps4 = getps(XB)
                    B.op("pe", lambda e: e.matmul(ps4[:, :], bones, rkb, start=True, stop=True),
                         r=[tk["rkb"], t_c], w=[tps4])
                    B.op("act", lambda e: e.activation(out=vb, in_=v_t, func=AF.Copy), r=[tk["v"]], w=[stk["vb"]])
                    yield
                    B.op("act", lambda e: e.activation(out=Pb, in_=tA, func=AF.Exp, scale=C0), r=[tk["A"]], w=[tPb])
                    B.op("act", lambda e: e.activation(out=Pinvb, in_=tA, func=AF.Exp, scale=-C0), r=[tk["A"]], w=[tPinvb])
                    B.op("act", lambda e: e.activation(out=PCt.rearrange("p (c x) -> p c x", x=1), in_=v3(tA)[:, :, 63:64],
                                                       func=AF.Exp, scale=C0), r=[tk["A"]], w=[stk["PC"]])
                    B.op("dve", lambda e: e.tensor_tensor(out=tD, in0=tA, in1=sgw, op=ALU.subtract),
                         r=[tk["A"], tk["sgw"]], w=[tk["D"]])
                    B.op("dve", lambda e: e.tensor_tensor(out=v3(tB), in0=v3(tA), in1=v3(tA)[:, :, 63:64].to_broadcast([P, 8, 64]),
                                                          op=ALU.subtract), r=[tk["A"]], w=[tk["B"]])
                    yield
                    B.op("act", lambda e: e.activation(out=Pexb, in_=tD, func=AF.Exp, scale=C0), r=[tk["D"]], w=[tPexb])
                    B.op("act", lambda e: e.activation(out=PCrb, in_=tB, func=AF.Exp, scale=-C0), r=[tk["B"]], w=[tPCrb])
                    B.op("act", lambda e: e.activation(out=rn, in_=ps4[:, :], func=AF.Ln, bias=smallc[:, 3:4]), r=[tps4, t_c], w=[t_raw[0]])
                    B.op("act", lambda e: e.activation(out=rn, in_=rn, func=AF.Exp, scale=-0.5), w=[t_raw[0]])
                    B.op("dve", lambda e: e.tensor_tensor(out=art3[:, :, 64:128], in0=v3(r_t), in1=v3(Pb), op=ALU.mult),
                         r=[tk["r"], tPb], w=[stk["art"]])
                    B.op("act", lambda e: e.activation(out=tE, in_=av, func=AF.Identity, scale=pvc("ka", hp), bias=pdc("omka", hp)),
                         r=[tk["av"], t_pv, t_pd], w=[tk["E"]])
                    yield
                    B.op("dve", lambda e: e.tensor_tensor(out=tE, in0=tE, in1=k_t, op=ALU.mult), r=[tk["k"]], w=[tk["E"]])
                    B.op("dve", lambda e: e.tensor_tensor(out=tC, in0=tC, in1=rn, op=ALU.mult), r=[t_raw[0]], w=[tk["C"]])
                    yield
                    B.op("dve", lambda e: e.scalar_tensor_tensor(out=art3[:, :, 0:64], in0=v3(tC), scalar=-1.0, in1=v3(Pexb),
                                                                 op0=ALU.mult, op1=ALU.mult),
                         r=[tk["C"], tPexb], w=[stk["art"]])
                    B.op("dve", lambda e: e.scalar_tensor_tensor(out=rkb, in0=r_t, scalar=pvc("rk", hp), in1=tE,
                                                                 op0=ALU.mult, op1=ALU.mult),
                         r=[tk["r"], tk["E"], t_pv], w=[tk["rkb"]])
                    ps5, tps5 = getps(XB)
                    B.op("pe", lambda e: e.matmul(ps5[:, :], bones, rkb, start=True, stop=True),
                         r=[tk["rkb"], t_c], w=[tps5])
                    yield
                    B.op("dve", lambda e: e.tensor_tensor(out=tC, in0=tC, in1=av, op=ALU.mult), r=[tk["av"]], w=[tk["C"]])
                    B.op("dve", lambda e: e.tensor_tensor(out=kt_, in0=tE, in1=Pinvb, op=ALU.mult),
                         r=[tk["E"], tPinvb], w=[stk["kt"]])
                    yield
                    B.op("dve", lambda e: e.tensor_tensor(out=Kh, in0=tE, in1=PCrb, op=ALU.mult),
                         r=[tk["E"], tPCrb], w=[stk["Kh"]])
                    B.op("dve", lambda e: e.tensor_tensor(out=bt_, in0=tC, in1=Pinvb, op=ALU.mult),
                         r=[tk["C"], tPinvb], w=[stk["bt"]])
                    yield
                    B.op("dve", lambda e: e.tensor_tensor(out=Bh, in0=tC, in1=PCrb, op=ALU.mult),
                         r=[tk["C"], tPCrb], w=[stk["Bh"]])
                    B.op("dve", lambda e: e.tensor_tensor(out=bonus, in0=ps5[:, :], in1=v_t, op=ALU.mult),
                         r=[tps5, tk["v"]], w=[stk["bonus"]])
                    yield

                def ysetup(hp):
                    S = {**SETS[hp % 2], **CH[hp]}
                    stk = {**SETS[hp % 2]["tk"], **CH[hp]["tk"]}
                    return S, stk

                def y2a(hp):
                    S, stk = ysetup(hp)
                    TK = lambda n: stk[n] if n in stk else tk[n]
                    art, bt_, kt_, Bh, Kh, vb = (S[n] for n in ["art", "bt", "kt", "Bh", "Kh", "vb"])
                    VT, BhT, KhT, LakT, LrbT, LrkT, TTf = (S[n] for n in ["VT", "BhT", "KhT", "LakT", "LrbT", "LrkT", "TT"])
                    for (src, sk, dst, dtk) in [(vb, "vb", VT, "VT"), (Bh, "Bh", BhT, "BhT"), (Kh, "Kh", KhT, "KhT")]:
                        ps, tps = getps(YB)
                        for c in range(8):
                            for e_ in range(2):
                                B.op("pe", lambda e, ps=ps, c=c, e_=e_, src=src: e.matmul(
                                    ps[eh[e_], c * 64:(c + 1) * 64], src[eh[e_], c * 64:(c + 1) * 64],
                                    ident[eh[e_], e_ * 64:(e_ + 1) * 64], start=True, stop=True,
                                    tile_position=(64 * e_, 64 * e_)), r=[stk[sk], t_c], w=[tps])
                        B.op("act", lambda e, ps=ps, dst=dst: e.activation(out=dst, in_=ps[:, :], func=AF.Copy),
                             r=[tps], w=[stk[dtk]])
                        yield
                    for (stat, sk, dA, dAk, dB, dBk) in [(bt_, "bt", Rm[0], "R0", LrbT, "LrbT"),
                                                        (kt_, "kt", LakT, "LakT", LrkT, "LrkT")]:
                        for half in range(2):
                            ps, tps = getps(YB)
                            for c4 in range(4):
                                c = half * 4 + c4
                                for e_ in range(2):
                                    B.op("pe", lambda e, ps=ps, c=c, c4=c4, e_=e_, stat=stat: e.matmul(
                                        ps[eh[e_], c4 * 128:(c4 + 1) * 128], stat[eh[e_], c * 64:(c + 1) * 64],
                                        art[eh[e_], c * 128:(c + 1) * 128], start=True, stop=True,
                                        tile_position=(64 * e_, 64 * e_)), r=[stk[sk], stk["art"]], w=[tps])
                            ps3 = ps[:, :].rearrange("p (c x) -> p c x", c=4)
                            B.op("dve", lambda e, ps3=ps3, dA=dA, half=half: e.tensor_tensor(
                                out=v3(dA)[:, half * 4:(half + 1) * 4, :], in0=ps3[:, :, 0:64], in1=mk3(msu, 4), op=ALU.mult),
                                r=[tps, t_c], w=[TK(dAk)])
                            B.op("dve", lambda e, ps3=ps3, dB=dB, half=half: e.tensor_tensor(
                                out=v3(dB)[:, half * 4:(half + 1) * 4, :], in0=ps3[:, :, 64:128], in1=mk3(miu, 4), op=ALU.mult),
                                r=[tps, t_c], w=[TK(dBk)])
                            yield
                    ps, tps = getps(YB)
                    for c in range(8):
                        for e_ in range(2):
                            B.op("pe", lambda e, ps=ps, c=c, e_=e_: e.matmul(
                                ps[eh[e_], c * 64:(c + 1) * 64], art[eh[e_], c * 128: c * 128 + 64],
                                bt_[eh[e_], c * 64:(c + 1) * 64], start=True, stop=True,
                                tile_position=(64 * e_, 64 * e_)), r=[stk["bt"], stk["art"]], w=[tps])
                    B.op("dve", lambda e, ps=ps: e.tensor_tensor(out=Lm[0], in0=ps[:, :], in1=msl, op=ALU.mult),
                         r=[tps, t_c], w=[tk["L0"]])
                    B.op("dve", lambda e: e.tensor_tensor(out=Gm[0], in0=Rm[0], in1=irep, op=ALU.add),
                         r=[tk["R0"], t_c], w=[tk["G0"]])
                    yield
                    cur = 0
                    for lev in range(1, 6):
                        nxt = 1 - cur
                        Rc, Lc, Gc = Rm[cur], Lm[cur], Gm[cur]
                        Rn, Ln, Gn = Rm[nxt], Lm[nxt], Gm[nxt]
                        tRc, tLc, tGc = tk["R%d" % cur], tk["L%d" % cur], tk["G%d" % cur]
                        tRn, tLn, tGn = tk["R%d" % nxt], tk["L%d" % nxt], tk["G%d" % nxt]
                        if lev == 5:
                            Gn, tGn = TTf, stk["TT"]
                        ps, tps = getps(YB)
                        for c in range(8):
                            for e_ in range(2):
                                sl = slice(c * 64, (c + 1) * 64)
                                B.op("pe", lambda e, ps=ps, sl=sl, e_=e_: e.matmul(
                                    ps[eh[e_], sl], Rc[eh[e_], sl], Lc[eh[e_], sl], start=True, stop=True,
                                    tile_position=(64 * e_, 64 * e_)), r=[tRc, tLc], w=[tps])
                        B.op("act", lambda e, ps=ps: e.activation(out=Ln, in_=ps[:, :], func=AF.Copy),
                             r=[tps], w=[tLn])
                        if lev < 5:
                            ps2, tps2 = getps(YB)
                            for c in range(8):
                                for e_ in range(2):
                                    sl = slice(c * 64, (c + 1) * 64)
                                    B.op("pe", lambda e, ps2=ps2, sl=sl, e_=e_: e.matmul(
                                        ps2[eh[e_], sl], Lc[eh[e_], sl], Rc[eh[e_], sl], start=True, stop=True,
                                        tile_position=(64 * e_, 64 * e_)), r=[tRc, tLc], w=[tps2])
                            B.op("dve", lambda e, ps2=ps2: e.tensor_copy(Rn, ps2[:, :]), r=[tps2], w=[tRn])
                        yield
                        ps3_, tps3 = getps(YB)
                        for c in range(8):
                            for e_ in range(2):
                                sl = slice(c * 64, (c + 1) * 64)
                                B.op("pe", lambda e, ps3_=ps3_, sl=sl, e_=e_: e.matmul(
                                    ps3_[eh[e_], sl], Ln[eh[e_], sl], Gc[eh[e_], sl], start=True, stop=True,
                                    tile_position=(64 * e_, 64 * e_)), r=[tLn, tGc], w=[tps3])
                        B.op("dve", lambda e, ps3_=ps3_: e.tensor_tensor(out=Gn, in0=ps3_[:, :], in1=Gc, op=ALU.add),
                             r=[tps3, tGc], w=[tGn])
                        yield
                        cur = nxt

                PSY = {}

                def y_chain(hp):
                    S, stk = ysetup(hp)
                    art, bonus, gg, PCt = (S[n] for n in ["art", "bonus", "gg", "PC"])
                    VT, BhT, KhT, LakT, LrbT, LrkT, TTm, Xb, Ub = (S[n] for n in ["VT", "BhT", "KhT", "LakT", "LrbT", "LrkT", "TT", "Xb", "Ub"])
                    tTT = stk["TT"]
                    YB = (0, 1, 2, 3)
                    psY, tpsY = getps((4 + hp,), hold=True)
                    PSY[hp] = (psY, tpsY)
                    Sb = STb[:, hp * 64:(hp + 1) * 64]
                    S32 = ST32[:, hp * 64:(hp + 1) * 64]
                    tS = t_ST[hp]
                    for c in range(8):
                        sl = slice(c * 64, (c + 1) * 64)
                        psX, tpsX = getps(YB)
                        for e_ in range(2):
                            B.op("pe", lambda e, e_=e_: e.matmul(
                                psX[eh[e_], 0:64], art[eh[e_], c * 128:c * 128 + 64], Sb[eh[e_], :], start=True, stop=False,
                                tile_position=(64 * e_, 64 * e_)), r=[stk["art"], tS], w=[tpsX])
                            B.op("pe", lambda e, e_=e_: e.matmul(
                                psX[eh[e_], 0:64], LakT[eh[e_], sl], VT[eh[e_], sl], start=False, stop=True,
                                tile_position=(64 * e_, 64 * e_)), r=[stk["LakT"], stk["VT"]], w=[tpsX])
                        B.op("act", lambda e: e.activation(out=Xb, in_=psX[:, 0:64], func=AF.Copy),
                             r=[tpsX], w=[stk["Xb"]])
                        yield
                        psU, tpsU = getps(YB)
                        for e_ in range(2):
                            B.op("pe", lambda e, e_=e_: e.matmul(
                                psU[eh[e_], 0:64], TTm[eh[e_], sl], Xb[eh[e_], :], start=True, stop=True,
                                tile_position=(64 * e_, 64 * e_)), r=[tTT, stk["Xb"]], w=[tpsU])
                        B.op("dve", lambda e: e.tensor_copy(Ub, psU[:, 0:64]), r=[tpsU], w=[stk["Ub"]])
                        yield
                        for e_ in range(2):
                            B.op("pe", lambda e, e_=e_: e.matmul(
                                psY[eh[e_], sl], Sb[eh[e_], :], art[eh[e_], c * 128 + 64:(c + 1) * 128], start=True, stop=False,
                                tile_position=(64 * e_, 64 * e_)), r=[tS, stk["art"]], w=[tpsY])
                            B.op("pe", lambda e, e_=e_: e.matmul(
                                psY[eh[e_], sl], Ub[eh[e_], :], LrbT[eh[e_], sl], start=False, stop=False,
                                tile_position=(64 * e_, 64 * e_)), r=[stk["Ub"], stk["LrbT"]], w=[tpsY])
                            B.op("pe", lambda e, e_=e_: e.matmul(
                                psY[eh[e_], sl], VT[eh[e_], sl], LrkT[eh[e_], sl], start=False, stop=True,
                                tile_position=(64 * e_, 64 * e_)), r=[stk["VT"], stk["LrkT"]], w=[tpsY])
                        psS, tpsS = getps(YB)
                        for e_ in range(2):
                            B.op("pe", lambda e, e_=e_: e.matmul(
                                psS[eh[e_], 0:64], BhT[eh[e_], sl], Ub[eh[e_], :], start=True, stop=False,
                                tile_position=(64 * e_, 64 * e_)), r=[stk["BhT"], stk["Ub"]], w=[tpsS])
                            B.op("pe", lambda e, e_=e_: e.matmul(
                                psS[eh[e_], 0:64], KhT[eh[e_], sl], VT[eh[e_], sl], start=False, stop=True,
                                tile_position=(64 * e_, 64 * e_)), r=[stk["KhT"], stk["VT"]], w=[tpsS])
                        B.op("dve", lambda e: e.scalar_tensor_tensor(
                            out=S32, in0=S32, scalar=PCt[:, c:c + 1], in1=psS[:, 0:64], op0=ALU.mult, op1=ALU.add),
                            r=[tpsS, stk["PC"]], w=[tS])
                        B.op("act", lambda e: e.activation(out=Sb, in_=S32, func=AF.Copy), w=[tS])
                        yield

                TAIL3 = [(r_t, "r", k_t, "k", v_t, "v"), (sgw, "sgw", av, "av", tE, "E")]

                def y_tail(hp):
                    S, stk = ysetup(hp)
                    bonus, gg, VT, BhT = (S[n] for n in ["bonus", "gg", "VT", "BhT"])
                    YB = (0, 1, 2, 3)
                    psY, tpsY = PSY[hp]
                    gA, kA, gB, kB, gC, kC = TAIL3[hp % 2]
                    tgA, tgB, tgC = tk[kA], tk[kB], tk[kC]
                    B.op("act", lambda e: e.activation(out=gA, in_=psY[:, :], func=AF.Copy), r=[tpsY], w=[tgA])
                    B.op("act", lambda e: e.activation(out=VT, in_=psY[:, :], func=AF.Copy), r=[tpsY], w=[stk["VT"]])
                    B.op("act", lambda e: e.activation(out=BhT, in_=psY[:, :], func=AF.Square), r=[tpsY], w=[stk["BhT"]])
                    release(psY)
                    yield
                    psm, tpsm = getps(YB)
                    psq, tpsq = getps(YB)
                    B.op("pe", lambda e: e.matmul(psm[:, :], bones, VT, start=True, stop=True),
                         r=[stk["VT"], t_c], w=[tpsm])
                    B.op("pe", lambda e: e.matmul(psq[:, :], bones, BhT, start=True, stop=True),
                         r=[stk["BhT"], t_c], w=[tpsq])
                    B.op("dve", lambda e: e.tensor_scalar(out=gB, in0=psm[:, :], scalar1=1.0 / 64, scalar2=None, op0=ALU.mult),
                         r=[tpsm], w=[tgB])
                    B.op("dve", lambda e: e.tensor_tensor(out=gC, in0=gB, in1=gB, op=ALU.mult), r=[tgB], w=[tgC])
                    B.op("dve", lambda e: e.scalar_tensor_tensor(out=gC, in0=psq[:, :], scalar=1.0 / 64, in1=gC,
                                                                 op0=ALU.mult, op1=ALU.subtract),
                         r=[tpsq], w=[tgC])
                    yield
                    B.op("act", lambda e: e.activation(out=gC, in_=gC, func=AF.Ln, bias=eps_gn), r=[t_c], w=[tgC])
                    B.op("act", lambda e: e.activation(out=gC, in_=gC, func=AF.Exp, scale=-0.5), w=[tgC])
                    B.op("dve", lambda e: e.tensor_tensor(out=gA, in0=gA, in1=gB, op=ALU.subtract), r=[tgB], w=[tgA])
                    yield
                    B.op("dve", lambda e: e.tensor_tensor(out=gA, in0=gA, in1=gC, op=ALU.mult), r=[tgC], w=[tgA])
                    B.op("act", lambda e: e.activation(out=gA, in_=gA, func=AF.Identity, scale=pvc("lng", hp), bias=pvc("lnb", hp)),
                         r=[t_pv], w=[tgA])
                    yield
                    B.op("dve", lambda e: e.tensor_tensor(out=gA, in0=gA, in1=bonus, op=ALU.add), r=[stk["bonus"]], w=[tgA])
                    B.op("dve", lambda e: e.tensor_tensor(out=mixcat[:, hp * TT:(hp + 1) * TT], in0=gA, in1=gg, op=ALU.mult),
                         r=[tgA, stk["gg"]], w=[t_mix[hp]])
                    yield

                def chain_gens(gs):
                    for g_ in gs:
                        yield from g_

                def interleave(gs):
                    gs = list(gs)
                    while gs:
                        for g_ in list(gs):
                            try:
                                next(g_)
                            except StopIteration:
                                gs.remove(g_)
                for _ in x_lowrank():
                    pass
                for _ in x_prep(0):
                    pass
                for hp in range(1, 4):
                    interleave([x_prep(hp), y2a(hp - 1)])
                interleave([chain_gens([x_sc(0), x_sc(1)]), y2a(3)])
                for dg in range(2):
                    B.dma("pool", wos[dg][:, :], wo_d[l, dg], w=[t_wos[dg]])
                XB = (0, 1, 2, 3)
                interleave([y_chain(0), y_chain(1), y_chain(2), y_chain(3), chain_gens([x_cfall(), x_cfln()])])
                interleave([y_tail(0), y_tail(1)])
                interleave([y_tail(2), y_tail(3)])
                B.barrier()

                if dbg and l == 0 and tt == 0:
                    B.dma("pool", dbg_d[:, :], mixcat[:, :], r=t_mix)

                ar.off = base_off
                wos4 = [wos[0][:, :], wos[1][:, :], ar.alloc(KC * 128, BF16), ar.alloc(KC * 128, BF16)]
                t_wos4 = [t_wos[0], t_wos[1], Tok(), Tok()]
                for dg in (2, 3):
                    B.dma("pool", wos4[dg], wo_d[l, dg], w=[t_wos4[dg]])
                pss, tss = getps(hold=True)
                for dg in range(KC):
                    si = dg % 4
                    if dg >= 4:
                        B.dma("pool", wos4[si], wo_d[l, dg], w=[t_wos4[si]])
                    ps, tps = getps()
                    for kc in range(KC):
                        B.op("pe", lambda e, ps=ps, kc=kc, si=si: e.matmul(
                            ps[:, :], wos4[si][:, kc * 128:(kc + 1) * 128], mixcat[:, kc * TT:(kc + 1) * TT],
                            start=(kc == 0), stop=(kc == KC - 1)), r=[t_wos4[si], t_mix[kc]], w=[tps])
                    B.op("act", lambda e, ps=ps, dg=dg: e.activation(out=fo[:, dg * TT:(dg + 1) * TT], in_=ps[:, :], func=AF.Copy),
                         r=[tps], w=[t_fo[dg]])
                    i = dg % 2
                    B.op("act", lambda e, ps=ps, i=i: e.activation(out=NORM["sq"][i], in_=ps[:, :], func=AF.Square),
                         r=[tps], w=[NORM["t_sq"][i]])
                    B.op("pe", lambda e, i=i, dg=dg, pss=pss: e.matmul(pss[:, :], ones, NORM["sq"][i], start=(dg == 0), stop=(dg == KC - 1)),
                         r=[NORM["t_sq"][i], t_c], w=[tss])
                release(pss)
                post_norm_residual(tt, "postmix", pss, tss)

                B.barrier()
                ar.off = base_off
                act_t = ar.alloc(NJ * TT, BF16)
                t_act = [Tok() for _ in range(NJ)]
                NGUS, NWDS = 5, 3
                gus = [ar.alloc(2 * KC * 128, BF16) for _ in range(NGUS)]
                t_gus = [Tok() for _ in range(NGUS)]
                t_guu = [Tok() for _ in range(NGUS)]
                wds = [ar.alloc(NJ * 128, BF16) for _ in range(NWDS)]
                t_wds = [Tok() for _ in range(NWDS)]
                sil = [ar.alloc(TT, F32), ar.alloc(TT, F32)]
                t_sil = [Tok(), Tok()]
                for dg in range(NWDS):
                    B.dma("pool", wds[dg], wd_d[l, dg], w=[t_wds[dg]])
                rmsnorm_to_hT(tt, "preffn")
                for j in range(NJ):
                    si = rr("gus", NGUS)
                    B.dma("pool", gus[si][:, 0:KC * 128], wg_d[l, j], w=[t_gus[si]])
                    B.dma("pool", gus[si][:, KC * 128:2 * KC * 128], wu_d[l, j], w=[t_guu[si]])
                    psg, tpsg = getps()
                    psu, tpsu = getps()
                    for kc in range(KC):
                        B.op("pe", lambda e, kc=kc, si=si, psg=psg: e.matmul(
                            psg[:, :], gus[si][:, kc * 128:(kc + 1) * 128], hs(kc, 1, 513),
                            start=(kc == 0), stop=(kc == KC - 1)), r=[t_gus[si], t_hT[kc]], w=[tpsg])
                    for kc in range(KC):
                        B.op("pe", lambda e, kc=kc, si=si, psu=psu: e.matmul(
                            psu[:, :], gus[si][:, (KC + kc) * 128:(KC + kc + 1) * 128], hs(kc, 1, 513),
                            start=(kc == 0), stop=(kc == KC - 1)), r=[t_guu[si], t_hT[kc]], w=[tpsu])
                    i = j % 2
                    B.op("act", lambda e, i=i, psg=psg: e.activation(out=sil[i], in_=psg[:, :], func=AF.Silu),
                         r=[tpsg], w=[t_sil[i]])
                    B.op("dve", lambda e, i=i, j=j, psu=psu: e.tensor_tensor(
                        out=act_t[:, j * TT:(j + 1) * TT], in0=psu[:, :], in1=sil[i], op=ALU.mult),
                        r=[tpsu, t_sil[i]], w=[t_act[j]])
                pss, tss = getps(hold=True)
                for dg in range(KC):
                    si = dg % NWDS
                    if dg >= NWDS:
                        B.dma("pool", wds[si], wd_d[l, dg], w=[t_wds[si]])
                    ps, tps = getps()
                    for j in range(NJ):
                        B.op("pe", lambda e, ps=ps, j=j, si=si: e.matmul(
                            ps[:, :], wds[si][:, j * 128:(j + 1) * 128], act_t[:, j * TT:(j + 1) * TT],
                            start=(j == 0), stop=(j == NJ - 1)), r=[t_wds[si], t_act[j]], w=[tps])
                    B.op("act", lambda e, ps=ps, dg=dg: e.activation(out=fo[:, dg * TT:(dg + 1) * TT], in_=ps[:, :], func=AF.Copy),
                         r=[tps], w=[t_fo[dg]])
                    i = dg % 2
                    B.op("act", lambda e, ps=ps, i=i: e.activation(out=NORM["sq"][i], in_=ps[:, :], func=AF.Square),
                         r=[tps], w=[NORM["t_sq"][i]])
                    B.op("pe", lambda e, i=i, dg=dg, pss=pss: e.matmul(pss[:, :], ones, NORM["sq"][i], start=(dg == 0), stop=(dg == KC - 1)),
                         r=[NORM["t_sq"][i], t_c], w=[tss])
                release(pss)
                post_norm_residual(tt, "postffn", pss, tss)

        t_out = Tok()
        for tt in range(NT):
            for kc in range(KC):
                B.dma("sp", y_d[:, kc * T + tt * TT: kc * T + (tt + 1) * TT], xs(kc, tt), r=[t_x[tt][kc]], w=[Tok()])
        B.barrier()

        with nc.Block() as block:
            B.emit(block)
    return nc


def _consts():
    c = np.zeros((P, NCF), np.float32)
    p = np.arange(P)[:, None]
    c[:, CF["ident"]:CF["ident"] + 128] = (p == np.arange(128)[None, :])
    c[:, CF["ones"]:CF["ones"] + 128] = 1.0
    c[:, CF["bones"]:CF["bones"] + 128] = ((p // 64) == (np.arange(128)[None, :] // 64))
    col = (np.arange(512) % 64)[None, :]
    row = p % 64
    c[:, CF["msu"]:CF["msu"] + 512] = (row < col)
    c[:, CF["miu"]:CF["miu"] + 512] = (row <= col)
    c[:, CF["msl"]:CF["msl"] + 512] = (col < row)
    c[:, CF["irep"]:CF["irep"] + 512] = (row == col)
    c[:, CF["scan"]:CF["scan"] + 512] = (col != 0) * np.ones((P, 1))
    return c


def _colvec(v):
    return np.ascontiguousarray(np.asarray(v, np.float32).reshape(-1, P).T)


def _prep_inputs(inp, L):
    f = lambda a: np.asarray(a, np.float32)
    out = {}

    def grp_cols(w, ncol_groups):
        Lw, K, N = w.shape
        a = w.reshape(Lw, K // P, P, N // P, P)
        a = a.transpose(0, 3, 2, 1, 4)
        return np.ascontiguousarray(a.reshape(Lw, N // P, P, (K // P) * P))
    out["w_in"] = grp_cols(f(inp["w_in"])[:L], NJ)
    out["w_gate"] = grp_cols(f(inp["w_gate"])[:L], NJ)
    out["w_up"] = grp_cols(f(inp["w_up"])[:L], NJ)
    out["w_o"] = grp_cols(f(inp["w_o"])[:L], KC)
    out["w_down"] = grp_cols(f(inp["w_down"])[:L], KC)
    wl = np.concatenate([f(inp["w1"]), f(inp["a1"]), f(inp["g1"])], axis=2)[:L]
    wl = wl.reshape(L, KC, P, 256).transpose(0, 2, 1, 3).reshape(L, P, KC * 256)
    out["wlr"] = np.ascontiguousarray(wl)
    out["w2a2"] = np.ascontiguousarray(np.concatenate([f(inp["w2"]), f(inp["a2"])], axis=1)[:L])
    out["g2"] = np.ascontiguousarray(f(inp["g2"])[:L])
    pv = np.zeros((L, P, NPV), np.float32)
    for l in range(L):
        def put(name, arr):
            a = _colvec(arr)
            pv[l, :, PV[name]:PV[name] + a.shape[1]] = a
        put("premix", inp["pre_mix_g"][l]); put("postmix", inp["post_mix_g"][l])
        put("preffn", inp["pre_ffn_g"][l]); put("postffn", inp["post_ffn_g"][l])
        put("muw", inp["mu_wag"][l][0]); put("mua", inp["mu_wag"][l][1]); put("mug", inp["mu_wag"][l][2])
        put("murkv", inp["mu_rkv"][l])
        put("w0", inp["w0"][l]); put("a0", inp["a0"][l]); put("kk", inp["k_k"][l]); put("ka", inp["k_a"][l])
        put("rk", np.asarray(inp["r_k"][l]).reshape(-1)); put("lng", inp["lnx_g"][l]); put("lnb", inp["lnx_b"][l])
        scw = f(inp["sc_conv_w"][l])
        for k_ in range(3):
            pv[l, :, PV["scw"] + k_ * 2: PV["scw"] + k_ * 2 + 2] = _colvec(scw[k_])
        cfw = f(inp["cf_conv_w"][l])
        for k_ in range(31):
            pv[l, :, PV["cfw"] + k_ * 2: PV["cfw"] + k_ * 2 + 2] = _colvec(cfw[k_])
        put("cfb", inp["cf_conv_b"][l]); put("cflg", inp["cf_ln_g"][l]); put("cflb", inp["cf_ln_b"][l])
    out["pv"] = pv
    out["cf"] = _consts()
    return out


_CACHE = {}


def run(inputs, L=4, dbg=False):
    x = np.asarray(inputs["x"], np.float32)
    shared = _prep_inputs(inputs, L)
    key = (L, dbg)
    if key not in _CACHE:
        _CACHE[key] = build_program(L, dbg)
    nc = _CACHE[key]
    in_maps = []
    for b in range(NCORES):
        xt = np.ascontiguousarray(x[b].T.reshape(KC, P, T).transpose(1, 0, 2).reshape(P, KC * T))
        m = dict(shared)
        m["xT"] = xt
        in_maps.append(m)
    res = run_bass_kernel_spmd(nc, in_maps, core_ids=list(range(NCORES)))
    outs = []
    for b in range(NCORES):
        yt = np.asarray(res.results[b]["yT"]).reshape(P, KC, T).transpose(1, 0, 2).reshape(D, T)
        outs.append(yt.T)
    y = np.stack(outs, 0).astype(np.float32)
    if dbg:
        return y, [np.asarray(r["dbg"]) for r in res.results]
    return y


def kernel(**inputs):
    return run(inputs, L=4)
```

```python
import contextlib
import numpy as np
import concourse.bass as bass
import concourse.mybir as mybir
from concourse.bass_utils import run_bass_kernel_spmd

F32 = mybir.dt.float32
BF16 = mybir.dt.bfloat16
AF = mybir.ActivationFunctionType
ALU = mybir.AluOpType

P = 128
T = 2048
D = 1024
TT = 512
NT = T // TT
KC = 8
NJ = 22
NCORES = 8
C0 = -0.6065306597126334
SAME_ENGINE_SYNC = True

PV = {}
_o = 0
for _n, _c in [("premix", 8), ("postmix", 8), ("preffn", 8), ("postffn", 8), ("muw", 8), ("mua", 8),
               ("mug", 8), ("murkv", 12), ("w0", 4), ("a0", 4), ("kk", 4), ("ka", 4), ("rk", 4),
               ("lng", 4), ("lnb", 4), ("scw", 6), ("cfw", 62), ("cfb", 2), ("cflg", 2), ("cflb", 2)]:
    PV[_n] = _o
    _o += _c
NPV = _o
PD = {"omuw": 0, "omua": 8, "omug": 16, "omurkv": 24, "omka": 36}
NPD = 40
CF = {"ident": 0, "ones": 128, "bones": 256, "msu": 384, "miu": 896, "msl": 1408, "irep": 1920, "scan": 2432}
NCF = 2944


class Tok:
    __slots__ = ("w", "r")

    def __init__(self):
        self.w = None
        self.r = []


class _Rec:
    def __init__(self):
        self.calls = []

    def __getattr__(self, name):
        def f(*a, **k):
            self.calls.append((name, a, k))
        return f


class Builder:
    ENGS = ("pe", "act", "dve", "pool", "sp")

    def __init__(self, nc, es, ndma=48):
        self.nc = nc
        self.q = {e: [] for e in self.ENGS}
        self.cnt = {e: 0 for e in self.ENGS}
        self.seen = {e: {} for e in self.ENGS}
        self.sems = {}
        for e in ("pe", "act", "dve", "pool"):
            self.sems[e] = es.enter_context(nc.semaphore("s_" + e))
        self.ndma = ndma
        self.dma_uses = [0] * ndma
        self.dma_next = 0
        for k in range(ndma):
            self.sems["dma%d" % k] = es.enter_context(nc.semaphore("s_dma%d" % k))

    def _collect(self, r, w):
        deps = {}

        def add(d):
            if d is None:
                return
            k, v = d
            if deps.get(k, 0) < v:
                deps[k] = v
        for t in r:
            add(t.w)
        for t in w:
            add(t.w)
            for d in t.r:
                add(d)
        return deps

    def _waits(self, eng, deps):
        out = []
        seen = self.seen[eng]
        for k, v in deps.items():
            if k == eng and (eng == "pe" or not SAME_ENGINE_SYNC):
                continue
            if seen.get(k, 0) >= v:
                continue
            seen[k] = v
            out.append((k, v))
        return out

    def op(self, eng, fn, r=(), w=()):
        rec = _Rec()
        fn(rec)
        assert len(rec.calls) == 1
        call = rec.calls[0]
        fn = lambda e, call=call: getattr(e, call[0])(*call[1], **call[2])
        deps = self._collect(r, w)
        waits = self._waits(eng, deps)
        self.cnt[eng] += 1
        me = (eng, self.cnt[eng])
        self.q[eng].append((fn, waits, (eng, 1)))
        for t in r:
            t.r.append(me)
        for t in w:
            t.w = me
            t.r = []

    def dma(self, qe, out_ap, in_ap, r=(), w=()):
        deps = self._collect(r, w)
        k = self.dma_next
        self.dma_next = (k + 1) % self.ndma
        key = "dma%d" % k
        if self.dma_uses[k] > 0:
            v = 16 * self.dma_uses[k]
            if deps.get(key, 0) < v:
                deps[key] = v
        waits = self._waits(qe, deps)
        self.dma_uses[k] += 1
        me = (key, 16 * self.dma_uses[k])
        self.q[qe].append((lambda e: e.dma_start(out=out_ap, in_=in_ap), waits, (key, 16)))
        for t in r:
            t.r.append(me)
        for t in w:
            t.w = me
            t.r = []

    def wait_only(self, eng, toks):
        deps = self._collect(toks, ())
        waits = self._waits(eng, deps)
        self.q[eng].append((None, waits, None))

    def barrier(self):
        allv = {e: self.cnt[e] for e in ("pe", "act", "dve", "pool") if self.cnt[e] > 0}
        for k in range(self.ndma):
            if self.dma_uses[k] > 0:
                allv["dma%d" % k] = 16 * self.dma_uses[k]
        for e in self.ENGS:
            deps = {k: v for k, v in allv.items() if k != e}
            waits = self._waits(e, deps)
            if waits:
                self.q[e].append((None, waits, None))

    def emit(self, block):
        sems = self.sems

        def run(e, lst):
            for fn, waits, inc in lst:
                for k, v in waits:
                    e.wait_ge(sems[k], v)
                if fn is not None:
                    ins = fn(e)
                    if inc is not None:
                        ins.then_inc(sems[inc[0]], inc[1])

        @block.tensor
        def _(e):
            run(e, self.q["pe"])

        @block.scalar
        def _(e):
            run(e, self.q["act"])

        @block.vector
        def _(e):
            run(e, self.q["dve"])

        @block.gpsimd
        def _(e):
            run(e, self.q["pool"])

        @block.sync
        def _(e):
            run(e, self.q["sp"])


class Arena:
    def __init__(self, ap, nbytes):
        self.ap = ap
        self.n = nbytes
        self.off = 0

    def alloc(self, cols, dtype):
        sz = cols * (4 if dtype == F32 else 2)
        sz = (sz + 3) // 4 * 4
        assert self.off + sz <= self.n, ("arena overflow", self.off, sz, self.n)
        a = self.ap[:, self.off // 4:(self.off + sz) // 4]
        self.off += sz
        if dtype != F32:
            a = a.bitcast(dtype)
            a = a[:, 0:cols]
        return a


def build_program(L=4, dbg=False):
    nc = bass.Bass("TRN2", target_bir_lowering=False)
    dt_in = lambda n, s: nc.dram_tensor(n, s, F32, kind="ExternalInput").ap()
    x_d = dt_in("xT", [P, KC * T])
    win_d = dt_in("w_in", [L, NJ, P, KC * 128])
    wg_d = dt_in("w_gate", [L, NJ, P, KC * 128])
    wu_d = dt_in("w_up", [L, NJ, P, KC * 128])
    wo_d = dt_in("w_o", [L, KC, P, KC * 128])
    wd_d = dt_in("w_down", [L, KC, P, NJ * 128])
    wlr_d = dt_in("wlr", [L, P, KC * 256])
    w2a2_d = dt_in("w2a2", [L, P, 512])
    g2_d = dt_in("g2", [L, P, 512])
    pv_d = dt_in("pv", [L, P, NPV])
    cf_d = dt_in("cf", [P, NCF])
    y_d = nc.dram_tensor("yT", [P, KC * T], F32, kind="ExternalOutput").ap()
    if dbg:
        dbg_d = nc.dram_tensor("dbg", [P, KC * TT], F32, kind="ExternalOutput").ap()

    with contextlib.ExitStack() as es:
        def sb(name, cols, dtype):
            return es.enter_context(nc.sbuf_tensor(name, [P, cols], dtype))
        xT = sb("xT_sb", KC * T, F32)
        pv = sb("pv_sb", NPV, F32)
        pd = sb("pd_sb", NPD, F32)
        cb = sb("cb", NCF, BF16)
        cmask = cb[:, 384:NCF]
        smallc = sb("smallc", 4, F32)
        wlra = sb("wlra", KC * 256, BF16)
        wlrb = sb("wlrb", KC * 256, BF16)
        w2a2 = sb("w2a2_sb", 512, BF16)
        g2 = sb("g2_sb", 512, BF16)
        rkvcarry = sb("rkvcarry", 12, F32)
        cu = sb("cu", 2 * 514, F32)
        cfu = sb("cfu", 2 * 542, F32)
        ST32 = sb("ST32", 4 * 64, F32)
        STb = sb("STb", 4 * 64, BF16)
        hT = sb("hT", KC * 513, BF16)
        hcarry = sb("hcarry", KC, BF16)
        mixcat = sb("mixcat", KC * TT, BF16)
        fo = sb("fo", KC * TT, F32)
        wlr = fo[:, 0:KC * 128].bitcast(BF16)
        wos = [sb("wos%d" % i, KC * 128, BF16) for i in range(2)]
        ARENA_BYTES = 80 * 1024
        arena_t = sb("arena", ARENA_BYTES // 4, F32)
        psum = [es.enter_context(nc.psum_tensor("ps%d" % i, [P, 512], F32)) for i in range(8)]

        B = Builder(nc, es)
        pst = [Tok() for _ in range(8)]
        ps_rr = [0]

        held = set()

        ALLB = tuple(range(8))
        ps_ptr = {}

        def getps(banks=None, hold=False):
            key = banks or ALLB
            while True:
                p_ = ps_ptr.get(key, 0)
                ps_ptr[key] = p_ + 1
                i = key[p_ % len(key)]
                if i not in held:
                    break
            if hold:
                held.add(i)
            return psum[i], pst[i]

        def release(ps):
            for i in range(8):
                if psum[i] is ps:
                    held.discard(i)

        t_x = [[Tok() for _ in range(KC)] for _ in range(NT)]
        t_pv, t_pd, t_c, t_wlr, t_wlrab, t_w2, t_g2 = Tok(), Tok(), Tok(), Tok(), Tok(), Tok(), Tok()
        t_carry, t_cu, t_cfu = Tok(), [Tok(), Tok()], [Tok(), Tok()]
        t_ST = [Tok() for _ in range(4)]
        t_hT, t_mix, t_fo = [Tok() for _ in range(KC)], [Tok() for _ in range(KC)], [Tok() for _ in range(KC)]
        t_wos = [Tok(), Tok()]
        t_hc = [Tok() for _ in range(KC)]

        def pvc(name, i=0):
            o = PV[name] + i
            return pv[:, o:o + 1]

        def pdc(name, i=0):
            o = PD[name] + i
            return pd[:, o:o + 1]

        ident = cb[:, 0:128]
        ones = cb[:, 128:256]
        bones = cb[:, 256:384]
        msu = cmask[:, 0:512]
        miu = cmask[:, 512:1024]
        msl = cmask[:, 1024:1536]
        irep = cmask[:, 1536:2048]
        scanm = cmask[:, 2048:2560]
        eps_n = smallc[:, 0:1]
        eps_ln = smallc[:, 1:2]
        eps_gn = smallc[:, 2:3]

        def xs(kc, tt):
            return xT[:, kc * T + tt * TT: kc * T + (tt + 1) * TT]

        def hs(kc, a, b):
            return hT[:, kc * 513 + a: kc * 513 + b]

        for kc in range(KC):
            for tt in range(NT):
                B.dma("sp", xs(kc, tt), x_d[:, kc * T + tt * TT: kc * T + (tt + 1) * TT], w=[t_x[tt][kc]])
        B.dma("pool", cb[:, :], cf_d[:, :], w=[t_c])
        B.op("dve", lambda e: e.memset(smallc[:, 0:1], 1e-6), w=[t_c])
        B.op("dve", lambda e: e.memset(smallc[:, 1:2], 1e-5), w=[t_c])
        B.op("dve", lambda e: e.memset(smallc[:, 2:3], 64e-5), w=[t_c])
        B.op("dve", lambda e: e.memset(smallc[:, 3:4], 1e-24), w=[t_c])

        wslot_rr = {}

        def rr(name, n):
            i = wslot_rr.get(name, 0)
            wslot_rr[name] = (i + 1) % n
            return i

        def rmsnorm_to_hT(tt, gname):
            pss, tss = getps()
            sq, t_sq = NORM["sq"], NORM["t_sq"]
            for kc in range(KC):
                i = kc % 2
                B.op("act", lambda e, kc=kc, i=i: e.activation(out=sq[i], in_=xs(kc, tt), func=AF.Square),
                     r=[t_x[tt][kc]], w=[t_sq[i]])
                B.op("pe", lambda e, kc=kc, i=i: e.matmul(pss[:, :], ones, sq[i], start=(kc == 0), stop=(kc == KC - 1)),
                     r=[t_sq[i], t_c], w=[tss])
            rstd, t_rstd = NORM["rstd"], NORM["t_rstd"]
            B.op("act", lambda e: e.activation(out=rstd, in_=pss[:, :], func=AF.Ln, scale=1.0 / D, bias=eps_n),
                 r=[tss, t_c], w=[t_rstd])
            B.op("act", lambda e: e.activation(out=rstd, in_=rstd, func=AF.Exp, scale=-0.5), r=[t_rstd], w=[t_rstd])
            for kc in range(KC):
                B.op("dve", lambda e, kc=kc: e.scalar_tensor_tensor(
                    out=hs(kc, 1, 513), in0=xs(kc, tt), scalar=pvc(gname, kc), in1=rstd,
                    op0=ALU.mult, op1=ALU.mult), r=[t_x[tt][kc], t_rstd, t_pv], w=[t_hT[kc]])

        def post_norm_residual(tt, gname, pss, tss):
            rstd, t_rstd = NORM["rstd"], NORM["t_rstd"]
            B.op("act", lambda e: e.activation(out=rstd, in_=pss[:, :], func=AF.Ln, scale=1.0 / D, bias=eps_n),
                 r=[tss, t_c], w=[t_rstd])
            B.op("act", lambda e: e.activation(out=rstd, in_=rstd, func=AF.Exp, scale=-0.5), r=[t_rstd], w=[t_rstd])
            for dg in range(KC):
                f = fo[:, dg * TT:(dg + 1) * TT]
                B.op("dve", lambda e, f=f, dg=dg: e.scalar_tensor_tensor(
                    out=f, in0=f, scalar=pvc(gname, dg), in1=rstd, op0=ALU.mult, op1=ALU.mult),
                    r=[t_rstd, t_pv], w=[t_fo[dg]])
                B.op("dve", lambda e, f=f, dg=dg: e.tensor_tensor(out=xs(dg, tt), in0=xs(dg, tt), in1=f, op=ALU.add),
                     r=[t_fo[dg]], w=[t_x[tt][dg]])

        NORM = {}

        for l in range(L):
            B.barrier()
            ar = Arena(arena_t[:, :], ARENA_BYTES)
            NORM["sq"] = [ar.alloc(TT, BF16), ar.alloc(TT, BF16)]
            NORM["t_sq"] = [Tok(), Tok()]
            NORM["rstd"] = ar.alloc(TT, F32)
            NORM["t_rstd"] = Tok()
            base_off = ar.off
            B.dma("sp", pv[:, :], pv_d[l], w=[t_pv])
            B.dma("pool", wlr[:, :], wlr_d[l], w=[t_wlr])
            B.dma("pool", w2a2[:, :], w2a2_d[l], w=[t_w2])
            B.dma("pool", g2[:, :], g2_d[l], w=[t_g2])
            for nm, src, n in [("omuw", "muw", 8), ("omua", "mua", 8), ("omug", "mug", 8), ("omurkv", "murkv", 12),
                               ("omka", "ka", 4)]:
                B.op("dve", lambda e, nm=nm, src=src, n=n: e.tensor_scalar(
                    out=pd[:, PD[nm]:PD[nm] + n], in0=pv[:, PV[src]:PV[src] + n], scalar1=-1.0, scalar2=1.0,
                    op0=ALU.mult, op1=ALU.add), r=[t_pv], w=[t_pd])
            for kc in range(KC):
                for (c0, c1, mn, on) in [(0, 64, "muw", "omuw"), (64, 128, "mua", "omua"), (128, 256, "mug", "omug")]:
                    src = wlr[:, kc * 256 + c0: kc * 256 + c1]
                    B.op("dve", lambda e, src=src, kc=kc, c0=c0, c1=c1, on=on: e.tensor_scalar(
                        out=wlra[:, kc * 256 + c0: kc * 256 + c1], in0=src, scalar1=pdc(on, kc), scalar2=None,
                        op0=ALU.mult), r=[t_wlr, t_pd], w=[t_wlrab])
                    B.op("dve", lambda e, src=src, kc=kc, c0=c0, c1=c1, mn=mn: e.tensor_scalar(
                        out=wlrb[:, kc * 256 + c0: kc * 256 + c1], in0=src, scalar1=pvc(mn, kc), scalar2=None,
                        op0=ALU.mult), r=[t_wlr, t_pv], w=[t_wlrab])
            B.op("dve", lambda e: e.memset(rkvcarry[:, :], 0.0), w=[t_carry])
            B.op("dve", lambda e: e.memset(cu[:, :], 0.0), w=t_cu)
            B.op("dve", lambda e: e.memset(cfu[:, :], 0.0), w=t_cfu)
            B.op("dve", lambda e: e.memset(ST32[:, :], 0.0), w=t_ST)
            B.op("dve", lambda e: e.memset(STb[:, :], 0.0), w=t_ST)
            B.op("dve", lambda e: e.memset(hT[:, :], 0.0), w=t_hT)

            for tt in range(NT):
                B.barrier()
                ar.off = base_off
                if tt > 0:
                    for kc in range(KC):
                        B.op("dve", lambda e, kc=kc: e.tensor_copy(hs(kc, 0, 1), hcarry[:, kc:kc + 1]), r=[t_hc[kc]], w=[t_hT[kc]])
                rmsnorm_to_hT(tt, "premix")
                for kc in range(KC):
                    B.op("dve", lambda e, kc=kc: e.tensor_copy(hcarry[:, kc:kc + 1], hs(kc, 512, 513)), r=[t_hT[kc]], w=[t_hc[kc]])

                ar2 = Arena(fo[:, :], KC * TT * 4)

                def A_(cols, dtype):
                    sz = (cols * (4 if dtype == F32 else 2) + 3) // 4 * 4
                    if ar.off + sz <= ar.n:
                        return ar.alloc(cols, dtype)
                    return ar2.alloc(cols, dtype)
                wins = [A_(KC * 128, BF16) for _ in range(2)]
                t_wins = [Tok() for _ in range(2)]
                raw = [A_(513, F32)]
                t_raw = [Tok()]
                f32t = lambda: A_(TT, F32)
                bft = lambda: A_(TT, BF16)
                r_t, k_t, v_t, sgw, av = f32t(), f32t(), f32t(), f32t(), f32t()
                twa, tg = bft(), bft()
                tA, tB, tC, tD, tE = f32t(), f32t(), f32t(), f32t(), f32t()
                rkb = bft()
                scr2 = tg
                SETS = []
                for _i in range(2):
                    S = dict(bt=bft(), kt=bft(), Bh=bft(), Kh=bft(), vb=bft())
                    S["tk"] = {n: Tok() for n in ["bt", "kt", "Bh", "Kh", "vb"]}
                    SETS.append(S)
                Rm = [bft(), bft()]
                Lm = [bft(), bft()]
                Gm = [bft(), bft()]
                CH = []
                for _i in range(4):
                    S = dict(art=A_(8 * 128, BF16), bonus=bft(), gg=bft(), PC=A_(8, F32), TT=bft(),
                             LakT=bft(), LrbT=bft(), LrkT=bft(), VT=bft(), BhT=bft(), KhT=bft(),
                             Xb=A_(64, BF16), Ub=A_(64, BF16))
                    S["tk"] = {n: Tok() for n in ["art", "bonus", "gg", "PC", "TT", "LakT", "LrbT", "LrkT", "VT", "BhT", "KhT", "Xb", "Ub"]}
                    CH.append(S)
                gA, gB, gC = tE, r_t, k_t
                tk = {n: Tok() for n in ["r", "k", "v", "sgw", "av", "twa", "tg", "A", "B", "C", "D", "E", "rkb",
                                         "R0", "R1", "L0", "L1", "G0", "G1"]}
                tk["scr2"] = tk["tg"]
                tk["gA"], tk["gB"], tk["gC"] = tk["E"], tk["r"], tk["k"]
                XB = (0, 1, 2)
                YB = (3, 4, 5, 6, 7)
                eh = [slice(0, 64), slice(64, 128)]
                v3 = lambda a: a.rearrange("p (c x) -> p c x", c=8)
                mk3 = lambda m, n: m[:, 0:n * 64].rearrange("p (c x) -> p c x", c=n)

                def proj_group(g, consume):
                    si = rr("win", 2)
                    B.dma("pool", wins[si], win_d[l, g], w=[t_wins[si]])
                    ps, tps = getps(XB)
                    for kc in range(KC):
                        B.op("pe", lambda e, kc=kc, ps=ps, si=si: e.matmul(
                            ps[:, :], wins[si][:, kc * 128:(kc + 1) * 128], hs(kc, 1, 513),
                            start=(kc == 0), stop=(kc == KC - 1)), r=[t_wins[si], t_hT[kc]], w=[tps])
                    consume(ps, tps)

                def lowrank(c0, c1, consume):
                    ps, tps = getps(XB)
                    n = 0
                    for kc in range(KC):
                        for (wsb, a, b) in [(wlra, 1, 513), (wlrb, 0, 512)]:
                            B.op("pe", lambda e, kc=kc, wsb=wsb, a=a, b=b, n=n, ps=ps: e.matmul(
                                ps[:, :], wsb[:, kc * 256 + c0: kc * 256 + c1], hs(kc, a, b),
                                start=(n == 0), stop=(n == 2 * KC - 1)), r=[t_wlrab, t_hT[kc]], w=[tps])
                            n += 1
                    consume(ps, tps)

                def x_lowrank():
                    def lr0(ps, tps):
                        B.op("act", lambda e: e.activation(out=twa[0:64, :], in_=ps[0:64, :], func=AF.Tanh),
                             r=[tps], w=[tk["twa"]])
                        B.op("act", lambda e: e.activation(out=twa[64:128, :], in_=ps[64:128, :], func=AF.Copy), r=[tps], w=[tk["twa"]])
                    lowrank(0, 128, lr0)
                    yield

                    def lr1(ps, tps):
                        B.op("act", lambda e: e.activation(out=tg, in_=ps[:, :], func=AF.Sigmoid), r=[tps], w=[tk["tg"]])
                    lowrank(128, 256, lr1)
                    yield

                def x_sc(gi):
                    cuv = lambda a, b: cu[:, gi * 514 + a: gi * 514 + b]
                    csb = tA
                    acc = tB

                    def c_cons(ps, tps):
                        B.op("act", lambda e: e.activation(out=csb, in_=ps[:, :], func=AF.Copy), r=[tps], w=[tk["A"]])
                    proj_group(14 + gi, c_cons)
                    yield

                    def u_cons(ps, tps):
                        B.op("dve", lambda e: e.tensor_tensor(out=cuv(2, 514), in0=ps[:, :], in1=csb, op=ALU.mult),
                             r=[tps, tk["A"]], w=[t_cu[gi]])
                    proj_group(16 + gi, u_cons)
                    yield
                    B.op("dve", lambda e: e.tensor_scalar(
                        out=acc, in0=cuv(0, 512), scalar1=pvc("scw", 0 * 2 + gi), scalar2=None, op0=ALU.mult),
                        r=[t_cu[gi], t_pv], w=[tk["B"]])
                    for kk_ in (1, 2):
                        B.op("dve", lambda e, kk_=kk_: e.scalar_tensor_tensor(
                            out=acc, in0=cuv(kk_, kk_ + 512), scalar=pvc("scw", kk_ * 2 + gi), in1=acc,
                            op0=ALU.mult, op1=ALU.add), r=[t_cu[gi], t_pv], w=[tk["B"]])
                    B.op("dve", lambda e: e.tensor_copy(cuv(0, 2), cuv(512, 514)), w=[t_cu[gi]])
                    yield

                    def b_cons(ps, tps):
                        B.op("dve", lambda e: e.tensor_tensor(
                            out=mixcat[:, (4 + gi) * TT:(5 + gi) * TT], in0=ps[:, :], in1=acc, op=ALU.mult),
                            r=[tps, tk["B"]], w=[t_mix[4 + gi]])
                    proj_group(12 + gi, b_cons)
                    yield

                cres = [tC, tD]
                tcres = [tk["C"], tk["D"]]

                def x_cfall():
                    sg = tA
                    fvs = []
                    for gi in range(2):
                        fv = lambda a_, b_, gi=gi: cfu[:, gi * 542 + a_: gi * 542 + b_]
                        fvs.append(fv)

                        def g_cons(ps, tps):
                            B.op("act", lambda e: e.activation(out=sg, in_=ps[:, :], func=AF.Sigmoid), r=[tps], w=[tk["A"]])
                        proj_group(20 + gi, g_cons)
                        yield

                        def v_cons(ps, tps, gi=gi, fv=fv):
                            B.op("dve", lambda e: e.tensor_tensor(out=fv(30, 542), in0=ps[:, :], in1=sg, op=ALU.mult),
                                 r=[tps, tk["A"]], w=[t_cfu[gi]])
                        proj_group(18 + gi, v_cons)
                        yield
                    for gi in range(2):
                        B.op("dve", lambda e, gi=gi: e.tensor_scalar(
                            out=cres[gi], in0=fvs[gi](0, 512), scalar1=pvc("cfw", gi), scalar2=pvc("cfb", gi),
                            op0=ALU.mult, op1=ALU.add), r=[t_cfu[gi], t_pv], w=[tcres[gi]])
                    for kk_ in range(1, 31):
                        for gi in range(2):
                            B.op("dve", lambda e, kk_=kk_, gi=gi: e.scalar_tensor_tensor(
                                out=cres[gi], in0=fvs[gi](kk_, kk_ + 512), scalar=pvc("cfw", kk_ * 2 + gi), in1=cres[gi],
                                op0=ALU.mult, op1=ALU.add), r=[t_cfu[gi], t_pv], w=[tcres[gi]])
                        yield
                    for gi in range(2):
                        B.op("dve", lambda e, gi=gi: e.tensor_copy(fvs[gi](0, 30), fvs[gi](512, 542)), w=[t_cfu[gi]])
                    yield

                def x_cfln():
                    psm, tpsm = getps(XB)
                    psq, tpsq = getps(XB)
                    for gi in range(2):
                        B.op("act", lambda e, gi=gi: e.activation(out=rkb, in_=cres[gi], func=AF.Copy),
                             r=[tcres[gi]], w=[tk["rkb"]])
                        B.op("pe", lambda e, gi=gi: e.matmul(psm[:, :], ones, rkb, start=(gi == 0), stop=(gi == 1)),
                             r=[tk["rkb"], t_c], w=[tpsm])
                        B.op("act", lambda e, gi=gi: e.activation(out=scr2, in_=cres[gi], func=AF.Square),
                             r=[tcres[gi]], w=[tk["scr2"]])
                        B.op("pe", lambda e, gi=gi: e.matmul(psq[:, :], ones, scr2, start=(gi == 0), stop=(gi == 1)),
                             r=[tk["scr2"], t_c], w=[tpsq])
                        yield
                    B.op("dve", lambda e: e.tensor_scalar(out=tA, in0=psm[:, :], scalar1=1.0 / 256, scalar2=None, op0=ALU.mult),
                         r=[tpsm], w=[tk["A"]])
                    B.op("dve", lambda e: e.tensor_tensor(out=tB, in0=tA, in1=tA, op=ALU.mult), r=[tk["A"]], w=[tk["B"]])
                    B.op("dve", lambda e: e.scalar_tensor_tensor(out=tB, in0=psq[:, :], scalar=1.0 / 256, in1=tB,
                                                                 op0=ALU.mult, op1=ALU.subtract),
                         r=[tpsq], w=[tk["B"]])
                    yield
                    B.op("act", lambda e: e.activation(out=tB, in_=tB, func=AF.Ln, bias=eps_ln), r=[t_c], w=[tk["B"]])
                    B.op("act", lambda e: e.activation(out=tB, in_=tB, func=AF.Exp, scale=-0.5), w=[tk["B"]])
                    yield
                    for gi in range(2):
                        B.op("dve", lambda e, gi=gi: e.tensor_tensor(out=cres[gi], in0=cres[gi], in1=tA, op=ALU.subtract),
                             r=[tk["A"]], w=[tcres[gi]])
                        B.op("dve", lambda e, gi=gi: e.tensor_tensor(out=cres[gi], in0=cres[gi], in1=tB, op=ALU.mult),
                             r=[tk["B"]], w=[tcres[gi]])
                        B.op("act", lambda e, gi=gi: e.activation(
                            out=mixcat[:, (6 + gi) * TT:(7 + gi) * TT], in_=cres[gi], func=AF.Silu,
                            scale=pvc("cflg", gi), bias=pvc("cflb", gi)), r=[tcres[gi], t_pv], w=[t_mix[6 + gi]])
                        yield

                def x_prep(hp):
                    S = {**SETS[hp % 2], **CH[hp]}
                    stk = {**SETS[hp % 2]["tk"], **CH[hp]["tk"]}
                    art, bt_, kt_, Bh, Kh, vb, bonus, gg, PCt = (S[n] for n in ["art", "bt", "kt", "Bh", "Kh", "vb", "bonus", "gg", "PC"])
                    art3 = art.rearrange("p (c x) -> p c x", c=8)
                    ps, tps = getps(XB)
                    B.op("pe", lambda e: e.matmul(ps[:, :], w2a2[0:64, hp * 128:(hp + 1) * 128], twa[0:64, :],
                                                  start=True, stop=True), r=[t_w2, tk["twa"]], w=[tps])
                    B.op("act", lambda e: e.activation(out=sgw, in_=ps[:, :], func=AF.Sigmoid, bias=pvc("w0", hp)),
                         r=[tps, t_pv], w=[tk["sgw"]])
                    ps2, tps2 = getps(XB)
                    B.op("pe", lambda e: e.matmul(ps2[:, :], w2a2[64:128, hp * 128:(hp + 1) * 128], twa[64:128, :],
                                                  start=True, stop=True), r=[t_w2, tk["twa"]], w=[tps2])
                    B.op("act", lambda e: e.activation(out=av, in_=ps2[:, :], func=AF.Sigmoid, bias=pvc("a0", hp)),
                         r=[tps2, t_pv], w=[tk["av"]])
                    ps3, tps3 = getps(XB)
                    B.op("pe", lambda e: e.matmul(ps3[:, :], g2[:, hp * 128:(hp + 1) * 128], tg,
                                                  start=True, stop=True), r=[t_g2, tk["tg"]], w=[tps3])
                    B.op("act", lambda e: e.activation(out=gg, in_=ps3[:, :], func=AF.Copy), r=[tps3], w=[stk["gg"]])
                    yield
                    for (which, dst, tkn) in [(0, r_t, "r"), (1, k_t, "k"), (2, v_t, "v")]:
                        g = which * 4 + hp

                        def rkv_cons(ps, tps, g=g, dst=dst, tkn=tkn):
                            ri = 0
                            rw = raw[ri]
                            B.op("dve", lambda e: e.tensor_copy(rw[:, 0:1], rkvcarry[:, g:g + 1]),
                                 r=[t_carry], w=[t_raw[ri]])
                            B.op("act", lambda e: e.activation(out=rw[:, 1:513], in_=ps[:, :], func=AF.Copy),
                                 r=[tps], w=[t_raw[ri]])
                            B.op("dve", lambda e: e.tensor_copy(rkvcarry[:, g:g + 1], rw[:, 512:513]),
                                 r=[t_raw[ri]], w=[t_carry])
                            B.op("act", lambda e: e.activation(out=dst, in_=rw[:, 0:512], func=AF.Identity, scale=pvc("murkv", g)),
                                 r=[t_raw[ri], t_pv], w=[tk[tkn]])
                            B.op("dve", lambda e: e.scalar_tensor_tensor(
                                out=dst, in0=rw[:, 1:513], scalar=pdc("omurkv", g), in1=dst, op0=ALU.mult, op1=ALU.add),
                                r=[t_raw[ri], t_pd], w=[tk[tkn]])
                        proj_group(g, rkv_cons)
                        yield
                    Pb, Pexb, Pinvb, PCrb = S["VT"], S["BhT"], S["KhT"], S["TT"]
                    tPb, tPexb, tPinvb, tPCrb = stk["VT"], stk["BhT"], stk["KhT"], stk["TT"]
                    rn = raw[0][:, 0:512]
                    B.op("dve", lambda e: e.tensor_tensor_scan(tA, scanm, sgw, 0.0, ALU.mult, ALU.add),
                         r=[tk["sgw"], t_c], w=[tk["A"]])
                    B.op("act", lambda e: e.activation(out=tC, in_=k_t, func=AF.Identity, scale=pvc("kk", hp)),
                         r=[tk["k"], t_pv], w=[tk["C"]])
                    B.op("act", lambda e: e.activation(out=rkb, in_=tC, func=AF.Square), r=[tk["C"]], w=[tk["rkb"]])
                    ps4, tps4 = getps(XB)
                    B.op("pe", lambda e: e.matmul(ps4[:, :], bones, rkb, start=True, stop=True),
                         r=[tk["rkb"], t_c], w=[tps4])
                    B.op("act", lambda e: e.activation(out=vb, in_=v_t, func=AF.Copy), r=[tk["v"]], w=[stk["vb"]])
                    yield
                    B.op("act", lambda e: e.activation(out=Pb, in_=tA, func=AF.Exp, scale=C0), r=[tk["A"]], w=[tPb])
                    B.op("act", lambda e: e.activation(out=Pinvb, in_=tA, func=AF.Exp, scale=-C0), r=[tk["A"]], w=[tPinvb])
                    B.op("act", lambda e: e.activation(out=PCt.rearrange("p (c x) -> p c x", x=1), in_=v3(tA)[:, :, 63:64],
                                                       func=AF.Exp, scale=C0), r=[tk["A"]], w=[stk["PC"]])
                    B.op("dve", lambda e: e.tensor_tensor(out=tD, in0=tA, in1=sgw, op=ALU.subtract),
                         r=[tk["A"], tk["sgw"]], w=[tk["D"]])
                    B.op("dve", lambda e: e.tensor_tensor(out=v3(tB), in0=v3(tA), in1=v3(tA)[:, :, 63:64].to_broadcast([P, 8, 64]),
                                                          op=ALU.subtract), r=[tk["A"]], w=[tk["B"]])
                    yield
                    B.op("act", lambda e: e.activation(out=Pexb, in_=tD, func=AF.Exp, scale=C0), r=[tk["D"]], w=[tPexb])
                    B.op("act", lambda e: e.activation(out=PCrb, in_=tB, func=AF.Exp, scale=-C0), r=[tk["B"]], w=[tPCrb])
                    B.op("act", lambda e: e.activation(out=rn, in_=ps4[:, :], func=AF.Ln, bias=smallc[:, 3:4]), r=[tps4, t_c], w=[t_raw[0]])
                    B.op("act", lambda e: e.activation(out=rn, in_=rn, func=AF.Exp, scale=-0.5), w=[t_raw[0]])
                    B.op("dve", lambda e: e.tensor_tensor(out=art3[:, :, 64:128], in0=v3(r_t), in1=v3(Pb), op=ALU.mult),
                         r=[tk["r"], tPb], w=[stk["art"]])
                    B.op("act", lambda e: e.activation(out=tE, in_=av, func=AF.Identity, scale=pvc("ka", hp), bias=pdc("omka", hp)),
                         r=[tk["av"], t_pv, t_pd], w=[tk["E"]])
                    yield
                    B.op("dve", lambda e: e.tensor_tensor(out=tE, in0=tE, in1=k_t, op=ALU.mult), r=[tk["k"]], w=[tk["E"]])
                    B.op("dve", lambda e: e.tensor_tensor(out=tC, in0=tC, in1=rn, op=ALU.mult), r=[t_raw[0]], w=[tk["C"]])
                    yield
                    B.op("dve", lambda e: e.scalar_tensor_tensor(out=art3[:, :, 0:64], in0=v3(tC), scalar=-1.0, in1=v3(Pexb),
                                                                 op0=ALU.mult, op1=ALU.mult),
                         r=[tk["C"], tPexb], w=[stk["art"]])
                    B.op("dve", lambda e: e.scalar_tensor_tensor(out=rkb, in0=r_t, scalar=pvc("rk", hp), in1=tE,
                                                                 op0=ALU.mult, op1=ALU.mult),
                         r=[tk["r"], tk["E"], t_pv], w=[tk["rkb"]])
                    ps5, tps5 = getps(XB)
                    B.op("pe", lambda e: e.matmul(ps5[:, :], bones, rkb, start=True, stop=True),
                         r=[tk["rkb"], t_c], w=[tps5])
                    yield
                    B.op("dve", lambda e: e.tensor_tensor(out=tC, in0=tC, in1=av, op=ALU.mult), r=[tk["av"]], w=[tk["C"]])
                    B.op("pool", lambda e: e.tensor_tensor(out=kt_, in0=tE, in1=Pinvb, op=ALU.mult),
                         r=[tk["E"], tPinvb], w=[stk["kt"]])
                    yield
                    B.op("pool", lambda e: e.tensor_tensor(out=Kh, in0=tE, in1=PCrb, op=ALU.mult),
                         r=[tk["E"], tPCrb], w=[stk["Kh"]])
                    B.op("pool", lambda e: e.tensor_tensor(out=bt_, in0=tC, in1=Pinvb, op=ALU.mult),
                         r=[tk["C"], tPinvb], w=[stk["bt"]])
                    yield
                    B.op("pool", lambda e: e.tensor_tensor(out=Bh, in0=tC, in1=PCrb, op=ALU.mult),
                         r=[tk["C"], tPCrb], w=[stk["Bh"]])
                    B.op("dve", lambda e: e.tensor_tensor(out=bonus, in0=ps5[:, :], in1=v_t, op=ALU.mult),
                         r=[tps5, tk["v"]], w=[stk["bonus"]])
                    yield

                def ysetup(hp):
                    S = {**SETS[hp % 2], **CH[hp]}
                    stk = {**SETS[hp % 2]["tk"], **CH[hp]["tk"]}
                    return S, stk

                def y2a(hp):
                    S, stk = ysetup(hp)
                    TK = lambda n: stk[n] if n in stk else tk[n]
                    art, bt_, kt_, Bh, Kh, vb = (S[n] for n in ["art", "bt", "kt", "Bh", "Kh", "vb"])
                    VT, BhT, KhT, LakT, LrbT, LrkT, TTf = (S[n] for n in ["VT", "BhT", "KhT", "LakT", "LrbT", "LrkT", "TT"])
                    for (src, sk, dst, dtk) in [(vb, "vb", VT, "VT"), (Bh, "Bh", BhT, "BhT"), (Kh, "Kh", KhT, "KhT")]:
                        ps, tps = getps(YB)
                        for c in range(8):
                            for e_ in range(2):
                                B.op("pe", lambda e, ps=ps, c=c, e_=e_, src=src: e.matmul(
                                    ps[eh[e_], c * 64:(c + 1) * 64], src[eh[e_], c * 64:(c + 1) * 64],
                                    ident[eh[e_], e_ * 64:(e_ + 1) * 64], start=True, stop=True,
                                    tile_position=(64 * e_, 64 * e_)), r=[stk[sk], t_c], w=[tps])
                        B.op("act", lambda e, ps=ps, dst=dst: e.activation(out=dst, in_=ps[:, :], func=AF.Copy),
                             r=[tps], w=[stk[dtk]])
                        yield
                    for (stat, sk, dA, dAk, dB, dBk) in [(bt_, "bt", Rm[0], "R0", LrbT, "LrbT"),
                                                        (kt_, "kt", LakT, "LakT", LrkT, "LrkT")]:
                        for half in range(2):
                            ps, tps = getps(YB)
                            for c4 in range(4):
                                c = half * 4 + c4
                                for e_ in range(2):
                                    B.op("pe", lambda e, ps=ps, c=c, c4=c4, e_=e_, stat=stat: e.matmul(
                                        ps[eh[e_], c4 * 128:(c4 + 1) * 128], stat[eh[e_], c * 64:(c + 1) * 64],
                                        art[eh[e_], c * 128:(c + 1) * 128], start=True, stop=True,
                                        tile_position=(64 * e_, 64 * e_)), r=[stk[sk], stk["art"]], w=[tps])
                            ps3 = ps[:, :].rearrange("p (c x) -> p c x", c=4)
                            B.op("dve", lambda e, ps3=ps3, dA=dA, half=half: e.tensor_tensor(
                                out=v3(dA)[:, half * 4:(half + 1) * 4, :], in0=ps3[:, :, 0:64], in1=mk3(msu, 4), op=ALU.mult),
                                r=[tps, t_c], w=[TK(dAk)])
                            B.op("dve", lambda e, ps3=ps3, dB=dB, half=half: e.tensor_tensor(
                                out=v3(dB)[:, half * 4:(half + 1) * 4, :], in0=ps3[:, :, 64:128], in1=mk3(miu, 4), op=ALU.mult),
                                r=[tps, t_c], w=[TK(dBk)])
                            yield
                    ps, tps = getps(YB)
                    for c in range(8):
                        for e_ in range(2):
                            B.op("pe", lambda e, ps=ps, c=c, e_=e_: e.matmul(
                                ps[eh[e_], c * 64:(c + 1) * 64], art[eh[e_], c * 128: c * 128 + 64],
                                bt_[eh[e_], c * 64:(c + 1) * 64], start=True, stop=True,
                                tile_position=(64 * e_, 64 * e_)), r=[stk["bt"], stk["art"]], w=[tps])
                    B.op("dve", lambda e, ps=ps: e.tensor_tensor(out=Lm[0], in0=ps[:, :], in1=msl, op=ALU.mult),
                         r=[tps, t_c], w=[tk["L0"]])
                    B.op("dve", lambda e: e.tensor_tensor(out=Gm[0], in0=Rm[0], in1=irep, op=ALU.add),
                         r=[tk["R0"], t_c], w=[tk["G0"]])
                    yield
                    cur = 0
                    for lev in range(1, 6):
                        nxt = 1 - cur
                        Rc, Lc, Gc = Rm[cur], Lm[cur], Gm[cur]
                        Rn, Ln, Gn = Rm[nxt], Lm[nxt], Gm[nxt]
                        tRc, tLc, tGc = tk["R%d" % cur], tk["L%d" % cur], tk["G%d" % cur]
                        tRn, tLn, tGn = tk["R%d" % nxt], tk["L%d" % nxt], tk["G%d" % nxt]
                        if lev == 5:
                            Gn, tGn = TTf, stk["TT"]
                        ps, tps = getps(YB)
                        for c in range(8):
                            for e_ in range(2):
                                sl = slice(c * 64, (c + 1) * 64)
                                B.op("pe", lambda e, ps=ps, sl=sl, e_=e_: e.matmul(
                                    ps[eh[e_], sl], Rc[eh[e_], sl], Lc[eh[e_], sl], start=True, stop=True,
                                    tile_position=(64 * e_, 64 * e_)), r=[tRc, tLc], w=[tps])
                        B.op("act", lambda e, ps=ps: e.activation(out=Ln, in_=ps[:, :], func=AF.Copy),
                             r=[tps], w=[tLn])
                        if lev < 5:
                            ps2, tps2 = getps(YB)
                            for c in range(8):
                                for e_ in range(2):
                                    sl = slice(c * 64, (c + 1) * 64)
                                    B.op("pe", lambda e, ps2=ps2, sl=sl, e_=e_: e.matmul(
                                        ps2[eh[e_], sl], Lc[eh[e_], sl], Rc[eh[e_], sl], start=True, stop=True,
                                        tile_position=(64 * e_, 64 * e_)), r=[tRc, tLc], w=[tps2])
                            B.op("dve", lambda e, ps2=ps2: e.tensor_copy(Rn, ps2[:, :]), r=[tps2], w=[tRn])
                        yield
                        ps3_, tps3 = getps(YB)
                        for c in range(8):
                            for e_ in range(2):
                                sl = slice(c * 64, (c + 1) * 64)
                                B.op("pe", lambda e, ps3_=ps3_, sl=sl, e_=e_: e.matmul(
                                    ps3_[eh[e_], sl], Ln[eh[e_], sl], Gc[eh[e_], sl], start=True, stop=True,
                                    tile_position=(64 * e_, 64 * e_)), r=[tLn, tGc], w=[tps3])
                        B.op("dve", lambda e, ps3_=ps3_: e.tensor_tensor(out=Gn, in0=ps3_[:, :], in1=Gc, op=ALU.add),
                             r=[tps3, tGc], w=[tGn])
                        yield
                        cur = nxt

                PSY = {}

                def y_chain(hp):
                    S, stk = ysetup(hp)
                    art, bonus, gg, PCt = (S[n] for n in ["art", "bonus", "gg", "PC"])
                    VT, BhT, KhT, LakT, LrbT, LrkT, TTm, Xb, Ub = (S[n] for n in ["VT", "BhT", "KhT", "LakT", "LrbT", "LrkT", "TT", "Xb", "Ub"])
                    tTT = stk["TT"]
                    YB = (0, 1, 2, 3)
                    psY, tpsY = getps((4 + hp,), hold=True)
                    PSY[hp] = (psY, tpsY)
                    Sb = STb[:, hp * 64:(hp + 1) * 64]
                    S32 = ST32[:, hp * 64:(hp + 1) * 64]
                    tS = t_ST[hp]
                    for c in range(8):
                        sl = slice(c * 64, (c + 1) * 64)
                        psX, tpsX = getps(YB)
                        for e_ in range(2):
                            B.op("pe", lambda e, e_=e_: e.matmul(
                                psX[eh[e_], 0:64], art[eh[e_], c * 128:c * 128 + 64], Sb[eh[e_], :], start=True, stop=False,
                                tile_position=(64 * e_, 64 * e_)), r=[stk["art"], tS], w=[tpsX])
                            B.op("pe", lambda e, e_=e_: e.matmul(
                                psX[eh[e_], 0:64], LakT[eh[e_], sl], VT[eh[e_], sl], start=False, stop=True,
                                tile_position=(64 * e_, 64 * e_)), r=[stk["LakT"], stk["VT"]], w=[tpsX])
                        B.op("act", lambda e: e.activation(out=Xb, in_=psX[:, 0:64], func=AF.Copy),
                             r=[tpsX], w=[stk["Xb"]])
                        yield
                        psU, tpsU = getps(YB)
                        for e_ in range(2):
                            B.op("pe", lambda e, e_=e_: e.matmul(
                                psU[eh[e_], 0:64], TTm[eh[e_], sl], Xb[eh[e_], :], start=True, stop=True,
                                tile_position=(64 * e_, 64 * e_)), r=[tTT, stk["Xb"]], w=[tpsU])
                        B.op("dve", lambda e: e.tensor_copy(Ub, psU[:, 0:64]), r=[tpsU], w=[stk["Ub"]])
                        yield
                        for e_ in range(2):
                            B.op("pe", lambda e, e_=e_: e.matmul(
                                psY[eh[e_], sl], Sb[eh[e_], :], art[eh[e_], c * 128 + 64:(c + 1) * 128], start=True, stop=False,
                                tile_position=(64 * e_, 64 * e_)), r=[tS, stk["art"]], w=[tpsY])
                            B.op("pe", lambda e, e_=e_: e.matmul(
                                psY[eh[e_], sl], Ub[eh[e_], :], LrbT[eh[e_], sl], start=False, stop=False,
                                tile_position=(64 * e_, 64 * e_)), r=[stk["Ub"], stk["LrbT"]], w=[tpsY])
                            B.op("pe", lambda e, e_=e_: e.matmul(
                                psY[eh[e_], sl], VT[eh[e_], sl], LrkT[eh[e_], sl], start=False, stop=True,
                                tile_position=(64 * e_, 64 * e_)), r=[stk["VT"], stk["LrkT"]], w=[tpsY])
                        psS, tpsS = getps(YB)
                        for e_ in range(2):
                            B.op("pe", lambda e, e_=e_: e.matmul(
                                psS[eh[e_], 0:64], BhT[eh[e_], sl], Ub[eh[e_], :], start=True, stop=False,
                                tile_position=(64 * e_, 64 * e_)), r=[stk["BhT"], stk["Ub"]], w=[tpsS])
                            B.op("pe", lambda e, e_=e_: e.matmul(
                                psS[eh[e_], 0:64], KhT[eh[e_], sl], VT[eh[e_], sl], start=False, stop=True,
                                tile_position=(64 * e_, 64 * e_)), r=[stk["KhT"], stk["VT"]], w=[tpsS])
                        B.op("dve", lambda e: e.scalar_tensor_tensor(
                            out=S32, in0=S32, scalar=PCt[:, c:c + 1], in1=psS[:, 0:64], op0=ALU.mult, op1=ALU.add),
                            r=[tpsS, stk["PC"]], w=[tS])
                        B.op("act", lambda e: e.activation(out=Sb, in_=S32, func=AF.Copy), w=[tS])
                        yield

                TAIL3 = [(r_t, "r", k_t, "k", v_t, "v"), (sgw, "sgw", av, "av", tE, "E")]

                def y_tail(hp):
                    S, stk = ysetup(hp)
                    bonus, gg, VT, BhT = (S[n] for n in ["bonus", "gg", "VT", "BhT"])
                    YB = (0, 1, 2, 3)
                    psY, tpsY = PSY[hp]
                    gA, kA, gB, kB, gC, kC = TAIL3[hp % 2]
                    tgA, tgB, tgC = tk[kA], tk[kB], tk[kC]
                    B.op("act", lambda e: e.activation(out=gA, in_=psY[:, :], func=AF.Copy), r=[tpsY], w=[tgA])
                    B.op("act", lambda e: e.activation(out=VT, in_=psY[:, :], func=AF.Copy), r=[tpsY], w=[stk["VT"]])
                    B.op("act", lambda e: e.activation(out=BhT, in_=psY[:, :], func=AF.Square), r=[tpsY], w=[stk["BhT"]])
                    release(psY)
                    yield
                    psm, tpsm = getps(YB)
                    psq, tpsq = getps(YB)
                    B.op("pe", lambda e: e.matmul(psm[:, :], bones, VT, start=True, stop=True),
                         r=[stk["VT"], t_c], w=[tpsm])
                    B.op("pe", lambda e: e.matmul(psq[:, :], bones, BhT, start=True, stop=True),
                         r=[stk["BhT"], t_c], w=[tpsq])
                    B.op("dve", lambda e: e.tensor_scalar(out=gB, in0=psm[:, :], scalar1=1.0 / 64, scalar2=None, op0=ALU.mult),
                         r=[tpsm], w=[tgB])
                    B.op("dve", lambda e: e.tensor_tensor(out=gC, in0=gB, in1=gB, op=ALU.mult), r=[tgB], w=[tgC])
                    B.op("dve", lambda e: e.scalar_tensor_tensor(out=gC, in0=psq[:, :], scalar=1.0 / 64, in1=gC,
                                                                 op0=ALU.mult, op1=ALU.subtract),
                         r=[tpsq], w=[tgC])
                    yield
                    B.op("act", lambda e: e.activation(out=gC, in_=gC, func=AF.Ln, bias=eps_gn), r=[t_c], w=[tgC])
                    B.op("act", lambda e: e.activation(out=gC, in_=gC, func=AF.Exp, scale=-0.5), w=[tgC])
                    B.op("dve", lambda e: e.tensor_tensor(out=gA, in0=gA, in1=gB, op=ALU.subtract), r=[tgB], w=[tgA])
                    yield
                    B.op("dve", lambda e: e.tensor_tensor(out=gA, in0=gA, in1=gC, op=ALU.mult), r=[tgC], w=[tgA])
                    B.op("act", lambda e: e.activation(out=gA, in_=gA, func=AF.Identity, scale=pvc("lng", hp), bias=pvc("lnb", hp)),
                         r=[t_pv], w=[tgA])
                    yield
                    B.op("dve", lambda e: e.tensor_tensor(out=gA, in0=gA, in1=bonus, op=ALU.add), r=[stk["bonus"]], w=[tgA])
                    B.op("dve", lambda e: e.tensor_tensor(out=mixcat[:, hp * TT:(hp + 1) * TT], in0=gA, in1=gg, op=ALU.mult),
                         r=[tgA, stk["gg"]], w=[t_mix[hp]])
                    yield

                def chain_gens(gs):
                    for g_ in gs:
                        yield from g_

                def interleave(gs):
                    gs = list(gs)
                    while gs:
                        for g_ in list(gs):
                            try:
                                next(g_)
                            except StopIteration:
                                gs.remove(g_)
                for _ in x_lowrank():
                    pass
                for _ in x_prep(0):
                    pass
                for hp in range(1, 4):
                    interleave([x_prep(hp), y2a(hp - 1)])
                interleave([chain_gens([x_sc(0), x_sc(1)]), y2a(3)])
                for dg in range(2):
                    B.dma("pool", wos[dg][:, :], wo_d[l, dg], w=[t_wos[dg]])
                XB = (0, 1, 2, 3)
                interleave([y_chain(0), y_chain(1), y_chain(2), y_chain(3), chain_gens([x_cfall(), x_cfln()])])
                interleave([y_tail(0), y_tail(1)])
                interleave([y_tail(2), y_tail(3)])
                B.barrier()

                if dbg and l == 0 and tt == 0:
                    B.dma("pool", dbg_d[:, :], mixcat[:, :], r=t_mix)

                ar.off = base_off
                wos4 = [wos[0][:, :], wos[1][:, :], ar.alloc(KC * 128, BF16), ar.alloc(KC * 128, BF16)]
                t_wos4 = [t_wos[0], t_wos[1], Tok(), Tok()]
                for dg in (2, 3):
                    B.dma("pool", wos4[dg], wo_d[l, dg], w=[t_wos4[dg]])
                pss, tss = getps(hold=True)
                for dg in range(KC):
                    si = dg % 4
                    if dg >= 4:
                        B.dma("pool", wos4[si], wo_d[l, dg], w=[t_wos4[si]])
                    ps, tps = getps()
                    for kc in range(KC):
                        B.op("pe", lambda e, ps=ps, kc=kc, si=si: e.matmul(
                            ps[:, :], wos4[si][:, kc * 128:(kc + 1) * 128], mixcat[:, kc * TT:(kc + 1) * TT],
                            start=(kc == 0), stop=(kc == KC - 1)), r=[t_wos4[si], t_mix[kc]], w=[tps])
                    B.op("act", lambda e, ps=ps, dg=dg: e.activation(out=fo[:, dg * TT:(dg + 1) * TT], in_=ps[:, :], func=AF.Copy),
                         r=[tps], w=[t_fo[dg]])
                    i = dg % 2
                    B.op("act", lambda e, ps=ps, i=i: e.activation(out=NORM["sq"][i], in_=ps[:, :], func=AF.Square),
                         r=[tps], w=[NORM["t_sq"][i]])
                    B.op("pe", lambda e, i=i, dg=dg, pss=pss: e.matmul(pss[:, :], ones, NORM["sq"][i], start=(dg == 0), stop=(dg == KC - 1)),
                         r=[NORM["t_sq"][i], t_c], w=[tss])
                release(pss)
                post_norm_residual(tt, "postmix", pss, tss)

                B.barrier()
                ar.off = base_off
                act_t = ar.alloc(NJ * TT, BF16)
                t_act = [Tok() for _ in range(NJ)]
                NGUS, NWDS = 5, 3
                gus = [ar.alloc(2 * KC * 128, BF16) for _ in range(NGUS)]
                t_gus = [Tok() for _ in range(NGUS)]
                t_guu = [Tok() for _ in range(NGUS)]
                wds = [ar.alloc(NJ * 128, BF16) for _ in range(NWDS)]
                t_wds = [Tok() for _ in range(NWDS)]
                sil = [ar.alloc(TT, F32), ar.alloc(TT, F32)]
                t_sil = [Tok(), Tok()]
                rmsnorm_to_hT(tt, "preffn")
                for j in range(NJ):
                    si = rr("gus", NGUS)
                    B.dma("pool", gus[si][:, 0:KC * 128], wg_d[l, j], w=[t_gus[si]])
                    B.dma("pool", gus[si][:, KC * 128:2 * KC * 128], wu_d[l, j], w=[t_guu[si]])
                    if j == NGUS - 1:
                        for dg in range(NWDS):
                            B.dma("pool", wds[dg], wd_d[l, dg], w=[t_wds[dg]])
                    psg, tpsg = getps()
                    psu, tpsu = getps()
                    for kc in range(KC):
                        B.op("pe", lambda e, kc=kc, si=si, psg=psg: e.matmul(
                            psg[:, :], gus[si][:, kc * 128:(kc + 1) * 128], hs(kc, 1, 513),
                            start=(kc == 0), stop=(kc == KC - 1)), r=[t_gus[si], t_hT[kc]], w=[tpsg])
                    for kc in range(KC):
                        B.op("pe", lambda e, kc=kc, si=si, psu=psu: e.matmul(
                            psu[:, :], gus[si][:, (KC + kc) * 128:(KC + kc + 1) * 128], hs(kc, 1, 513),
                            start=(kc == 0), stop=(kc == KC - 1)), r=[t_guu[si], t_hT[kc]], w=[tpsu])
                    i = j % 2
                    B.op("act", lambda e, i=i, psg=psg: e.activation(out=sil[i], in_=psg[:, :], func=AF.Silu),
                         r=[tpsg], w=[t_sil[i]])
                    B.op("dve", lambda e, i=i, j=j, psu=psu: e.tensor_tensor(
                        out=act_t[:, j * TT:(j + 1) * TT], in0=psu[:, :], in1=sil[i], op=ALU.mult),
                        r=[tpsu, t_sil[i]], w=[t_act[j]])
                pss, tss = getps(hold=True)
                for dg in range(KC):
                    si = dg % NWDS
                    if dg >= NWDS:
                        B.dma("pool", wds[si], wd_d[l, dg], w=[t_wds[si]])
                    ps, tps = getps()
                    for j in range(NJ):
                        B.op("pe", lambda e, ps=ps, j=j, si=si: e.matmul(
                            ps[:, :], wds[si][:, j * 128:(j + 1) * 128], act_t[:, j * TT:(j + 1) * TT],
                            start=(j == 0), stop=(j == NJ - 1)), r=[t_wds[si], t_act[j]], w=[tps])
                    B.op("act", lambda e, ps=ps, dg=dg: e.activation(out=fo[:, dg * TT:(dg + 1) * TT], in_=ps[:, :], func=AF.Copy),
                         r=[tps], w=[t_fo[dg]])
                    i = dg % 2
                    B.op("act", lambda e, ps=ps, i=i: e.activation(out=NORM["sq"][i], in_=ps[:, :], func=AF.Square),
                         r=[tps], w=[NORM["t_sq"][i]])
                    B.op("pe", lambda e, i=i, dg=dg, pss=pss: e.matmul(pss[:, :], ones, NORM["sq"][i], start=(dg == 0), stop=(dg == KC - 1)),
                         r=[NORM["t_sq"][i], t_c], w=[tss])
                release(pss)
                post_norm_residual(tt, "postffn", pss, tss)

        t_out = Tok()
        for tt in range(NT):
            for kc in range(KC):
                B.dma("sp", y_d[:, kc * T + tt * TT: kc * T + (tt + 1) * TT], xs(kc, tt), r=[t_x[tt][kc]], w=[Tok()])
        B.barrier()

        with nc.Block() as block:
            B.emit(block)
    return nc


def _consts():
    c = np.zeros((P, NCF), np.float32)
    p = np.arange(P)[:, None]
    c[:, CF["ident"]:CF["ident"] + 128] = (p == np.arange(128)[None, :])
    c[:, CF["ones"]:CF["ones"] + 128] = 1.0
    c[:, CF["bones"]:CF["bones"] + 128] = ((p // 64) == (np.arange(128)[None, :] // 64))
    col = (np.arange(512) % 64)[None, :]
    row = p % 64
    c[:, CF["msu"]:CF["msu"] + 512] = (row < col)
    c[:, CF["miu"]:CF["miu"] + 512] = (row <= col)
    c[:, CF["msl"]:CF["msl"] + 512] = (col < row)
    c[:, CF["irep"]:CF["irep"] + 512] = (row == col)
    c[:, CF["scan"]:CF["scan"] + 512] = (col != 0) * np.ones((P, 1))
    return c


def _colvec(v):
    return np.ascontiguousarray(np.asarray(v, np.float32).reshape(-1, P).T)


def _prep_inputs(inp, L):
    f = lambda a: np.asarray(a, np.float32)
    out = {}

    def grp_cols(w, ncol_groups):
        Lw, K, N = w.shape
        a = w.reshape(Lw, K // P, P, N // P, P)
        a = a.transpose(0, 3, 2, 1, 4)
        return np.ascontiguousarray(a.reshape(Lw, N // P, P, (K // P) * P))
    out["w_in"] = grp_cols(f(inp["w_in"])[:L], NJ)
    out["w_gate"] = grp_cols(f(inp["w_gate"])[:L], NJ)
    out["w_up"] = grp_cols(f(inp["w_up"])[:L], NJ)
    out["w_o"] = grp_cols(f(inp["w_o"])[:L], KC)
    out["w_down"] = grp_cols(f(inp["w_down"])[:L], KC)
    wl = np.concatenate([f(inp["w1"]), f(inp["a1"]), f(inp["g1"])], axis=2)[:L]
    wl = wl.reshape(L, KC, P, 256).transpose(0, 2, 1, 3).reshape(L, P, KC * 256)
    out["wlr"] = np.ascontiguousarray(wl)
    out["w2a2"] = np.ascontiguousarray(np.concatenate([f(inp["w2"]), f(inp["a2"])], axis=1)[:L])
    out["g2"] = np.ascontiguousarray(f(inp["g2"])[:L])
    pv = np.zeros((L, P, NPV), np.float32)
    for l in range(L):
        def put(name, arr):
            a = _colvec(arr)
            pv[l, :, PV[name]:PV[name] + a.shape[1]] = a
        put("premix", inp["pre_mix_g"][l]); put("postmix", inp["post_mix_g"][l])
        put("preffn", inp["pre_ffn_g"][l]); put("postffn", inp["post_ffn_g"][l])
        put("muw", inp["mu_wag"][l][0]); put("mua", inp["mu_wag"][l][1]); put("mug", inp["mu_wag"][l][2])
        put("murkv", inp["mu_rkv"][l])
        put("w0", inp["w0"][l]); put("a0", inp["a0"][l]); put("kk", inp["k_k"][l]); put("ka", inp["k_a"][l])
        put("rk", np.asarray(inp["r_k"][l]).reshape(-1)); put("lng", inp["lnx_g"][l]); put("lnb", inp["lnx_b"][l])
        scw = f(inp["sc_conv_w"][l])
        for k_ in range(3):
            pv[l, :, PV["scw"] + k_ * 2: PV["scw"] + k_ * 2 + 2] = _colvec(scw[k_])
        cfw = f(inp["cf_conv_w"][l])
        for k_ in range(31):
            pv[l, :, PV["cfw"] + k_ * 2: PV["cfw"] + k_ * 2 + 2] = _colvec(cfw[k_])
        put("cfb", inp["cf_conv_b"][l]); put("cflg", inp["cf_ln_g"][l]); put("cflb", inp["cf_ln_b"][l])
    out["pv"] = pv
    out["cf"] = _consts()
    return out


_CACHE = {}


def run(inputs, L=4, dbg=False):
    x = np.asarray(inputs["x"], np.float32)
    shared = _prep_inputs(inputs, L)
    key = (L, dbg)
    if key not in _CACHE:
        _CACHE[key] = build_program(L, dbg)
    nc = _CACHE[key]
    in_maps = []
    for b in range(NCORES):
        xt = np.ascontiguousarray(x[b].T.reshape(KC, P, T).transpose(1, 0, 2).reshape(P, KC * T))
        m = dict(shared)
        m["xT"] = xt
        in_maps.append(m)
    res = run_bass_kernel_spmd(nc, in_maps, core_ids=list(range(NCORES)))
    outs = []
    for b in range(NCORES):
        yt = np.asarray(res.results[b]["yT"]).reshape(P, KC, T).transpose(1, 0, 2).reshape(D, T)
        outs.append(yt.T)
    y = np.stack(outs, 0).astype(np.float32)
    if dbg:
        return y, [np.asarray(r["dbg"]) for r in res.results]
    return y


def kernel(**inputs):
    return run(inputs, L=4)
```

```python
import contextlib
import numpy as np
import concourse.bass as bass
import concourse.mybir as mybir
from concourse.bass_utils import run_bass_kernel_spmd

F32 = mybir.dt.float32
BF16 = mybir.dt.bfloat16
AF = mybir.ActivationFunctionType
ALU = mybir.AluOpType

P = 128
T = 2048
D = 1024
TT = 512
NT = T // TT
KC = 8
NJ = 22
NCORES = 8
C0 = -0.6065306597126334
SAME_ENGINE_SYNC = True

PV = {}
_o = 0
for _n, _c in [("premix", 8), ("postmix", 8), ("preffn", 8), ("postffn", 8), ("muw", 8), ("mua", 8),
               ("mug", 8), ("murkv", 12), ("w0", 4), ("a0", 4), ("kk", 4), ("ka", 4), ("rk", 4),
               ("lng", 4), ("lnb", 4), ("scw", 6), ("cfw", 62), ("cfb", 2), ("cflg", 2), ("cflb", 2)]:
    PV[_n] = _o
    _o += _c
NPV = _o
PD = {"omuw": 0, "omua": 8, "omug": 16, "omurkv": 24, "omka": 36}
NPD = 40
CF = {"ident": 0, "ones": 128, "bones": 256, "msu": 384, "miu": 896, "msl": 1408, "irep": 1920, "scan": 2432}
NCF = 2944


class Tok:
    __slots__ = ("w", "r")

    def __init__(self):
        self.w = None
        self.r = []


class _Rec:
    def __init__(self):
        self.calls = []

    def __getattr__(self, name):
        def f(*a, **k):
            self.calls.append((name, a, k))
        return f


class Builder:
    ENGS = ("pe", "act", "dve", "pool", "sp")

    def __init__(self, nc, es, ndma=48):
        self.nc = nc
        self.q = {e: [] for e in self.ENGS}
        self.cnt = {e: 0 for e in self.ENGS}
        self.seen = {e: {} for e in self.ENGS}
        self.sems = {}
        for e in ("pe", "act", "dve", "pool"):
            self.sems[e] = es.enter_context(nc.semaphore("s_" + e))
        self.ndma = ndma
        self.dma_uses = [0] * ndma
        self.dma_next = 0
        for k in range(ndma):
            self.sems["dma%d" % k] = es.enter_context(nc.semaphore("s_dma%d" % k))

    def _collect(self, r, w):
        deps = {}

        def add(d):
            if d is None:
                return
            k, v = d
            if deps.get(k, 0) < v:
                deps[k] = v
        for t in r:
            add(t.w)
        for t in w:
            add(t.w)
            for d in t.r:
                add(d)
        return deps

    def _waits(self, eng, deps):
        out = []
        seen = self.seen[eng]
        for k, v in deps.items():
            if k == eng and (eng == "pe" or not SAME_ENGINE_SYNC):
                continue
            if seen.get(k, 0) >= v:
                continue
            seen[k] = v
            out.append((k, v))
        return out

    def op(self, eng, fn, r=(), w=()):
        rec = _Rec()
        fn(rec)
        assert len(rec.calls) == 1
        call = rec.calls[0]
        fn = lambda e, call=call: getattr(e, call[0])(*call[1], **call[2])
        deps = self._collect(r, w)
        waits = self._waits(eng, deps)
        self.cnt[eng] += 1
        me = (eng, self.cnt[eng])
        self.q[eng].append((fn, waits, (eng, 1)))
        for t in r:
            t.r.append(me)
        for t in w:
            t.w = me
            t.r = []

    def dma(self, qe, out_ap, in_ap, r=(), w=()):
        deps = self._collect(r, w)
        k = self.dma_next
        self.dma_next = (k + 1) % self.ndma
        key = "dma%d" % k
        if self.dma_uses[k] > 0:
            v = 16 * self.dma_uses[k]
            if deps.get(key, 0) < v:
                deps[key] = v
        waits = self._waits(qe, deps)
        self.dma_uses[k] += 1
        me = (key, 16 * self.dma_uses[k])
        self.q[qe].append((lambda e: e.dma_start(out=out_ap, in_=in_ap), waits, (key, 16)))
        for t in r:
            t.r.append(me)
        for t in w:
            t.w = me
            t.r = []

    def wait_only(self, eng, toks):
        deps = self._collect(toks, ())
        waits = self._waits(eng, deps)
        self.q[eng].append((None, waits, None))

    def barrier(self):
        allv = {e: self.cnt[e] for e in ("pe", "act", "dve", "pool") if self.cnt[e] > 0}
        for k in range(self.ndma):
            if self.dma_uses[k] > 0:
                allv["dma%d" % k] = 16 * self.dma_uses[k]
        for e in self.ENGS:
            deps = {k: v for k, v in allv.items() if k != e}
            waits = self._waits(e, deps)
            if waits:
                self.q[e].append((None, waits, None))

    def emit(self, block):
        sems = self.sems

        def run(e, lst):
            for fn, waits, inc in lst:
                for k, v in waits:
                    e.wait_ge(sems[k], v)
                if fn is not None:
                    ins = fn(e)
                    if inc is not None:
                        ins.then_inc(sems[inc[0]], inc[1])

        @block.tensor
        def _(e):
            run(e, self.q["pe"])

        @block.scalar
        def _(e):
            run(e, self.q["act"])

        @block.vector
        def _(e):
            run(e, self.q["dve"])

        @block.gpsimd
        def _(e):
            run(e, self.q["pool"])

        @block.sync
        def _(e):
            run(e, self.q["sp"])


class Arena:
    def __init__(self, ap, nbytes):
        self.ap = ap
        self.n = nbytes
        self.off = 0

    def alloc(self, cols, dtype):
        sz = cols * (4 if dtype == F32 else 2)
        sz = (sz + 3) // 4 * 4
        assert self.off + sz <= self.n, ("arena overflow", self.off, sz, self.n)
        a = self.ap[:, self.off // 4:(self.off + sz) // 4]
        self.off += sz
        if dtype != F32:
            a = a.bitcast(dtype)
            a = a[:, 0:cols]
        return a


def build_program(L=4, dbg=False):
    nc = bass.Bass("TRN2", target_bir_lowering=False)
    dt_in = lambda n, s: nc.dram_tensor(n, s, F32, kind="ExternalInput").ap()
    x_d = dt_in("xT", [P, KC * T])
    win_d = dt_in("w_in", [L, NJ, P, KC * 128])
    wg_d = dt_in("w_gate", [L, NJ, P, KC * 128])
    wu_d = dt_in("w_up", [L, NJ, P, KC * 128])
    wo_d = dt_in("w_o", [L, KC, P, KC * 128])
    wd_d = dt_in("w_down", [L, KC, P, NJ * 128])
    wlr_d = dt_in("wlr", [L, P, KC * 256])
    w2a2_d = dt_in("w2a2", [L, P, 512])
    g2_d = dt_in("g2", [L, P, 512])
    pv_d = dt_in("pv", [L, P, NPV])
    cf_d = dt_in("cf", [P, NCF])
    y_d = nc.dram_tensor("yT", [P, KC * T], F32, kind="ExternalOutput").ap()
    if dbg:
        dbg_d = nc.dram_tensor("dbg", [P, KC * TT], F32, kind="ExternalOutput").ap()

    with contextlib.ExitStack() as es:
        def sb(name, cols, dtype):
            return es.enter_context(nc.sbuf_tensor(name, [P, cols], dtype))
        xT = sb("xT_sb", KC * T, F32)
        pv = sb("pv_sb", NPV, F32)
        pd = sb("pd_sb", NPD, F32)
        cb = sb("cb", NCF, BF16)
        cmask = cb[:, 384:NCF]
        smallc = sb("smallc", 4, F32)
        wlra = sb("wlra", KC * 256, BF16)
        wlrb = sb("wlrb", KC * 256, BF16)
        w2a2 = sb("w2a2_sb", 512, BF16)
        g2 = sb("g2_sb", 512, BF16)
        rkvcarry = sb("rkvcarry", 12, F32)
        cu = sb("cu", 2 * 514, F32)
        cfu = sb("cfu", 2 * 542, F32)
        ST32 = sb("ST32", 4 * 64, F32)
        STb = sb("STb", 4 * 64, BF16)
        hT = sb("hT", KC * 513, BF16)
        hcarry = sb("hcarry", KC, BF16)
        mixcat = sb("mixcat", KC * TT, BF16)
        fo = sb("fo", KC * TT, F32)
        wlr = fo[:, 0:KC * 128].bitcast(BF16)
        wos = [sb("wos%d" % i, KC * 128, BF16) for i in range(2)]
        ARENA_BYTES = 80 * 1024
        arena_t = sb("arena", ARENA_BYTES // 4, F32)
        psum = [es.enter_context(nc.psum_tensor("ps%d" % i, [P, 512], F32)) for i in range(8)]

        B = Builder(nc, es)
        pst = [Tok() for _ in range(8)]
        ps_rr = [0]

        held = set()

        ALLB = tuple(range(8))
        ps_ptr = {}

        def getps(banks=None, hold=False):
            key = banks or ALLB
            while True:
                p_ = ps_ptr.get(key, 0)
                ps_ptr[key] = p_ + 1
                i = key[p_ % len(key)]
                if i not in held:
                    break
            if hold:
                held.add(i)
            return psum[i], pst[i]

        def release(ps):
            for i in range(8):
                if psum[i] is ps:
                    held.discard(i)

        t_x = [[Tok() for _ in range(KC)] for _ in range(NT)]
        t_pv, t_pd, t_c, t_wlr, t_wlrab, t_w2, t_g2 = Tok(), Tok(), Tok(), Tok(), Tok(), Tok(), Tok()
        t_carry, t_cu, t_cfu = Tok(), [Tok(), Tok()], [Tok(), Tok()]
        t_ST = [Tok() for _ in range(4)]
        t_hT, t_mix, t_fo = [Tok() for _ in range(KC)], [Tok() for _ in range(KC)], [Tok() for _ in range(KC)]
        t_wos = [Tok(), Tok()]
        t_hc = [Tok() for _ in range(KC)]

        def pvc(name, i=0):
            o = PV[name] + i
            return pv[:, o:o + 1]

        def pdc(name, i=0):
            o = PD[name] + i
            return pd[:, o:o + 1]

        ident = cb[:, 0:128]
        ones = cb[:, 128:256]
        bones = cb[:, 256:384]
        msu = cmask[:, 0:512]
        miu = cmask[:, 512:1024]
        msl = cmask[:, 1024:1536]
        irep = cmask[:, 1536:2048]
        scanm = cmask[:, 2048:2560]
        eps_n = smallc[:, 0:1]
        eps_ln = smallc[:, 1:2]
        eps_gn = smallc[:, 2:3]

        def xs(kc, tt):
            return xT[:, kc * T + tt * TT: kc * T + (tt + 1) * TT]

        def hs(kc, a, b):
            return hT[:, kc * 513 + a: kc * 513 + b]

        for kc in range(KC):
            for tt in range(NT):
                B.dma("sp", xs(kc, tt), x_d[:, kc * T + tt * TT: kc * T + (tt + 1) * TT], w=[t_x[tt][kc]])
        B.dma("pool", cb[:, :], cf_d[:, :], w=[t_c])
        B.op("dve", lambda e: e.memset(smallc[:, 0:1], 1e-6), w=[t_c])
        B.op("dve", lambda e: e.memset(smallc[:, 1:2], 1e-5), w=[t_c])
        B.op("dve", lambda e: e.memset(smallc[:, 2:3], 64e-5), w=[t_c])
        B.op("dve", lambda e: e.memset(smallc[:, 3:4], 1e-24), w=[t_c])

        wslot_rr = {}

        def rr(name, n):
            i = wslot_rr.get(name, 0)
            wslot_rr[name] = (i + 1) % n
            return i

        def rmsnorm_to_hT(tt, gname):
            pss, tss = getps()
            sq, t_sq = NORM["sq"], NORM["t_sq"]
            for kc in range(KC):
                i = kc % 2
                B.op("act", lambda e, kc=kc, i=i: e.activation(out=sq[i], in_=xs(kc, tt), func=AF.Square),
                     r=[t_x[tt][kc]], w=[t_sq[i]])
                B.op("pe", lambda e, kc=kc, i=i: e.matmul(pss[:, :], ones, sq[i], start=(kc == 0), stop=(kc == KC - 1)),
                     r=[t_sq[i], t_c], w=[tss])
            rstd, t_rstd = NORM["rstd"], NORM["t_rstd"]
            B.op("act", lambda e: e.activation(out=rstd, in_=pss[:, :], func=AF.Ln, scale=1.0 / D, bias=eps_n),
                 r=[tss, t_c], w=[t_rstd])
            B.op("act", lambda e: e.activation(out=rstd, in_=rstd, func=AF.Exp, scale=-0.5), r=[t_rstd], w=[t_rstd])
            for kc in range(KC):
                B.op("dve", lambda e, kc=kc: e.scalar_tensor_tensor(
                    out=hs(kc, 1, 513), in0=xs(kc, tt), scalar=pvc(gname, kc), in1=rstd,
                    op0=ALU.mult, op1=ALU.mult), r=[t_x[tt][kc], t_rstd, t_pv], w=[t_hT[kc]])

        def post_norm_residual(tt, gname, pss, tss):
            rstd, t_rstd = NORM["rstd"], NORM["t_rstd"]
            B.op("act", lambda e: e.activation(out=rstd, in_=pss[:, :], func=AF.Ln, scale=1.0 / D, bias=eps_n),
                 r=[tss, t_c], w=[t_rstd])
            B.op("act", lambda e: e.activation(out=rstd, in_=rstd, func=AF.Exp, scale=-0.5), r=[t_rstd], w=[t_rstd])
            for dg in range(KC):
                f = fo[:, dg * TT:(dg + 1) * TT]
                B.op("dve", lambda e, f=f, dg=dg: e.scalar_tensor_tensor(
                    out=f, in0=f, scalar=pvc(gname, dg), in1=rstd, op0=ALU.mult, op1=ALU.mult),
                    r=[t_rstd, t_pv], w=[t_fo[dg]])
                B.op("pool", lambda e, f=f, dg=dg: e.tensor_tensor(out=xs(dg, tt), in0=xs(dg, tt), in1=f, op=ALU.add),
                     r=[t_fo[dg]], w=[t_x[tt][dg]])

        NORM = {}

        for l in range(L):
            B.barrier()
            ar = Arena(arena_t[:, :], ARENA_BYTES)
            NORM["sq"] = [ar.alloc(TT, BF16), ar.alloc(TT, BF16)]
            NORM["t_sq"] = [Tok(), Tok()]
            NORM["rstd"] = ar.alloc(TT, F32)
            NORM["t_rstd"] = Tok()
            base_off = ar.off
            B.dma("sp", pv[:, :], pv_d[l], w=[t_pv])
            B.dma("pool", wlr[:, :], wlr_d[l], w=[t_wlr])
            B.dma("pool", w2a2[:, :], w2a2_d[l], w=[t_w2])
            B.dma("pool", g2[:, :], g2_d[l], w=[t_g2])
            for nm, src, n in [("omuw", "muw", 8), ("omua", "mua", 8), ("omug", "mug", 8), ("omurkv", "murkv", 12),
                               ("omka", "ka", 4)]:
                B.op("dve", lambda e, nm=nm, src=src, n=n: e.tensor_scalar(
                    out=pd[:, PD[nm]:PD[nm] + n], in0=pv[:, PV[src]:PV[src] + n], scalar1=-1.0, scalar2=1.0,
                    op0=ALU.mult, op1=ALU.add), r=[t_pv], w=[t_pd])
            for kc in range(KC):
                for (c0, c1, mn, on) in [(0, 64, "muw", "omuw"), (64, 128, "mua", "omua"), (128, 256, "mug", "omug")]:
                    src = wlr[:, kc * 256 + c0: kc * 256 + c1]
                    B.op("dve", lambda e, src=src, kc=kc, c0=c0, c1=c1, on=on: e.tensor_scalar(
                        out=wlra[:, kc * 256 + c0: kc * 256 + c1], in0=src, scalar1=pdc(on, kc), scalar2=None,
                        op0=ALU.mult), r=[t_wlr, t_pd], w=[t_wlrab])
                    B.op("dve", lambda e, src=src, kc=kc, c0=c0, c1=c1, mn=mn: e.tensor_scalar(
                        out=wlrb[:, kc * 256 + c0: kc * 256 + c1], in0=src, scalar1=pvc(mn, kc), scalar2=None,
                        op0=ALU.mult), r=[t_wlr, t_pv], w=[t_wlrab])
            B.op("dve", lambda e: e.memset(rkvcarry[:, :], 0.0), w=[t_carry])
            B.op("dve", lambda e: e.memset(cu[:, :], 0.0), w=t_cu)
            B.op("dve", lambda e: e.memset(cfu[:, :], 0.0), w=t_cfu)
            B.op("dve", lambda e: e.memset(ST32[:, :], 0.0), w=t_ST)
            B.op("dve", lambda e: e.memset(STb[:, :], 0.0), w=t_ST)
            B.op("dve", lambda e: e.memset(hT[:, :], 0.0), w=t_hT)

            for tt in range(NT):
                B.barrier()
                ar.off = base_off
                if tt > 0:
                    for kc in range(KC):
                        B.op("dve", lambda e, kc=kc: e.tensor_copy(hs(kc, 0, 1), hcarry[:, kc:kc + 1]), r=[t_hc[kc]], w=[t_hT[kc]])
                rmsnorm_to_hT(tt, "premix")
                for kc in range(KC):
                    B.op("dve", lambda e, kc=kc: e.tensor_copy(hcarry[:, kc:kc + 1], hs(kc, 512, 513)), r=[t_hT[kc]], w=[t_hc[kc]])

                ar2 = Arena(fo[:, :], KC * TT * 4)

                def A_(cols, dtype):
                    sz = (cols * (4 if dtype == F32 else 2) + 3) // 4 * 4
                    if ar.off + sz <= ar.n:
                        return ar.alloc(cols, dtype)
                    return ar2.alloc(cols, dtype)
                wins = [A_(KC * 128, BF16) for _ in range(2)]
                t_wins = [Tok() for _ in range(2)]
                raw = [A_(513, F32)]
                t_raw = [Tok()]
                f32t = lambda: A_(TT, F32)
                bft = lambda: A_(TT, BF16)
                r_t, k_t, v_t, sgw, av = f32t(), f32t(), f32t(), f32t(), f32t()
                twa, tg = bft(), bft()
                tA, tB, tC, tD, tE = f32t(), f32t(), f32t(), f32t(), f32t()
                rkb = bft()
                scr2 = tg
                SETS = []
                for _i in range(2):
                    S = dict(bt=bft(), kt=bft(), Bh=bft(), Kh=bft(), vb=bft())
                    S["tk"] = {n: Tok() for n in ["bt", "kt", "Bh", "Kh", "vb"]}
                    SETS.append(S)
                Rm = [bft(), bft()]
                Lm = [bft(), bft()]
                Gm = [bft(), bft()]
                CH = []
                for _i in range(4):
                    S = dict(art=A_(8 * 128, BF16), bonus=bft(), gg=bft(), PC=A_(8, F32), TT=bft(),
                             LakT=bft(), LrbT=bft(), LrkT=bft(), VT=bft(), BhT=bft(), KhT=bft(),
                             Xb=A_(64, BF16), Ub=A_(64, BF16))
                    S["tk"] = {n: Tok() for n in ["art", "bonus", "gg", "PC", "TT", "LakT", "LrbT", "LrkT", "VT", "BhT", "KhT", "Xb", "Ub"]}
                    CH.append(S)
                gA, gB, gC = tE, r_t, k_t
                tk = {n: Tok() for n in ["r", "k", "v", "sgw", "av", "twa", "tg", "A", "B", "C", "D", "E", "rkb",
                                         "R0", "R1", "L0", "L1", "G0", "G1"]}
                tk["scr2"] = tk["tg"]
                tk["gA"], tk["gB"], tk["gC"] = tk["E"], tk["r"], tk["k"]
                XB = (0, 1, 2)
                YB = (3, 4, 5, 6, 7)
                eh = [slice(0, 64), slice(64, 128)]
                v3 = lambda a: a.rearrange("p (c x) -> p c x", c=8)
                mk3 = lambda m, n: m[:, 0:n * 64].rearrange("p (c x) -> p c x", c=n)

                def proj_group(g, consume):
                    si = rr("win", 2)
                    B.dma("pool", wins[si], win_d[l, g], w=[t_wins[si]])
                    ps, tps = getps(XB)
                    for kc in range(KC):
                        B.op("pe", lambda e, kc=kc, ps=ps, si=si: e.matmul(
                            ps[:, :], wins[si][:, kc * 128:(kc + 1) * 128], hs(kc, 1, 513),
                            start=(kc == 0), stop=(kc == KC - 1)), r=[t_wins[si], t_hT[kc]], w=[tps])
                    consume(ps, tps)

                def lowrank(c0, c1, consume):
                    ps, tps = getps(XB)
                    n = 0
                    for kc in range(KC):
                        for (wsb, a, b) in [(wlra, 1, 513), (wlrb, 0, 512)]:
                            B.op("pe", lambda e, kc=kc, wsb=wsb, a=a, b=b, n=n, ps=ps: e.matmul(
                                ps[:, :], wsb[:, kc * 256 + c0: kc * 256 + c1], hs(kc, a, b),
                                start=(n == 0), stop=(n == 2 * KC - 1)), r=[t_wlrab, t_hT[kc]], w=[tps])
                            n += 1
                    consume(ps, tps)

                def x_lowrank():
                    def lr0(ps, tps):
                        B.op("act", lambda e: e.activation(out=twa[0:64, :], in_=ps[0:64, :], func=AF.Tanh),
                             r=[tps], w=[tk["twa"]])
                        B.op("act", lambda e: e.activation(out=twa[64:128, :], in_=ps[64:128, :], func=AF.Copy), r=[tps], w=[tk["twa"]])
                    lowrank(0, 128, lr0)
                    yield

                    def lr1(ps, tps):
                        B.op("act", lambda e: e.activation(out=tg, in_=ps[:, :], func=AF.Sigmoid), r=[tps], w=[tk["tg"]])
                    lowrank(128, 256, lr1)
                    yield

                def x_sc(gi):
                    cuv = lambda a, b: cu[:, gi * 514 + a: gi * 514 + b]
                    csb = tA
                    acc = tB

                    def c_cons(ps, tps):
                        B.op("act", lambda e: e.activation(out=csb, in_=ps[:, :], func=AF.Copy), r=[tps], w=[tk["A"]])
                    proj_group(14 + gi, c_cons)
                    yield

                    def u_cons(ps, tps):
                        B.op("dve", lambda e: e.tensor_tensor(out=cuv(2, 514), in0=ps[:, :], in1=csb, op=ALU.mult),
                             r=[tps, tk["A"]], w=[t_cu[gi]])
                    proj_group(16 + gi, u_cons)
                    yield
                    B.op("dve", lambda e: e.tensor_scalar(
                        out=acc, in0=cuv(0, 512), scalar1=pvc("scw", 0 * 2 + gi), scalar2=None, op0=ALU.mult),
                        r=[t_cu[gi], t_pv], w=[tk["B"]])
                    for kk_ in (1, 2):
                        B.op("dve", lambda e, kk_=kk_: e.scalar_tensor_tensor(
                            out=acc, in0=cuv(kk_, kk_ + 512), scalar=pvc("scw", kk_ * 2 + gi), in1=acc,
                            op0=ALU.mult, op1=ALU.add), r=[t_cu[gi], t_pv], w=[tk["B"]])
                    B.op("dve", lambda e: e.tensor_copy(cuv(0, 2), cuv(512, 514)), w=[t_cu[gi]])
                    yield

                    def b_cons(ps, tps):
                        B.op("dve", lambda e: e.tensor_tensor(
                            out=mixcat[:, (4 + gi) * TT:(5 + gi) * TT], in0=ps[:, :], in1=acc, op=ALU.mult),
                            r=[tps, tk["B"]], w=[t_mix[4 + gi]])
                    proj_group(12 + gi, b_cons)
                    yield

                cres = [tC, tD]
                tcres = [tk["C"], tk["D"]]

                def x_cfall():
                    sg = tA
                    fvs = []
                    for gi in range(2):
                        fv = lambda a_, b_, gi=gi: cfu[:, gi * 542 + a_: gi * 542 + b_]
                        fvs.append(fv)

                        def g_cons(ps, tps):
                            B.op("act", lambda e: e.activation(out=sg, in_=ps[:, :], func=AF.Sigmoid), r=[tps], w=[tk["A"]])
                        proj_group(20 + gi, g_cons)
                        yield

                        def v_cons(ps, tps, gi=gi, fv=fv):
                            B.op("dve", lambda e: e.tensor_tensor(out=fv(30, 542), in0=ps[:, :], in1=sg, op=ALU.mult),
                                 r=[tps, tk["A"]], w=[t_cfu[gi]])
                        proj_group(18 + gi, v_cons)
                        yield
                    for gi in range(2):
                        B.op("dve", lambda e, gi=gi: e.tensor_scalar(
                            out=cres[gi], in0=fvs[gi](0, 512), scalar1=pvc("cfw", gi), scalar2=pvc("cfb", gi),
                            op0=ALU.mult, op1=ALU.add), r=[t_cfu[gi], t_pv], w=[tcres[gi]])
                    for kk_ in range(1, 31):
                        for gi in range(2):
                            B.op("dve", lambda e, kk_=kk_, gi=gi: e.scalar_tensor_tensor(
                                out=cres[gi], in0=fvs[gi](kk_, kk_ + 512), scalar=pvc("cfw", kk_ * 2 + gi), in1=cres[gi],
                                op0=ALU.mult, op1=ALU.add), r=[t_cfu[gi], t_pv], w=[tcres[gi]])
                        yield
                    for gi in range(2):
                        B.op("dve", lambda e, gi=gi: e.tensor_copy(fvs[gi](0, 30), fvs[gi](512, 542)), w=[t_cfu[gi]])
                    yield

                def x_cfln():
                    psm, tpsm = getps(XB)
                    psq, tpsq = getps(XB)
                    for gi in range(2):
                        B.op("act", lambda e, gi=gi: e.activation(out=rkb, in_=cres[gi], func=AF.Copy),
                             r=[tcres[gi]], w=[tk["rkb"]])
                        B.op("pe", lambda e, gi=gi: e.matmul(psm[:, :], ones, rkb, start=(gi == 0), stop=(gi == 1)),
                             r=[tk["rkb"], t_c], w=[tpsm])
                        B.op("act", lambda e, gi=gi: e.activation(out=scr2, in_=cres[gi], func=AF.Square),
                             r=[tcres[gi]], w=[tk["scr2"]])
                        B.op("pe", lambda e, gi=gi: e.matmul(psq[:, :], ones, scr2, start=(gi == 0), stop=(gi == 1)),
                             r=[tk["scr2"], t_c], w=[tpsq])
                        yield
                    B.op("dve", lambda e: e.tensor_scalar(out=tA, in0=psm[:, :], scalar1=1.0 / 256, scalar2=None, op0=ALU.mult),
                         r=[tpsm], w=[tk["A"]])
                    B.op("dve", lambda e: e.tensor_tensor(out=tB, in0=tA, in1=tA, op=ALU.mult), r=[tk["A"]], w=[tk["B"]])
                    B.op("dve", lambda e: e.scalar_tensor_tensor(out=tB, in0=psq[:, :], scalar=1.0 / 256, in1=tB,
                                                                 op0=ALU.mult, op1=ALU.subtract),
                         r=[tpsq], w=[tk["B"]])
                    yield
                    B.op("act", lambda e: e.activation(out=tB, in_=tB, func=AF.Ln, bias=eps_ln), r=[t_c], w=[tk["B"]])
                    B.op("act", lambda e: e.activation(out=tB, in_=tB, func=AF.Exp, scale=-0.5), w=[tk["B"]])
                    yield
                    for gi in range(2):
                        B.op("dve", lambda e, gi=gi: e.tensor_tensor(out=cres[gi], in0=cres[gi], in1=tA, op=ALU.subtract),
                             r=[tk["A"]], w=[tcres[gi]])
                        B.op("dve", lambda e, gi=gi: e.tensor_tensor(out=cres[gi], in0=cres[gi], in1=tB, op=ALU.mult),
                             r=[tk["B"]], w=[tcres[gi]])
                        B.op("act", lambda e, gi=gi: e.activation(
                            out=mixcat[:, (6 + gi) * TT:(7 + gi) * TT], in_=cres[gi], func=AF.Silu,
                            scale=pvc("cflg", gi), bias=pvc("cflb", gi)), r=[tcres[gi], t_pv], w=[t_mix[6 + gi]])
                        yield

                def x_prep(hp):
                    S = {**SETS[hp % 2], **CH[hp]}
                    stk = {**SETS[hp % 2]["tk"], **CH[hp]["tk"]}
                    art, bt_, kt_, Bh, Kh, vb, bonus, gg, PCt = (S[n] for n in ["art", "bt", "kt", "Bh", "Kh", "vb", "bonus", "gg", "PC"])
                    art3 = art.rearrange("p (c x) -> p c x", c=8)
                    ps, tps = getps(XB)
                    B.op("pe", lambda e: e.matmul(ps[:, :], w2a2[0:64, hp * 128:(hp + 1) * 128], twa[0:64, :],
                                                  start=True, stop=True), r=[t_w2, tk["twa"]], w=[tps])
                    B.op("act", lambda e: e.activation(out=sgw, in_=ps[:, :], func=AF.Sigmoid, bias=pvc("w0", hp)),
                         r=[tps, t_pv], w=[tk["sgw"]])
                    ps2, tps2 = getps(XB)
                    B.op("pe", lambda e: e.matmul(ps2[:, :], w2a2[64:128, hp * 128:(hp + 1) * 128], twa[64:128, :],
                                                  start=True, stop=True), r=[t_w2, tk["twa"]], w=[tps2])
                    B.op("act", lambda e: e.activation(out=av, in_=ps2[:, :], func=AF.Sigmoid, bias=pvc("a0", hp)),
                         r=[tps2, t_pv], w=[tk["av"]])
                    ps3, tps3 = getps(XB)
                    B.op("pe", lambda e: e.matmul(ps3[:, :], g2[:, hp * 128:(hp + 1) * 128], tg,
                                                  start=True, stop=True), r=[t_g2, tk["tg"]], w=[tps3])
                    B.op("act", lambda e: e.activation(out=gg, in_=ps3[:, :], func=AF.Copy), r=[tps3], w=[stk["gg"]])
                    yield
                    for (which, dst, tkn) in [(0, r_t, "r"), (1, k_t, "k"), (2, v_t, "v")]:
                        g = which * 4 + hp

                        def rkv_cons(ps, tps, g=g, dst=dst, tkn=tkn):
                            ri = 0
                            rw = raw[ri]
                            B.op("dve", lambda e: e.tensor_copy(rw[:, 0:1], rkvcarry[:, g:g + 1]),
                                 r=[t_carry], w=[t_raw[ri]])
                            B.op("act", lambda e: e.activation(out=rw[:, 1:513], in_=ps[:, :], func=AF.Copy),
                                 r=[tps], w=[t_raw[ri]])
                            B.op("dve", lambda e: e.tensor_copy(rkvcarry[:, g:g + 1], rw[:, 512:513]),
                                 r=[t_raw[ri]], w=[t_carry])
                            B.op("act", lambda e: e.activation(out=dst, in_=rw[:, 0:512], func=AF.Identity, scale=pvc("murkv", g)),
                                 r=[t_raw[ri], t_pv], w=[tk[tkn]])
                            B.op("dve", lambda e: e.scalar_tensor_tensor(
                                out=dst, in0=rw[:, 1:513], scalar=pdc("omurkv", g), in1=dst, op0=ALU.mult, op1=ALU.add),
                                r=[t_raw[ri], t_pd], w=[tk[tkn]])
                        proj_group(g, rkv_cons)
                        yield
                    Pb, Pexb, Pinvb, PCrb = S["VT"], S["BhT"], S["KhT"], S["TT"]
                    tPb, tPexb, tPinvb, tPCrb = stk["VT"], stk["BhT"], stk["KhT"], stk["TT"]
                    rn = raw[0][:, 0:512]
                    B.op("dve", lambda e: e.tensor_tensor_scan(tA, scanm, sgw, 0.0, ALU.mult, ALU.add),
                         r=[tk["sgw"], t_c], w=[tk["A"]])
                    B.op("act", lambda e: e.activation(out=tC, in_=k_t, func=AF.Identity, scale=pvc("kk", hp)),
                         r=[tk["k"], t_pv], w=[tk["C"]])
                    B.op("act", lambda e: e.activation(out=rkb, in_=tC, func=AF.Square), r=[tk["C"]], w=[tk["rkb"]])
                    ps4, tps4 = getps(XB)
                    B.op("pe", lambda e: e.matmul(ps4[:, :], bones, rkb, start=True, stop=True),
                         r=[tk["rkb"], t_c], w=[tps4])
                    B.op("act", lambda e: e.activation(out=vb, in_=v_t, func=AF.Copy), r=[tk["v"]], w=[stk["vb"]])
                    yield
                    B.op("act", lambda e: e.activation(out=Pb, in_=tA, func=AF.Exp, scale=C0), r=[tk["A"]], w=[tPb])
                    B.op("act", lambda e: e.activation(out=Pinvb, in_=tA, func=AF.Exp, scale=-C0), r=[tk["A"]], w=[tPinvb])
                    B.op("act", lambda e: e.activation(out=PCt.rearrange("p (c x) -> p c x", x=1), in_=v3(tA)[:, :, 63:64],
                                                       func=AF.Exp, scale=C0), r=[tk["A"]], w=[stk["PC"]])
                    B.op("dve", lambda e: e.tensor_tensor(out=tD, in0=tA, in1=sgw, op=ALU.subtract),
                         r=[tk["A"], tk["sgw"]], w=[tk["D"]])
                    B.op("dve", lambda e: e.tensor_tensor(out=v3(tB), in0=v3(tA), in1=v3(tA)[:, :, 63:64].to_broadcast([P, 8, 64]),
                                                          op=ALU.subtract), r=[tk["A"]], w=[tk["B"]])
                    yield
                    B.op("act", lambda e: e.activation(out=Pexb, in_=tD, func=AF.Exp, scale=C0), r=[tk["D"]], w=[tPexb])
                    B.op("act", lambda e: e.activation(out=PCrb, in_=tB, func=AF.Exp, scale=-C0), r=[tk["B"]], w=[tPCrb])
                    B.op("act", lambda e: e.activation(out=rn, in_=ps4[:, :], func=AF.Ln, bias=smallc[:, 3:4]), r=[tps4, t_c], w=[t_raw[0]])
                    B.op("act", lambda e: e.activation(out=rn, in_=rn, func=AF.Exp, scale=-0.5), w=[t_raw[0]])
                    B.op("dve", lambda e: e.tensor_tensor(out=art3[:, :, 64:128], in0=v3(r_t), in1=v3(Pb), op=ALU.mult),
                         r=[tk["r"], tPb], w=[stk["art"]])
                    B.op("act", lambda e: e.activation(out=tE, in_=av, func=AF.Identity, scale=pvc("ka", hp), bias=pdc("omka", hp)),
                         r=[tk["av"], t_pv, t_pd], w=[tk["E"]])
                    yield
                    B.op("dve", lambda e: e.tensor_tensor(out=tE, in0=tE, in1=k_t, op=ALU.mult), r=[tk["k"]], w=[tk["E"]])
                    B.op("dve", lambda e: e.tensor_tensor(out=tC, in0=tC, in1=rn, op=ALU.mult), r=[t_raw[0]], w=[tk["C"]])
                    yield
                    B.op("dve", lambda e: e.scalar_tensor_tensor(out=art3[:, :, 0:64], in0=v3(tC), scalar=-1.0, in1=v3(Pexb),
                                                                 op0=ALU.mult, op1=ALU.mult),
                         r=[tk["C"], tPexb], w=[stk["art"]])
                    B.op("dve", lambda e: e.scalar_tensor_tensor(out=rkb, in0=r_t, scalar=pvc("rk", hp), in1=tE,
                                                                 op0=ALU.mult, op1=ALU.mult),
                         r=[tk["r"], tk["E"], t_pv], w=[tk["rkb"]])
                    ps5, tps5 = getps(XB)
                    B.op("pe", lambda e: e.matmul(ps5[:, :], bones, rkb, start=True, stop=True),
                         r=[tk["rkb"], t_c], w=[tps5])
                    yield
                    B.op("dve", lambda e: e.tensor_tensor(out=tC, in0=tC, in1=av, op=ALU.mult), r=[tk["av"]], w=[tk["C"]])
                    B.op("pool", lambda e: e.tensor_tensor(out=kt_, in0=tE, in1=Pinvb, op=ALU.mult),
                         r=[tk["E"], tPinvb], w=[stk["kt"]])
                    yield
                    B.op("pool", lambda e: e.tensor_tensor(out=Kh, in0=tE, in1=PCrb, op=ALU.mult),
                         r=[tk["E"], tPCrb], w=[stk["Kh"]])
                    B.op("pool", lambda e: e.tensor_tensor(out=bt_, in0=tC, in1=Pinvb, op=ALU.mult),
                         r=[tk["C"], tPinvb], w=[stk["bt"]])
                    yield
                    B.op("pool", lambda e: e.tensor_tensor(out=Bh, in0=tC, in1=PCrb, op=ALU.mult),
                         r=[tk["C"], tPCrb], w=[stk["Bh"]])
                    B.op("dve", lambda e: e.tensor_tensor(out=bonus, in0=ps5[:, :], in1=v_t, op=ALU.mult),
                         r=[tps5, tk["v"]], w=[stk["bonus"]])
                    yield

                def ysetup(hp):
                    S = {**SETS[hp % 2], **CH[hp]}
                    stk = {**SETS[hp % 2]["tk"], **CH[hp]["tk"]}
                    return S, stk

                def y2a(hp):
                    S, stk = ysetup(hp)
                    TK = lambda n: stk[n] if n in stk else tk[n]
                    art, bt_, kt_, Bh, Kh, vb = (S[n] for n in ["art", "bt", "kt", "Bh", "Kh", "vb"])
                    VT, BhT, KhT, LakT, LrbT, LrkT, TTf = (S[n] for n in ["VT", "BhT", "KhT", "LakT", "LrbT", "LrkT", "TT"])
                    for (src, sk, dst, dtk) in [(vb, "vb", VT, "VT"), (Bh, "Bh", BhT, "BhT"), (Kh, "Kh", KhT, "KhT")]:
                        ps, tps = getps(YB)
                        for c in range(8):
                            for e_ in range(2):
                                B.op("pe", lambda e, ps=ps, c=c, e_=e_, src=src: e.matmul(
                                    ps[eh[e_], c * 64:(c + 1) * 64], src[eh[e_], c * 64:(c + 1) * 64],
                                    ident[eh[e_], e_ * 64:(e_ + 1) * 64], start=True, stop=True,
                                    tile_position=(64 * e_, 64 * e_)), r=[stk[sk], t_c], w=[tps])
                        B.op("act", lambda e, ps=ps, dst=dst: e.activation(out=dst, in_=ps[:, :], func=AF.Copy),
                             r=[tps], w=[stk[dtk]])
                        yield
                    for (stat, sk, dA, dAk, dB, dBk) in [(bt_, "bt", Rm[0], "R0", LrbT, "LrbT"),
                                                        (kt_, "kt", LakT, "LakT", LrkT, "LrkT")]:
                        for half in range(2):
                            ps, tps = getps(YB)
                            for c4 in range(4):
                                c = half * 4 + c4
                                for e_ in range(2):
                                    B.op("pe", lambda e, ps=ps, c=c, c4=c4, e_=e_, stat=stat: e.matmul(
                                        ps[eh[e_], c4 * 128:(c4 + 1) * 128], stat[eh[e_], c * 64:(c + 1) * 64],
                                        art[eh[e_], c * 128:(c + 1) * 128], start=True, stop=True,
                                        tile_position=(64 * e_, 64 * e_)), r=[stk[sk], stk["art"]], w=[tps])
                            ps3 = ps[:, :].rearrange("p (c x) -> p c x", c=4)
                            B.op("dve", lambda e, ps3=ps3, dA=dA, half=half: e.tensor_tensor(
                                out=v3(dA)[:, half * 4:(half + 1) * 4, :], in0=ps3[:, :, 0:64], in1=mk3(msu, 4), op=ALU.mult),
                                r=[tps, t_c], w=[TK(dAk)])
                            B.op("dve", lambda e, ps3=ps3, dB=dB, half=half: e.tensor_tensor(
                                out=v3(dB)[:, half * 4:(half + 1) * 4, :], in0=ps3[:, :, 64:128], in1=mk3(miu, 4), op=ALU.mult),
                                r=[tps, t_c], w=[TK(dBk)])
                            yield
                    ps, tps = getps(YB)
                    for c in range(8):
                        for e_ in range(2):
                            B.op("pe", lambda e, ps=ps, c=c, e_=e_: e.matmul(
                                ps[eh[e_], c * 64:(c + 1) * 64], art[eh[e_], c * 128: c * 128 + 64],
                                bt_[eh[e_], c * 64:(c + 1) * 64], start=True, stop=True,
                                tile_position=(64 * e_, 64 * e_)), r=[stk["bt"], stk["art"]], w=[tps])
                    B.op("dve", lambda e, ps=ps: e.tensor_tensor(out=Lm[0], in0=ps[:, :], in1=msl, op=ALU.mult),
                         r=[tps, t_c], w=[tk["L0"]])
                    B.op("dve", lambda e: e.tensor_tensor(out=Gm[0], in0=Rm[0], in1=irep, op=ALU.add),
                         r=[tk["R0"], t_c], w=[tk["G0"]])
                    yield
                    cur = 0
                    for lev in range(1, 6):
                        nxt = 1 - cur
                        Rc, Lc, Gc = Rm[cur], Lm[cur], Gm[cur]
                        Rn, Ln, Gn = Rm[nxt], Lm[nxt], Gm[nxt]
                        tRc, tLc, tGc = tk["R%d" % cur], tk["L%d" % cur], tk["G%d" % cur]
                        tRn, tLn, tGn = tk["R%d" % nxt], tk["L%d" % nxt], tk["G%d" % nxt]
                        if lev == 5:
                            Gn, tGn = TTf, stk["TT"]
                        ps, tps = getps(YB)
                        for c in range(8):
                            for e_ in range(2):
                                sl = slice(c * 64, (c + 1) * 64)
                                B.op("pe", lambda e, ps=ps, sl=sl, e_=e_: e.matmul(
                                    ps[eh[e_], sl], Rc[eh[e_], sl], Lc[eh[e_], sl], start=True, stop=True,
                                    tile_position=(64 * e_, 64 * e_)), r=[tRc, tLc], w=[tps])
                        B.op("act", lambda e, ps=ps: e.activation(out=Ln, in_=ps[:, :], func=AF.Copy),
                             r=[tps], w=[tLn])
                        if lev < 5:
                            ps2, tps2 = getps(YB)
                            for c in range(8):
                                for e_ in range(2):
                                    sl = slice(c * 64, (c + 1) * 64)
                                    B.op("pe", lambda e, ps2=ps2, sl=sl, e_=e_: e.matmul(
                                        ps2[eh[e_], sl], Lc[eh[e_], sl], Rc[eh[e_], sl], start=True, stop=True,
                                        tile_position=(64 * e_, 64 * e_)), r=[tRc, tLc], w=[tps2])
                            B.op("dve", lambda e, ps2=ps2: e.tensor_copy(Rn, ps2[:, :]), r=[tps2], w=[tRn])
                        yield
                        ps3_, tps3 = getps(YB)
                        for c in range(8):
                            for e_ in range(2):
                                sl = slice(c * 64, (c + 1) * 64)
                                B.op("pe", lambda e, ps3_=ps3_, sl=sl, e_=e_: e.matmul(
                                    ps3_[eh[e_], sl], Ln[eh[e_], sl], Gc[eh[e_], sl], start=True, stop=True,
                                    tile_position=(64 * e_, 64 * e_)), r=[tLn, tGc], w=[tps3])
                        B.op("dve", lambda e, ps3_=ps3_: e.tensor_tensor(out=Gn, in0=ps3_[:, :], in1=Gc, op=ALU.add),
                             r=[tps3, tGc], w=[tGn])
                        yield
                        cur = nxt

                PSY = {}

                def y_chain(hp):
                    S, stk = ysetup(hp)
                    art, bonus, gg, PCt = (S[n] for n in ["art", "bonus", "gg", "PC"])
                    VT, BhT, KhT, LakT, LrbT, LrkT, TTm, Xb, Ub = (S[n] for n in ["VT", "BhT", "KhT", "LakT", "LrbT", "LrkT", "TT", "Xb", "Ub"])
                    tTT = stk["TT"]
                    YB = (0, 1, 2, 3)
                    psY, tpsY = getps((4 + hp,), hold=True)
                    PSY[hp] = (psY, tpsY)
                    Sb = STb[:, hp * 64:(hp + 1) * 64]
                    S32 = ST32[:, hp * 64:(hp + 1) * 64]
                    tS = t_ST[hp]
                    for c in range(8):
                        sl = slice(c * 64, (c + 1) * 64)
                        psX, tpsX = getps(YB)
                        for e_ in range(2):
                            B.op("pe", lambda e, e_=e_: e.matmul(
                                psX[eh[e_], 0:64], art[eh[e_], c * 128:c * 128 + 64], Sb[eh[e_], :], start=True, stop=False,
                                tile_position=(64 * e_, 64 * e_)), r=[stk["art"], tS], w=[tpsX])
                            B.op("pe", lambda e, e_=e_: e.matmul(
                                psX[eh[e_], 0:64], LakT[eh[e_], sl], VT[eh[e_], sl], start=False, stop=True,
                                tile_position=(64 * e_, 64 * e_)), r=[stk["LakT"], stk["VT"]], w=[tpsX])
                        B.op("act", lambda e: e.activation(out=Xb, in_=psX[:, 0:64], func=AF.Copy),
                             r=[tpsX], w=[stk["Xb"]])
                        yield
                        psU, tpsU = getps(YB)
                        for e_ in range(2):
                            B.op("pe", lambda e, e_=e_: e.matmul(
                                psU[eh[e_], 0:64], TTm[eh[e_], sl], Xb[eh[e_], :], start=True, stop=True,
                                tile_position=(64 * e_, 64 * e_)), r=[tTT, stk["Xb"]], w=[tpsU])
                        B.op("dve", lambda e: e.tensor_copy(Ub, psU[:, 0:64]), r=[tpsU], w=[stk["Ub"]])
                        yield
                        for e_ in range(2):
                            B.op("pe", lambda e, e_=e_: e.matmul(
                                psY[eh[e_], sl], Sb[eh[e_], :], art[eh[e_], c * 128 + 64:(c + 1) * 128], start=True, stop=False,
                                tile_position=(64 * e_, 64 * e_)), r=[tS, stk["art"]], w=[tpsY])
                            B.op("pe", lambda e, e_=e_: e.matmul(
                                psY[eh[e_], sl], Ub[eh[e_], :], LrbT[eh[e_], sl], start=False, stop=False,
                                tile_position=(64 * e_, 64 * e_)), r=[stk["Ub"], stk["LrbT"]], w=[tpsY])
                            B.op("pe", lambda e, e_=e_: e.matmul(
                                psY[eh[e_], sl], VT[eh[e_], sl], LrkT[eh[e_], sl], start=False, stop=True,
                                tile_position=(64 * e_, 64 * e_)), r=[stk["VT"], stk["LrkT"]], w=[tpsY])
                        psS, tpsS = getps(YB)
                        for e_ in range(2):
                            B.op("pe", lambda e, e_=e_: e.matmul(
                                psS[eh[e_], 0:64], BhT[eh[e_], sl], Ub[eh[e_], :], start=True, stop=False,
                                tile_position=(64 * e_, 64 * e_)), r=[stk["BhT"], stk["Ub"]], w=[tpsS])
                            B.op("pe", lambda e, e_=e_: e.matmul(
                                psS[eh[e_], 0:64], KhT[eh[e_], sl], VT[eh[e_], sl], start=False, stop=True,
                                tile_position=(64 * e_, 64 * e_)), r=[stk["KhT"], stk["VT"]], w=[tpsS])
                        B.op("dve", lambda e: e.scalar_tensor_tensor(
                            out=S32, in0=S32, scalar=PCt[:, c:c + 1], in1=psS[:, 0:64], op0=ALU.mult, op1=ALU.add),
                            r=[tpsS, stk["PC"]], w=[tS])
                        B.op("act", lambda e: e.activation(out=Sb, in_=S32, func=AF.Copy), w=[tS])
                        yield

                TAIL3 = [(r_t, "r", k_t, "k", v_t, "v"), (sgw, "sgw", av, "av", tE, "E")]

                def y_tail(hp):
                    S, stk = ysetup(hp)
                    bonus, gg, VT, BhT = (S[n] for n in ["bonus", "gg", "VT", "BhT"])
                    YB = (0, 1, 2, 3)
                    psY, tpsY = PSY[hp]
                    gA, kA, gB, kB, gC, kC = TAIL3[hp % 2]
                    tgA, tgB, tgC = tk[kA], tk[kB], tk[kC]
                    B.op("act", lambda e: e.activation(out=gA, in_=psY[:, :], func=AF.Copy), r=[tpsY], w=[tgA])
                    B.op("act", lambda e: e.activation(out=VT, in_=psY[:, :], func=AF.Copy), r=[tpsY], w=[stk["VT"]])
                    B.op("act", lambda e: e.activation(out=BhT, in_=psY[:, :], func=AF.Square), r=[tpsY], w=[stk["BhT"]])
                    release(psY)
                    yield
                    psm, tpsm = getps(YB)
                    psq, tpsq = getps(YB)
                    B.op("pe", lambda e: e.matmul(psm[:, :], bones, VT, start=True, stop=True),
                         r=[stk["VT"], t_c], w=[tpsm])
                    B.op("pe", lambda e: e.matmul(psq[:, :], bones, BhT, start=True, stop=True),
                         r=[stk["BhT"], t_c], w=[tpsq])
                    B.op("dve", lambda e: e.tensor_scalar(out=gB, in0=psm[:, :], scalar1=1.0 / 64, scalar2=None, op0=ALU.mult),
                         r=[tpsm], w=[tgB])
                    B.op("dve", lambda e: e.tensor_tensor(out=gC, in0=gB, in1=gB, op=ALU.mult), r=[tgB], w=[tgC])
                    B.op("dve", lambda e: e.scalar_tensor_tensor(out=gC, in0=psq[:, :], scalar=1.0 / 64, in1=gC,
                                                                 op0=ALU.mult, op1=ALU.subtract),
                         r=[tpsq], w=[tgC])
                    yield
                    B.op("act", lambda e: e.activation(out=gC, in_=gC, func=AF.Ln, bias=eps_gn), r=[t_c], w=[tgC])
                    B.op("act", lambda e: e.activation(out=gC, in_=gC, func=AF.Exp, scale=-0.5), w=[tgC])
                    B.op("dve", lambda e: e.tensor_tensor(out=gA, in0=gA, in1=gB, op=ALU.subtract), r=[tgB], w=[tgA])
                    yield
                    B.op("dve", lambda e: e.tensor_tensor(out=gA, in0=gA, in1=gC, op=ALU.mult), r=[tgC], w=[tgA])
                    B.op("act", lambda e: e.activation(out=gA, in_=gA, func=AF.Identity, scale=pvc("lng", hp), bias=pvc("lnb", hp)),
                         r=[t_pv], w=[tgA])
                    yield
                    B.op("dve", lambda e: e.tensor_tensor(out=gA, in0=gA, in1=bonus, op=ALU.add), r=[stk["bonus"]], w=[tgA])
                    B.op("dve", lambda e: e.tensor_tensor(out=mixcat[:, hp * TT:(hp + 1) * TT], in0=gA, in1=gg, op=ALU.mult),
                         r=[tgA, stk["gg"]], w=[t_mix[hp]])
                    yield

                def chain_gens(gs):
                    for g_ in gs:
                        yield from g_

                def interleave(gs):
                    gs = list(gs)
                    while gs:
                        for g_ in list(gs):
                            try:
                                next(g_)
                            except StopIteration:
                                gs.remove(g_)
                for _ in x_lowrank():
                    pass
                for _ in x_prep(0):
                    pass
                for hp in range(1, 4):
                    interleave([x_prep(hp), y2a(hp - 1)])
                interleave([chain_gens([x_sc(0), x_sc(1)]), y2a(3)])
                for dg in range(2):
                    B.dma("pool", wos[dg][:, :], wo_d[l, dg], w=[t_wos[dg]])
                XB = (0, 1, 2, 3)
                interleave([y_chain(0), y_chain(1), y_chain(2), y_chain(3), chain_gens([x_cfall(), x_cfln()])])
                interleave([y_tail(0), y_tail(1)])
                interleave([y_tail(2), y_tail(3)])
                B.barrier()

                if dbg and l == 0 and tt == 0:
                    B.dma("pool", dbg_d[:, :], mixcat[:, :], r=t_mix)

                ar.off = base_off
                wos4 = [wos[0][:, :], wos[1][:, :], ar.alloc(KC * 128, BF16), ar.alloc(KC * 128, BF16)]
                t_wos4 = [t_wos[0], t_wos[1], Tok(), Tok()]
                for dg in (2, 3):
                    B.dma("pool", wos4[dg], wo_d[l, dg], w=[t_wos4[dg]])
                pss, tss = getps(hold=True)
                for dg in range(KC):
                    si = dg % 4
                    if dg >= 4:
                        B.dma("pool", wos4[si], wo_d[l, dg], w=[t_wos4[si]])
                    ps, tps = getps()
                    for kc in range(KC):
                        B.op("pe", lambda e, ps=ps, kc=kc, si=si: e.matmul(
                            ps[:, :], wos4[si][:, kc * 128:(kc + 1) * 128], mixcat[:, kc * TT:(kc + 1) * TT],
                            start=(kc == 0), stop=(kc == KC - 1)), r=[t_wos4[si], t_mix[kc]], w=[tps])
                    B.op("act", lambda e, ps=ps, dg=dg: e.activation(out=fo[:, dg * TT:(dg + 1) * TT], in_=ps[:, :], func=AF.Copy),
                         r=[tps], w=[t_fo[dg]])
                    i = dg % 2
                    B.op("act", lambda e, ps=ps, i=i: e.activation(out=NORM["sq"][i], in_=ps[:, :], func=AF.Square),
                         r=[tps], w=[NORM["t_sq"][i]])
                    B.op("pe", lambda e, i=i, dg=dg, pss=pss: e.matmul(pss[:, :], ones, NORM["sq"][i], start=(dg == 0), stop=(dg == KC - 1)),
                         r=[NORM["t_sq"][i], t_c], w=[tss])
                release(pss)
                post_norm_residual(tt, "postmix", pss, tss)

                B.barrier()
                ar.off = base_off
                act_t = ar.alloc(NJ * TT, BF16)
                t_act = [Tok() for _ in range(NJ)]
                NGUS, NWDS = 5, 3
                gus = [ar.alloc(2 * KC * 128, BF16) for _ in range(NGUS)]
                t_gus = [Tok() for _ in range(NGUS)]
                t_guu = [Tok() for _ in range(NGUS)]
                wds = [ar.alloc(NJ * 128, BF16) for _ in range(NWDS)]
                t_wds = [Tok() for _ in range(NWDS)]
                sil = [ar.alloc(TT, F32), ar.alloc(TT, F32)]
                t_sil = [Tok(), Tok()]
                rmsnorm_to_hT(tt, "preffn")
                for j in range(NJ):
                    si = rr("gus", NGUS)
                    B.dma("pool", gus[si][:, 0:KC * 128], wg_d[l, j], w=[t_gus[si]])
                    B.dma("pool", gus[si][:, KC * 128:2 * KC * 128], wu_d[l, j], w=[t_guu[si]])
                    if j == NGUS - 1:
                        for dg in range(NWDS):
                            B.dma("pool", wds[dg], wd_d[l, dg], w=[t_wds[dg]])
                    psg, tpsg = getps()
                    psu, tpsu = getps()
                    for kc in range(KC):
                        B.op("pe", lambda e, kc=kc, si=si, psg=psg: e.matmul(
                            psg[:, :], gus[si][:, kc * 128:(kc + 1) * 128], hs(kc, 1, 513),
                            start=(kc == 0), stop=(kc == KC - 1)), r=[t_gus[si], t_hT[kc]], w=[tpsg])
                    for kc in range(KC):
                        B.op("pe", lambda e, kc=kc, si=si, psu=psu: e.matmul(
                            psu[:, :], gus[si][:, (KC + kc) * 128:(KC + kc + 1) * 128], hs(kc, 1, 513),
                            start=(kc == 0), stop=(kc == KC - 1)), r=[t_guu[si], t_hT[kc]], w=[tpsu])
                    i = j % 2
                    B.op("act", lambda e, i=i, psg=psg: e.activation(out=sil[i], in_=psg[:, :], func=AF.Silu),
                         r=[tpsg], w=[t_sil[i]])
                    B.op("dve", lambda e, i=i, j=j, psu=psu: e.tensor_tensor(
                        out=act_t[:, j * TT:(j + 1) * TT], in0=psu[:, :], in1=sil[i], op=ALU.mult),
                        r=[tpsu, t_sil[i]], w=[t_act[j]])
                pss, tss = getps(hold=True)
                for dg in range(KC):
                    si = dg % NWDS
                    if dg >= NWDS:
                        B.dma("pool", wds[si], wd_d[l, dg], w=[t_wds[si]])
                    ps, tps = getps()
                    for j in range(NJ):
                        B.op("pe", lambda e, ps=ps, j=j, si=si: e.matmul(
                            ps[:, :], wds[si][:, j * 128:(j + 1) * 128], act_t[:, j * TT:(j + 1) * TT],
                            start=(j == 0), stop=(j == NJ - 1)), r=[t_wds[si], t_act[j]], w=[tps])
                    B.op("act", lambda e, ps=ps, dg=dg: e.activation(out=fo[:, dg * TT:(dg + 1) * TT], in_=ps[:, :], func=AF.Copy),
                         r=[tps], w=[t_fo[dg]])
                    i = dg % 2
                    B.op("act", lambda e, ps=ps, i=i: e.activation(out=NORM["sq"][i], in_=ps[:, :], func=AF.Square),
                         r=[tps], w=[NORM["t_sq"][i]])
                    B.op("pe", lambda e, i=i, dg=dg, pss=pss: e.matmul(pss[:, :], ones, NORM["sq"][i], start=(dg == 0), stop=(dg == KC - 1)),
                         r=[NORM["t_sq"][i], t_c], w=[tss])
                release(pss)
                post_norm_residual(tt, "postffn", pss, tss)

        t_out = Tok()
        for tt in range(NT):
            for kc in range(KC):
                B.dma("sp", y_d[:, kc * T + tt * TT: kc * T + (tt + 1) * TT], xs(kc, tt), r=[t_x[tt][kc]], w=[Tok()])
        B.barrier()

        with nc.Block() as block:
            B.emit(block)
    return nc


def _consts():
    c = np.zeros((P, NCF), np.float32)
    p = np.arange(P)[:, None]
    c[:, CF["ident"]:CF["ident"] + 128] = (p == np.arange(128)[None, :])
    c[:, CF["ones"]:CF["ones"] + 128] = 1.0
    c[:, CF["bones"]:CF["bones"] + 128] = ((p // 64) == (np.arange(128)[None, :] // 64))
    col = (np.arange(512) % 64)[None, :]
    row = p % 64
    c[:, CF["msu"]:CF["msu"] + 512] = (row < col)
    c[:, CF["miu"]:CF["miu"] + 512] = (row <= col)
    c[:, CF["msl"]:CF["msl"] + 512] = (col < row)
    c[:, CF["irep"]:CF["irep"] + 512] = (row == col)
    c[:, CF["scan"]:CF["scan"] + 512] = (col != 0) * np.ones((P, 1))
    return c


def _colvec(v):
    return np.ascontiguousarray(np.asarray(v, np.float32).reshape(-1, P).T)


def _prep_inputs(inp, L):
    f = lambda a: np.asarray(a, np.float32)
    out = {}

    def grp_cols(w, ncol_groups):
        Lw, K, N = w.shape
        a = w.reshape(Lw, K // P, P, N // P, P)
        a = a.transpose(0, 3, 2, 1, 4)
        return np.ascontiguousarray(a.reshape(Lw, N // P, P, (K // P) * P))
    out["w_in"] = grp_cols(f(inp["w_in"])[:L], NJ)
    out["w_gate"] = grp_cols(f(inp["w_gate"])[:L], NJ)
    out["w_up"] = grp_cols(f(inp["w_up"])[:L], NJ)
    out["w_o"] = grp_cols(f(inp["w_o"])[:L], KC)
    out["w_down"] = grp_cols(f(inp["w_down"])[:L], KC)
    wl = np.concatenate([f(inp["w1"]), f(inp["a1"]), f(inp["g1"])], axis=2)[:L]
    wl = wl.reshape(L, KC, P, 256).transpose(0, 2, 1, 3).reshape(L, P, KC * 256)
    out["wlr"] = np.ascontiguousarray(wl)
    out["w2a2"] = np.ascontiguousarray(np.concatenate([f(inp["w2"]), f(inp["a2"])], axis=1)[:L])
    out["g2"] = np.ascontiguousarray(f(inp["g2"])[:L])
    pv = np.zeros((L, P, NPV), np.float32)
    for l in range(L):
        def put(name, arr):
            a = _colvec(arr)
            pv[l, :, PV[name]:PV[name] + a.shape[1]] = a
        put("premix", inp["pre_mix_g"][l]); put("postmix", inp["post_mix_g"][l])
        put("preffn", inp["pre_ffn_g"][l]); put("postffn", inp["post_ffn_g"][l])
        put("muw", inp["mu_wag"][l][0]); put("mua", inp["mu_wag"][l][1]); put("mug", inp["mu_wag"][l][2])
        put("murkv", inp["mu_rkv"][l])
        put("w0", inp["w0"][l]); put("a0", inp["a0"][l]); put("kk", inp["k_k"][l]); put("ka", inp["k_a"][l])
        put("rk", np.asarray(inp["r_k"][l]).reshape(-1)); put("lng", inp["lnx_g"][l]); put("lnb", inp["lnx_b"][l])
        scw = f(inp["sc_conv_w"][l])
        for k_ in range(3):
            pv[l, :, PV["scw"] + k_ * 2: PV["scw"] + k_ * 2 + 2] = _colvec(scw[k_])
        cfw = f(inp["cf_conv_w"][l])
        for k_ in range(31):
            pv[l, :, PV["cfw"] + k_ * 2: PV["cfw"] + k_ * 2 + 2] = _colvec(cfw[k_])
        put("cfb", inp["cf_conv_b"][l]); put("cflg", inp["cf_ln_g"][l]); put("cflb", inp["cf_ln_b"][l])
    out["pv"] = pv
    out["cf"] = _consts()
    return out


_CACHE = {}


def run(inputs, L=4, dbg=False):
    x = np.asarray(inputs["x"], np.float32)
    shared = _prep_inputs(inputs, L)
    key = (L, dbg)
    if key not in _CACHE:
        _CACHE[key] = build_program(L, dbg)
    nc = _CACHE[key]
    in_maps = []
    for b in range(NCORES):
        xt = np.ascontiguousarray(x[b].T.reshape(KC, P, T).transpose(1, 0, 2).reshape(P, KC * T))
        m = dict(shared)
        m["xT"] = xt
        in_maps.append(m)
    res = run_bass_kernel_spmd(nc, in_maps, core_ids=list(range(NCORES)))
    outs = []
    for b in range(NCORES):
        yt = np.asarray(res.results[b]["yT"]).reshape(P, KC, T).transpose(1, 0, 2).reshape(D, T)
        outs.append(yt.T)
    y = np.stack(outs, 0).astype(np.float32)
    if dbg:
        return y, [np.asarray(r["dbg"]) for r in res.results]
    return y


def kernel(**inputs):
    return run(inputs, L=4)
```
